# Optimizing a Trainium2 kernel written in Bass

```python
import math
import jax, jax.numpy as jnp
from jax import lax
import numpy as np

D_MODEL = 2048
BATCH = 2
SEQ = 4096
DEPTH = 1

ATT_HEAD_DIM = 128
ATT_HEADS_PER_GROUP = 4
ATT_PATTERNS = ((128, 1), (512, 4), (2048, 16))
ATT_HEADS = ATT_HEADS_PER_GROUP * len(ATT_PATTERNS)
ATT_WIDTH = ATT_HEADS * ATT_HEAD_DIM
ATT_OUT_WIDTH = ATT_HEADS_PER_GROUP * ATT_HEAD_DIM
ATT_BLOCK = 128

M_HEADS = 4
M_QK_DIM = 128
M_V_DIM = 256
M_QK_WIDTH = M_HEADS * M_QK_DIM
M_V_WIDTH = M_HEADS * M_V_DIM
M_CONV = 4
M_CHUNK = 64

N_BRANCHES = 2
IN_PROJ_SPLITS = (ATT_WIDTH, ATT_WIDTH, ATT_WIDTH, 2 * M_QK_WIDTH, M_V_WIDTH, M_V_WIDTH, 2 * M_HEADS, N_BRANCHES * D_MODEL)
IN_PROJ_WIDTH = sum(IN_PROJ_SPLITS)

N_GROUPS = 4
EXPERTS_PER_GROUP = 8
N_EXPERTS = N_GROUPS * EXPERTS_PER_GROUP
TOP_K = 2
D_FF_EXPERT = 1408
MOE_BLOCK = 128

DEEPNORM_ALPHA = (2 * DEPTH) ** 0.25
DEEPNORM_BETA = (8 * DEPTH) ** -0.25
LN_EPS = 1e-5

kernel_name = 'hybrid_dilated_attn_mlstm_hmoe_deepnorm'


def alibi_slopes(n):
    def geometric(k):
        start = 2.0 ** (-8.0 / k)
        return [start ** (i + 1) for i in range(k)]
    c = 2 ** int(math.floor(math.log2(n)))
    s = geometric(c) if c == n else geometric(c) + geometric(2 * c)[0::2][: n - c]
    return np.array(sorted(s, reverse=True), dtype=np.float32)


def layer_norm(x, g, b):
    xf = x.astype(jnp.float32)
    mu = jnp.mean(xf, axis=-1, keepdims=True)
    var = jnp.mean(jnp.square(xf - mu), axis=-1, keepdims=True)
    y = (xf - mu) * lax.rsqrt(var + LN_EPS) * g.astype(jnp.float32) + b.astype(jnp.float32)
    return y.astype(x.dtype)


def dilated_window_attention(q, k, v, slopes, window, dilation):
    B, S, H, Dh = q.shape
    r = dilation
    n_win = window // r
    L = S // r
    nb = -(-L // ATT_BLOCK)
    Lp = nb * ATT_BLOCK

    def to_sub(t):
        t = t.reshape(B, L, r, H, Dh).transpose(0, 2, 3, 1, 4)
        return jnp.pad(t, ((0, 0), (0, 0), (0, 0), (0, Lp - L), (0, 0)))

    def band(t):
        prev = jnp.pad(t, ((0, 0), (0, 0), (0, 0), (ATT_BLOCK, 0), (0, 0)))[:, :, :, :Lp]
        return jnp.concatenate([prev.reshape(B, r, H, nb, ATT_BLOCK, Dh),
                                t.reshape(B, r, H, nb, ATT_BLOCK, Dh)], axis=4)

    qb = to_sub(q).reshape(B, r, H, nb, ATT_BLOCK, Dh)
    kb = band(to_sub(k))
    vb = band(to_sub(v))
    scores = jnp.einsum('bphnqd,bphnkd->bphnqk', qb, kb,
                        preferred_element_type=jnp.float32) * (Dh ** -0.5)
    qi = jnp.arange(ATT_BLOCK)[:, None]
    ki = jnp.arange(2 * ATT_BLOCK)[None, :]
    delta = ATT_BLOCK + qi - ki
    key_u = (jnp.arange(nb)[:, None, None] - 1) * ATT_BLOCK + ki[None]
    valid = (delta >= 0) & (delta <= n_win) & (key_u >= 0)
    alibi = -jnp.asarray(slopes, jnp.float32)[:, None, None, None] * (delta * r).astype(jnp.float32)
    scores = jnp.where(valid, scores + alibi, -jnp.inf)
    m = jnp.max(scores, axis=-1, keepdims=True)
    p = jnp.exp(scores - m)
    l = jnp.sum(p, axis=-1, keepdims=True)
    o = jnp.einsum('bphnqk,bphnkd->bphnqd', p, vb.astype(jnp.float32)) / l
    lse = (m + jnp.log(l))[..., 0]
    o = o.reshape(B, r, H, Lp, Dh)[:, :, :, :L].transpose(0, 3, 1, 2, 4).reshape(B, S, H, Dh)
    lse = lse.reshape(B, r, H, Lp)[..., :L].transpose(0, 3, 1, 2).reshape(B, S, H)
    return o, lse


def causal_depthwise_conv(x, w, b):
    K, C = w.shape
    y = lax.conv_general_dilated(x, w[:, None, :].astype(x.dtype), window_strides=(1,),
                                 padding=[(K - 1, 0)], dimension_numbers=('NWC', 'WIO', 'NWC'),
                                 feature_group_count=C)
    return y + b


def mlstm_chunkwise(q, k, v, i_pre, f_pre):
    B, NH, S, dqk = q.shape
    dv = v.shape[-1]
    nc = S // M_CHUNK
    f32 = jnp.float32

    def chunked(t):
        t = t.astype(f32).reshape(B, NH, nc, M_CHUNK, *t.shape[3:])
        return jnp.moveaxis(t, 2, 0)

    qc = chunked(q) * (dqk ** -0.5)
    kc = chunked(k)
    vc = chunked(v)
    ic = chunked(i_pre)
    bc = lax.cumsum(chunked(jax.nn.log_sigmoid(f_pre.astype(f32))), axis=3)
    causal = jnp.tril(jnp.ones((M_CHUNK, M_CHUNK), dtype=bool))

    def step(carry, xs):
        C, n, m = carry
        qj, kj, vj, ij, bj = xs
        dmat = jnp.where(causal, bj[..., :, None] - bj[..., None, :] + ij[..., None, :], -jnp.inf)
        inter = bj + m[..., None]
        m_t = jnp.maximum(inter, jnp.max(dmat, axis=-1))
        w_intra = jnp.exp(dmat - m_t[..., None])
        w_inter = jnp.exp(inter - m_t)
        qk = jnp.einsum('bhld,bhsd->bhls', qj, kj) * w_intra
        num = w_inter[..., None] * jnp.einsum('bhld,bhdv->bhlv', qj, C) + jnp.einsum('bhls,bhsv->bhlv', qk, vj)
        den = w_inter * jnp.einsum('bhld,bhd->bhl', qj, n) + jnp.sum(qk, axis=-1)
        h = num / jnp.maximum(jnp.abs(den), jnp.exp(-m_t))[..., None]
        b_last = bj[..., -1]
        w_log = b_last[..., None] - bj + ij
        m_new = jnp.maximum(b_last + m, jnp.max(w_log, axis=-1))
        wk = jnp.exp(w_log - m_new[..., None])
        decay = jnp.exp(b_last + m - m_new)
        C_new = decay[..., None, None] * C + jnp.einsum('bhs,bhsd,bhsv->bhdv', wk, kj, vj)
        n_new = decay[..., None] * n + jnp.einsum('bhs,bhsd->bhd', wk, kj)
        return (C_new, n_new, m_new), h

    init = (jnp.zeros((B, NH, dqk, dv), f32), jnp.zeros((B, NH, dqk), f32), jnp.zeros((B, NH), f32))
    _, hs = lax.scan(step, init, (qc, kc, vc, ic, bc))
    return jnp.moveaxis(hs, 0, 2).reshape(B, NH, S, dv)


def token_mixer(h, w_in, conv_w, conv_b, if_bias, norm_w, w_proj_att, w_proj_mlstm, w_out):
    B, S, _ = h.shape
    proj = h @ w_in
    cuts = [int(c) for c in np.cumsum(IN_PROJ_SPLITS)[:-1]]
    aq, ak, av, mqk, mv, mo, mif, gate_pre = jnp.split(proj, cuts, axis=-1)

    aq = aq.reshape(B, S, ATT_HEADS, ATT_HEAD_DIM)
    ak = ak.reshape(B, S, ATT_HEADS, ATT_HEAD_DIM)
    av = av.reshape(B, S, ATT_HEADS, ATT_HEAD_DIM)
    slopes = alibi_slopes(ATT_HEADS)
    outs, lses = [], []
    for g, (window, dilation) in enumerate(ATT_PATTERNS):
        hs = slice(g * ATT_HEADS_PER_GROUP, (g + 1) * ATT_HEADS_PER_GROUP)
        o, lse = dilated_window_attention(aq[:, :, hs], ak[:, :, hs], av[:, :, hs], slopes[hs], window, dilation)
        outs.append(o)
        lses.append(lse)
    mix_w = jax.nn.softmax(jnp.stack(lses, axis=0), axis=0)
    att = jnp.einsum('gbsh,gbshd->bshd', mix_w, jnp.stack(outs, axis=0))
    att = att.reshape(B, S, ATT_OUT_WIDTH).astype(h.dtype)

    mqk = jax.nn.silu(causal_depthwise_conv(mqk, conv_w, conv_b))
    mq, mk = jnp.split(mqk, 2, axis=-1)

    def heads(t, d):
        return t.reshape(B, S, M_HEADS, d).transpose(0, 2, 1, 3)

    mif = (mif + if_bias).reshape(B, S, 2, M_HEADS).transpose(2, 0, 3, 1)
    hm = mlstm_chunkwise(heads(mq, M_QK_DIM), heads(mk, M_QK_DIM), heads(mv, M_V_DIM), mif[0], mif[1])
    mu = jnp.mean(hm, axis=-1, keepdims=True)
    var = jnp.mean(jnp.square(hm - mu), axis=-1, keepdims=True)
    hm = ((hm - mu) * lax.rsqrt(var + LN_EPS)).transpose(0, 2, 1, 3) * norm_w
    hm = hm.reshape(B, S, M_V_WIDTH).astype(h.dtype) * jax.nn.sigmoid(mo)

    g_att, g_mlstm = jnp.split(jax.nn.sigmoid(gate_pre), 2, axis=-1)
    merged = g_att * (att @ w_proj_att) + g_mlstm * (hm @ w_proj_mlstm)
    return merged @ w_out


def hierarchical_moe(h, w_router_group, b_router_group, w_router_expert, b_router_expert, w_gate, w_up, w_down):
    B, S, D = h.shape
    T = B * S
    xf = h.reshape(T, D)
    g_logits = (xf @ w_router_group).astype(jnp.float32) + b_router_group.astype(jnp.float32)
    grp = jnp.argmax(g_logits, axis=-1)
    g_w = jnp.take_along_axis(jax.nn.softmax(g_logits, axis=-1), grp[:, None], axis=1)[:, 0]
    e_all = ((xf @ w_router_expert).astype(jnp.float32) + b_router_expert.astype(jnp.float32)).reshape(T, N_GROUPS, EXPERTS_PER_GROUP)
    e_logits = jnp.take_along_axis(e_all, grp[:, None, None], axis=1)[:, 0]
    top_v, top_i = lax.top_k(e_logits, TOP_K)
    weight = g_w[:, None] * jax.nn.softmax(top_v, axis=-1)
    expert = grp[:, None].astype(jnp.int32) * EXPERTS_PER_GROUP + top_i.astype(jnp.int32)

    M = T * TOP_K
    e_flat = expert.reshape(M)
    tok_flat = jnp.repeat(jnp.arange(T, dtype=jnp.int32), TOP_K)
    w_flat = weight.reshape(M)
    order = jnp.argsort(e_flat)
    e_sorted = e_flat[order]
    counts = jnp.bincount(e_flat, length=N_EXPERTS)
    padded = (counts + MOE_BLOCK - 1) // MOE_BLOCK * MOE_BLOCK
    start = jnp.cumsum(counts) - counts
    pstart = jnp.cumsum(padded) - padded
    pend = pstart + padded
    dest = pstart[e_sorted] + jnp.arange(M, dtype=jnp.int32) - start[e_sorted]
    NB = M // MOE_BLOCK + N_EXPERTS
    slot_tok = jnp.zeros((NB * MOE_BLOCK,), jnp.int32).at[dest].set(tok_flat[order])
    slot_w = jnp.zeros((NB * MOE_BLOCK,), jnp.float32).at[dest].set(w_flat[order])
    block_start = jnp.arange(NB, dtype=jnp.int32) * MOE_BLOCK
    block_expert = jnp.minimum(jnp.sum(pend[None, :] <= block_start[:, None], axis=-1), N_EXPERTS - 1)

    def expert_block(args):
        toks, e = args
        xb = xf[toks]
        hb = jax.nn.silu(xb @ w_gate[e]) * (xb @ w_up[e])
        return hb @ w_down[e]

    yb = lax.map(expert_block, (slot_tok.reshape(NB, MOE_BLOCK), block_expert))
    y = jax.ops.segment_sum(yb.reshape(NB * MOE_BLOCK, D) * slot_w[:, None].astype(yb.dtype),
                            slot_tok, num_segments=T)
    return y.reshape(B, S, D)


def setup_inputs(seed: int = 0) -> dict:
    key = jax.random.key(seed)
    ks = jax.random.split(key, 24)
    f32 = jnp.float32

    def nrm(k, shape, scale):
        return jax.random.normal(k, shape, f32) * scale

    Ld = DEPTH
    col_scale = jnp.concatenate([
        jnp.ones((2 * ATT_WIDTH,), f32),
        jnp.full((ATT_WIDTH,), DEEPNORM_BETA, f32),
        jnp.ones((2 * M_QK_WIDTH,), f32),
        jnp.full((M_V_WIDTH,), DEEPNORM_BETA, f32),
        jnp.ones((M_V_WIDTH + 2 * M_HEADS + N_BRANCHES * D_MODEL,), f32)])
    m_if_bias = jnp.concatenate([
        nrm(ks[6], (Ld, M_HEADS), 0.1),
        jnp.linspace(3.0, 6.0, M_HEADS, dtype=f32)[None, :] + nrm(ks[7], (Ld, M_HEADS), 0.1)], axis=-1)
    return {
        'x': nrm(ks[0], (BATCH, SEQ, D_MODEL), 1.0),
        'ln_in_g': 1.0 + nrm(ks[1], (D_MODEL,), 0.02),
        'ln_in_b': nrm(ks[2], (D_MODEL,), 0.02),
        'w_in': nrm(ks[3], (Ld, D_MODEL, IN_PROJ_WIDTH), D_MODEL ** -0.5) * col_scale,
        'm_conv_w': nrm(ks[4], (Ld, M_CONV, 2 * M_QK_WIDTH), M_CONV ** -0.5),
        'm_conv_b': nrm(ks[5], (Ld, 2 * M_QK_WIDTH), 0.02),
        'm_if_bias': m_if_bias,
        'm_norm_w': 1.0 + nrm(ks[8], (Ld, M_HEADS, M_V_DIM), 0.02),
        'w_proj_att': nrm(ks[9], (Ld, ATT_OUT_WIDTH, D_MODEL), ATT_OUT_WIDTH ** -0.5),
        'w_proj_mlstm': nrm(ks[10], (Ld, M_V_WIDTH, D_MODEL), M_V_WIDTH ** -0.5),
        'w_out': nrm(ks[11], (Ld, D_MODEL, D_MODEL), D_MODEL ** -0.5) * DEEPNORM_BETA,
        'ln1_g': 1.0 + nrm(ks[12], (Ld, D_MODEL), 0.02),
        'ln1_b': nrm(ks[13], (Ld, D_MODEL), 0.02),
        'w_router_group': nrm(ks[14], (Ld, D_MODEL, N_GROUPS), D_MODEL ** -0.5),
        'b_router_group': nrm(ks[15], (Ld, N_GROUPS), 0.01),
        'w_router_expert': nrm(ks[16], (Ld, D_MODEL, N_EXPERTS), D_MODEL ** -0.5),
        'b_router_expert': nrm(ks[17], (Ld, N_EXPERTS), 0.01),
        'w_gate': nrm(ks[18], (Ld, N_EXPERTS, D_MODEL, D_FF_EXPERT), D_MODEL ** -0.5),
        'w_up': nrm(ks[19], (Ld, N_EXPERTS, D_MODEL, D_FF_EXPERT), D_MODEL ** -0.5),
        'w_down': nrm(ks[20], (Ld, N_EXPERTS, D_FF_EXPERT, D_MODEL), D_FF_EXPERT ** -0.5) * DEEPNORM_BETA,
        'ln2_g': 1.0 + nrm(ks[21], (Ld, D_MODEL), 0.02),
        'ln2_b': nrm(ks[22], (Ld, D_MODEL), 0.02),
    }


def reference(x, ln_in_g, ln_in_b, w_in, m_conv_w, m_conv_b, m_if_bias, m_norm_w,
              w_proj_att, w_proj_mlstm, w_out, ln1_g, ln1_b,
              w_router_group, b_router_group, w_router_expert, b_router_expert,
              w_gate, w_up, w_down, ln2_g, ln2_b):
    h = layer_norm(x, ln_in_g, ln_in_b)
    for l in range(DEPTH):
        y = token_mixer(h, w_in[l], m_conv_w[l], m_conv_b[l], m_if_bias[l], m_norm_w[l],
                        w_proj_att[l], w_proj_mlstm[l], w_out[l])
        h = layer_norm(DEEPNORM_ALPHA * h + y, ln1_g[l], ln1_b[l])
        y = hierarchical_moe(h, w_router_group[l], b_router_group[l], w_router_expert[l],
                             b_router_expert[l], w_gate[l], w_up[l], w_down[l])
        h = layer_norm(DEEPNORM_ALPHA * h + y, ln2_g[l], ln2_b[l])
    return h
```

```python
import math
from contextlib import ExitStack
import numpy as np
import ml_dtypes
import concourse.bass as bass
import concourse.mybir as mybir
from concourse.bass_utils import run_bass_kernel_spmd

F32 = mybir.dt.float32
BF16 = mybir.dt.bfloat16
AF = mybir.ActivationFunctionType
ALU = mybir.AluOpType
AX = mybir.AxisListType

D = 2048
NW = 4096
NOWN = 1024
NPROJ = 11784
ALPHA = 2 ** 0.25
EPS = 1e-5
NEG = -30000.0
DFF = 1408
NEXP = 32
DEBUG = False


class Buf:
    __slots__ = ("w", "r")

    def __init__(self):
        self.w = None
        self.r = []


class Sched:
    ENG = ("pe", "act", "dve", "pool", "sp")

    def __init__(self, nc, es, n_dma_sems=20):
        self.nc = nc
        self.eobj = {"pe": nc.tensor, "act": nc.scalar, "dve": nc.vector, "pool": nc.gpsimd, "sp": nc.sync}
        self.cnt = {e: 0 for e in self.ENG}
        self.sem = {e: es.enter_context(nc.semaphore("c_" + e)) for e in self.ENG}
        self.seen = {e: {} for e in self.ENG}
        self.pending = {e: [] for e in self.ENG}
        self.dsems, self.dtot, self.dnext = {}, {}, {}
        for q in ("sp", "pool", "act"):
            self.dsems[q] = [es.enter_context(nc.semaphore(f"d_{q}_{i}")) for i in range(n_dma_sems)]
            self.dnext[q] = 0
        self.final_events = []

    def _deps(self, reads, writes):
        deps = []
        for b in reads:
            if b.w is not None:
                deps.append(b.w)
        for b in writes:
            if b.w is not None:
                deps.append(b.w)
            deps.extend(b.r)
        return deps

    def _filter(self, eng, deps):
        seen = self.seen[eng]
        own = self.sem[eng]
        best = {}
        for (sem, val) in deps + self.pending[eng]:
            if sem is own and eng in ("pe", "sp"):
                continue
            k = id(sem)
            if seen.get(k, 0) >= val:
                continue
            seen[k] = val
            best[k] = (sem, val)
        self.pending[eng] = []
        return list(best.values())

    def _emit(self, ename, waits, fn, sem, inc):
        eng = self.eobj[ename]
        for (s_, v) in waits:
            eng.wait_ge(s_, v)
        fn(eng).then_inc(sem, inc)

    def op(self, eng, fn, reads=(), writes=()):
        waits = self._filter(eng, self._deps(reads, writes))
        self.cnt[eng] += 1
        ev = (self.sem[eng], self.cnt[eng])
        for b in reads:
            b.r.append(ev)
        for b in writes:
            b.w = ev
            b.r = []
        self._emit(eng, waits, fn, self.sem[eng], 1)
        return ev

    def dma(self, q, out, in_, reads=(), writes=(), final=False):
        deps = self._deps(reads, writes)
        i = self.dnext[q]
        self.dnext[q] = (i + 1) % len(self.dsems[q])
        dsem = self.dsems[q][i]
        prev = self.dtot.get(id(dsem), 0)
        if prev:
            deps.append((dsem, prev))
        waits = self._filter(q, deps)
        tot = prev + 16
        self.dtot[id(dsem)] = tot
        ev = (dsem, tot)
        for b in reads:
            b.r.append(ev)
        for b in writes:
            b.w = ev
            b.r = []
        self._emit(q, waits, (lambda e: e.dma_start(out=out, in_=in_)), dsem, 16)
        if final:
            self.final_events.append(ev)
        return ev

    def barrier(self):
        evs = [(self.sem[e], self.cnt[e]) for e in self.ENG if self.cnt[e] > 0]
        for q in self.dsems:
            for s in self.dsems[q]:
                t = self.dtot.get(id(s), 0)
                if t:
                    evs.append((s, t))
        for e in self.ENG:
            self.pending[e].extend(evs)

    def finish(self):
        for (s_, v) in self._filter("sp", list(self.final_events)):
            self.nc.sync.wait_ge(s_, v)


def ss(start, n, r):
    return slice(start, start + (n - 1) * r + 1, r)


def alibi_slopes(n):
    def geometric(k):
        start = 2.0 ** (-8.0 / k)
        return [start ** (i + 1) for i in range(k)]
    c = 2 ** int(math.floor(math.log2(n)))
    s = geometric(c) if c == n else geometric(c) + geometric(2 * c)[0::2][: n - c]
    return np.array(sorted(s, reverse=True), dtype=np.float32)


C_AQ, C_AK, C_AV = 0, 1536, 3072
C_MQ, C_MK = 4608, 5120
C_MV, C_MO = 5632, 6656
C_IF = 7680
C_GA, C_GM = 7688, 9736


def build_program(upto=9):
    nc = bass.Bass("TRN2", target_bir_lowering=False)

    def din(name, shape, dt=F32):
        return nc.dram_tensor(name, list(shape), dt, kind="ExternalInput").ap()

    def dscr(name, shape, dt=F32):
        return nc.dram_tensor(name, list(shape), dt, kind="Internal").ap()

    x_win = din("x_win", [NW, D])
    w_in = din("w_in", [D, NPROJ])
    padneg_b = din("padneg_b", [128, NW])
    valid_b = din("valid_b", [128, NW])
    gmask = din("gmask", [4, 2, NW])
    lng_col = din("lng_col", [128, 16]); lnb_col = din("lnb_col", [128, 16])
    lng_b = din("lng_b", [128, D]); lnb_b = din("lnb_b", [128, D])
    cw = din("cw", [128, 8, 4]); cb = din("cb", [128, 8])
    ifb = din("ifb", [4, 2])
    mnw_b = din("mnw_b", [128, 1024])
    w_pa = din("w_pa", [512, D]); w_pm = din("w_pm", [1024, D]); w_out = din("w_out", [D, D])
    ln1g_b = din("ln1g_b", [128, D]); ln1b_b = din("ln1b_b", [128, D])
    ln2g_b = din("ln2g_b", [128, D]); ln2b_b = din("ln2b_b", [128, D])
    wr = din("wr", [128, 16, 36]); br_b = din("br_b", [128, 36])
    if upto >= 6:
        w_gate = din("w_gate", [NEXP, D, DFF]); w_up = din("w_up", [NEXP, D, DFF]); w_down = din("w_down", [NEXP, DFF, D])
    abias = din("abias", [12, 128, 256])
    ident_f = din("ident_f", [128, 128]); ident_h = din("ident_h", [128, 128], BF16)
    tri_neg = din("tri_neg", [128, 128])
    ltri = din("ltri", [128, 128])
    ones_f = din("ones_f", [128, 128])
    iota1 = din("iota1", [128, 128])
    iota1c = din("iota1c", [128, 1])
    sel4 = din("sel4", [4, 4, 128])
    sel32 = din("sel32", [32, 32, 128])
    out_d = nc.dram_tensor("out", [NOWN, D], F32, kind="ExternalOutput").ap()

    h_d = dscr("h_d", [NOWN, D])
    h1_d = dscr("h1_d", [NOWN, D])
    kT_d = dscr("kT_d", [12, 128, NW], BF16)
    qT_d = dscr("qT_d", [12, 128, NOWN], BF16)
    v_d = dscr("v_d", [NW, 1536], BF16)
    hT_d = dscr("hT_d", [16, 128, NOWN], BF16)
    b_hT_d = Buf()
    hmT_d = dscr("hmT_d", [8, 128, NOWN], BF16); attT_d = dscr("attT_d", [4, 128, NOWN], BF16)
    mT_d = dscr("mT_d", [16, 128, NOWN], BF16); h1bf_d = dscr("h1bf_d", [NOWN, D], BF16)
    b_hmT_d, b_attT_d, b_mT_d, b_h1bf_d = (Buf() for _ in range(4))
    o_d = dscr("o_d", [3, NOWN, 512])
    lse_d = dscr("lse_d", [3, NOWN, 4])
    b_h_d, b_h1_d, b_kT_d, b_qT_d, b_v_d, b_o_d, b_lse_d = (Buf() for _ in range(7))
    dbg = {}
    if DEBUG:
        for nm, shp in (("dbg_hm", [NOWN, 1024]), ("dbg_att", [NOWN, 512]), ("dbg_h1", [NOWN, D]), ("dbg_y2", [NOWN, D])):
            dbg[nm] = nc.dram_tensor(nm, shp, F32, kind="ExternalOutput").ap()

    with ExitStack() as es:
        S = Sched(nc, es)
        cnt = [0]

        def sb(es_, shape, dt=F32):
            cnt[0] += 1
            t = es_.enter_context(nc.sbuf_tensor(f"t{cnt[0]}", list(shape), dt))
            return t, Buf()

        psT = [es.enter_context(nc.psum_tensor(f"ps{i}", [128, 512], F32)) for i in range(8)]
        psB = [Buf() for _ in range(8)]
        psi = [0]

        def ps():
            i = psi[0]
            psi[0] = (i + 1) % 8
            return psT[i], psB[i]

        rr = [0]

        def evac_eng():
            rr[0] += 1
            return "act" if rr[0] % 2 else "dve"

        def copy(eng, out, in_, reads, writes):
            if eng == "act":
                S.op("act", lambda e: e.copy(out=out, in_=in_), reads, writes)
            else:
                S.op(eng, lambda e: e.tensor_copy(out=out, in_=in_), reads, writes)

        def load_const(es_, src, shape, dt=F32, q="sp"):
            t, b = sb(es_, shape, dt)
            idx = tuple(slice(None) for _ in shape)
            S.dma(q, t[idx], src, writes=[b])
            return t, b

        idf, b_idf = load_const(es, ident_f, [128, 128])
        idh, b_idh = load_const(es, ident_h, [128, 128], BF16)
        bC = Buf()
        def lc(src, shape, dt=F32):
            t, _ = sb(es, shape, dt)
            idx = tuple(slice(None) for _ in shape)
            S.dma("sp", t[idx], src, writes=[bC])
            return t
        c_lng = lc(lng_col, [128, 16]); c_lnb = lc(lnb_col, [128, 16])

        def layer_norm_stats(x_ap, b_x, st, b_st, mvt, b_mv, sd, b_sd, nchunk=4, csz=512):
            for i in range(nchunk):
                S.op("dve", lambda e, i=i: e.bn_stats(out=st[:, 6 * i:6 * i + 6], in_=x_ap[:, csz * i:csz * i + csz]),
                     reads=[b_x], writes=[b_st])
            S.op("dve", lambda e: e.bn_aggr(out=mvt[:, 0:2], in_=st[:, 0:6 * nchunk]), reads=[b_st], writes=[b_mv])
            S.op("dve", lambda e: e.tensor_scalar(out=sd[:, 0:1], in0=mvt[:, 1:2], scalar1=EPS, scalar2=None, op0=ALU.add),
                 reads=[b_mv], writes=[b_sd])
            S.op("act", lambda e: e.activation(out=sd[:, 0:1], in_=sd[:, 0:1], func=AF.Sqrt), reads=[b_sd], writes=[b_sd])
            S.op("dve", lambda e: e.reciprocal(out=sd[:, 0:1], in_=sd[:, 0:1]), reads=[b_sd], writes=[b_sd])
            S.op("dve", lambda e: e.tensor_scalar(out=sd[:, 1:2], in0=mvt[:, 0:1], scalar1=sd[:, 0:1], scalar2=-1.0,
                                                  op0=ALU.mult, op1=ALU.mult), reads=[b_mv, b_sd], writes=[b_sd])

        Wt_all, b_Wt = sb(es, [128, 8, 32]); mk_all, b_mk = sb(es, [128, 8, 32]); cumm, b_cumm = sb(es, [128, 8, 32])

        with ExitStack() as e1:
            bC1 = Buf()
            def lc1(src, shape, dt=F32):
                t, _ = sb(e1, shape, dt)
                idx = tuple(slice(None) for _ in shape)
                S.dma("sp", t[idx], src, writes=[bC1])
                return t
            c_cw = lc1(cw, [128, 8, 4]); c_cb = lc1(cb, [128, 8]); c_ifb = lc1(ifb, [4, 2])
            c_mnw = lc1(mnw_b, [128, 1024]); c_tri = lc1(tri_neg, [128, 128]); c_sel4 = lc1(sel4, [4, 4, 128])
            xs = [sb(e1, [128, D]) for _ in range(2)]
            xn_l = [sb(e1, [128, D]) for _ in range(2)]
            ln_l = [(sb(e1, [128, 24]), sb(e1, [128, 2]), sb(e1, [128, 2])) for _ in range(2)]
            hT_st, b_hT_st = sb(e1, [128, 16, 512], BF16)
            wbuf = [sb(e1, [128, 16, 512], BF16) for _ in range(2)]
            wif, b_wif = sb(e1, [128, 16, 8], BF16)
            S.dma("pool", wif[:, :, :], w_in[:, C_IF:C_IF + 8].rearrange("(k p) n -> p k n", p=128), writes=[b_wif])
            wi = [0]
            stage_o, b_stage_o = sb(e1, [128, 4, 512], BF16)
            stage_t, b_stage_t = sb(e1, [128, 4, 512], BF16)
            halo, b_halo = sb(e1, [128, 8, 3])
            S.op("pool", lambda e: e.memset(halo[:, :, :], 0.0), writes=[b_halo])
            rw, b_rw = sb(e1, [128, 515])
            cacc, b_cacc = sb(e1, [128, 512])
            kT_st, b_kT_st = sb(e1, [128, 4, 512], BF16)
            qT_st, b_qT_st = sb(e1, [128, 4, 512], BF16)
            vext, b_vext = sb(e1, [128, 4, 4, 258], BF16)
            S.op("pool", lambda e: e.memset(vext[:, :, :, 256:257], 1.0), writes=[b_vext])
            S.op("pool", lambda e: e.memset(vext[:, :, :, 257:258], 0.0), writes=[b_vext])
            smo, b_smo = sb(e1, [128, 4, 1024], BF16)
            gI, b_gI = sb(e1, [4, 512]); gF, b_gF = sb(e1, [4, 512])
            gB, b_gB = sb(e1, [4, 512]); gMg, b_gMg = sb(e1, [4, 512])
            gm_sb, b_gm = sb(e1, [4, 2, 512])
            QS, b_QS = sb(e1, [128, 512])
            S.op("pool", lambda e: e.memset(QS[:, :], 0.0), writes=[b_QS])
            vmask, b_vmask = sb(e1, [128, 512])
            ones512, b_ones512 = sb(e1, [4, 512])
            S.op("pool", lambda e: e.memset(ones512[:, :], 1.0), writes=[b_ones512])
            carryB, b_cB = sb(e1, [4, 1]); carryM, b_cM = sb(e1, [4, 1])
            S.op("pool", lambda e: e.memset(carryB[:, :], 0.0), writes=[b_cB])
            S.op("pool", lambda e: e.memset(carryM[:, :], 0.0), writes=[b_cM])
            QT, b_QT = sb(e1, [128, 4, 128])
            NMGB, b_NMGB = sb(e1, [128, 4, 513])
            S.op("pool", lambda e: e.memset(NMGB[:, :, :], 0.0), writes=[b_NMGB])
            Cst, b_Cst = sb(e1, [128, 4, 258]); Cbf, b_Cbf = sb(e1, [128, 4, 258], BF16)
            S.op("pool", lambda e: e.memset(Cst[:, :, :], 0.0), writes=[b_Cst])
            S.op("pool", lambda e: e.memset(Cbf[:, :, :], 0.0), writes=[b_Cbf])
            sc_l = [sb(e1, [128, 16]) for _ in range(4)]
            kw_l = [sb(e1, [128, 128], BF16) for _ in range(4)]
            arg_l = [sb(e1, [128, 128]) for _ in range(4)]; Pt_l = [sb(e1, [128, 128], BF16) for _ in range(4)]
            intra_l = [sb(e1, [128, 258]) for _ in range(4)]; Rn_l = [sb(e1, [128, 258]) for _ in range(4)]
            hraw_l = [sb(e1, [128, 256]) for _ in range(4)]
            hm_t, b_hm_t = sb(e1, [128, 1024]); hm_h, b_hm_h = sb(e1, [128, 1024], BF16)
            hmT_c, b_hmT_c = sb(e1, [128, 8, 128], BF16)
            st2_l = [sb(e1, [128, 6]) for _ in range(4)]; mv2_l = [sb(e1, [128, 2]) for _ in range(4)]; sd2_l = [sb(e1, [128, 2]) for _ in range(4)]

            def wload(c0, ncols):
                t, b = wbuf[wi[0] % 2]
                wi[0] += 1
                S.dma("pool", t[:, :, 0:ncols], w_in[:, c0:c0 + ncols].rearrange("(k p) n -> p k n", p=128), writes=[b])
                return t, b

            def proj_fm(wt, bw, ncol_chunks, consume):
                for cc in range(ncol_chunks):
                    p_, bp = ps()
                    for kc in range(16):
                        S.op("pe", lambda e, kc=kc, cc=cc, p_=p_: e.matmul(p_[:, :], lhsT=wt[:, kc, 128 * cc:128 * cc + 128],
                                                                         rhs=hT_st[:, kc, :], start=(kc == 0), stop=(kc == 15)),
                             reads=[bw, b_hT_st], writes=[bp])
                    consume(cc, p_, bp)

            def proj_tm(wt, bw, ncols, consume):
                for tt in range(4):
                    p_, bp = ps()
                    for kc in range(16):
                        S.op("pe", lambda e, kc=kc, tt=tt, p_=p_: e.matmul(p_[:, 0:ncols], lhsT=hT_st[:, kc, 128 * tt:128 * tt + 128],
                                                                         rhs=wt[:, kc, 0:ncols], start=(kc == 0), stop=(kc == 15)),
                             reads=[bw, b_hT_st], writes=[bp])
                    consume(tt, p_, bp)

            for sti in range(8):
                own = sti >= 6
                t0 = 512 * sti
                for tt in range(4):
                    w0 = t0 + 128 * tt
                    xt, b_xt = xs[tt % 2]
                    xn, b_xn = xn_l[tt % 2]
                    (st, b_st), (mvt, b_mv), (sd, b_sd) = ln_l[tt % 2]
                    S.dma("sp", xt[:, :], x_win[w0:w0 + 128, :], writes=[b_xt])
                    layer_norm_stats(xt, b_xt, st, b_st, mvt, b_mv, sd, b_sd)
                    S.op("act", lambda e, xt=xt: e.activation(out=xn[:, :], in_=xt[:, :], func=AF.Identity,
                                                            scale=sd[:, 0:1], bias=sd[:, 1:2]),
                         reads=[b_xt, b_sd], writes=[b_xn])
                    if own:
                        o0 = w0 - 3072
                        S.dma("sp", h_d[o0:o0 + 128, :], xn[:, :], reads=[b_xn], writes=[b_h_d])
                    for g in range(4):
                        p_, bp = ps()
                        for j in range(4):
                            kc = 4 * g + j
                            S.op("pe", lambda e, kc=kc, j=j, p_=p_: e.transpose(out=p_[:, 128 * j:128 * j + 128],
                                                                              in_=xn[:, 128 * kc:128 * kc + 128], identity=idf[:, :]),
                                 reads=[b_xn, b_idf], writes=[bp])
                        for j in range(4):
                            kc = 4 * g + j
                            eng = evac_eng()
                            if eng == "act":
                                S.op("act", lambda e, kc=kc, j=j, p_=p_, tt=tt: e.activation(
                                    out=hT_st[:, kc, 128 * tt:128 * tt + 128], in_=p_[:, 128 * j:128 * j + 128], func=AF.Identity,
                                    scale=c_lng[:, kc:kc + 1], bias=c_lnb[:, kc:kc + 1]), reads=[bp, bC], writes=[b_hT_st])
                            else:
                                S.op("dve", lambda e, kc=kc, j=j, p_=p_, tt=tt: e.tensor_scalar(
                                    out=hT_st[:, kc, 128 * tt:128 * tt + 128], in0=p_[:, 128 * j:128 * j + 128],
                                    scalar1=c_lng[:, kc:kc + 1], scalar2=c_lnb[:, kc:kc + 1], op0=ALU.mult, op1=ALU.add),
                                     reads=[bp, bC], writes=[b_hT_st])
                if own:
                    o0 = t0 - 3072
                    S.dma("sp", hT_d[:, :, o0:o0 + 512].rearrange("k p t -> p k t"), hT_st[:, :, :], reads=[b_hT_st], writes=[b_hT_d])

                att_groups = [g for g in range(3) if t0 >= (1024 if g == 2 else 2560)]
                for g in att_groups:
                    wt, bw = wload(C_AK + 512 * g, 512)
                    def cons_k(cc, p_, bp, g=g):
                        copy(evac_eng(), stage_o[:, cc, :], p_[:, :], [bp], [b_stage_o])
                    proj_fm(wt, bw, 4, cons_k)
                    S.dma("sp", kT_d[4 * g:4 * g + 4, :, t0:t0 + 512].rearrange("h p t -> p h t"), stage_o[:, :, :],
                          reads=[b_stage_o], writes=[b_kT_d])
                    wt, bw = wload(C_AV + 512 * g, 512)
                    def cons_v(tt, p_, bp, g=g):
                        copy(evac_eng(), stage_t[:, tt, :], p_[:, :], [bp], [b_stage_t])
                    proj_tm(wt, bw, 512, cons_v)
                    S.dma("sp", v_d[t0:t0 + 512, 512 * g:512 * g + 512].rearrange("(t p) c -> p t c", p=128), stage_t[:, :, :],
                          reads=[b_stage_t], writes=[b_v_d])
                    if own:
                        wt, bw = wload(C_AQ + 512 * g, 512)
                        proj_fm(wt, bw, 4, cons_k)
                        o0 = t0 - 3072
                        S.dma("sp", qT_d[4 * g:4 * g + 4, :, o0:o0 + 512].rearrange("h p t -> p h t"), stage_o[:, :, :],
                              reads=[b_stage_o], writes=[b_qT_d])

                S.dma("sp", gm_sb[:, :, :], gmask[:, :, t0:t0 + 512], writes=[b_gm])
                S.dma("sp", vmask[:, :], valid_b[:, t0:t0 + 512], writes=[b_vmask])
                for which in ((1, 0) if sti >= 5 else (1,)):
                    wt, bw = wload(C_MK if which else C_MQ, 512)
                    dstT, b_dstT = (kT_st, b_kT_st) if which else (qT_st, b_qT_st)
                    def cons_c(cc, p_, bp, which=which, dstT=dstT, b_dstT=b_dstT):
                        ch = 4 * which + cc
                        S.op("pool", lambda e: e.tensor_copy(out=rw[:, 0:3], in_=halo[:, ch, :]), reads=[b_halo], writes=[b_rw])
                        S.op("dve", lambda e: e.tensor_tensor(out=rw[:, 3:515], in0=p_[:, :], in1=vmask[:, :], op=ALU.mult),
                             reads=[bp, b_vmask], writes=[b_rw])
                        S.op("dve", lambda e: e.tensor_scalar(out=cacc[:, :], in0=rw[:, 3:515], scalar1=c_cw[:, ch, 3:4],
                                                              scalar2=c_cb[:, ch:ch + 1], op0=ALU.mult, op1=ALU.add),
                             reads=[b_rw, bC1], writes=[b_cacc])
                        for j in range(3):
                            S.op("dve", lambda e, j=j: e.scalar_tensor_tensor(out=cacc[:, :], in0=rw[:, j:j + 512],
                                                                              scalar=c_cw[:, ch, j:j + 1], in1=cacc[:, :],
                                                                              op0=ALU.mult, op1=ALU.add),
                                 reads=[b_rw, bC1, b_cacc], writes=[b_cacc])
                        S.op("act", lambda e: e.activation(out=dstT[:, cc, :], in_=cacc[:, :], func=AF.Silu),
                             reads=[b_cacc], writes=[b_dstT])
                        S.op("pool", lambda e: e.tensor_copy(out=halo[:, ch, :], in_=rw[:, 512:515]), reads=[b_rw], writes=[b_halo])
                    proj_fm(wt, bw, 4, cons_c)
                for half in range(2):
                    wt, bw = wload(C_MV + 512 * half, 512)
                    def cons_mv(tt, p_, bp, half=half):
                        copy(evac_eng(), vext[:, tt, 2 * half:2 * half + 2, 0:256],
                             p_[:, :].rearrange("p (h c) -> p h c", c=256), [bp], [b_vext])
                    proj_tm(wt, bw, 512, cons_mv)
                if own:
                    for half in range(2):
                        wt, bw = wload(C_MO + 512 * half, 512)
                        def cons_mo(tt, p_, bp, half=half):
                            S.op("act", lambda e: e.activation(out=smo[:, tt, 512 * half:512 * half + 512], in_=p_[:, :], func=AF.Sigmoid),
                                 reads=[bp], writes=[b_smo])
                        proj_tm(wt, bw, 512, cons_mo)
                for gi_, (dst, b_dst) in enumerate(((gI, b_gI), (gF, b_gF))):
                    p_, bp = ps()
                    for kc in range(16):
                        S.op("pe", lambda e, kc=kc, p_=p_, gi_=gi_: e.matmul(p_[0:4, :], lhsT=wif[:, kc, 4 * gi_:4 * gi_ + 4],
                                                                           rhs=hT_st[:, kc, :], start=(kc == 0), stop=(kc == 15)),
                             reads=[b_wif, b_hT_st], writes=[bp])
                    S.op("dve", lambda e, p_=p_, gi_=gi_, dst=dst: e.scalar_tensor_tensor(
                        out=dst[:, :], in0=p_[0:4, :], scalar=c_ifb[:, gi_:gi_ + 1], in1=gm_sb[:, gi_, :], op0=ALU.add, op1=ALU.add),
                         reads=[bp, bC1, b_gm], writes=[b_dst])
                S.op("act", lambda e: e.activation(out=gF[:, :], in_=gF[:, :], func=AF.Exp, scale=-1.0), reads=[b_gF], writes=[b_gF])
                S.op("dve", lambda e: e.tensor_scalar(out=gF[:, :], in0=gF[:, :], scalar1=1.0, scalar2=None, op0=ALU.add),
                     reads=[b_gF], writes=[b_gF])
                S.op("act", lambda e: e.activation(out=gF[:, :], in_=gF[:, :], func=AF.Ln), reads=[b_gF], writes=[b_gF])
                S.op("dve", lambda e: e.tensor_tensor_scan(out=gB[:, :], data0=ones512[:, :], data1=gF[:, :], initial=carryB[:, 0:1],
                                                           op0=ALU.mult, op1=ALU.subtract),
                     reads=[b_gF, b_cB, b_ones512], writes=[b_gB])
                S.op("dve", lambda e: e.tensor_copy(out=carryB[:, :], in_=gB[:, 511:512]), reads=[b_gB], writes=[b_cB])
                S.op("dve", lambda e: e.tensor_tensor(out=QS[0:4, :], in0=gI[:, :], in1=gB[:, :], op=ALU.subtract),
                     reads=[b_gI, b_gB], writes=[b_QS])
                S.op("dve", lambda e: e.tensor_tensor_scan(out=gMg[:, :], data0=QS[0:4, :], data1=QS[0:4, :], initial=carryM[:, 0:1],
                                                           op0=ALU.max, op1=ALU.max),
                     reads=[b_QS, b_cM], writes=[b_gMg])
                S.op("dve", lambda e: e.tensor_copy(out=carryM[:, :], in_=gMg[:, 511:512]), reads=[b_gMg], writes=[b_cM])
                S.op("dve", lambda e: e.tensor_scalar(out=QS[32:36, :], in0=gMg[:, :], scalar1=-1.0, scalar2=None, op0=ALU.mult),
                     reads=[b_gMg], writes=[b_QS])
                S.op("dve", lambda e: e.tensor_tensor(out=gI[:, :], in0=gB[:, :], in1=gMg[:, :], op=ALU.add),
                     reads=[b_gB, b_gMg], writes=[b_gI])
                S.op("act", lambda e: e.activation(out=QS[64:68, :], in_=gI[:, :], func=AF.Exp, scale=-1.0), reads=[b_gI], writes=[b_QS])
                S.op("dve", lambda e: e.tensor_scalar(out=gF[:, :], in0=gMg[:, :], scalar1=-1.0, scalar2=None, op0=ALU.mult),
                     reads=[b_gMg], writes=[b_gF])
                S.op("dve", lambda e: e.tensor_copy(out=NMGB[:, :, 0:1], in_=NMGB[:, :, 512:513]), reads=[b_NMGB], writes=[b_NMGB])
                for hd in range(4):
                    p_, bp = ps()
                    S.op("pe", lambda e, hd=hd, p_=p_: e.matmul(p_[:, :], lhsT=c_sel4[:, hd, :], rhs=gF[:, :], start=True, stop=True),
                         reads=[b_gF, bC1], writes=[bp])
                    copy(evac_eng(), NMGB[:, hd, 1:513], p_[:, :], [bp], [b_NMGB])
                for c in range(4):
                    p_, bp = ps()
                    S.op("pe", lambda e, c=c, p_=p_: e.transpose(out=p_[:, 0:128], in_=QS[:, 128 * c:128 * c + 128], identity=idf[:, :]),
                         reads=[b_QS, b_idf], writes=[bp])
                    copy(evac_eng(), QT[:, c, :], p_[:, 0:128], [bp], [b_QT])

                HD = range(4)
                for c in range(4):
                    cs_ = slice(128 * c, 128 * c + 128)
                    def nmgL(hd): return NMGB[:, hd, 128 * c + 128:128 * c + 129]
                    def nmgS(hd): return NMGB[:, hd, 128 * c:128 * c + 1]
                    def Acol(hd): return QT[:, c, hd:hd + 1]
                    if own:
                        pS_l = {}; pI_l = {}; pX_l = {}
                        for hd in HD:
                            pS, bpS = ps(); pS_l[hd] = (pS, bpS)
                            S.op("pe", lambda e, pS=pS, hd=hd: e.matmul(pS[:, 0:128], lhsT=kT_st[:, hd, cs_], rhs=qT_st[:, hd, cs_], start=True, stop=True),
                                 reads=[b_kT_st, b_qT_st], writes=[bpS])
                        for hd in HD:
                            arg, b_arg = arg_l[hd]
                            S.op("dve", lambda e, hd=hd, arg=arg: e.scalar_tensor_tensor(
                                out=arg[:, :], in0=NMGB[:, hd, 128 * c + 1:128 * c + 129], scalar=Acol(hd), in1=c_tri[:, :],
                                op0=ALU.add, op1=ALU.add), reads=[b_NMGB, b_QT, bC1], writes=[b_arg])
                        for hd in HD:
                            arg, b_arg = arg_l[hd]
                            S.op("act", lambda e, arg=arg: e.activation(out=arg[:, :], in_=arg[:, :], func=AF.Exp), reads=[b_arg], writes=[b_arg])
                        for hd in HD:
                            arg, b_arg = arg_l[hd]; Pt, b_Pt = Pt_l[hd]; pS, bpS = pS_l[hd]
                            S.op("dve", lambda e, pS=pS, Pt=Pt, arg=arg: e.scalar_tensor_tensor(out=Pt[:, :], in0=pS[:, 0:128], scalar=128 ** -0.5,
                                                                                              in1=arg[:, :], op0=ALU.mult, op1=ALU.mult),
                                 reads=[bpS, b_arg], writes=[b_Pt])
                        for hd in HD:
                            Pt, b_Pt = Pt_l[hd]
                            pI, bpI = ps(); pI_l[hd] = (pI, bpI)
                            S.op("pe", lambda e, pI=pI, hd=hd, Pt=Pt: e.matmul(pI[:, 0:258], lhsT=Pt[:, :], rhs=vext[:, c, hd, :], start=True, stop=True),
                                 reads=[b_Pt, b_vext], writes=[bpI])
                        for hd in HD:
                            pX, bpX = ps(); pX_l[hd] = (pX, bpX)
                            S.op("pe", lambda e, pX=pX, hd=hd: e.matmul(pX[:, 0:258], lhsT=qT_st[:, hd, cs_], rhs=Cbf[:, hd, :], start=True, stop=True),
                                 reads=[b_qT_st, b_Cbf], writes=[bpX])
                        for hd in HD:
                            sc, b_sc = sc_l[hd]
                            S.op("dve", lambda e, hd=hd, sc=sc: e.tensor_scalar(out=sc[:, 0:1], in0=nmgS(hd), scalar1=-1.0, scalar2=None, op0=ALU.mult),
                                 reads=[b_NMGB], writes=[b_sc])
                        for hd in HD:
                            sc, b_sc = sc_l[hd]; intra, b_intra = intra_l[hd]; pI, bpI = pI_l[hd]
                            S.op("act", lambda e, pI=pI, intra=intra: e.copy(out=intra[:, :], in_=pI[:, 0:258]), reads=[bpI], writes=[b_intra])
                            S.op("act", lambda e, hd=hd, sc=sc: e.activation(out=sc[:, 1:2], in_=QT[:, c, 32 + hd:33 + hd], func=AF.Exp, bias=sc[:, 0:1]),
                                 reads=[b_QT, b_sc], writes=[b_sc])
                        for hd in HD:
                            sc, b_sc = sc_l[hd]
                            S.op("dve", lambda e, sc=sc: e.tensor_scalar(out=sc[:, 1:2], in0=sc[:, 1:2], scalar1=128 ** -0.5, scalar2=None, op0=ALU.mult),
                                 reads=[b_sc], writes=[b_sc])
                        for hd in HD:
                            sc, b_sc = sc_l[hd]; intra, b_intra = intra_l[hd]; Rn, b_Rn = Rn_l[hd]; pX, bpX = pX_l[hd]
                            S.op("dve", lambda e, pX=pX, sc=sc, intra=intra, Rn=Rn: e.scalar_tensor_tensor(out=Rn[:, :], in0=pX[:, 0:258], scalar=sc[:, 1:2],
                                                                                                         in1=intra[:, :], op0=ALU.mult, op1=ALU.add),
                                 reads=[bpX, b_sc, b_intra], writes=[b_Rn])
                        for hd in HD:
                            sc, b_sc = sc_l[hd]; Rn, b_Rn = Rn_l[hd]
                            S.op("dve", lambda e, sc=sc, Rn=Rn: e.tensor_scalar(out=sc[:, 3:4], in0=Rn[:, 256:257], scalar1=-1.0, scalar2=None, op0=ALU.mult),
                                 reads=[b_Rn], writes=[b_sc])
                        for hd in HD:
                            sc, b_sc = sc_l[hd]; Rn, b_Rn = Rn_l[hd]
                            S.op("dve", lambda e, sc=sc, Rn=Rn: e.tensor_tensor(out=sc[:, 2:3], in0=sc[:, 3:4], in1=Rn[:, 256:257], op=ALU.max),
                                 reads=[b_Rn, b_sc], writes=[b_sc])
                        for hd in HD:
                            sc, b_sc = sc_l[hd]
                            S.op("dve", lambda e, hd=hd, sc=sc: e.tensor_tensor(out=sc[:, 2:3], in0=sc[:, 2:3], in1=QT[:, c, 64 + hd:65 + hd], op=ALU.max),
                                 reads=[b_sc, b_QT], writes=[b_sc])
                        for hd in HD:
                            sc, b_sc = sc_l[hd]
                            S.op("dve", lambda e, sc=sc: e.reciprocal(out=sc[:, 2:3], in_=sc[:, 2:3]), reads=[b_sc], writes=[b_sc])
                        for hd in HD:
                            sc, b_sc = sc_l[hd]; Rn, b_Rn = Rn_l[hd]; hraw, b_hraw = hraw_l[hd]
                            S.op("dve", lambda e, sc=sc, Rn=Rn, hraw=hraw: e.tensor_scalar(out=hraw[:, :], in0=Rn[:, 0:256], scalar1=sc[:, 2:3], scalar2=None, op0=ALU.mult),
                                 reads=[b_Rn, b_sc], writes=[b_hraw])
                        for hd in HD:
                            hraw, b_hraw = hraw_l[hd]; st2, b_st2 = st2_l[hd]
                            S.op("dve", lambda e, hraw=hraw, st2=st2: e.bn_stats(out=st2[:, 0:6], in_=hraw[:, :]), reads=[b_hraw], writes=[b_st2])
                        for hd in HD:
                            st2, b_st2 = st2_l[hd]; mv2, b_mv2 = mv2_l[hd]
                            S.op("dve", lambda e, st2=st2, mv2=mv2: e.bn_aggr(out=mv2[:, 0:2], in_=st2[:, 0:6]), reads=[b_st2], writes=[b_mv2])
                        for hd in HD:
                            mv2, b_mv2 = mv2_l[hd]; sd2, b_sd2 = sd2_l[hd]
                            S.op("dve", lambda e, mv2=mv2, sd2=sd2: e.tensor_scalar(out=sd2[:, 0:1], in0=mv2[:, 1:2], scalar1=EPS, scalar2=None, op0=ALU.add),
                                 reads=[b_mv2], writes=[b_sd2])
                        for hd in HD:
                            sd2, b_sd2 = sd2_l[hd]
                            S.op("act", lambda e, sd2=sd2: e.activation(out=sd2[:, 0:1], in_=sd2[:, 0:1], func=AF.Sqrt), reads=[b_sd2], writes=[b_sd2])
                        for hd in HD:
                            sd2, b_sd2 = sd2_l[hd]
                            S.op("dve", lambda e, sd2=sd2: e.reciprocal(out=sd2[:, 0:1], in_=sd2[:, 0:1]), reads=[b_sd2], writes=[b_sd2])
                        for hd in HD:
                            mv2, b_mv2 = mv2_l[hd]; sd2, b_sd2 = sd2_l[hd]
                            S.op("dve", lambda e, mv2=mv2, sd2=sd2: e.tensor_scalar(out=sd2[:, 1:2], in0=mv2[:, 0:1], scalar1=sd2[:, 0:1], scalar2=-1.0,
                                                                                  op0=ALU.mult, op1=ALU.mult), reads=[b_mv2, b_sd2], writes=[b_sd2])
                        for hd in HD:
                            hraw, b_hraw = hraw_l[hd]; sd2, b_sd2 = sd2_l[hd]
                            S.op("act", lambda e, hraw=hraw, sd2=sd2: e.activation(out=hraw[:, :], in_=hraw[:, :], func=AF.Identity, scale=sd2[:, 0:1], bias=sd2[:, 1:2]),
                                 reads=[b_hraw, b_sd2], writes=[b_hraw])
                        for hd in HD:
                            hraw, b_hraw = hraw_l[hd]
                            S.op("dve", lambda e, hd=hd, hraw=hraw: e.tensor_tensor(out=hm_t[:, 256 * hd:256 * hd + 256], in0=hraw[:, :],
                                                                                  in1=c_mnw[:, 256 * hd:256 * hd + 256], op=ALU.mult),
                                 reads=[b_hraw, bC1], writes=[b_hm_t])
                    pK_l = {}; pU_l = {}
                    for hd in HD:
                        sc, b_sc = sc_l[hd]
                        S.op("act", lambda e, hd=hd, sc=sc: e.activation(out=sc[:, 4:5], in_=Acol(hd), func=AF.Exp, bias=nmgL(hd)),
                             reads=[b_QT, b_NMGB], writes=[b_sc])
                        S.op("act", lambda e, hd=hd, sc=sc: e.activation(out=sc[:, 5:6], in_=nmgS(hd), func=AF.Exp, scale=-1.0, bias=nmgL(hd)),
                             reads=[b_NMGB], writes=[b_sc])
                    for hd in HD:
                        pK, bpK = ps(); pKh = pK.bitcast(BF16); pK_l[hd] = (pKh, bpK)
                        S.op("pe", lambda e, pKh=pKh, hd=hd: e.transpose(out=pKh[:, 0:128], in_=kT_st[:, hd, cs_], identity=idh[:, :]),
                             reads=[b_kT_st, b_idh], writes=[bpK])
                    for hd in HD:
                        sc, b_sc = sc_l[hd]; kw, b_kw = kw_l[hd]; pKh, bpK = pK_l[hd]
                        S.op("dve", lambda e, pKh=pKh, sc=sc, kw=kw: e.tensor_scalar(out=kw[:, :], in0=pKh[:, 0:128], scalar1=sc[:, 4:5], scalar2=None, op0=ALU.mult),
                             reads=[bpK, b_sc], writes=[b_kw])
                    for hd in HD:
                        kw, b_kw = kw_l[hd]
                        pU, bpU = ps(); pU_l[hd] = (pU, bpU)
                        S.op("pe", lambda e, pU=pU, hd=hd, kw=kw: e.matmul(pU[:, 0:258], lhsT=kw[:, :], rhs=vext[:, c, hd, :], start=True, stop=True),
                             reads=[b_kw, b_vext], writes=[bpU])
                    for hd in HD:
                        sc, b_sc = sc_l[hd]; pU, bpU = pU_l[hd]
                        S.op("dve", lambda e, pU=pU, hd=hd, sc=sc: e.scalar_tensor_tensor(out=Cst[:, hd, :], in0=Cst[:, hd, :], scalar=sc[:, 5:6],
                                                                                        in1=pU[:, 0:258], op0=ALU.mult, op1=ALU.add),
                             reads=[b_Cst, b_sc, bpU], writes=[b_Cst])
                    if sti >= 5:
                        S.op("pool", lambda e: e.tensor_copy(out=Cbf[:, :, :], in_=Cst[:, :, :]), reads=[b_Cst], writes=[b_Cbf])
                    if own:
                        S.op("dve", lambda e, c=c: e.tensor_tensor(out=hm_t[:, :], in0=hm_t[:, :], in1=smo[:, c, :], op=ALU.mult),
                             reads=[b_hm_t, b_smo], writes=[b_hm_t])
                        o0 = t0 - 3072 + 128 * c
                        if DEBUG:
                            S.dma("sp", dbg["dbg_hm"][o0:o0 + 128, :], hm_t[:, :], reads=[b_hm_t], final=True)
                        S.op("act", lambda e: e.copy(out=hm_h[:, :], in_=hm_t[:, :]), reads=[b_hm_t], writes=[b_hm_h])
                        for half in range(2):
                            p_, bp = ps()
                            ph = p_.bitcast(BF16)
                            for j in range(4):
                                kc = 4 * half + j
                                S.op("pe", lambda e, ph=ph, kc=kc, j=j: e.transpose(out=ph[:, 128 * j:128 * j + 128],
                                                                                  in_=hm_h[:, 128 * kc:128 * kc + 128], identity=idh[:, :]),
                                     reads=[b_hm_h, b_idh], writes=[bp])
                            copy(evac_eng(), hmT_c[:, 4 * half:4 * half + 4, :],
                                 ph[:, 0:512].rearrange("p (k t) -> p k t", t=128), [bp], [b_hmT_c])
                        S.dma("sp", hmT_d[:, :, o0:o0 + 128].rearrange("k p t -> p k t"), hmT_c[:, :, :], reads=[b_hmT_c], writes=[b_hmT_d])
        S.barrier()
        if upto >= 2:
          with ExitStack() as e2:
            qT_sb, b_qT_sb = sb(e2, [128, 4, NOWN], BF16)
            kT_sb, b_kT_sb = sb(e2, [128, 4, 3072], BF16)
            pad_sb, b_pad_sb = sb(e2, [128, 3072])
            S.dma("sp", pad_sb[:, :], padneg_b[:, 1024:4096], writes=[b_pad_sb])
            ab_sb, b_ab_sb = sb(e2, [128, 4, 256])
            Vb = [sb(e2, [128, 2, 512], BF16) for _ in range(2)]
            S2_l = [sb(e2, [128, 256]) for _ in range(4)]; Pb_l = [sb(e2, [128, 256], BF16) for _ in range(4)]
            PT_l = [sb(e2, [128, 2, 128], BF16) for _ in range(4)]
            a_sc_l = [sb(e2, [128, 8]) for _ in range(4)]
            o_sb = [sb(e2, [128, 4, 128]) for _ in range(2)]
            l_sb = [sb(e2, [128, 4]) for _ in range(2)]
            ui = 0
            for g in range(3):
                r = (1, 4, 16)[g]
                w_lo = 1024 if g == 2 else 2560
                nq = min(128, 1024 // r)
                nk = 128 + nq
                nch = (1024 // r) // nq
                S.dma("sp", qT_sb[:, :, :], qT_d[4 * g:4 * g + 4, :, :].rearrange("h p t -> p h t"), reads=[b_qT_d], writes=[b_qT_sb])
                S.dma("sp", kT_sb[:, :, 0:4096 - w_lo], kT_d[4 * g:4 * g + 4, :, w_lo:4096].rearrange("h p t -> p h t"),
                      reads=[b_kT_d], writes=[b_kT_sb])
                S.dma("sp", ab_sb[:, :, :], abias[4 * g:4 * g + 4, :, :].rearrange("h p k -> p h k"), writes=[b_ab_sb])
                for p in range(r):
                    for ci in range(nch):
                        j0 = 3072 // r + ci * nq
                        wk0 = p + r * (j0 - 128)
                        kc0 = wk0 - w_lo
                        qc0 = p + r * j0 - 3072
                        vt, b_vt = Vb[ui % 2]
                        ot, b_ot = o_sb[ui % 2]
                        lt, b_lt = l_sb[ui % 2]
                        ui += 1
                        S.dma("sp", vt[:, 0, :], v_d[ss(wk0, 128, r), 512 * g:512 * g + 512], reads=[b_v_d], writes=[b_vt])
                        S.dma("sp", vt[0:nq, 1, :], v_d[ss(wk0 + 128 * r, nq, r), 512 * g:512 * g + 512], reads=[b_v_d], writes=[b_vt])
                        HD = range(4)
                        pS_l = {}; pT_l = {}; pO_l = {}
                        for hd in HD:
                            pS, bpS = ps(); pS_l[hd] = (pS, bpS)
                            S.op("pe", lambda e, pS=pS, hd=hd: e.matmul(pS[0:nq, 0:nk], lhsT=qT_sb[:, hd, ss(qc0, nq, r)],
                                                                      rhs=kT_sb[:, hd, ss(kc0, nk, r)], start=True, stop=True),
                                 reads=[b_qT_sb, b_kT_sb], writes=[bpS])
                        for hd in HD:
                            S2, b_S2 = S2_l[hd]; pS, bpS = pS_l[hd]
                            S.op("dve", lambda e, pS=pS, hd=hd, S2=S2: e.scalar_tensor_tensor(out=S2[0:nq, 0:nk], in0=pS[0:nq, 0:nk], scalar=128 ** -0.5,
                                                                                            in1=ab_sb[0:nq, hd, 0:nk], op0=ALU.mult, op1=ALU.add),
                                 reads=[bpS, b_ab_sb], writes=[b_S2])
                            S.op("pool", lambda e, S2=S2: e.tensor_tensor(out=S2[0:nq, 0:nk], in0=S2[0:nq, 0:nk], in1=pad_sb[0:nq, ss(wk0 - 1024, nk, r)], op=ALU.add),
                                 reads=[b_S2, b_pad_sb], writes=[b_S2])
                        for hd in HD:
                            S2, b_S2 = S2_l[hd]; a_sc, b_a_sc = a_sc_l[hd]
                            S.op("dve", lambda e, S2=S2, a_sc=a_sc: e.tensor_reduce(out=a_sc[0:nq, 1:2], in_=S2[0:nq, 0:nk], axis=AX.X, op=ALU.max, negate=True),
                                 reads=[b_S2], writes=[b_a_sc])
                        for hd in HD:
                            S2, b_S2 = S2_l[hd]; a_sc, b_a_sc = a_sc_l[hd]; Pb, b_Pb = Pb_l[hd]
                            S.op("act", lambda e, S2=S2, a_sc=a_sc, Pb=Pb: e.activation(out=Pb[0:nq, 0:nk], in_=S2[0:nq, 0:nk], func=AF.Exp, bias=a_sc[0:nq, 1:2],
                                                                                       accum_out=a_sc[0:nq, 2:3]), reads=[b_S2, b_a_sc], writes=[b_Pb, b_a_sc])
                        for hd in HD:
                            Pb, b_Pb = Pb_l[hd]
                            pT_, bpT = ps(); pTh = pT_.bitcast(BF16); pT_l[hd] = (pTh, bpT)
                            S.op("pe", lambda e, pTh=pTh, Pb=Pb: e.transpose(out=pTh[:, 0:nq], in_=Pb[0:nq, 0:128], identity=idh[0:nq, 0:nq]),
                                 reads=[b_Pb, b_idh], writes=[bpT])
                            S.op("pe", lambda e, pTh=pTh, Pb=Pb: e.transpose(out=pTh[0:nq, 128:128 + nq], in_=Pb[0:nq, 128:128 + nq], identity=idh[0:nq, 0:nq]),
                                 reads=[b_Pb, b_idh], writes=[bpT])
                        for hd in HD:
                            PT, b_PT = PT_l[hd]; pTh, bpT = pT_l[hd]
                            copy("act", PT[:, 0, 0:nq], pTh[:, 0:nq], [bpT], [b_PT])
                            copy("dve", PT[0:nq, 1, 0:nq], pTh[0:nq, 128:128 + nq], [bpT], [b_PT])
                        for hd in HD:
                            PT, b_PT = PT_l[hd]
                            pO, bpO = ps(); pO_l[hd] = (pO, bpO)
                            S.op("pe", lambda e, pO=pO, hd=hd, vt=vt, PT=PT: e.matmul(pO[0:nq, 0:128], lhsT=PT[:, 0, 0:nq], rhs=vt[:, 0, 128 * hd:128 * hd + 128],
                                                                                    start=True, stop=False), reads=[b_PT, b_vt], writes=[bpO])
                            S.op("pe", lambda e, pO=pO, hd=hd, vt=vt, PT=PT: e.matmul(pO[0:nq, 0:128], lhsT=PT[0:nq, 1, 0:nq], rhs=vt[0:nq, 1, 128 * hd:128 * hd + 128],
                                                                                    start=False, stop=True), reads=[b_PT, b_vt], writes=[bpO])
                        for hd in HD:
                            a_sc, b_a_sc = a_sc_l[hd]
                            S.op("dve", lambda e, a_sc=a_sc: e.reciprocal(out=a_sc[0:nq, 3:4], in_=a_sc[0:nq, 2:3]), reads=[b_a_sc], writes=[b_a_sc])
                            S.op("act", lambda e, a_sc=a_sc: e.activation(out=a_sc[0:nq, 4:5], in_=a_sc[0:nq, 2:3], func=AF.Ln), reads=[b_a_sc], writes=[b_a_sc])
                        for hd in HD:
                            a_sc, b_a_sc = a_sc_l[hd]; pO, bpO = pO_l[hd]
                            S.op("dve", lambda e, pO=pO, hd=hd, ot=ot, a_sc=a_sc: e.tensor_scalar(out=ot[0:nq, hd, :], in0=pO[0:nq, 0:128], scalar1=a_sc[0:nq, 3:4],
                                                                                                scalar2=None, op0=ALU.mult), reads=[bpO, b_a_sc], writes=[b_ot])
                            S.op("dve", lambda e, hd=hd, lt=lt, a_sc=a_sc: e.tensor_tensor(out=lt[0:nq, hd:hd + 1], in0=a_sc[0:nq, 4:5], in1=a_sc[0:nq, 1:2], op=ALU.subtract),
                                 reads=[b_a_sc], writes=[b_lt])
                        S.dma("sp", o_d[g, ss(qc0, nq, r), :], ot[0:nq, :, :].rearrange("p h d -> p (h d)"), reads=[b_ot], writes=[b_o_d])
                        S.dma("sp", lse_d[g, ss(qc0, nq, r), :], lt[0:nq, :], reads=[b_lt], writes=[b_lse_d])
            o3 = [sb(e2, [128, 3, 512]) for _ in range(2)]
            l3, b_l3 = sb(e2, [128, 3, 4]); e3, b_e3 = sb(e2, [128, 3, 4]); m_sc, b_m_sc = sb(e2, [128, 12])
            att_t, b_att_t = sb(e2, [128, 512]); att_h, b_att_h = sb(e2, [128, 512], BF16)
            attT_c, b_attT_c = sb(e2, [128, 4, 128], BF16)
            for t in range(8):
                ot3, b_ot3 = o3[t % 2]
                S.dma("sp", ot3[:, :, :], o_d[:, 128 * t:128 * t + 128, :].rearrange("g p c -> p g c"), reads=[b_o_d], writes=[b_ot3])
                S.dma("sp", l3[:, :, :], lse_d[:, 128 * t:128 * t + 128, :].rearrange("g p c -> p g c"), reads=[b_lse_d], writes=[b_l3])
                S.op("dve", lambda e: e.tensor_tensor(out=m_sc[:, 0:4], in0=l3[:, 0, :], in1=l3[:, 1, :], op=ALU.max), reads=[b_l3], writes=[b_m_sc])
                S.op("dve", lambda e: e.tensor_tensor(out=m_sc[:, 0:4], in0=m_sc[:, 0:4], in1=l3[:, 2, :], op=ALU.max), reads=[b_l3, b_m_sc], writes=[b_m_sc])
                for g in range(3):
                    S.op("dve", lambda e, g=g: e.tensor_tensor(out=e3[:, g, :], in0=l3[:, g, :], in1=m_sc[:, 0:4], op=ALU.subtract),
                         reads=[b_l3, b_m_sc], writes=[b_e3])
                S.op("act", lambda e: e.activation(out=e3[:, :, :], in_=e3[:, :, :], func=AF.Exp), reads=[b_e3], writes=[b_e3])
                S.op("dve", lambda e: e.tensor_tensor(out=m_sc[:, 4:8], in0=e3[:, 0, :], in1=e3[:, 1, :], op=ALU.add), reads=[b_e3], writes=[b_m_sc])
                S.op("dve", lambda e: e.tensor_tensor(out=m_sc[:, 4:8], in0=m_sc[:, 4:8], in1=e3[:, 2, :], op=ALU.add), reads=[b_e3, b_m_sc], writes=[b_m_sc])
                S.op("dve", lambda e: e.reciprocal(out=m_sc[:, 8:12], in_=m_sc[:, 4:8]), reads=[b_m_sc], writes=[b_m_sc])
                for g in range(3):
                    S.op("dve", lambda e, g=g: e.tensor_tensor(out=e3[:, g, :], in0=e3[:, g, :], in1=m_sc[:, 8:12], op=ALU.mult),
                         reads=[b_e3, b_m_sc], writes=[b_e3])
                for sl in range(4):
                    S.op("dve", lambda e, sl=sl, ot3=ot3: e.tensor_scalar(out=att_t[:, 128 * sl:128 * sl + 128], in0=ot3[:, 0, 128 * sl:128 * sl + 128],
                                                                        scalar1=e3[:, 0, sl:sl + 1], scalar2=None, op0=ALU.mult),
                         reads=[b_ot3, b_e3], writes=[b_att_t])
                    for g in (1, 2):
                        S.op("dve", lambda e, sl=sl, g=g, ot3=ot3: e.scalar_tensor_tensor(
                            out=att_t[:, 128 * sl:128 * sl + 128], in0=ot3[:, g, 128 * sl:128 * sl + 128], scalar=e3[:, g, sl:sl + 1],
                            in1=att_t[:, 128 * sl:128 * sl + 128], op0=ALU.mult, op1=ALU.add), reads=[b_ot3, b_e3, b_att_t], writes=[b_att_t])
                if DEBUG:
                    S.dma("sp", dbg["dbg_att"][128 * t:128 * t + 128, :], att_t[:, :], reads=[b_att_t], final=True)
                S.op("act", lambda e: e.copy(out=att_h[:, :], in_=att_t[:, :]), reads=[b_att_t], writes=[b_att_h])
                p_, bp = ps()
                ph = p_.bitcast(BF16)
                for j in range(4):
                    S.op("pe", lambda e, ph=ph, j=j: e.transpose(out=ph[:, 128 * j:128 * j + 128], in_=att_h[:, 128 * j:128 * j + 128], identity=idh[:, :]),
                         reads=[b_att_h, b_idh], writes=[bp])
                copy(evac_eng(), attT_c[:, :, :], ph[:, 0:512].rearrange("p (k t) -> p k t", t=128), [bp], [b_attT_c])
                S.dma("sp", attT_d[:, :, 128 * t:128 * t + 128].rearrange("k p t -> p k t"), attT_c[:, :, :], reads=[b_attT_c], writes=[b_attT_d])
          S.barrier()

        if upto >= 4:
          if True:
            with ExitStack() as e4a:
                mergedT, b_mergedT = sb(e4a, [128, 16, NOWN], BF16)
                hmT, b_hmT = sb(e4a, [128, 8, NOWN], BF16); attT, b_attT = sb(e4a, [128, 4, NOWN], BF16)
                S.dma("sp", hmT[:, :, :], hmT_d[:, :, :].rearrange("k p t -> p k t"), reads=[b_hmT_d], writes=[b_hmT])
                S.dma("sp", attT[:, :, :], attT_d[:, :, :].rearrange("k p t -> p k t"), reads=[b_attT_d], writes=[b_attT])
                hTo, b_hTo = sb(e4a, [128, 16, NOWN], BF16)
                S.dma("sp", hTo[:, :, :], hT_d[:, :, :].rearrange("k p t -> p k t"), reads=[b_hT_d], writes=[b_hTo])
                wpa, b_wpa = sb(e4a, [128, 4, D], BF16); wpm, b_wpm = sb(e4a, [128, 8, D], BF16)
                for kc in range(4):
                    S.dma("pool", wpa[:, kc, :], w_pa[128 * kc:128 * kc + 128, :], writes=[b_wpa])
                for kc in range(8):
                    S.dma("pool", wpm[:, kc, :], w_pm[128 * kc:128 * kc + 128, :], writes=[b_wpm])
                wg = [sb(e4a, [128, 2, 16, 256], BF16) for _ in range(2)]
                sg_l = [(sb(e4a, [128, 512]), sb(e4a, [128, 512])) for _ in range(2)]
                sgi = [0]
                for blk in range(8):
                    wgt, b_wgt = wg[blk % 2]
                    for a_, c0 in enumerate((C_GA, C_GM)):
                        S.dma("pool", wgt[:, a_, :, :], w_in[:, c0 + 256 * blk:c0 + 256 * blk + 256].rearrange("(k p) n -> p k n", p=128), writes=[b_wgt])
                    for sub in range(2):
                        cc = 2 * blk + sub
                        for th in range(2):
                            tk = slice(512 * th, 512 * th + 512)
                            pA, bpA = ps(); pM, bpM = ps(); pGA, bpGA = ps(); pGM, bpGM = ps()
                            (sga, b_sga), (sgm, b_sgm) = sg_l[sgi[0] % 2]; sgi[0] += 1
                            for kc in range(4):
                                S.op("pe", lambda e, kc=kc, pA=pA, cc=cc, tk=tk: e.matmul(pA[:, :], lhsT=wpa[:, kc, 128 * cc:128 * cc + 128], rhs=attT[:, kc, tk],
                                                                                       start=(kc == 0), stop=(kc == 3)), reads=[b_wpa, b_attT], writes=[bpA])
                            for kc in range(8):
                                S.op("pe", lambda e, kc=kc, pM=pM, cc=cc, tk=tk: e.matmul(pM[:, :], lhsT=wpm[:, kc, 128 * cc:128 * cc + 128], rhs=hmT[:, kc, tk],
                                                                                       start=(kc == 0), stop=(kc == 7)), reads=[b_wpm, b_hmT], writes=[bpM])
                            for a_, (pG, bpG) in enumerate(((pGA, bpGA), (pGM, bpGM))):
                                for kc in range(16):
                                    S.op("pe", lambda e, kc=kc, pG=pG, a_=a_, sub=sub, tk=tk, wgt=wgt: e.matmul(
                                        pG[:, :], lhsT=wgt[:, a_, kc, 128 * sub:128 * sub + 128], rhs=hTo[:, kc, tk], start=(kc == 0), stop=(kc == 15)),
                                         reads=[b_wgt, b_hTo], writes=[bpG])
                            S.op("act", lambda e, pGA=pGA: e.activation(out=sga[:, :], in_=pGA[:, :], func=AF.Sigmoid), reads=[bpGA], writes=[b_sga])
                            S.op("act", lambda e, pGM=pGM: e.activation(out=sgm[:, :], in_=pGM[:, :], func=AF.Sigmoid), reads=[bpGM], writes=[b_sgm])
                            S.op("dve", lambda e, pA=pA: e.tensor_tensor(out=sga[:, :], in0=sga[:, :], in1=pA[:, :], op=ALU.mult), reads=[b_sga, bpA], writes=[b_sga])
                            S.op("dve", lambda e, pM=pM: e.tensor_tensor(out=sgm[:, :], in0=sgm[:, :], in1=pM[:, :], op=ALU.mult), reads=[b_sgm, bpM], writes=[b_sgm])
                            S.op("dve", lambda e, cc=cc, tk=tk: e.tensor_tensor(out=mergedT[:, cc, tk], in0=sga[:, :], in1=sgm[:, :], op=ALU.add),
                                 reads=[b_sga, b_sgm], writes=[b_mergedT])
                S.dma("sp", mT_d[:, :, :].rearrange("k p t -> p k t"), mergedT[:, :, :], reads=[b_mergedT], writes=[b_mT_d])
            S.barrier()
            with ExitStack() as e5:
                mergedT, b_mergedT = sb(e5, [128, 16, NOWN], BF16)
                S.dma("sp", mergedT[:, :, :], mT_d[:, :, :].rearrange("k p t -> p k t"), reads=[b_mT_d], writes=[b_mergedT])
                h1c, b_h1c = sb(e5, [128, D], BF16)
                wo, b_wo = sb(e5, [128, 16, D], BF16)
                for kc in range(16):
                    S.dma("pool", wo[:, kc, :], w_out[128 * kc:128 * kc + 128, :], writes=[b_wo])
                cA, b_cA = sb(e5, [128, D]); cB, b_cB = sb(e5, [128, D])
                cG1, b_cG1 = sb(e5, [128, D]); cB1, b_cB1 = sb(e5, [128, D])
                S.dma("sp", cA[:, :], lng_b, writes=[b_cA]); S.dma("sp", cB[:, :], lnb_b, writes=[b_cB])
                S.dma("sp", cG1[:, :], ln1g_b, writes=[b_cG1]); S.dma("sp", cB1[:, :], ln1b_b, writes=[b_cB1])
                h1t, b_h1t = None, None
                c_wr, b_c_wr = sb(e5, [128, 16, 36]); c_br, b_c_br = sb(e5, [128, 36])
                S.dma("sp", c_wr[:, :, :], wr, writes=[b_c_wr]); S.dma("sp", c_br[:, :], br_b, writes=[b_c_br])
                xz_l = [(sb(e5, [128, D]), sb(e5, [128, D])) for _ in range(2)]
                h1T, b_h1T = sb(e5, [128, 16, 128])
                st, b_st = sb(e5, [128, 24]); mvt, b_mv = sb(e5, [128, 2]); sd, b_sd = sb(e5, [128, 2])
                lg, b_lg = sb(e5, [128, 36]); r_sc, b_r_sc = sb(e5, [128, 16]); goh, b_goh = sb(e5, [128, 4])
                esel, b_esel = sb(e5, [128, 8]); top8, b_top8 = sb(e5, [128, 8]); oh, b_oh = sb(e5, [128, 2, 8]); wsel, b_wsel = sb(e5, [128, 8])
                for t in range(8):
                    tk = slice(128 * t, 128 * t + 128)
                    (xnt, b_xnt), (z, b_z) = xz_l[t % 2]
                    h1t, b_h1t = z, b_z
                    S.dma("sp", xnt[:, :], h_d[tk, :], reads=[b_h_d], writes=[b_xnt])
                    S.op("dve", lambda e: e.tensor_tensor(out=xnt[:, :], in0=xnt[:, :], in1=cA[:, :], op=ALU.mult), reads=[b_xnt, b_cA], writes=[b_xnt])
                    S.op("dve", lambda e: e.tensor_tensor(out=xnt[:, :], in0=xnt[:, :], in1=cB[:, :], op=ALU.add), reads=[b_xnt, b_cB], writes=[b_xnt])
                    for cb_ in range(4):
                        cs = slice(512 * cb_, 512 * cb_ + 512)
                        pY, bpY = ps()
                        for kc in range(16):
                            S.op("pe", lambda e, kc=kc, pY=pY, cs=cs, tk=tk: e.matmul(pY[:, :], lhsT=mergedT[:, kc, tk], rhs=wo[:, kc, cs],
                                                                                   start=(kc == 0), stop=(kc == 15)), reads=[b_mergedT, b_wo], writes=[bpY])
                        S.op("dve", lambda e, pY=pY, cs=cs: e.scalar_tensor_tensor(out=z[:, cs], in0=xnt[:, cs], scalar=ALPHA, in1=pY[:, :],
                                                                                 op0=ALU.mult, op1=ALU.add), reads=[b_xnt, bpY], writes=[b_z])
                    layer_norm_stats(z, b_z, st, b_st, mvt, b_mv, sd, b_sd)
                    S.op("act", lambda e: e.activation(out=z[:, :], in_=z[:, :], func=AF.Identity, scale=sd[:, 0:1], bias=sd[:, 1:2]),
                         reads=[b_z, b_sd], writes=[b_z])
                    S.op("dve", lambda e: e.tensor_tensor(out=z[:, :], in0=z[:, :], in1=cG1[:, :], op=ALU.mult), reads=[b_z, b_cG1], writes=[b_z])
                    S.op("dve", lambda e: e.tensor_tensor(out=h1t[:, :], in0=h1t[:, :], in1=cB1[:, :], op=ALU.add), reads=[b_h1t, b_cB1], writes=[b_h1t])
                    S.dma("sp", h1_d[tk, :], h1t[:, :], reads=[b_h1t], writes=[b_h1_d])
                    if DEBUG:
                        S.dma("sp", dbg["dbg_h1"][tk, :], h1t[:, :], reads=[b_h1t], final=True)
                    S.op("act", lambda e: e.copy(out=h1c[:, :], in_=h1t[:, :]), reads=[b_h1t], writes=[b_h1c])
                    S.dma("sp", h1bf_d[tk, :], h1c[:, :], reads=[b_h1c], writes=[b_h1bf_d])
                    for g4 in range(4):
                        p_, bp = ps()
                        for j in range(4):
                            kc = 4 * g4 + j
                            S.op("pe", lambda e, p_=p_, kc=kc, j=j: e.transpose(out=p_[:, 128 * j:128 * j + 128], in_=h1t[:, 128 * kc:128 * kc + 128], identity=idf[:, :]),
                                 reads=[b_h1t, b_idf], writes=[bp])
                        copy(evac_eng(), h1T[:, 4 * g4:4 * g4 + 4, :], p_[:, :].rearrange("p (k t) -> p k t", t=128), [bp], [b_h1T])
                    pL, bpL = ps()
                    for kc in range(16):
                        S.op("pe", lambda e, kc=kc, pL=pL: e.matmul(pL[:, 0:36], lhsT=h1T[:, kc, :], rhs=c_wr[:, kc, :], start=(kc == 0), stop=(kc == 15)),
                             reads=[b_h1T, b_c_wr], writes=[bpL])
                    S.op("dve", lambda e, pL=pL: e.tensor_tensor(out=lg[:, :], in0=pL[:, 0:36], in1=c_br[:, :], op=ALU.add), reads=[bpL, b_c_br], writes=[b_lg])
                    S.op("dve", lambda e: e.tensor_reduce(out=r_sc[:, 0:1], in_=lg[:, 0:4], axis=AX.X, op=ALU.max), reads=[b_lg], writes=[b_r_sc])
                    S.op("dve", lambda e: e.tensor_scalar(out=goh[:, :], in0=lg[:, 0:4], scalar1=r_sc[:, 0:1], scalar2=None, op0=ALU.is_equal),
                         reads=[b_lg, b_r_sc], writes=[b_goh])
                    S.op("dve", lambda e: e.tensor_scalar(out=r_sc[:, 1:2], in0=r_sc[:, 0:1], scalar1=-1.0, scalar2=None, op0=ALU.mult), reads=[b_r_sc], writes=[b_r_sc])
                    S.op("act", lambda e: e.activation(out=wsel[:, 0:4], in_=lg[:, 0:4], func=AF.Exp, bias=r_sc[:, 1:2], accum_out=r_sc[:, 2:3]),
                         reads=[b_lg, b_r_sc], writes=[b_wsel, b_r_sc])
                    S.op("dve", lambda e: e.reciprocal(out=r_sc[:, 3:4], in_=r_sc[:, 2:3]), reads=[b_r_sc], writes=[b_r_sc])
                    S.op("dve", lambda e: e.tensor_scalar(out=esel[:, :], in0=lg[:, 4:12], scalar1=goh[:, 0:1], scalar2=None, op0=ALU.mult),
                         reads=[b_lg, b_goh], writes=[b_esel])
                    for g in (1, 2, 3):
                        S.op("dve", lambda e, g=g: e.scalar_tensor_tensor(out=esel[:, :], in0=lg[:, 4 + 8 * g:12 + 8 * g], scalar=goh[:, g:g + 1], in1=esel[:, :],
                                                                          op0=ALU.mult, op1=ALU.add), reads=[b_lg, b_goh, b_esel], writes=[b_esel])
                    S.op("dve", lambda e: e.max(out=top8[:, :], in_=esel[:, :]), reads=[b_esel], writes=[b_top8])
                    S.op("dve", lambda e: e.tensor_tensor(out=r_sc[:, 4:5], in0=top8[:, 1:2], in1=top8[:, 0:1], op=ALU.subtract), reads=[b_top8], writes=[b_r_sc])
                    S.op("act", lambda e: e.activation(out=r_sc[:, 5:6], in_=r_sc[:, 4:5], func=AF.Exp), reads=[b_r_sc], writes=[b_r_sc])
                    S.op("dve", lambda e: e.tensor_scalar(out=r_sc[:, 6:7], in0=r_sc[:, 5:6], scalar1=1.0, scalar2=None, op0=ALU.add), reads=[b_r_sc], writes=[b_r_sc])
                    S.op("dve", lambda e: e.reciprocal(out=r_sc[:, 6:7], in_=r_sc[:, 6:7]), reads=[b_r_sc], writes=[b_r_sc])
                    S.op("dve", lambda e: e.tensor_tensor(out=r_sc[:, 7:8], in0=r_sc[:, 5:6], in1=r_sc[:, 6:7], op=ALU.mult), reads=[b_r_sc], writes=[b_r_sc])
                    S.op("dve", lambda e: e.tensor_scalar(out=r_sc[:, 6:8], in0=r_sc[:, 6:8], scalar1=r_sc[:, 3:4], scalar2=None, op0=ALU.mult),
                         reads=[b_r_sc], writes=[b_r_sc])
                    for j in range(2):
                        S.op("dve", lambda e, j=j: e.tensor_scalar(out=oh[:, j, :], in0=esel[:, :], scalar1=top8[:, j:j + 1], scalar2=None, op0=ALU.is_equal),
                             reads=[b_esel, b_top8], writes=[b_oh])
                    S.op("dve", lambda e: e.tensor_scalar(out=wsel[:, :], in0=oh[:, 0, :], scalar1=r_sc[:, 6:7], scalar2=None, op0=ALU.mult),
                         reads=[b_oh, b_r_sc], writes=[b_wsel])
                    S.op("dve", lambda e: e.scalar_tensor_tensor(out=wsel[:, :], in0=oh[:, 1, :], scalar=r_sc[:, 7:8], in1=wsel[:, :], op0=ALU.mult, op1=ALU.add),
                         reads=[b_oh, b_r_sc, b_wsel], writes=[b_wsel])
                    for g in range(4):
                        S.op("dve", lambda e, g=g, t=t: e.tensor_scalar(out=Wt_all[:, t, 8 * g:8 * g + 8], in0=wsel[:, :], scalar1=goh[:, g:g + 1], scalar2=None, op0=ALU.mult),
                             reads=[b_wsel, b_goh], writes=[b_Wt])
                    S.op("dve", lambda e, t=t: e.tensor_scalar(out=mk_all[:, t, :], in0=Wt_all[:, t, :], scalar1=0.0, scalar2=None, op0=ALU.is_gt),
                         reads=[b_Wt], writes=[b_mk])
                c_ones, b_c_ones = sb(e5, [128, 128]); c_ltri, b_c_ltri = sb(e5, [128, 128])
                S.dma("sp", c_ones[:, :], ones_f, writes=[b_c_ones]); S.dma("sp", c_ltri[:, :], ltri, writes=[b_c_ltri])
                for t in range(8):
                    pC, bpC = ps()
                    for t2 in range(t):
                        S.op("pe", lambda e, pC=pC, t2=t2: e.matmul(pC[:, 0:32], lhsT=c_ones[:, :], rhs=mk_all[:, t2, :], start=(t2 == 0), stop=False),
                             reads=[b_c_ones, b_mk], writes=[bpC])
                    S.op("pe", lambda e, pC=pC, t=t: e.matmul(pC[:, 0:32], lhsT=c_ltri[:, :], rhs=mk_all[:, t, :], start=(t == 0), stop=True),
                         reads=[b_c_ltri, b_mk], writes=[bpC])
                    S.op("dve", lambda e, pC=pC, t=t: e.tensor_tensor(out=cumm[:, t, :], in0=pC[:, 0:32], in1=mk_all[:, t, :], op=ALU.mult),
                         reads=[bpC, b_mk], writes=[b_cumm])
          S.barrier()

        if upto >= 6:
          with ExitStack() as e6:
            y_acc, b_y_acc = sb(e6, [128, 8, D])
            S.op("pool", lambda e: e.memset(y_acc[:, :, :], 0.0), writes=[b_y_acc])
            e6a = ExitStack()
            e6_outer = e6
            e6 = e6a
            h1_bf, b_h1_bf = sb(e6, [128, 8, D], BF16)
            S.dma("sp", h1_bf[:, :, :], h1bf_d.rearrange("(t p) c -> p t c", p=128), reads=[b_h1bf_d], writes=[b_h1_bf])
            c_iota, b_c_iota = sb(e6, [128, 128])
            S.dma("sp", c_iota[:, :], iota1, writes=[b_c_iota])
            wgu = [sb(e6, [128, 16, 512], BF16) for _ in range(3)]
            wdn = [sb(e6, [128, 11, 512], BF16) for _ in range(2)]
            Sel, b_Sel = sb(e6, [128, 8, 128], BF16); SelT, b_SelT = sb(e6, [128, NOWN], BF16)
            XeT, b_XeT = sb(e6, [128, 16, 128], BF16)
            sg, b_sg = sb(e6, [128, DFF]); Hs, b_Hs = sb(e6, [128, DFF], BF16)
            HT, b_HT = sb(e6, [128, 11, 128], BF16); Yb, b_Yb = sb(e6, [128, D], BF16)
            gi_ = [0]; di_ = [0]
            for ex in range(NEXP):
                for t in range(8):
                    S.op("dve", lambda e, t=t, ex=ex: e.tensor_scalar(out=Sel[:, t, :], in0=c_iota[:, :], scalar1=cumm[:, t, ex:ex + 1], scalar2=None, op0=ALU.is_equal),
                         reads=[b_c_iota, b_cumm], writes=[b_Sel])
                pB, bpB = ps()
                pBh = pB.bitcast(BF16)
                for t in range(8):
                    S.op("pe", lambda e, pBh=pBh, t=t: e.transpose(out=pBh[:, 128 * t:128 * t + 128], in_=Sel[:, t, :], identity=idh[:, :]),
                         reads=[b_Sel, b_idh], writes=[bpB])
                copy(evac_eng(), SelT[:, :], pBh[:, :], [bpB], [b_SelT])
                for g4 in range(4):
                    pX_, bpX_ = ps()
                    for j in range(4):
                        kc = 4 * g4 + j
                        for t in range(8):
                            S.op("pe", lambda e, pX_=pX_, kc=kc, j=j, t=t: e.matmul(pX_[:, 128 * j:128 * j + 128], lhsT=h1_bf[:, t, 128 * kc:128 * kc + 128], rhs=Sel[:, t, :],
                                                                                 start=(t == 0), stop=(t == 7)), reads=[b_h1_bf, b_Sel], writes=[bpX_])
                    copy(evac_eng(), XeT[:, 4 * g4:4 * g4 + 4, :], pX_[:, :].rearrange("p (k t) -> p k t", t=128), [bpX_], [b_XeT])
                for which, wsrc in enumerate((w_gate, w_up)):
                    for blk in range(3):
                        c0 = 512 * blk
                        ncol = min(512, DFF - c0)
                        wt_, bw_ = wgu[gi_[0] % 3]; gi_[0] += 1
                        S.dma("pool", wt_[:, :, 0:ncol], wsrc[ex, :, c0:c0 + ncol].rearrange("(k p) n -> p k n", p=128), writes=[bw_])
                        pG, bpG = ps()
                        for kc in range(16):
                            S.op("pe", lambda e, pG=pG, wt_=wt_, kc=kc: e.matmul(pG[:, 0:ncol], lhsT=XeT[:, kc, :], rhs=wt_[:, kc, 0:ncol], start=(kc == 0), stop=(kc == 15)),
                                 reads=[b_XeT, bw_], writes=[bpG])
                        if which == 0:
                            S.op("act", lambda e, pG=pG, c0=c0: e.activation(out=sg[:, c0:c0 + ncol], in_=pG[:, 0:ncol], func=AF.Silu), reads=[bpG], writes=[b_sg])
                        else:
                            S.op("dve", lambda e, pG=pG, c0=c0: e.tensor_tensor(out=Hs[:, c0:c0 + ncol], in0=sg[:, c0:c0 + ncol], in1=pG[:, 0:ncol], op=ALU.mult),
                                 reads=[b_sg, bpG], writes=[b_Hs])
                for g3 in range(3):
                    pH, bpH = ps()
                    pHh = pH.bitcast(BF16)
                    nj = min(4, 11 - 4 * g3)
                    for j in range(nj):
                        fc = 4 * g3 + j
                        S.op("pe", lambda e, pHh=pHh, j=j, fc=fc: e.transpose(out=pHh[:, 128 * j:128 * j + 128], in_=Hs[:, 128 * fc:128 * fc + 128], identity=idh[:, :]),
                             reads=[b_Hs, b_idh], writes=[bpH])
                    copy(evac_eng(), HT[:, 4 * g3:4 * g3 + nj, :], pHh[:, 0:128 * nj].rearrange("p (k t) -> p k t", t=128), [bpH], [b_HT])
                for cb_ in range(4):
                    cs = slice(512 * cb_, 512 * cb_ + 512)
                    wd_, bwd_ = wdn[di_[0] % 2]; di_[0] += 1
                    S.dma("pool", wd_[:, :, :], w_down[ex, :, cs].rearrange("(k p) n -> p k n", p=128), writes=[bwd_])
                    pY, bpY = ps()
                    for fc in range(11):
                        S.op("pe", lambda e, pY=pY, fc=fc, wd_=wd_: e.matmul(pY[:, :], lhsT=HT[:, fc, :], rhs=wd_[:, fc, :], start=(fc == 0), stop=(fc == 10)),
                             reads=[b_HT, bwd_], writes=[bpY])
                    copy(evac_eng(), Yb[:, cs], pY[:, :], [bpY], [b_Yb])
                for t in range(8):
                    for cb_ in range(4):
                        cs = slice(512 * cb_, 512 * cb_ + 512)
                        pZ, bpZ = ps()
                        S.op("pe", lambda e, pZ=pZ, t=t, cs=cs: e.matmul(pZ[:, :], lhsT=SelT[:, 128 * t:128 * t + 128], rhs=Yb[:, cs], start=True, stop=True),
                             reads=[b_SelT, b_Yb], writes=[bpZ])
                        S.op("dve", lambda e, pZ=pZ, t=t, cs=cs, ex=ex: e.scalar_tensor_tensor(out=y_acc[:, t, cs], in0=pZ[:, :], scalar=Wt_all[:, t, ex:ex + 1],
                                                                                           in1=y_acc[:, t, cs], op0=ALU.mult, op1=ALU.add),
                             reads=[bpZ, b_Wt, b_y_acc], writes=[b_y_acc])
            e6a.close()
            e6 = e6_outer
            S.barrier()
            cG2, b_cG2 = sb(e6, [128, D]); cB2, b_cB2 = sb(e6, [128, D])
            S.dma("sp", cG2[:, :], ln2g_b, writes=[b_cG2]); S.dma("sp", cB2[:, :], ln2b_b, writes=[b_cB2])
            h1r = [sb(e6, [128, D]) for _ in range(2)]
            st, b_st = sb(e6, [128, 24]); mvt, b_mv = sb(e6, [128, 2]); sd, b_sd = sb(e6, [128, 2])
            for t in range(8):
                tk = slice(128 * t, 128 * t + 128)
                ht_, b_ht_ = h1r[t % 2]
                if DEBUG:
                    S.dma("sp", dbg["dbg_y2"][tk, :], y_acc[:, t, :], reads=[b_y_acc], final=True)
                S.dma("sp", ht_[:, :], h1_d[tk, :], reads=[b_h1_d], writes=[b_ht_])
                S.op("dve", lambda e, t=t, ht_=ht_: e.scalar_tensor_tensor(out=ht_[:, :], in0=ht_[:, :], scalar=ALPHA, in1=y_acc[:, t, :], op0=ALU.mult, op1=ALU.add),
                     reads=[b_ht_, b_y_acc], writes=[b_ht_])
                layer_norm_stats(ht_, b_ht_, st, b_st, mvt, b_mv, sd, b_sd)
                S.op("act", lambda e, ht_=ht_: e.activation(out=ht_[:, :], in_=ht_[:, :], func=AF.Identity, scale=sd[:, 0:1], bias=sd[:, 1:2]),
                     reads=[b_ht_, b_sd], writes=[b_ht_])
                S.op("dve", lambda e, ht_=ht_: e.tensor_tensor(out=ht_[:, :], in0=ht_[:, :], in1=cG2[:, :], op=ALU.mult), reads=[b_ht_, b_cG2], writes=[b_ht_])
                S.op("dve", lambda e, ht_=ht_: e.tensor_tensor(out=ht_[:, :], in0=ht_[:, :], in1=cB2[:, :], op=ALU.add), reads=[b_ht_, b_cB2], writes=[b_ht_])
                S.dma("sp", out_d[tk, :], ht_[:, :], reads=[b_ht_], final=True)
        S.barrier()
        S.finish()
    return nc


def _bcast(v, n=128):
    v = np.asarray(v, np.float32).reshape(1, -1)
    return np.ascontiguousarray(np.broadcast_to(v, (n, v.shape[1])))


def prep_inputs(inp):
    f32 = np.float32
    x = np.asarray(inp["x"], f32)
    shared = {}
    shared["w_in"] = np.ascontiguousarray(np.asarray(inp["w_in"], f32)[0])
    g = np.asarray(inp["ln_in_g"], f32); b = np.asarray(inp["ln_in_b"], f32)
    shared["lng_col"] = np.ascontiguousarray(g.reshape(16, 128).T); shared["lnb_col"] = np.ascontiguousarray(b.reshape(16, 128).T)
    shared["lng_b"] = _bcast(g); shared["lnb_b"] = _bcast(b)
    cwv = np.asarray(inp["m_conv_w"], f32)[0]
    shared["cw"] = np.ascontiguousarray(cwv.reshape(4, 8, 128).transpose(2, 1, 0))
    shared["cb"] = np.ascontiguousarray(np.asarray(inp["m_conv_b"], f32)[0].reshape(8, 128).T)
    shared["ifb"] = np.ascontiguousarray(np.asarray(inp["m_if_bias"], f32)[0].reshape(2, 4).T)
    shared["mnw_b"] = _bcast(np.asarray(inp["m_norm_w"], f32)[0].reshape(-1))
    shared["w_pa"] = np.ascontiguousarray(np.asarray(inp["w_proj_att"], f32)[0])
    shared["w_pm"] = np.ascontiguousarray(np.asarray(inp["w_proj_mlstm"], f32)[0])
    shared["w_out"] = np.ascontiguousarray(np.asarray(inp["w_out"], f32)[0])
    shared["ln1g_b"] = _bcast(np.asarray(inp["ln1_g"], f32)[0]); shared["ln1b_b"] = _bcast(np.asarray(inp["ln1_b"], f32)[0])
    shared["ln2g_b"] = _bcast(np.asarray(inp["ln2_g"], f32)[0]); shared["ln2b_b"] = _bcast(np.asarray(inp["ln2_b"], f32)[0])
    wrc = np.concatenate([np.asarray(inp["w_router_group"], f32)[0], np.asarray(inp["w_router_expert"], f32)[0]], axis=1)
    shared["wr"] = np.ascontiguousarray(wrc.reshape(16, 128, 36).transpose(1, 0, 2))
    shared["br_b"] = _bcast(np.concatenate([np.asarray(inp["b_router_group"], f32)[0], np.asarray(inp["b_router_expert"], f32)[0]]))
    shared["w_gate"] = np.ascontiguousarray(np.asarray(inp["w_gate"], f32)[0])
    shared["w_up"] = np.ascontiguousarray(np.asarray(inp["w_up"], f32)[0])
    shared["w_down"] = np.ascontiguousarray(np.asarray(inp["w_down"], f32)[0])
    slopes = alibi_slopes(12)
    qi = np.arange(128)[:, None]; ki = np.arange(256)[None, :]
    delta = 128 + qi - ki
    ab = np.zeros((12, 128, 256), f32)
    for h in range(12):
        r = (1, 4, 16)[h // 4]
        ab[h] = np.where((delta >= 0) & (delta <= 128), -slopes[h] * (delta * r).astype(f32), NEG)
    shared["abias"] = ab
    shared["ident_f"] = np.eye(128, dtype=f32)
    shared["ident_h"] = np.eye(128, dtype=f32).astype(ml_dtypes.bfloat16)
    s_ = np.arange(128)[:, None]; l_ = np.arange(128)[None, :]
    shared["tri_neg"] = np.where(s_ <= l_, 0.0, NEG).astype(f32)
    shared["ltri"] = (s_ <= l_).astype(f32)
    shared["ones_f"] = np.ones((128, 128), f32)
    shared["iota1"] = np.ascontiguousarray(np.broadcast_to(np.arange(1, 129, dtype=f32)[None, :], (128, 128)))
    shared["iota1c"] = np.arange(1, 129, dtype=f32).reshape(128, 1)
    sel4 = np.zeros((4, 4, 128), f32)
    for k in range(4):
        sel4[k, k, :] = 1.0
    shared["sel4"] = sel4
    sel32 = np.zeros((32, 32, 128), f32)
    for k in range(32):
        sel32[k, k, :] = 1.0
    shared["sel32"] = sel32
    in_maps = []
    for c in range(8):
        b_, k_ = c // 4, c % 4
        base = 1024 * k_ - 3072
        xw = np.zeros((NW, D), f32)
        lo = max(0, -base)
        xw[lo:] = x[b_, base + lo: base + NW]
        valid = (np.arange(NW) + base >= 0).astype(f32)
        m = dict(shared)
        m["x_win"] = xw
        m["padneg_b"] = _bcast((valid - 1.0) * (-NEG))
        m["valid_b"] = _bcast(valid)
        gm = np.zeros((4, 2, NW), f32)
        gm[:, 0, :] = (1.0 - valid) * NEG
        gm[:, 1, :] = (1.0 - valid) * (-NEG)
        m["gmask"] = gm
        in_maps.append(m)
    return in_maps


_NC = None


def kernel(**inputs):
    global _NC
    if _NC is None:
        _NC = build_program()
    in_maps = prep_inputs(inputs)
    res = run_bass_kernel_spmd(_NC, in_maps, core_ids=list(range(8)))
    out = np.zeros((2, 4096, D), np.float32)
    for c in range(8):
        b_, k_ = c // 4, c % 4
        out[b_, 1024 * k_:1024 * k_ + 1024] = res.results[c]["out"]
    return out
```

```python
import math
from contextlib import ExitStack
import numpy as np
import ml_dtypes
import concourse.bass as bass
import concourse.mybir as mybir
from concourse.bass_utils import run_bass_kernel_spmd

F32 = mybir.dt.float32
BF16 = mybir.dt.bfloat16
AF = mybir.ActivationFunctionType
ALU = mybir.AluOpType
AX = mybir.AxisListType

D = 2048
NW = 4096
NOWN = 1024
NPROJ = 11784
ALPHA = 2 ** 0.25
EPS = 1e-5
NEG = -30000.0
DFF = 1408
NEXP = 32
DEBUG = False


class Buf:
    __slots__ = ("w", "r")

    def __init__(self):
        self.w = None
        self.r = []


class Sched:
    ENG = ("pe", "act", "dve", "pool", "sp")

    def __init__(self, nc, es, n_dma_sems=20):
        self.nc = nc
        self.eobj = {"pe": nc.tensor, "act": nc.scalar, "dve": nc.vector, "pool": nc.gpsimd, "sp": nc.sync}
        self.cnt = {e: 0 for e in self.ENG}
        self.sem = {e: es.enter_context(nc.semaphore("c_" + e)) for e in self.ENG}
        self.seen = {e: {} for e in self.ENG}
        self.pending = {e: [] for e in self.ENG}
        self.dsems, self.dtot, self.dnext = {}, {}, {}
        for q in ("sp", "pool", "act"):
            self.dsems[q] = [es.enter_context(nc.semaphore(f"d_{q}_{i}")) for i in range(n_dma_sems)]
            self.dnext[q] = 0
        self.final_events = []

    def _deps(self, reads, writes):
        deps = []
        for b in reads:
            if b.w is not None:
                deps.append(b.w)
        for b in writes:
            if b.w is not None:
                deps.append(b.w)
            deps.extend(b.r)
        return deps

    def _filter(self, eng, deps):
        seen = self.seen[eng]
        own = self.sem[eng]
        best = {}
        for (sem, val) in deps + self.pending[eng]:
            if sem is own and eng in ("pe", "sp"):
                continue
            k = id(sem)
            if seen.get(k, 0) >= val:
                continue
            seen[k] = val
            best[k] = (sem, val)
        self.pending[eng] = []
        return list(best.values())

    def _emit(self, ename, waits, fn, sem, inc):
        eng = self.eobj[ename]
        for (s_, v) in waits:
            eng.wait_ge(s_, v)
        fn(eng).then_inc(sem, inc)

    def op(self, eng, fn, reads=(), writes=()):
        waits = self._filter(eng, self._deps(reads, writes))
        self.cnt[eng] += 1
        ev = (self.sem[eng], self.cnt[eng])
        for b in reads:
            b.r.append(ev)
        for b in writes:
            b.w = ev
            b.r = []
        self._emit(eng, waits, fn, self.sem[eng], 1)
        return ev

    def dma(self, q, out, in_, reads=(), writes=(), final=False):
        deps = self._deps(reads, writes)
        i = self.dnext[q]
        self.dnext[q] = (i + 1) % len(self.dsems[q])
        dsem = self.dsems[q][i]
        prev = self.dtot.get(id(dsem), 0)
        if prev:
            deps.append((dsem, prev))
        waits = self._filter(q, deps)
        tot = prev + 16
        self.dtot[id(dsem)] = tot
        ev = (dsem, tot)
        for b in reads:
            b.r.append(ev)
        for b in writes:
            b.w = ev
            b.r = []
        self._emit(q, waits, (lambda e: e.dma_start(out=out, in_=in_)), dsem, 16)
        if final:
            self.final_events.append(ev)
        return ev

    def barrier(self):
        evs = [(self.sem[e], self.cnt[e]) for e in self.ENG if self.cnt[e] > 0]
        for q in self.dsems:
            for s in self.dsems[q]:
                t = self.dtot.get(id(s), 0)
                if t:
                    evs.append((s, t))
        for e in self.ENG:
            self.pending[e].extend(evs)

    def finish(self):
        for (s_, v) in self._filter("sp", list(self.final_events)):
            self.nc.sync.wait_ge(s_, v)


def ss(start, n, r):
    return slice(start, start + (n - 1) * r + 1, r)


def alibi_slopes(n):
    def geometric(k):
        start = 2.0 ** (-8.0 / k)
        return [start ** (i + 1) for i in range(k)]
    c = 2 ** int(math.floor(math.log2(n)))
    s = geometric(c) if c == n else geometric(c) + geometric(2 * c)[0::2][: n - c]
    return np.array(sorted(s, reverse=True), dtype=np.float32)


C_AQ, C_AK, C_AV = 0, 1536, 3072
C_MQ, C_MK = 4608, 5120
C_MV, C_MO = 5632, 6656
C_IF = 7680
C_GA, C_GM = 7688, 9736


def build_program(upto=9):
    nc = bass.Bass("TRN2", target_bir_lowering=False)

    def din(name, shape, dt=F32):
        return nc.dram_tensor(name, list(shape), dt, kind="ExternalInput").ap()

    def dscr(name, shape, dt=F32):
        return nc.dram_tensor(name, list(shape), dt, kind="Internal").ap()

    x_win = din("x_win", [NW, D])
    w_in = din("w_in", [D, NPROJ])
    padneg_b = din("padneg_b", [128, NW])
    valid_b = din("valid_b", [128, NW])
    gmask = din("gmask", [4, 2, NW])
    lng_col = din("lng_col", [128, 16]); lnb_col = din("lnb_col", [128, 16])
    lng_b = din("lng_b", [128, D]); lnb_b = din("lnb_b", [128, D])
    cw = din("cw", [128, 8, 4]); cb = din("cb", [128, 8])
    ifb = din("ifb", [4, 2])
    mnw_b = din("mnw_b", [128, 1024])
    w_pa = din("w_pa", [512, D]); w_pm = din("w_pm", [1024, D]); w_out = din("w_out", [D, D])
    ln1g_b = din("ln1g_b", [128, D]); ln1b_b = din("ln1b_b", [128, D])
    ln2g_b = din("ln2g_b", [128, D]); ln2b_b = din("ln2b_b", [128, D])
    wr = din("wr", [128, 16, 36]); br_b = din("br_b", [128, 36])
    if upto >= 6:
        w_gate = din("w_gate", [NEXP, D, DFF]); w_up = din("w_up", [NEXP, D, DFF]); w_down = din("w_down", [NEXP, DFF, D])
    abias = din("abias", [12, 128, 256])
    ident_f = din("ident_f", [128, 128]); ident_h = din("ident_h", [128, 128], BF16)
    tri_neg = din("tri_neg", [128, 128])
    ltri = din("ltri", [128, 128])
    ones_f = din("ones_f", [128, 128])
    iota1 = din("iota1", [128, 128])
    iota1c = din("iota1c", [128, 1])
    sel4 = din("sel4", [4, 4, 128])
    sel32 = din("sel32", [32, 32, 128])
    out_d = nc.dram_tensor("out", [NOWN, D], F32, kind="ExternalOutput").ap()

    h_d = dscr("h_d", [NOWN, D])
    h1_d = dscr("h1_d", [NOWN, D])
    kT_d = dscr("kT_d", [12, 128, NW], BF16)
    qT_d = dscr("qT_d", [12, 128, NOWN], BF16)
    v_d = dscr("v_d", [NW, 1536], BF16)
    hT_d = dscr("hT_d", [16, 128, NOWN], BF16)
    b_hT_d = Buf()
    hmT_d = dscr("hmT_d", [8, 128, NOWN], BF16); attT_d = dscr("attT_d", [4, 128, NOWN], BF16)
    mT_d = dscr("mT_d", [16, 128, NOWN], BF16); h1bf_d = dscr("h1bf_d", [NOWN, D], BF16)
    b_hmT_d, b_attT_d, b_mT_d, b_h1bf_d = (Buf() for _ in range(4))
    o_d = dscr("o_d", [3, NOWN, 512])
    lse_d = dscr("lse_d", [3, NOWN, 4])
    b_h_d, b_h1_d, b_kT_d, b_qT_d, b_v_d, b_o_d, b_lse_d = (Buf() for _ in range(7))
    dbg = {}
    if DEBUG:
        for nm, shp in (("dbg_hm", [NOWN, 1024]), ("dbg_att", [NOWN, 512]), ("dbg_h1", [NOWN, D]), ("dbg_y2", [NOWN, D])):
            dbg[nm] = nc.dram_tensor(nm, shp, F32, kind="ExternalOutput").ap()

    with ExitStack() as es:
        S = Sched(nc, es)
        cnt = [0]

        def sb(es_, shape, dt=F32):
            cnt[0] += 1
            t = es_.enter_context(nc.sbuf_tensor(f"t{cnt[0]}", list(shape), dt))
            return t, Buf()

        psT = [es.enter_context(nc.psum_tensor(f"ps{i}", [128, 512], F32)) for i in range(8)]
        psB = [Buf() for _ in range(8)]
        psi = [0]

        def ps():
            i = psi[0]
            psi[0] = (i + 1) % 8
            return psT[i], psB[i]

        rr = [0]

        def evac_eng():
            rr[0] += 1
            return "act" if rr[0] % 2 else "dve"

        def copy(eng, out, in_, reads, writes):
            if eng == "act":
                S.op("act", lambda e: e.copy(out=out, in_=in_), reads, writes)
            else:
                S.op(eng, lambda e: e.tensor_copy(out=out, in_=in_), reads, writes)

        def load_const(es_, src, shape, dt=F32, q="sp"):
            t, b = sb(es_, shape, dt)
            idx = tuple(slice(None) for _ in shape)
            S.dma(q, t[idx], src, writes=[b])
            return t, b

        idf, b_idf = load_const(es, ident_f, [128, 128])
        idh, b_idh = load_const(es, ident_h, [128, 128], BF16)
        bC = Buf()
        def lc(src, shape, dt=F32):
            t, _ = sb(es, shape, dt)
            idx = tuple(slice(None) for _ in shape)
            S.dma("sp", t[idx], src, writes=[bC])
            return t
        c_lng = lc(lng_col, [128, 16]); c_lnb = lc(lnb_col, [128, 16])

        def layer_norm_stats(x_ap, b_x, st, b_st, mvt, b_mv, sd, b_sd, nchunk=4, csz=512):
            for i in range(nchunk):
                S.op("dve", lambda e, i=i: e.bn_stats(out=st[:, 6 * i:6 * i + 6], in_=x_ap[:, csz * i:csz * i + csz]),
                     reads=[b_x], writes=[b_st])
            S.op("dve", lambda e: e.bn_aggr(out=mvt[:, 0:2], in_=st[:, 0:6 * nchunk]), reads=[b_st], writes=[b_mv])
            S.op("dve", lambda e: e.tensor_scalar(out=sd[:, 0:1], in0=mvt[:, 1:2], scalar1=EPS, scalar2=None, op0=ALU.add),
                 reads=[b_mv], writes=[b_sd])
            S.op("act", lambda e: e.activation(out=sd[:, 0:1], in_=sd[:, 0:1], func=AF.Sqrt), reads=[b_sd], writes=[b_sd])
            S.op("dve", lambda e: e.reciprocal(out=sd[:, 0:1], in_=sd[:, 0:1]), reads=[b_sd], writes=[b_sd])
            S.op("dve", lambda e: e.tensor_scalar(out=sd[:, 1:2], in0=mvt[:, 0:1], scalar1=sd[:, 0:1], scalar2=-1.0,
                                                  op0=ALU.mult, op1=ALU.mult), reads=[b_mv, b_sd], writes=[b_sd])

        Wt_all, b_Wt = sb(es, [128, 8, 32]); mk_all, b_mk = sb(es, [128, 8, 32]); cumm, b_cumm = sb(es, [128, 8, 32])

        with ExitStack() as e1:
            bC1 = Buf()
            def lc1(src, shape, dt=F32):
                t, _ = sb(e1, shape, dt)
                idx = tuple(slice(None) for _ in shape)
                S.dma("sp", t[idx], src, writes=[bC1])
                return t
            c_cw = lc1(cw, [128, 8, 4]); c_cb = lc1(cb, [128, 8]); c_ifb = lc1(ifb, [4, 2])
            c_mnw = lc1(mnw_b, [128, 1024]); c_tri = lc1(tri_neg, [128, 128]); c_sel4 = lc1(sel4, [4, 4, 128])
            xs = [sb(e1, [128, D]) for _ in range(2)]
            xn_l = [sb(e1, [128, D]) for _ in range(2)]
            ln_l = [(sb(e1, [128, 24]), sb(e1, [128, 2]), sb(e1, [128, 2])) for _ in range(2)]
            hT_st, b_hT_st = sb(e1, [128, 16, 512], BF16)
            wbuf = [sb(e1, [128, 16, 512], BF16) for _ in range(2)]
            wif, b_wif = sb(e1, [128, 16, 8], BF16)
            S.dma("pool", wif[:, :, :], w_in[:, C_IF:C_IF + 8].rearrange("(k p) n -> p k n", p=128), writes=[b_wif])
            wi = [0]
            stage_o, b_stage_o = sb(e1, [128, 4, 512], BF16)
            stage_t, b_stage_t = sb(e1, [128, 4, 512], BF16)
            halo, b_halo = sb(e1, [128, 8, 3])
            S.op("pool", lambda e: e.memset(halo[:, :, :], 0.0), writes=[b_halo])
            rw, b_rw = sb(e1, [128, 515])
            cacc, b_cacc = sb(e1, [128, 512])
            kT_st, b_kT_st = sb(e1, [128, 4, 512], BF16)
            qT_st, b_qT_st = sb(e1, [128, 4, 512], BF16)
            vext, b_vext = sb(e1, [128, 4, 4, 258], BF16)
            S.op("pool", lambda e: e.memset(vext[:, :, :, 256:257], 1.0), writes=[b_vext])
            S.op("pool", lambda e: e.memset(vext[:, :, :, 257:258], 0.0), writes=[b_vext])
            smo, b_smo = sb(e1, [128, 4, 1024], BF16)
            gI, b_gI = sb(e1, [4, 512]); gF, b_gF = sb(e1, [4, 512])
            gB, b_gB = sb(e1, [4, 512]); gMg, b_gMg = sb(e1, [4, 512])
            gm_sb, b_gm = sb(e1, [4, 2, 512])
            QS, b_QS = sb(e1, [128, 512])
            S.op("pool", lambda e: e.memset(QS[:, :], 0.0), writes=[b_QS])
            vmask, b_vmask = sb(e1, [128, 512])
            ones512, b_ones512 = sb(e1, [4, 512])
            S.op("pool", lambda e: e.memset(ones512[:, :], 1.0), writes=[b_ones512])
            carryB, b_cB = sb(e1, [4, 1]); carryM, b_cM = sb(e1, [4, 1])
            S.op("pool", lambda e: e.memset(carryB[:, :], 0.0), writes=[b_cB])
            S.op("pool", lambda e: e.memset(carryM[:, :], 0.0), writes=[b_cM])
            QT, b_QT = sb(e1, [128, 4, 128])
            NMGB, b_NMGB = sb(e1, [128, 4, 513])
            S.op("pool", lambda e: e.memset(NMGB[:, :, :], 0.0), writes=[b_NMGB])
            Cst, b_Cst = sb(e1, [128, 4, 258]); Cbf, b_Cbf = sb(e1, [128, 4, 258], BF16)
            S.op("pool", lambda e: e.memset(Cst[:, :, :], 0.0), writes=[b_Cst])
            S.op("pool", lambda e: e.memset(Cbf[:, :, :], 0.0), writes=[b_Cbf])
            sc_l = [sb(e1, [128, 16]) for _ in range(4)]
            kw_l = [sb(e1, [128, 128], BF16) for _ in range(4)]
            arg_l = [sb(e1, [128, 128]) for _ in range(4)]; Pt_l = [sb(e1, [128, 128], BF16) for _ in range(4)]
            intra_l = [sb(e1, [128, 258]) for _ in range(4)]; Rn_l = [sb(e1, [128, 258]) for _ in range(4)]
            hraw_l = [sb(e1, [128, 256]) for _ in range(4)]
            hm_t, b_hm_t = sb(e1, [128, 1024]); hm_h, b_hm_h = sb(e1, [128, 1024], BF16)
            hmT_c, b_hmT_c = sb(e1, [128, 8, 128], BF16)
            st2_l = [sb(e1, [128, 6]) for _ in range(4)]; mv2_l = [sb(e1, [128, 2]) for _ in range(4)]; sd2_l = [sb(e1, [128, 2]) for _ in range(4)]

            def wload(c0, ncols):
                t, b = wbuf[wi[0] % 2]
                wi[0] += 1
                S.dma("pool", t[:, :, 0:ncols], w_in[:, c0:c0 + ncols].rearrange("(k p) n -> p k n", p=128), writes=[b])
                return t, b

            def proj_fm(wt, bw, ncol_chunks, consume):
                for cc in range(ncol_chunks):
                    p_, bp = ps()
                    for kc in range(16):
                        S.op("pe", lambda e, kc=kc, cc=cc, p_=p_: e.matmul(p_[:, :], lhsT=wt[:, kc, 128 * cc:128 * cc + 128],
                                                                         rhs=hT_st[:, kc, :], start=(kc == 0), stop=(kc == 15)),
                             reads=[bw, b_hT_st], writes=[bp])
                    consume(cc, p_, bp)

            def proj_tm(wt, bw, ncols, consume):
                for tt in range(4):
                    p_, bp = ps()
                    for kc in range(16):
                        S.op("pe", lambda e, kc=kc, tt=tt, p_=p_: e.matmul(p_[:, 0:ncols], lhsT=hT_st[:, kc, 128 * tt:128 * tt + 128],
                                                                         rhs=wt[:, kc, 0:ncols], start=(kc == 0), stop=(kc == 15)),
                             reads=[bw, b_hT_st], writes=[bp])
                    consume(tt, p_, bp)

            for sti in range(8):
                own = sti >= 6
                t0 = 512 * sti
                for tt in range(4):
                    w0 = t0 + 128 * tt
                    xt, b_xt = xs[tt % 2]
                    xn, b_xn = xn_l[tt % 2]
                    (st, b_st), (mvt, b_mv), (sd, b_sd) = ln_l[tt % 2]
                    S.dma("sp", xt[:, :], x_win[w0:w0 + 128, :], writes=[b_xt])
                    layer_norm_stats(xt, b_xt, st, b_st, mvt, b_mv, sd, b_sd)
                    S.op("act", lambda e, xt=xt: e.activation(out=xn[:, :], in_=xt[:, :], func=AF.Identity,
                                                            scale=sd[:, 0:1], bias=sd[:, 1:2]),
                         reads=[b_xt, b_sd], writes=[b_xn])
                    if own:
                        o0 = w0 - 3072
                        S.dma("sp", h_d[o0:o0 + 128, :], xn[:, :], reads=[b_xn], writes=[b_h_d])
                    for g in range(4):
                        p_, bp = ps()
                        for j in range(4):
                            kc = 4 * g + j
                            S.op("pe", lambda e, kc=kc, j=j, p_=p_: e.transpose(out=p_[:, 128 * j:128 * j + 128],
                                                                              in_=xn[:, 128 * kc:128 * kc + 128], identity=idf[:, :]),
                                 reads=[b_xn, b_idf], writes=[bp])
                        for j in range(4):
                            kc = 4 * g + j
                            eng = evac_eng()
                            if eng == "act":
                                S.op("act", lambda e, kc=kc, j=j, p_=p_, tt=tt: e.activation(
                                    out=hT_st[:, kc, 128 * tt:128 * tt + 128], in_=p_[:, 128 * j:128 * j + 128], func=AF.Identity,
                                    scale=c_lng[:, kc:kc + 1], bias=c_lnb[:, kc:kc + 1]), reads=[bp, bC], writes=[b_hT_st])
                            else:
                                S.op("dve", lambda e, kc=kc, j=j, p_=p_, tt=tt: e.tensor_scalar(
                                    out=hT_st[:, kc, 128 * tt:128 * tt + 128], in0=p_[:, 128 * j:128 * j + 128],
                                    scalar1=c_lng[:, kc:kc + 1], scalar2=c_lnb[:, kc:kc + 1], op0=ALU.mult, op1=ALU.add),
                                     reads=[bp, bC], writes=[b_hT_st])
                if own:
                    o0 = t0 - 3072
                    S.dma("sp", hT_d[:, :, o0:o0 + 512].rearrange("k p t -> p k t"), hT_st[:, :, :], reads=[b_hT_st], writes=[b_hT_d])

                att_groups = [g for g in range(3) if t0 >= (1024 if g == 2 else 2560)]
                for g in att_groups:
                    wt, bw = wload(C_AK + 512 * g, 512)
                    def cons_k(cc, p_, bp, g=g):
                        copy(evac_eng(), stage_o[:, cc, :], p_[:, :], [bp], [b_stage_o])
                    proj_fm(wt, bw, 4, cons_k)
                    S.dma("sp", kT_d[4 * g:4 * g + 4, :, t0:t0 + 512].rearrange("h p t -> p h t"), stage_o[:, :, :],
                          reads=[b_stage_o], writes=[b_kT_d])
                    wt, bw = wload(C_AV + 512 * g, 512)
                    def cons_v(tt, p_, bp, g=g):
                        copy(evac_eng(), stage_t[:, tt, :], p_[:, :], [bp], [b_stage_t])
                    proj_tm(wt, bw, 512, cons_v)
                    S.dma("sp", v_d[t0:t0 + 512, 512 * g:512 * g + 512].rearrange("(t p) c -> p t c", p=128), stage_t[:, :, :],
                          reads=[b_stage_t], writes=[b_v_d])
                    if own:
                        wt, bw = wload(C_AQ + 512 * g, 512)
                        proj_fm(wt, bw, 4, cons_k)
                        o0 = t0 - 3072
                        S.dma("sp", qT_d[4 * g:4 * g + 4, :, o0:o0 + 512].rearrange("h p t -> p h t"), stage_o[:, :, :],
                              reads=[b_stage_o], writes=[b_qT_d])

                S.dma("sp", gm_sb[:, :, :], gmask[:, :, t0:t0 + 512], writes=[b_gm])
                S.dma("sp", vmask[:, :], valid_b[:, t0:t0 + 512], writes=[b_vmask])
                for which in ((1, 0) if sti >= 5 else (1,)):
                    wt, bw = wload(C_MK if which else C_MQ, 512)
                    dstT, b_dstT = (kT_st, b_kT_st) if which else (qT_st, b_qT_st)
                    def cons_c(cc, p_, bp, which=which, dstT=dstT, b_dstT=b_dstT):
                        ch = 4 * which + cc
                        S.op("pool", lambda e: e.tensor_copy(out=rw[:, 0:3], in_=halo[:, ch, :]), reads=[b_halo], writes=[b_rw])
                        S.op("dve", lambda e: e.tensor_tensor(out=rw[:, 3:515], in0=p_[:, :], in1=vmask[:, :], op=ALU.mult),
                             reads=[bp, b_vmask], writes=[b_rw])
                        S.op("dve", lambda e: e.tensor_scalar(out=cacc[:, :], in0=rw[:, 3:515], scalar1=c_cw[:, ch, 3:4],
                                                              scalar2=c_cb[:, ch:ch + 1], op0=ALU.mult, op1=ALU.add),
                             reads=[b_rw, bC1], writes=[b_cacc])
                        for j in range(3):
                            S.op("dve", lambda e, j=j: e.scalar_tensor_tensor(out=cacc[:, :], in0=rw[:, j:j + 512],
                                                                              scalar=c_cw[:, ch, j:j + 1], in1=cacc[:, :],
                                                                              op0=ALU.mult, op1=ALU.add),
                                 reads=[b_rw, bC1, b_cacc], writes=[b_cacc])
                        S.op("act", lambda e: e.activation(out=dstT[:, cc, :], in_=cacc[:, :], func=AF.Silu),
                             reads=[b_cacc], writes=[b_dstT])
                        S.op("pool", lambda e: e.tensor_copy(out=halo[:, ch, :], in_=rw[:, 512:515]), reads=[b_rw], writes=[b_halo])
                    proj_fm(wt, bw, 4, cons_c)
                for half in range(2):
                    wt, bw = wload(C_MV + 512 * half, 512)
                    def cons_mv(tt, p_, bp, half=half):
                        copy(evac_eng(), vext[:, tt, 2 * half:2 * half + 2, 0:256],
                             p_[:, :].rearrange("p (h c) -> p h c", c=256), [bp], [b_vext])
                    proj_tm(wt, bw, 512, cons_mv)
                if own:
                    for half in range(2):
                        wt, bw = wload(C_MO + 512 * half, 512)
                        def cons_mo(tt, p_, bp, half=half):
                            S.op("act", lambda e: e.activation(out=smo[:, tt, 512 * half:512 * half + 512], in_=p_[:, :], func=AF.Sigmoid),
                                 reads=[bp], writes=[b_smo])
                        proj_tm(wt, bw, 512, cons_mo)
                for gi_, (dst, b_dst) in enumerate(((gI, b_gI), (gF, b_gF))):
                    p_, bp = ps()
                    for kc in range(16):
                        S.op("pe", lambda e, kc=kc, p_=p_, gi_=gi_: e.matmul(p_[0:4, :], lhsT=wif[:, kc, 4 * gi_:4 * gi_ + 4],
                                                                           rhs=hT_st[:, kc, :], start=(kc == 0), stop=(kc == 15)),
                             reads=[b_wif, b_hT_st], writes=[bp])
                    S.op("dve", lambda e, p_=p_, gi_=gi_, dst=dst: e.scalar_tensor_tensor(
                        out=dst[:, :], in0=p_[0:4, :], scalar=c_ifb[:, gi_:gi_ + 1], in1=gm_sb[:, gi_, :], op0=ALU.add, op1=ALU.add),
                         reads=[bp, bC1, b_gm], writes=[b_dst])
                S.op("act", lambda e: e.activation(out=gF[:, :], in_=gF[:, :], func=AF.Exp, scale=-1.0), reads=[b_gF], writes=[b_gF])
                S.op("dve", lambda e: e.tensor_scalar(out=gF[:, :], in0=gF[:, :], scalar1=1.0, scalar2=None, op0=ALU.add),
                     reads=[b_gF], writes=[b_gF])
                S.op("act", lambda e: e.activation(out=gF[:, :], in_=gF[:, :], func=AF.Ln), reads=[b_gF], writes=[b_gF])
                S.op("dve", lambda e: e.tensor_tensor_scan(out=gB[:, :], data0=ones512[:, :], data1=gF[:, :], initial=carryB[:, 0:1],
                                                           op0=ALU.mult, op1=ALU.subtract),
                     reads=[b_gF, b_cB, b_ones512], writes=[b_gB])
                S.op("dve", lambda e: e.tensor_copy(out=carryB[:, :], in_=gB[:, 511:512]), reads=[b_gB], writes=[b_cB])
                S.op("dve", lambda e: e.tensor_tensor(out=QS[0:4, :], in0=gI[:, :], in1=gB[:, :], op=ALU.subtract),
                     reads=[b_gI, b_gB], writes=[b_QS])
                S.op("dve", lambda e: e.tensor_tensor_scan(out=gMg[:, :], data0=QS[0:4, :], data1=QS[0:4, :], initial=carryM[:, 0:1],
                                                           op0=ALU.max, op1=ALU.max),
                     reads=[b_QS, b_cM], writes=[b_gMg])
                S.op("dve", lambda e: e.tensor_copy(out=carryM[:, :], in_=gMg[:, 511:512]), reads=[b_gMg], writes=[b_cM])
                S.op("dve", lambda e: e.tensor_scalar(out=QS[32:36, :], in0=gMg[:, :], scalar1=-1.0, scalar2=None, op0=ALU.mult),
                     reads=[b_gMg], writes=[b_QS])
                S.op("dve", lambda e: e.tensor_tensor(out=gI[:, :], in0=gB[:, :], in1=gMg[:, :], op=ALU.add),
                     reads=[b_gB, b_gMg], writes=[b_gI])
                S.op("act", lambda e: e.activation(out=QS[64:68, :], in_=gI[:, :], func=AF.Exp, scale=-1.0), reads=[b_gI], writes=[b_QS])
                S.op("dve", lambda e: e.tensor_scalar(out=gF[:, :], in0=gMg[:, :], scalar1=-1.0, scalar2=None, op0=ALU.mult),
                     reads=[b_gMg], writes=[b_gF])
                S.op("dve", lambda e: e.tensor_copy(out=NMGB[:, :, 0:1], in_=NMGB[:, :, 512:513]), reads=[b_NMGB], writes=[b_NMGB])
                for hd in range(4):
                    p_, bp = ps()
                    S.op("pe", lambda e, hd=hd, p_=p_: e.matmul(p_[:, :], lhsT=c_sel4[:, hd, :], rhs=gF[:, :], start=True, stop=True),
                         reads=[b_gF, bC1], writes=[bp])
                    copy(evac_eng(), NMGB[:, hd, 1:513], p_[:, :], [bp], [b_NMGB])
                for c in range(4):
                    p_, bp = ps()
                    S.op("pe", lambda e, c=c, p_=p_: e.transpose(out=p_[:, 0:128], in_=QS[:, 128 * c:128 * c + 128], identity=idf[:, :]),
                         reads=[b_QS, b_idf], writes=[bp])
                    copy(evac_eng(), QT[:, c, :], p_[:, 0:128], [bp], [b_QT])

                HD = range(4)
                for c in range(4):
                    cs_ = slice(128 * c, 128 * c + 128)
                    def nmgL(hd): return NMGB[:, hd, 128 * c + 128:128 * c + 129]
                    def nmgS(hd): return NMGB[:, hd, 128 * c:128 * c + 1]
                    def Acol(hd): return QT[:, c, hd:hd + 1]
                    if own:
                        pS_l = {}; pI_l = {}; pX_l = {}
                        for hd in HD:
                            pS, bpS = ps(); pS_l[hd] = (pS, bpS)
                            S.op("pe", lambda e, pS=pS, hd=hd: e.matmul(pS[:, 0:128], lhsT=kT_st[:, hd, cs_], rhs=qT_st[:, hd, cs_], start=True, stop=True),
                                 reads=[b_kT_st, b_qT_st], writes=[bpS])
                        for hd in HD:
                            arg, b_arg = arg_l[hd]
                            S.op("dve", lambda e, hd=hd, arg=arg: e.scalar_tensor_tensor(
                                out=arg[:, :], in0=NMGB[:, hd, 128 * c + 1:128 * c + 129], scalar=Acol(hd), in1=c_tri[:, :],
                                op0=ALU.add, op1=ALU.add), reads=[b_NMGB, b_QT, bC1], writes=[b_arg])
                        for hd in HD:
                            arg, b_arg = arg_l[hd]
                            S.op("act", lambda e, arg=arg: e.activation(out=arg[:, :], in_=arg[:, :], func=AF.Exp), reads=[b_arg], writes=[b_arg])
                        for hd in HD:
                            arg, b_arg = arg_l[hd]; Pt, b_Pt = Pt_l[hd]; pS, bpS = pS_l[hd]
                            S.op("dve", lambda e, pS=pS, Pt=Pt, arg=arg: e.scalar_tensor_tensor(out=Pt[:, :], in0=pS[:, 0:128], scalar=128 ** -0.5,
                                                                                              in1=arg[:, :], op0=ALU.mult, op1=ALU.mult),
                                 reads=[bpS, b_arg], writes=[b_Pt])
                        for hd in HD:
                            Pt, b_Pt = Pt_l[hd]
                            pI, bpI = ps(); pI_l[hd] = (pI, bpI)
                            S.op("pe", lambda e, pI=pI, hd=hd, Pt=Pt: e.matmul(pI[:, 0:258], lhsT=Pt[:, :], rhs=vext[:, c, hd, :], start=True, stop=True),
                                 reads=[b_Pt, b_vext], writes=[bpI])
                        for hd in HD:
                            pX, bpX = ps(); pX_l[hd] = (pX, bpX)
                            S.op("pe", lambda e, pX=pX, hd=hd: e.matmul(pX[:, 0:258], lhsT=qT_st[:, hd, cs_], rhs=Cbf[:, hd, :], start=True, stop=True),
                                 reads=[b_qT_st, b_Cbf], writes=[bpX])
                        for hd in HD:
                            sc, b_sc = sc_l[hd]
                            S.op("dve", lambda e, hd=hd, sc=sc: e.tensor_scalar(out=sc[:, 0:1], in0=nmgS(hd), scalar1=-1.0, scalar2=None, op0=ALU.mult),
                                 reads=[b_NMGB], writes=[b_sc])
                        for hd in HD:
                            sc, b_sc = sc_l[hd]; intra, b_intra = intra_l[hd]; pI, bpI = pI_l[hd]
                            S.op("act", lambda e, pI=pI, intra=intra: e.copy(out=intra[:, :], in_=pI[:, 0:258]), reads=[bpI], writes=[b_intra])
                            S.op("act", lambda e, hd=hd, sc=sc: e.activation(out=sc[:, 1:2], in_=QT[:, c, 32 + hd:33 + hd], func=AF.Exp, bias=sc[:, 0:1]),
                                 reads=[b_QT, b_sc], writes=[b_sc])
                        for hd in HD:
                            sc, b_sc = sc_l[hd]
                            S.op("dve", lambda e, sc=sc: e.tensor_scalar(out=sc[:, 1:2], in0=sc[:, 1:2], scalar1=128 ** -0.5, scalar2=None, op0=ALU.mult),
                                 reads=[b_sc], writes=[b_sc])
                        for hd in HD:
                            sc, b_sc = sc_l[hd]; intra, b_intra = intra_l[hd]; Rn, b_Rn = Rn_l[hd]; pX, bpX = pX_l[hd]
                            S.op("dve", lambda e, pX=pX, sc=sc, intra=intra, Rn=Rn: e.scalar_tensor_tensor(out=Rn[:, :], in0=pX[:, 0:258], scalar=sc[:, 1:2],
                                                                                                         in1=intra[:, :], op0=ALU.mult, op1=ALU.add),
                                 reads=[bpX, b_sc, b_intra], writes=[b_Rn])
                        for hd in HD:
                            sc, b_sc = sc_l[hd]; Rn, b_Rn = Rn_l[hd]
                            S.op("dve", lambda e, sc=sc, Rn=Rn: e.tensor_scalar(out=sc[:, 3:4], in0=Rn[:, 256:257], scalar1=-1.0, scalar2=None, op0=ALU.mult),
                                 reads=[b_Rn], writes=[b_sc])
                        for hd in HD:
                            sc, b_sc = sc_l[hd]; Rn, b_Rn = Rn_l[hd]
                            S.op("dve", lambda e, sc=sc, Rn=Rn: e.tensor_tensor(out=sc[:, 2:3], in0=sc[:, 3:4], in1=Rn[:, 256:257], op=ALU.max),
                                 reads=[b_Rn, b_sc], writes=[b_sc])
                        for hd in HD:
                            sc, b_sc = sc_l[hd]
                            S.op("dve", lambda e, hd=hd, sc=sc: e.tensor_tensor(out=sc[:, 2:3], in0=sc[:, 2:3], in1=QT[:, c, 64 + hd:65 + hd], op=ALU.max),
                                 reads=[b_sc, b_QT], writes=[b_sc])
                        for hd in HD:
                            sc, b_sc = sc_l[hd]
                            S.op("dve", lambda e, sc=sc: e.reciprocal(out=sc[:, 2:3], in_=sc[:, 2:3]), reads=[b_sc], writes=[b_sc])
                        for hd in HD:
                            sc, b_sc = sc_l[hd]; Rn, b_Rn = Rn_l[hd]; hraw, b_hraw = hraw_l[hd]
                            S.op("dve", lambda e, sc=sc, Rn=Rn, hraw=hraw: e.tensor_scalar(out=hraw[:, :], in0=Rn[:, 0:256], scalar1=sc[:, 2:3], scalar2=None, op0=ALU.mult),
                                 reads=[b_Rn, b_sc], writes=[b_hraw])
                        for hd in HD:
                            hraw, b_hraw = hraw_l[hd]; st2, b_st2 = st2_l[hd]
                            S.op("dve", lambda e, hraw=hraw, st2=st2: e.bn_stats(out=st2[:, 0:6], in_=hraw[:, :]), reads=[b_hraw], writes=[b_st2])
                        for hd in HD:
                            st2, b_st2 = st2_l[hd]; mv2, b_mv2 = mv2_l[hd]
                            S.op("dve", lambda e, st2=st2, mv2=mv2: e.bn_aggr(out=mv2[:, 0:2], in_=st2[:, 0:6]), reads=[b_st2], writes=[b_mv2])
                        for hd in HD:
                            mv2, b_mv2 = mv2_l[hd]; sd2, b_sd2 = sd2_l[hd]
                            S.op("dve", lambda e, mv2=mv2, sd2=sd2: e.tensor_scalar(out=sd2[:, 0:1], in0=mv2[:, 1:2], scalar1=EPS, scalar2=None, op0=ALU.add),
                                 reads=[b_mv2], writes=[b_sd2])
                        for hd in HD:
                            sd2, b_sd2 = sd2_l[hd]
                            S.op("act", lambda e, sd2=sd2: e.activation(out=sd2[:, 0:1], in_=sd2[:, 0:1], func=AF.Sqrt), reads=[b_sd2], writes=[b_sd2])
                        for hd in HD:
                            sd2, b_sd2 = sd2_l[hd]
                            S.op("dve", lambda e, sd2=sd2: e.reciprocal(out=sd2[:, 0:1], in_=sd2[:, 0:1]), reads=[b_sd2], writes=[b_sd2])
                        for hd in HD:
                            mv2, b_mv2 = mv2_l[hd]; sd2, b_sd2 = sd2_l[hd]
                            S.op("dve", lambda e, mv2=mv2, sd2=sd2: e.tensor_scalar(out=sd2[:, 1:2], in0=mv2[:, 0:1], scalar1=sd2[:, 0:1], scalar2=-1.0,
                                                                                  op0=ALU.mult, op1=ALU.mult), reads=[b_mv2, b_sd2], writes=[b_sd2])
                        for hd in HD:
                            hraw, b_hraw = hraw_l[hd]; sd2, b_sd2 = sd2_l[hd]
                            S.op("act", lambda e, hraw=hraw, sd2=sd2: e.activation(out=hraw[:, :], in_=hraw[:, :], func=AF.Identity, scale=sd2[:, 0:1], bias=sd2[:, 1:2]),
                                 reads=[b_hraw, b_sd2], writes=[b_hraw])
                        for hd in HD:
                            hraw, b_hraw = hraw_l[hd]
                            S.op("dve", lambda e, hd=hd, hraw=hraw: e.tensor_tensor(out=hm_t[:, 256 * hd:256 * hd + 256], in0=hraw[:, :],
                                                                                  in1=c_mnw[:, 256 * hd:256 * hd + 256], op=ALU.mult),
                                 reads=[b_hraw, bC1], writes=[b_hm_t])
                    pK_l = {}; pU_l = {}
                    for hd in HD:
                        sc, b_sc = sc_l[hd]
                        S.op("act", lambda e, hd=hd, sc=sc: e.activation(out=sc[:, 4:5], in_=Acol(hd), func=AF.Exp, bias=nmgL(hd)),
                             reads=[b_QT, b_NMGB], writes=[b_sc])
                        S.op("act", lambda e, hd=hd, sc=sc: e.activation(out=sc[:, 5:6], in_=nmgS(hd), func=AF.Exp, scale=-1.0, bias=nmgL(hd)),
                             reads=[b_NMGB], writes=[b_sc])
                    for hd in HD:
                        pK, bpK = ps(); pKh = pK.bitcast(BF16); pK_l[hd] = (pKh, bpK)
                        S.op("pe", lambda e, pKh=pKh, hd=hd: e.transpose(out=pKh[:, 0:128], in_=kT_st[:, hd, cs_], identity=idh[:, :]),
                             reads=[b_kT_st, b_idh], writes=[bpK])
                    for hd in HD:
                        sc, b_sc = sc_l[hd]; kw, b_kw = kw_l[hd]; pKh, bpK = pK_l[hd]
                        S.op("dve", lambda e, pKh=pKh, sc=sc, kw=kw: e.tensor_scalar(out=kw[:, :], in0=pKh[:, 0:128], scalar1=sc[:, 4:5], scalar2=None, op0=ALU.mult),
                             reads=[bpK, b_sc], writes=[b_kw])
                    for hd in HD:
                        kw, b_kw = kw_l[hd]
                        pU, bpU = ps(); pU_l[hd] = (pU, bpU)
                        S.op("pe", lambda e, pU=pU, hd=hd, kw=kw: e.matmul(pU[:, 0:258], lhsT=kw[:, :], rhs=vext[:, c, hd, :], start=True, stop=True),
                             reads=[b_kw, b_vext], writes=[bpU])
                    for hd in HD:
                        sc, b_sc = sc_l[hd]; pU, bpU = pU_l[hd]
                        S.op("dve", lambda e, pU=pU, hd=hd, sc=sc: e.scalar_tensor_tensor(out=Cst[:, hd, :], in0=Cst[:, hd, :], scalar=sc[:, 5:6],
                                                                                        in1=pU[:, 0:258], op0=ALU.mult, op1=ALU.add),
                             reads=[b_Cst, b_sc, bpU], writes=[b_Cst])
                    if sti >= 5:
                        S.op("pool", lambda e: e.tensor_copy(out=Cbf[:, :, :], in_=Cst[:, :, :]), reads=[b_Cst], writes=[b_Cbf])
                    if own:
                        S.op("dve", lambda e, c=c: e.tensor_tensor(out=hm_t[:, :], in0=hm_t[:, :], in1=smo[:, c, :], op=ALU.mult),
                             reads=[b_hm_t, b_smo], writes=[b_hm_t])
                        o0 = t0 - 3072 + 128 * c
                        if DEBUG:
                            S.dma("sp", dbg["dbg_hm"][o0:o0 + 128, :], hm_t[:, :], reads=[b_hm_t], final=True)
                        S.op("act", lambda e: e.copy(out=hm_h[:, :], in_=hm_t[:, :]), reads=[b_hm_t], writes=[b_hm_h])
                        for half in range(2):
                            p_, bp = ps()
                            ph = p_.bitcast(BF16)
                            for j in range(4):
                                kc = 4 * half + j
                                S.op("pe", lambda e, ph=ph, kc=kc, j=j: e.transpose(out=ph[:, 128 * j:128 * j + 128],
                                                                                  in_=hm_h[:, 128 * kc:128 * kc + 128], identity=idh[:, :]),
                                     reads=[b_hm_h, b_idh], writes=[bp])
                            copy(evac_eng(), hmT_c[:, 4 * half:4 * half + 4, :],
                                 ph[:, 0:512].rearrange("p (k t) -> p k t", t=128), [bp], [b_hmT_c])
                        S.dma("sp", hmT_d[:, :, o0:o0 + 128].rearrange("k p t -> p k t"), hmT_c[:, :, :], reads=[b_hmT_c], writes=[b_hmT_d])
        S.barrier()
        if upto >= 2:
          with ExitStack() as e2:
            qT_sb, b_qT_sb = sb(e2, [128, 4, NOWN], BF16)
            kT_sb, b_kT_sb = sb(e2, [128, 4, 3072], BF16)
            pad_sb, b_pad_sb = sb(e2, [128, 3072])
            S.dma("sp", pad_sb[:, :], padneg_b[:, 1024:4096], writes=[b_pad_sb])
            ab_sb, b_ab_sb = sb(e2, [128, 4, 256])
            Vb = [sb(e2, [128, 2, 512], BF16) for _ in range(2)]
            S2_l = [sb(e2, [128, 256]) for _ in range(4)]; Pb_l = [sb(e2, [128, 256], BF16) for _ in range(4)]
            PT_l = [sb(e2, [128, 2, 128], BF16) for _ in range(4)]
            a_sc_l = [sb(e2, [128, 8]) for _ in range(4)]
            o_sb = [sb(e2, [128, 4, 128]) for _ in range(2)]
            l_sb = [sb(e2, [128, 4]) for _ in range(2)]
            ui = 0
            for g in range(3):
                r = (1, 4, 16)[g]
                w_lo = 1024 if g == 2 else 2560
                nq = min(128, 1024 // r)
                nk = 128 + nq
                nch = (1024 // r) // nq
                S.dma("sp", qT_sb[:, :, :], qT_d[4 * g:4 * g + 4, :, :].rearrange("h p t -> p h t"), reads=[b_qT_d], writes=[b_qT_sb])
                S.dma("sp", kT_sb[:, :, 0:4096 - w_lo], kT_d[4 * g:4 * g + 4, :, w_lo:4096].rearrange("h p t -> p h t"),
                      reads=[b_kT_d], writes=[b_kT_sb])
                S.dma("sp", ab_sb[:, :, :], abias[4 * g:4 * g + 4, :, :].rearrange("h p k -> p h k"), writes=[b_ab_sb])
                for p in range(r):
                    for ci in range(nch):
                        j0 = 3072 // r + ci * nq
                        wk0 = p + r * (j0 - 128)
                        kc0 = wk0 - w_lo
                        qc0 = p + r * j0 - 3072
                        vt, b_vt = Vb[ui % 2]
                        ot, b_ot = o_sb[ui % 2]
                        lt, b_lt = l_sb[ui % 2]
                        ui += 1
                        S.dma("sp", vt[:, 0, :], v_d[ss(wk0, 128, r), 512 * g:512 * g + 512], reads=[b_v_d], writes=[b_vt])
                        S.dma("sp", vt[0:nq, 1, :], v_d[ss(wk0 + 128 * r, nq, r), 512 * g:512 * g + 512], reads=[b_v_d], writes=[b_vt])
                        HD = range(4)
                        pS_l = {}; pT_l = {}; pO_l = {}
                        for hd in HD:
                            pS, bpS = ps(); pS_l[hd] = (pS, bpS)
                            S.op("pe", lambda e, pS=pS, hd=hd: e.matmul(pS[0:nq, 0:nk], lhsT=qT_sb[:, hd, ss(qc0, nq, r)],
                                                                      rhs=kT_sb[:, hd, ss(kc0, nk, r)], start=True, stop=True),
                                 reads=[b_qT_sb, b_kT_sb], writes=[bpS])
                        for hd in HD:
                            S2, b_S2 = S2_l[hd]; pS, bpS = pS_l[hd]
                            S.op("dve", lambda e, pS=pS, hd=hd, S2=S2: e.scalar_tensor_tensor(out=S2[0:nq, 0:nk], in0=pS[0:nq, 0:nk], scalar=128 ** -0.5,
                                                                                            in1=ab_sb[0:nq, hd, 0:nk], op0=ALU.mult, op1=ALU.add),
                                 reads=[bpS, b_ab_sb], writes=[b_S2])
                            S.op("pool", lambda e, S2=S2: e.tensor_tensor(out=S2[0:nq, 0:nk], in0=S2[0:nq, 0:nk], in1=pad_sb[0:nq, ss(wk0 - 1024, nk, r)], op=ALU.add),
                                 reads=[b_S2, b_pad_sb], writes=[b_S2])
                        for hd in HD:
                            S2, b_S2 = S2_l[hd]; a_sc, b_a_sc = a_sc_l[hd]
                            S.op("dve", lambda e, S2=S2, a_sc=a_sc: e.tensor_reduce(out=a_sc[0:nq, 1:2], in_=S2[0:nq, 0:nk], axis=AX.X, op=ALU.max, negate=True),
                                 reads=[b_S2], writes=[b_a_sc])
                        for hd in HD:
                            S2, b_S2 = S2_l[hd]; a_sc, b_a_sc = a_sc_l[hd]; Pb, b_Pb = Pb_l[hd]
                            S.op("act", lambda e, S2=S2, a_sc=a_sc, Pb=Pb: e.activation(out=Pb[0:nq, 0:nk], in_=S2[0:nq, 0:nk], func=AF.Exp, bias=a_sc[0:nq, 1:2],
                                                                                       accum_out=a_sc[0:nq, 2:3]), reads=[b_S2, b_a_sc], writes=[b_Pb, b_a_sc])
                        for hd in HD:
                            Pb, b_Pb = Pb_l[hd]
                            pT_, bpT = ps(); pTh = pT_.bitcast(BF16); pT_l[hd] = (pTh, bpT)
                            S.op("pe", lambda e, pTh=pTh, Pb=Pb: e.transpose(out=pTh[:, 0:nq], in_=Pb[0:nq, 0:128], identity=idh[0:nq, 0:nq]),
                                 reads=[b_Pb, b_idh], writes=[bpT])
                            S.op("pe", lambda e, pTh=pTh, Pb=Pb: e.transpose(out=pTh[0:nq, 128:128 + nq], in_=Pb[0:nq, 128:128 + nq], identity=idh[0:nq, 0:nq]),
                                 reads=[b_Pb, b_idh], writes=[bpT])
                        for hd in HD:
                            PT, b_PT = PT_l[hd]; pTh, bpT = pT_l[hd]
                            copy("act", PT[:, 0, 0:nq], pTh[:, 0:nq], [bpT], [b_PT])
                            copy("dve", PT[0:nq, 1, 0:nq], pTh[0:nq, 128:128 + nq], [bpT], [b_PT])
                        for hd in HD:
                            PT, b_PT = PT_l[hd]
                            pO, bpO = ps(); pO_l[hd] = (pO, bpO)
                            S.op("pe", lambda e, pO=pO, hd=hd, vt=vt, PT=PT: e.matmul(pO[0:nq, 0:128], lhsT=PT[:, 0, 0:nq], rhs=vt[:, 0, 128 * hd:128 * hd + 128],
                                                                                    start=True, stop=False), reads=[b_PT, b_vt], writes=[bpO])
                            S.op("pe", lambda e, pO=pO, hd=hd, vt=vt, PT=PT: e.matmul(pO[0:nq, 0:128], lhsT=PT[0:nq, 1, 0:nq], rhs=vt[0:nq, 1, 128 * hd:128 * hd + 128],
                                                                                    start=False, stop=True), reads=[b_PT, b_vt], writes=[bpO])
                        for hd in HD:
                            a_sc, b_a_sc = a_sc_l[hd]
                            S.op("dve", lambda e, a_sc=a_sc: e.reciprocal(out=a_sc[0:nq, 3:4], in_=a_sc[0:nq, 2:3]), reads=[b_a_sc], writes=[b_a_sc])
                            S.op("act", lambda e, a_sc=a_sc: e.activation(out=a_sc[0:nq, 4:5], in_=a_sc[0:nq, 2:3], func=AF.Ln), reads=[b_a_sc], writes=[b_a_sc])
                        for hd in HD:
                            a_sc, b_a_sc = a_sc_l[hd]; pO, bpO = pO_l[hd]
                            S.op("dve", lambda e, pO=pO, hd=hd, ot=ot, a_sc=a_sc: e.tensor_scalar(out=ot[0:nq, hd, :], in0=pO[0:nq, 0:128], scalar1=a_sc[0:nq, 3:4],
                                                                                                scalar2=None, op0=ALU.mult), reads=[bpO, b_a_sc], writes=[b_ot])
                            S.op("dve", lambda e, hd=hd, lt=lt, a_sc=a_sc: e.tensor_tensor(out=lt[0:nq, hd:hd + 1], in0=a_sc[0:nq, 4:5], in1=a_sc[0:nq, 1:2], op=ALU.subtract),
                                 reads=[b_a_sc], writes=[b_lt])
                        S.dma("sp", o_d[g, ss(qc0, nq, r), :], ot[0:nq, :, :].rearrange("p h d -> p (h d)"), reads=[b_ot], writes=[b_o_d])
                        S.dma("sp", lse_d[g, ss(qc0, nq, r), :], lt[0:nq, :], reads=[b_lt], writes=[b_lse_d])
            o3 = [sb(e2, [128, 3, 512]) for _ in range(2)]
            l3, b_l3 = sb(e2, [128, 3, 4]); e3, b_e3 = sb(e2, [128, 3, 4]); m_sc, b_m_sc = sb(e2, [128, 12])
            att_t, b_att_t = sb(e2, [128, 512]); att_h, b_att_h = sb(e2, [128, 512], BF16)
            attT_c, b_attT_c = sb(e2, [128, 4, 128], BF16)
            for t in range(8):
                ot3, b_ot3 = o3[t % 2]
                S.dma("sp", ot3[:, :, :], o_d[:, 128 * t:128 * t + 128, :].rearrange("g p c -> p g c"), reads=[b_o_d], writes=[b_ot3])
                S.dma("sp", l3[:, :, :], lse_d[:, 128 * t:128 * t + 128, :].rearrange("g p c -> p g c"), reads=[b_lse_d], writes=[b_l3])
                S.op("dve", lambda e: e.tensor_tensor(out=m_sc[:, 0:4], in0=l3[:, 0, :], in1=l3[:, 1, :], op=ALU.max), reads=[b_l3], writes=[b_m_sc])
                S.op("dve", lambda e: e.tensor_tensor(out=m_sc[:, 0:4], in0=m_sc[:, 0:4], in1=l3[:, 2, :], op=ALU.max), reads=[b_l3, b_m_sc], writes=[b_m_sc])
                for g in range(3):
                    S.op("dve", lambda e, g=g: e.tensor_tensor(out=e3[:, g, :], in0=l3[:, g, :], in1=m_sc[:, 0:4], op=ALU.subtract),
                         reads=[b_l3, b_m_sc], writes=[b_e3])
                S.op("act", lambda e: e.activation(out=e3[:, :, :], in_=e3[:, :, :], func=AF.Exp), reads=[b_e3], writes=[b_e3])
                S.op("dve", lambda e: e.tensor_tensor(out=m_sc[:, 4:8], in0=e3[:, 0, :], in1=e3[:, 1, :], op=ALU.add), reads=[b_e3], writes=[b_m_sc])
                S.op("dve", lambda e: e.tensor_tensor(out=m_sc[:, 4:8], in0=m_sc[:, 4:8], in1=e3[:, 2, :], op=ALU.add), reads=[b_e3, b_m_sc], writes=[b_m_sc])
                S.op("dve", lambda e: e.reciprocal(out=m_sc[:, 8:12], in_=m_sc[:, 4:8]), reads=[b_m_sc], writes=[b_m_sc])
                for g in range(3):
                    S.op("dve", lambda e, g=g: e.tensor_tensor(out=e3[:, g, :], in0=e3[:, g, :], in1=m_sc[:, 8:12], op=ALU.mult),
                         reads=[b_e3, b_m_sc], writes=[b_e3])
                for sl in range(4):
                    S.op("dve", lambda e, sl=sl, ot3=ot3: e.tensor_scalar(out=att_t[:, 128 * sl:128 * sl + 128], in0=ot3[:, 0, 128 * sl:128 * sl + 128],
                                                                        scalar1=e3[:, 0, sl:sl + 1], scalar2=None, op0=ALU.mult),
                         reads=[b_ot3, b_e3], writes=[b_att_t])
                    for g in (1, 2):
                        S.op("dve", lambda e, sl=sl, g=g, ot3=ot3: e.scalar_tensor_tensor(
                            out=att_t[:, 128 * sl:128 * sl + 128], in0=ot3[:, g, 128 * sl:128 * sl + 128], scalar=e3[:, g, sl:sl + 1],
                            in1=att_t[:, 128 * sl:128 * sl + 128], op0=ALU.mult, op1=ALU.add), reads=[b_ot3, b_e3, b_att_t], writes=[b_att_t])
                if DEBUG:
                    S.dma("sp", dbg["dbg_att"][128 * t:128 * t + 128, :], att_t[:, :], reads=[b_att_t], final=True)
                S.op("act", lambda e: e.copy(out=att_h[:, :], in_=att_t[:, :]), reads=[b_att_t], writes=[b_att_h])
                p_, bp = ps()
                ph = p_.bitcast(BF16)
                for j in range(4):
                    S.op("pe", lambda e, ph=ph, j=j: e.transpose(out=ph[:, 128 * j:128 * j + 128], in_=att_h[:, 128 * j:128 * j + 128], identity=idh[:, :]),
                         reads=[b_att_h, b_idh], writes=[bp])
                copy(evac_eng(), attT_c[:, :, :], ph[:, 0:512].rearrange("p (k t) -> p k t", t=128), [bp], [b_attT_c])
                S.dma("sp", attT_d[:, :, 128 * t:128 * t + 128].rearrange("k p t -> p k t"), attT_c[:, :, :], reads=[b_attT_c], writes=[b_attT_d])
          S.barrier()

        if upto >= 4:
          if True:
            with ExitStack() as e4a:
                mergedT, b_mergedT = sb(e4a, [128, 16, NOWN], BF16)
                hmT, b_hmT = sb(e4a, [128, 8, NOWN], BF16); attT, b_attT = sb(e4a, [128, 4, NOWN], BF16)
                S.dma("sp", hmT[:, :, :], hmT_d[:, :, :].rearrange("k p t -> p k t"), reads=[b_hmT_d], writes=[b_hmT])
                S.dma("sp", attT[:, :, :], attT_d[:, :, :].rearrange("k p t -> p k t"), reads=[b_attT_d], writes=[b_attT])
                hTo, b_hTo = sb(e4a, [128, 16, NOWN], BF16)
                S.dma("sp", hTo[:, :, :], hT_d[:, :, :].rearrange("k p t -> p k t"), reads=[b_hT_d], writes=[b_hTo])
                wpa, b_wpa = sb(e4a, [128, 4, D], BF16); wpm, b_wpm = sb(e4a, [128, 8, D], BF16)
                for kc in range(4):
                    S.dma("pool", wpa[:, kc, :], w_pa[128 * kc:128 * kc + 128, :], writes=[b_wpa])
                for kc in range(8):
                    S.dma("pool", wpm[:, kc, :], w_pm[128 * kc:128 * kc + 128, :], writes=[b_wpm])
                wg = [sb(e4a, [128, 2, 16, 256], BF16) for _ in range(2)]
                sg_l = [(sb(e4a, [128, 512]), sb(e4a, [128, 512])) for _ in range(2)]
                sgi = [0]
                for blk in range(8):
                    wgt, b_wgt = wg[blk % 2]
                    for a_, c0 in enumerate((C_GA, C_GM)):
                        S.dma("pool", wgt[:, a_, :, :], w_in[:, c0 + 256 * blk:c0 + 256 * blk + 256].rearrange("(k p) n -> p k n", p=128), writes=[b_wgt])
                    for sub in range(2):
                        cc = 2 * blk + sub
                        for th in range(2):
                            tk = slice(512 * th, 512 * th + 512)
                            pA, bpA = ps(); pM, bpM = ps(); pGA, bpGA = ps(); pGM, bpGM = ps()
                            (sga, b_sga), (sgm, b_sgm) = sg_l[sgi[0] % 2]; sgi[0] += 1
                            for kc in range(4):
                                S.op("pe", lambda e, kc=kc, pA=pA, cc=cc, tk=tk: e.matmul(pA[:, :], lhsT=wpa[:, kc, 128 * cc:128 * cc + 128], rhs=attT[:, kc, tk],
                                                                                       start=(kc == 0), stop=(kc == 3)), reads=[b_wpa, b_attT], writes=[bpA])
                            for kc in range(8):
                                S.op("pe", lambda e, kc=kc, pM=pM, cc=cc, tk=tk: e.matmul(pM[:, :], lhsT=wpm[:, kc, 128 * cc:128 * cc + 128], rhs=hmT[:, kc, tk],
                                                                                       start=(kc == 0), stop=(kc == 7)), reads=[b_wpm, b_hmT], writes=[bpM])
                            for a_, (pG, bpG) in enumerate(((pGA, bpGA), (pGM, bpGM))):
                                for kc in range(16):
                                    S.op("pe", lambda e, kc=kc, pG=pG, a_=a_, sub=sub, tk=tk, wgt=wgt: e.matmul(
                                        pG[:, :], lhsT=wgt[:, a_, kc, 128 * sub:128 * sub + 128], rhs=hTo[:, kc, tk], start=(kc == 0), stop=(kc == 15)),
                                         reads=[b_wgt, b_hTo], writes=[bpG])
                            S.op("act", lambda e, pGA=pGA: e.activation(out=sga[:, :], in_=pGA[:, :], func=AF.Sigmoid), reads=[bpGA], writes=[b_sga])
                            S.op("act", lambda e, pGM=pGM: e.activation(out=sgm[:, :], in_=pGM[:, :], func=AF.Sigmoid), reads=[bpGM], writes=[b_sgm])
                            S.op("dve", lambda e, pA=pA: e.tensor_tensor(out=sga[:, :], in0=sga[:, :], in1=pA[:, :], op=ALU.mult), reads=[b_sga, bpA], writes=[b_sga])
                            S.op("dve", lambda e, pM=pM: e.tensor_tensor(out=sgm[:, :], in0=sgm[:, :], in1=pM[:, :], op=ALU.mult), reads=[b_sgm, bpM], writes=[b_sgm])
                            S.op("dve", lambda e, cc=cc, tk=tk: e.tensor_tensor(out=mergedT[:, cc, tk], in0=sga[:, :], in1=sgm[:, :], op=ALU.add),
                                 reads=[b_sga, b_sgm], writes=[b_mergedT])
                S.dma("sp", mT_d[:, :, :].rearrange("k p t -> p k t"), mergedT[:, :, :], reads=[b_mergedT], writes=[b_mT_d])
            S.barrier()
            with ExitStack() as e5:
                mergedT, b_mergedT = sb(e5, [128, 16, NOWN], BF16)
                S.dma("sp", mergedT[:, :, :], mT_d[:, :, :].rearrange("k p t -> p k t"), reads=[b_mT_d], writes=[b_mergedT])
                h1c, b_h1c = sb(e5, [128, D], BF16)
                wo, b_wo = sb(e5, [128, 16, D], BF16)
                for kc in range(16):
                    S.dma("pool", wo[:, kc, :], w_out[128 * kc:128 * kc + 128, :], writes=[b_wo])
                cA, b_cA = sb(e5, [128, D]); cB, b_cB = sb(e5, [128, D])
                cG1, b_cG1 = sb(e5, [128, D]); cB1, b_cB1 = sb(e5, [128, D])
                S.dma("sp", cA[:, :], lng_b, writes=[b_cA]); S.dma("sp", cB[:, :], lnb_b, writes=[b_cB])
                S.dma("sp", cG1[:, :], ln1g_b, writes=[b_cG1]); S.dma("sp", cB1[:, :], ln1b_b, writes=[b_cB1])
                h1t, b_h1t = None, None
                c_wr, b_c_wr = sb(e5, [128, 16, 36]); c_br, b_c_br = sb(e5, [128, 36])
                S.dma("sp", c_wr[:, :, :], wr, writes=[b_c_wr]); S.dma("sp", c_br[:, :], br_b, writes=[b_c_br])
                xz_l = [(sb(e5, [128, D]), sb(e5, [128, D])) for _ in range(2)]
                h1T, b_h1T = sb(e5, [128, 16, 128])
                st, b_st = sb(e5, [128, 24]); mvt, b_mv = sb(e5, [128, 2]); sd, b_sd = sb(e5, [128, 2])
                lg, b_lg = sb(e5, [128, 36]); r_sc, b_r_sc = sb(e5, [128, 16]); goh, b_goh = sb(e5, [128, 4])
                esel, b_esel = sb(e5, [128, 8]); top8, b_top8 = sb(e5, [128, 8]); oh, b_oh = sb(e5, [128, 2, 8]); wsel, b_wsel = sb(e5, [128, 8])
                for t in range(8):
                    tk = slice(128 * t, 128 * t + 128)
                    (xnt, b_xnt), (z, b_z) = xz_l[t % 2]
                    h1t, b_h1t = z, b_z
                    S.dma("sp", xnt[:, :], h_d[tk, :], reads=[b_h_d], writes=[b_xnt])
                    S.op("dve", lambda e: e.tensor_tensor(out=xnt[:, :], in0=xnt[:, :], in1=cA[:, :], op=ALU.mult), reads=[b_xnt, b_cA], writes=[b_xnt])
                    S.op("dve", lambda e: e.tensor_tensor(out=xnt[:, :], in0=xnt[:, :], in1=cB[:, :], op=ALU.add), reads=[b_xnt, b_cB], writes=[b_xnt])
                    for cb_ in range(4):
                        cs = slice(512 * cb_, 512 * cb_ + 512)
                        pY, bpY = ps()
                        for kc in range(16):
                            S.op("pe", lambda e, kc=kc, pY=pY, cs=cs, tk=tk: e.matmul(pY[:, :], lhsT=mergedT[:, kc, tk], rhs=wo[:, kc, cs],
                                                                                   start=(kc == 0), stop=(kc == 15)), reads=[b_mergedT, b_wo], writes=[bpY])
                        S.op("dve", lambda e, pY=pY, cs=cs: e.scalar_tensor_tensor(out=z[:, cs], in0=xnt[:, cs], scalar=ALPHA, in1=pY[:, :],
                                                                                 op0=ALU.mult, op1=ALU.add), reads=[b_xnt, bpY], writes=[b_z])
                    layer_norm_stats(z, b_z, st, b_st, mvt, b_mv, sd, b_sd)
                    S.op("act", lambda e: e.activation(out=z[:, :], in_=z[:, :], func=AF.Identity, scale=sd[:, 0:1], bias=sd[:, 1:2]),
                         reads=[b_z, b_sd], writes=[b_z])
                    S.op("dve", lambda e: e.tensor_tensor(out=z[:, :], in0=z[:, :], in1=cG1[:, :], op=ALU.mult), reads=[b_z, b_cG1], writes=[b_z])
                    S.op("dve", lambda e: e.tensor_tensor(out=h1t[:, :], in0=h1t[:, :], in1=cB1[:, :], op=ALU.add), reads=[b_h1t, b_cB1], writes=[b_h1t])
                    S.dma("sp", h1_d[tk, :], h1t[:, :], reads=[b_h1t], writes=[b_h1_d])
                    if DEBUG:
                        S.dma("sp", dbg["dbg_h1"][tk, :], h1t[:, :], reads=[b_h1t], final=True)
                    S.op("act", lambda e: e.copy(out=h1c[:, :], in_=h1t[:, :]), reads=[b_h1t], writes=[b_h1c])
                    S.dma("sp", h1bf_d[tk, :], h1c[:, :], reads=[b_h1c], writes=[b_h1bf_d])
                    for g4 in range(4):
                        p_, bp = ps()
                        for j in range(4):
                            kc = 4 * g4 + j
                            S.op("pe", lambda e, p_=p_, kc=kc, j=j: e.transpose(out=p_[:, 128 * j:128 * j + 128], in_=h1t[:, 128 * kc:128 * kc + 128], identity=idf[:, :]),
                                 reads=[b_h1t, b_idf], writes=[bp])
                        copy(evac_eng(), h1T[:, 4 * g4:4 * g4 + 4, :], p_[:, :].rearrange("p (k t) -> p k t", t=128), [bp], [b_h1T])
                    pL, bpL = ps()
                    for kc in range(16):
                        S.op("pe", lambda e, kc=kc, pL=pL: e.matmul(pL[:, 0:36], lhsT=h1T[:, kc, :], rhs=c_wr[:, kc, :], start=(kc == 0), stop=(kc == 15)),
                             reads=[b_h1T, b_c_wr], writes=[bpL])
                    S.op("dve", lambda e, pL=pL: e.tensor_tensor(out=lg[:, :], in0=pL[:, 0:36], in1=c_br[:, :], op=ALU.add), reads=[bpL, b_c_br], writes=[b_lg])
                    S.op("dve", lambda e: e.tensor_reduce(out=r_sc[:, 0:1], in_=lg[:, 0:4], axis=AX.X, op=ALU.max), reads=[b_lg], writes=[b_r_sc])
                    S.op("dve", lambda e: e.tensor_scalar(out=goh[:, :], in0=lg[:, 0:4], scalar1=r_sc[:, 0:1], scalar2=None, op0=ALU.is_equal),
                         reads=[b_lg, b_r_sc], writes=[b_goh])
                    S.op("dve", lambda e: e.tensor_scalar(out=r_sc[:, 1:2], in0=r_sc[:, 0:1], scalar1=-1.0, scalar2=None, op0=ALU.mult), reads=[b_r_sc], writes=[b_r_sc])
                    S.op("act", lambda e: e.activation(out=wsel[:, 0:4], in_=lg[:, 0:4], func=AF.Exp, bias=r_sc[:, 1:2], accum_out=r_sc[:, 2:3]),
                         reads=[b_lg, b_r_sc], writes=[b_wsel, b_r_sc])
                    S.op("dve", lambda e: e.reciprocal(out=r_sc[:, 3:4], in_=r_sc[:, 2:3]), reads=[b_r_sc], writes=[b_r_sc])
                    S.op("dve", lambda e: e.tensor_scalar(out=esel[:, :], in0=lg[:, 4:12], scalar1=goh[:, 0:1], scalar2=None, op0=ALU.mult),
                         reads=[b_lg, b_goh], writes=[b_esel])
                    for g in (1, 2, 3):
                        S.op("dve", lambda e, g=g: e.scalar_tensor_tensor(out=esel[:, :], in0=lg[:, 4 + 8 * g:12 + 8 * g], scalar=goh[:, g:g + 1], in1=esel[:, :],
                                                                          op0=ALU.mult, op1=ALU.add), reads=[b_lg, b_goh, b_esel], writes=[b_esel])
                    S.op("dve", lambda e: e.max(out=top8[:, :], in_=esel[:, :]), reads=[b_esel], writes=[b_top8])
                    S.op("dve", lambda e: e.tensor_tensor(out=r_sc[:, 4:5], in0=top8[:, 1:2], in1=top8[:, 0:1], op=ALU.subtract), reads=[b_top8], writes=[b_r_sc])
                    S.op("act", lambda e: e.activation(out=r_sc[:, 5:6], in_=r_sc[:, 4:5], func=AF.Exp), reads=[b_r_sc], writes=[b_r_sc])
                    S.op("dve", lambda e: e.tensor_scalar(out=r_sc[:, 6:7], in0=r_sc[:, 5:6], scalar1=1.0, scalar2=None, op0=ALU.add), reads=[b_r_sc], writes=[b_r_sc])
                    S.op("dve", lambda e: e.reciprocal(out=r_sc[:, 6:7], in_=r_sc[:, 6:7]), reads=[b_r_sc], writes=[b_r_sc])
                    S.op("dve", lambda e: e.tensor_tensor(out=r_sc[:, 7:8], in0=r_sc[:, 5:6], in1=r_sc[:, 6:7], op=ALU.mult), reads=[b_r_sc], writes=[b_r_sc])
                    S.op("dve", lambda e: e.tensor_scalar(out=r_sc[:, 6:8], in0=r_sc[:, 6:8], scalar1=r_sc[:, 3:4], scalar2=None, op0=ALU.mult),
                         reads=[b_r_sc], writes=[b_r_sc])
                    for j in range(2):
                        S.op("dve", lambda e, j=j: e.tensor_scalar(out=oh[:, j, :], in0=esel[:, :], scalar1=top8[:, j:j + 1], scalar2=None, op0=ALU.is_equal),
                             reads=[b_esel, b_top8], writes=[b_oh])
                    S.op("dve", lambda e: e.tensor_scalar(out=wsel[:, :], in0=oh[:, 0, :], scalar1=r_sc[:, 6:7], scalar2=None, op0=ALU.mult),
                         reads=[b_oh, b_r_sc], writes=[b_wsel])
                    S.op("dve", lambda e: e.scalar_tensor_tensor(out=wsel[:, :], in0=oh[:, 1, :], scalar=r_sc[:, 7:8], in1=wsel[:, :], op0=ALU.mult, op1=ALU.add),
                         reads=[b_oh, b_r_sc, b_wsel], writes=[b_wsel])
                    for g in range(4):
                        S.op("dve", lambda e, g=g, t=t: e.tensor_scalar(out=Wt_all[:, t, 8 * g:8 * g + 8], in0=wsel[:, :], scalar1=goh[:, g:g + 1], scalar2=None, op0=ALU.mult),
                             reads=[b_wsel, b_goh], writes=[b_Wt])
                    S.op("dve", lambda e, t=t: e.tensor_scalar(out=mk_all[:, t, :], in0=Wt_all[:, t, :], scalar1=0.0, scalar2=None, op0=ALU.is_gt),
                         reads=[b_Wt], writes=[b_mk])
                c_ones, b_c_ones = sb(e5, [128, 128]); c_ltri, b_c_ltri = sb(e5, [128, 128])
                S.dma("sp", c_ones[:, :], ones_f, writes=[b_c_ones]); S.dma("sp", c_ltri[:, :], ltri, writes=[b_c_ltri])
                for t in range(8):
                    pC, bpC = ps()
                    for t2 in range(t):
                        S.op("pe", lambda e, pC=pC, t2=t2: e.matmul(pC[:, 0:32], lhsT=c_ones[:, :], rhs=mk_all[:, t2, :], start=(t2 == 0), stop=False),
                             reads=[b_c_ones, b_mk], writes=[bpC])
                    S.op("pe", lambda e, pC=pC, t=t: e.matmul(pC[:, 0:32], lhsT=c_ltri[:, :], rhs=mk_all[:, t, :], start=(t == 0), stop=True),
                         reads=[b_c_ltri, b_mk], writes=[bpC])
                    S.op("dve", lambda e, pC=pC, t=t: e.tensor_tensor(out=cumm[:, t, :], in0=pC[:, 0:32], in1=mk_all[:, t, :], op=ALU.mult),
                         reads=[bpC, b_mk], writes=[b_cumm])
          S.barrier()

        if upto >= 6:
          with ExitStack() as e6:
            y_acc, b_y_acc = sb(e6, [128, 8, D])
            S.op("pool", lambda e: e.memset(y_acc[:, :, :], 0.0), writes=[b_y_acc])
            e6a = ExitStack()
            e6_outer = e6
            e6 = e6a
            h1_bf, b_h1_bf = sb(e6, [128, 8, D], BF16)
            S.dma("sp", h1_bf[:, :, :], h1bf_d.rearrange("(t p) c -> p t c", p=128), reads=[b_h1bf_d], writes=[b_h1_bf])
            c_iota, b_c_iota = sb(e6, [128, 128])
            S.dma("sp", c_iota[:, :], iota1, writes=[b_c_iota])
            wgu = [sb(e6, [128, 16, 512], BF16) for _ in range(3)]
            wdn = [sb(e6, [128, 11, 512], BF16) for _ in range(2)]
            Sel, b_Sel = sb(e6, [128, 8, 128], BF16); SelT_l = [sb(e6, [128, NOWN], BF16) for _ in range(2)]
            XeT, b_XeT = sb(e6, [128, 16, 128], BF16)
            sg, b_sg = sb(e6, [128, DFF]); Hs, b_Hs = sb(e6, [128, DFF], BF16)
            HT, b_HT = sb(e6, [128, 11, 128], BF16); Yb_l = [sb(e6, [128, D], BF16) for _ in range(2)]
            pending = []
            gi_ = [0]; di_ = [0]
            for ex in range(NEXP):
                SelT, b_SelT = SelT_l[ex % 2]; Yb, b_Yb = Yb_l[ex % 2]
                for t in range(8):
                    S.op("dve", lambda e, t=t, ex=ex: e.tensor_scalar(out=Sel[:, t, :], in0=c_iota[:, :], scalar1=cumm[:, t, ex:ex + 1], scalar2=None, op0=ALU.is_equal),
                         reads=[b_c_iota, b_cumm], writes=[b_Sel])
                pB, bpB = ps()
                pBh = pB.bitcast(BF16)
                for t in range(8):
                    S.op("pe", lambda e, pBh=pBh, t=t: e.transpose(out=pBh[:, 128 * t:128 * t + 128], in_=Sel[:, t, :], identity=idh[:, :]),
                         reads=[b_Sel, b_idh], writes=[bpB])
                copy(evac_eng(), SelT[:, :], pBh[:, :], [bpB], [b_SelT])
                for g4 in range(4):
                    pX_, bpX_ = ps()
                    for j in range(4):
                        kc = 4 * g4 + j
                        for t in range(8):
                            S.op("pe", lambda e, pX_=pX_, kc=kc, j=j, t=t: e.matmul(pX_[:, 128 * j:128 * j + 128], lhsT=h1_bf[:, t, 128 * kc:128 * kc + 128], rhs=Sel[:, t, :],
                                                                                 start=(t == 0), stop=(t == 7)), reads=[b_h1_bf, b_Sel], writes=[bpX_])
                    copy(evac_eng(), XeT[:, 4 * g4:4 * g4 + 4, :], pX_[:, :].rearrange("p (k t) -> p k t", t=128), [bpX_], [b_XeT])
                for which, wsrc in enumerate((w_gate, w_up)):
                    for blk in range(3):
                        c0 = 512 * blk
                        ncol = min(512, DFF - c0)
                        wt_, bw_ = wgu[gi_[0] % 3]; gi_[0] += 1
                        S.dma("pool", wt_[:, :, 0:ncol], wsrc[ex, :, c0:c0 + ncol].rearrange("(k p) n -> p k n", p=128), writes=[bw_])
                        pG, bpG = ps()
                        for kc in range(16):
                            S.op("pe", lambda e, pG=pG, wt_=wt_, kc=kc: e.matmul(pG[:, 0:ncol], lhsT=XeT[:, kc, :], rhs=wt_[:, kc, 0:ncol], start=(kc == 0), stop=(kc == 15)),
                                 reads=[b_XeT, bw_], writes=[bpG])
                        if which == 0:
                            S.op("act", lambda e, pG=pG, c0=c0: e.activation(out=sg[:, c0:c0 + ncol], in_=pG[:, 0:ncol], func=AF.Silu), reads=[bpG], writes=[b_sg])
                            if blk == 2:
                                while pending:
                                    pending.pop(0)()
                        else:
                            S.op("dve", lambda e, pG=pG, c0=c0: e.tensor_tensor(out=Hs[:, c0:c0 + ncol], in0=sg[:, c0:c0 + ncol], in1=pG[:, 0:ncol], op=ALU.mult),
                                 reads=[b_sg, bpG], writes=[b_Hs])
                for g3 in range(3):
                    pH, bpH = ps()
                    pHh = pH.bitcast(BF16)
                    nj = min(4, 11 - 4 * g3)
                    for j in range(nj):
                        fc = 4 * g3 + j
                        S.op("pe", lambda e, pHh=pHh, j=j, fc=fc: e.transpose(out=pHh[:, 128 * j:128 * j + 128], in_=Hs[:, 128 * fc:128 * fc + 128], identity=idh[:, :]),
                             reads=[b_Hs, b_idh], writes=[bpH])
                    copy(evac_eng(), HT[:, 4 * g3:4 * g3 + nj, :], pHh[:, 0:128 * nj].rearrange("p (k t) -> p k t", t=128), [bpH], [b_HT])
                for cb_ in range(4):
                    cs = slice(512 * cb_, 512 * cb_ + 512)
                    wd_, bwd_ = wdn[di_[0] % 2]; di_[0] += 1
                    S.dma("pool", wd_[:, :, :], w_down[ex, :, cs].rearrange("(k p) n -> p k n", p=128), writes=[bwd_])
                    pY, bpY = ps()
                    for fc in range(11):
                        S.op("pe", lambda e, pY=pY, fc=fc, wd_=wd_: e.matmul(pY[:, :], lhsT=HT[:, fc, :], rhs=wd_[:, fc, :], start=(fc == 0), stop=(fc == 10)),
                             reads=[b_HT, bwd_], writes=[bpY])
                    copy(evac_eng(), Yb[:, cs], pY[:, :], [bpY], [b_Yb])
                def combine(ex=ex, SelT=SelT, b_SelT=b_SelT, Yb=Yb, b_Yb=b_Yb):
                    for t in range(8):
                        for cb_ in range(4):
                            cs = slice(512 * cb_, 512 * cb_ + 512)
                            pZ, bpZ = ps()
                            S.op("pe", lambda e, pZ=pZ, t=t, cs=cs: e.matmul(pZ[:, :], lhsT=SelT[:, 128 * t:128 * t + 128], rhs=Yb[:, cs], start=True, stop=True),
                                 reads=[b_SelT, b_Yb], writes=[bpZ])
                            S.op("dve", lambda e, pZ=pZ, t=t, cs=cs: e.scalar_tensor_tensor(out=y_acc[:, t, cs], in0=pZ[:, :], scalar=Wt_all[:, t, ex:ex + 1],
                                                                                          in1=y_acc[:, t, cs], op0=ALU.mult, op1=ALU.add),
                                 reads=[bpZ, b_Wt, b_y_acc], writes=[b_y_acc])
                pending.append(combine)
            while pending:
                pending.pop(0)()
            e6a.close()
            e6 = e6_outer
            S.barrier()
            cG2, b_cG2 = sb(e6, [128, D]); cB2, b_cB2 = sb(e6, [128, D])
            S.dma("sp", cG2[:, :], ln2g_b, writes=[b_cG2]); S.dma("sp", cB2[:, :], ln2b_b, writes=[b_cB2])
            h1r = [sb(e6, [128, D]) for _ in range(2)]
            st, b_st = sb(e6, [128, 24]); mvt, b_mv = sb(e6, [128, 2]); sd, b_sd = sb(e6, [128, 2])
            for t in range(8):
                tk = slice(128 * t, 128 * t + 128)
                ht_, b_ht_ = h1r[t % 2]
                if DEBUG:
                    S.dma("sp", dbg["dbg_y2"][tk, :], y_acc[:, t, :], reads=[b_y_acc], final=True)
                S.dma("sp", ht_[:, :], h1_d[tk, :], reads=[b_h1_d], writes=[b_ht_])
                S.op("dve", lambda e, t=t, ht_=ht_: e.scalar_tensor_tensor(out=ht_[:, :], in0=ht_[:, :], scalar=ALPHA, in1=y_acc[:, t, :], op0=ALU.mult, op1=ALU.add),
                     reads=[b_ht_, b_y_acc], writes=[b_ht_])
                layer_norm_stats(ht_, b_ht_, st, b_st, mvt, b_mv, sd, b_sd)
                S.op("act", lambda e, ht_=ht_: e.activation(out=ht_[:, :], in_=ht_[:, :], func=AF.Identity, scale=sd[:, 0:1], bias=sd[:, 1:2]),
                     reads=[b_ht_, b_sd], writes=[b_ht_])
                S.op("dve", lambda e, ht_=ht_: e.tensor_tensor(out=ht_[:, :], in0=ht_[:, :], in1=cG2[:, :], op=ALU.mult), reads=[b_ht_, b_cG2], writes=[b_ht_])
                S.op("dve", lambda e, ht_=ht_: e.tensor_tensor(out=ht_[:, :], in0=ht_[:, :], in1=cB2[:, :], op=ALU.add), reads=[b_ht_, b_cB2], writes=[b_ht_])
                S.dma("sp", out_d[tk, :], ht_[:, :], reads=[b_ht_], final=True)
        S.barrier()
        S.finish()
    return nc


def _bcast(v, n=128):
    v = np.asarray(v, np.float32).reshape(1, -1)
    return np.ascontiguousarray(np.broadcast_to(v, (n, v.shape[1])))


def prep_inputs(inp):
    f32 = np.float32
    x = np.asarray(inp["x"], f32)
    shared = {}
    shared["w_in"] = np.ascontiguousarray(np.asarray(inp["w_in"], f32)[0])
    g = np.asarray(inp["ln_in_g"], f32); b = np.asarray(inp["ln_in_b"], f32)
    shared["lng_col"] = np.ascontiguousarray(g.reshape(16, 128).T); shared["lnb_col"] = np.ascontiguousarray(b.reshape(16, 128).T)
    shared["lng_b"] = _bcast(g); shared["lnb_b"] = _bcast(b)
    cwv = np.asarray(inp["m_conv_w"], f32)[0]
    shared["cw"] = np.ascontiguousarray(cwv.reshape(4, 8, 128).transpose(2, 1, 0))
    shared["cb"] = np.ascontiguousarray(np.asarray(inp["m_conv_b"], f32)[0].reshape(8, 128).T)
    shared["ifb"] = np.ascontiguousarray(np.asarray(inp["m_if_bias"], f32)[0].reshape(2, 4).T)
    shared["mnw_b"] = _bcast(np.asarray(inp["m_norm_w"], f32)[0].reshape(-1))
    shared["w_pa"] = np.ascontiguousarray(np.asarray(inp["w_proj_att"], f32)[0])
    shared["w_pm"] = np.ascontiguousarray(np.asarray(inp["w_proj_mlstm"], f32)[0])
    shared["w_out"] = np.ascontiguousarray(np.asarray(inp["w_out"], f32)[0])
    shared["ln1g_b"] = _bcast(np.asarray(inp["ln1_g"], f32)[0]); shared["ln1b_b"] = _bcast(np.asarray(inp["ln1_b"], f32)[0])
    shared["ln2g_b"] = _bcast(np.asarray(inp["ln2_g"], f32)[0]); shared["ln2b_b"] = _bcast(np.asarray(inp["ln2_b"], f32)[0])
    wrc = np.concatenate([np.asarray(inp["w_router_group"], f32)[0], np.asarray(inp["w_router_expert"], f32)[0]], axis=1)
    shared["wr"] = np.ascontiguousarray(wrc.reshape(16, 128, 36).transpose(1, 0, 2))
    shared["br_b"] = _bcast(np.concatenate([np.asarray(inp["b_router_group"], f32)[0], np.asarray(inp["b_router_expert"], f32)[0]]))
    shared["w_gate"] = np.ascontiguousarray(np.asarray(inp["w_gate"], f32)[0])
    shared["w_up"] = np.ascontiguousarray(np.asarray(inp["w_up"], f32)[0])
    shared["w_down"] = np.ascontiguousarray(np.asarray(inp["w_down"], f32)[0])
    slopes = alibi_slopes(12)
    qi = np.arange(128)[:, None]; ki = np.arange(256)[None, :]
    delta = 128 + qi - ki
    ab = np.zeros((12, 128, 256), f32)
    for h in range(12):
        r = (1, 4, 16)[h // 4]
        ab[h] = np.where((delta >= 0) & (delta <= 128), -slopes[h] * (delta * r).astype(f32), NEG)
    shared["abias"] = ab
    shared["ident_f"] = np.eye(128, dtype=f32)
    shared["ident_h"] = np.eye(128, dtype=f32).astype(ml_dtypes.bfloat16)
    s_ = np.arange(128)[:, None]; l_ = np.arange(128)[None, :]
    shared["tri_neg"] = np.where(s_ <= l_, 0.0, NEG).astype(f32)
    shared["ltri"] = (s_ <= l_).astype(f32)
    shared["ones_f"] = np.ones((128, 128), f32)
    shared["iota1"] = np.ascontiguousarray(np.broadcast_to(np.arange(1, 129, dtype=f32)[None, :], (128, 128)))
    shared["iota1c"] = np.arange(1, 129, dtype=f32).reshape(128, 1)
    sel4 = np.zeros((4, 4, 128), f32)
    for k in range(4):
        sel4[k, k, :] = 1.0
    shared["sel4"] = sel4
    sel32 = np.zeros((32, 32, 128), f32)
    for k in range(32):
        sel32[k, k, :] = 1.0
    shared["sel32"] = sel32
    in_maps = []
    for c in range(8):
        b_, k_ = c // 4, c % 4
        base = 1024 * k_ - 3072
        xw = np.zeros((NW, D), f32)
        lo = max(0, -base)
        xw[lo:] = x[b_, base + lo: base + NW]
        valid = (np.arange(NW) + base >= 0).astype(f32)
        m = dict(shared)
        m["x_win"] = xw
        m["padneg_b"] = _bcast((valid - 1.0) * (-NEG))
        m["valid_b"] = _bcast(valid)
        gm = np.zeros((4, 2, NW), f32)
        gm[:, 0, :] = (1.0 - valid) * NEG
        gm[:, 1, :] = (1.0 - valid) * (-NEG)
        m["gmask"] = gm
        in_maps.append(m)
    return in_maps


_NC = None


def kernel(**inputs):
    global _NC
    if _NC is None:
        _NC = build_program()
    in_maps = prep_inputs(inputs)
    res = run_bass_kernel_spmd(_NC, in_maps, core_ids=list(range(8)))
    out = np.zeros((2, 4096, D), np.float32)
    for c in range(8):
        b_, k_ = c // 4, c % 4
        out[b_, 1024 * k_:1024 * k_ + 1024] = res.results[c]["out"]
    return out
```

```python
import math
from contextlib import ExitStack
import numpy as np
import ml_dtypes
import concourse.bass as bass
import concourse.mybir as mybir
from concourse.bass_utils import run_bass_kernel_spmd

F32 = mybir.dt.float32
BF16 = mybir.dt.bfloat16
AF = mybir.ActivationFunctionType
ALU = mybir.AluOpType
AX = mybir.AxisListType

D = 2048
NW = 4096
NOWN = 1024
NPROJ = 11784
ALPHA = 2 ** 0.25
EPS = 1e-5
NEG = -30000.0
DFF = 1408
NEXP = 32
DEBUG = False


class Buf:
    __slots__ = ("w", "r")

    def __init__(self):
        self.w = None
        self.r = []


class Sched:
    ENG = ("pe", "act", "dve", "pool", "sp")

    def __init__(self, nc, es, n_dma_sems=20):
        self.nc = nc
        self.eobj = {"pe": nc.tensor, "act": nc.scalar, "dve": nc.vector, "pool": nc.gpsimd, "sp": nc.sync}
        self.cnt = {e: 0 for e in self.ENG}
        self.sem = {e: es.enter_context(nc.semaphore("c_" + e)) for e in self.ENG}
        self.seen = {e: {} for e in self.ENG}
        self.pending = {e: [] for e in self.ENG}
        self.dsems, self.dtot, self.dnext = {}, {}, {}
        for q in ("sp", "pool", "act"):
            self.dsems[q] = [es.enter_context(nc.semaphore(f"d_{q}_{i}")) for i in range(n_dma_sems)]
            self.dnext[q] = 0
        self.final_events = []

    def _deps(self, reads, writes):
        deps = []
        for b in reads:
            if b.w is not None:
                deps.append(b.w)
        for b in writes:
            if b.w is not None:
                deps.append(b.w)
            deps.extend(b.r)
        return deps

    def _filter(self, eng, deps):
        seen = self.seen[eng]
        own = self.sem[eng]
        best = {}
        for (sem, val) in deps + self.pending[eng]:
            if sem is own and eng in ("pe", "sp"):
                continue
            k = id(sem)
            if seen.get(k, 0) >= val:
                continue
            seen[k] = val
            best[k] = (sem, val)
        self.pending[eng] = []
        return list(best.values())

    def _emit(self, ename, waits, fn, sem, inc):
        eng = self.eobj[ename]
        for (s_, v) in waits:
            eng.wait_ge(s_, v)
        fn(eng).then_inc(sem, inc)

    def op(self, eng, fn, reads=(), writes=()):
        waits = self._filter(eng, self._deps(reads, writes))
        self.cnt[eng] += 1
        ev = (self.sem[eng], self.cnt[eng])
        for b in reads:
            b.r.append(ev)
        for b in writes:
            b.w = ev
            b.r = []
        self._emit(eng, waits, fn, self.sem[eng], 1)
        return ev

    def dma(self, q, out, in_, reads=(), writes=(), final=False):
        deps = self._deps(reads, writes)
        i = self.dnext[q]
        self.dnext[q] = (i + 1) % len(self.dsems[q])
        dsem = self.dsems[q][i]
        prev = self.dtot.get(id(dsem), 0)
        if prev:
            deps.append((dsem, prev))
        waits = self._filter(q, deps)
        tot = prev + 16
        self.dtot[id(dsem)] = tot
        ev = (dsem, tot)
        for b in reads:
            b.r.append(ev)
        for b in writes:
            b.w = ev
            b.r = []
        self._emit(q, waits, (lambda e: e.dma_start(out=out, in_=in_)), dsem, 16)
        if final:
            self.final_events.append(ev)
        return ev

    def barrier(self):
        evs = [(self.sem[e], self.cnt[e]) for e in self.ENG if self.cnt[e] > 0]
        for q in self.dsems:
            for s in self.dsems[q]:
                t = self.dtot.get(id(s), 0)
                if t:
                    evs.append((s, t))
        for e in self.ENG:
            self.pending[e].extend(evs)

    def finish(self):
        for (s_, v) in self._filter("sp", list(self.final_events)):
            self.nc.sync.wait_ge(s_, v)


def ss(start, n, r):
    return slice(start, start + (n - 1) * r + 1, r)


def alibi_slopes(n):
    def geometric(k):
        start = 2.0 ** (-8.0 / k)
        return [start ** (i + 1) for i in range(k)]
    c = 2 ** int(math.floor(math.log2(n)))
    s = geometric(c) if c == n else geometric(c) + geometric(2 * c)[0::2][: n - c]
    return np.array(sorted(s, reverse=True), dtype=np.float32)


C_AQ, C_AK, C_AV = 0, 1536, 3072
C_MQ, C_MK = 4608, 5120
C_MV, C_MO = 5632, 6656
C_IF = 7680
C_GA, C_GM = 7688, 9736


def build_program(upto=9):
    nc = bass.Bass("TRN2", target_bir_lowering=False)

    def din(name, shape, dt=F32):
        return nc.dram_tensor(name, list(shape), dt, kind="ExternalInput").ap()

    def dscr(name, shape, dt=F32):
        return nc.dram_tensor(name, list(shape), dt, kind="Internal").ap()

    x_win = din("x_win", [NW, D])
    w_in = din("w_in", [D, NPROJ])
    padneg_b = din("padneg_b", [128, NW])
    valid_b = din("valid_b", [128, NW])
    gmask = din("gmask", [4, 2, NW])
    lng_col = din("lng_col", [128, 16]); lnb_col = din("lnb_col", [128, 16])
    lng_b = din("lng_b", [128, D]); lnb_b = din("lnb_b", [128, D])
    cw = din("cw", [128, 8, 4]); cb = din("cb", [128, 8])
    ifb = din("ifb", [4, 2])
    mnw_b = din("mnw_b", [128, 1024])
    w_pa = din("w_pa", [512, D]); w_pm = din("w_pm", [1024, D]); w_out = din("w_out", [D, D])
    ln1g_b = din("ln1g_b", [128, D]); ln1b_b = din("ln1b_b", [128, D])
    ln2g_b = din("ln2g_b", [128, D]); ln2b_b = din("ln2b_b", [128, D])
    wr = din("wr", [128, 16, 36]); br_b = din("br_b", [128, 36])
    if upto >= 6:
        w_gate = din("w_gate", [NEXP, D, DFF]); w_up = din("w_up", [NEXP, D, DFF]); w_down = din("w_down", [NEXP, DFF, D])
    abias = din("abias", [12, 128, 256])
    ident_f = din("ident_f", [128, 128]); ident_h = din("ident_h", [128, 128], BF16)
    tri_neg = din("tri_neg", [128, 128])
    ltri = din("ltri", [128, 128])
    ones_f = din("ones_f", [128, 128])
    iota1 = din("iota1", [128, 128])
    iota1c = din("iota1c", [128, 1])
    sel4 = din("sel4", [4, 4, 128])
    sel32 = din("sel32", [32, 32, 128])
    out_d = nc.dram_tensor("out", [NOWN, D], F32, kind="ExternalOutput").ap()

    h_d = dscr("h_d", [NOWN, D])
    h1_d = dscr("h1_d", [NOWN, D])
    kT_d = dscr("kT_d", [12, 128, NW], BF16)
    qT_d = dscr("qT_d", [12, 128, NOWN], BF16)
    v_d = dscr("v_d", [NW, 1536], BF16)
    hT_d = dscr("hT_d", [16, 128, NOWN], BF16)
    b_hT_d = Buf()
    hmT_d = dscr("hmT_d", [8, 128, NOWN], BF16); attT_d = dscr("attT_d", [4, 128, NOWN], BF16)
    mT_d = dscr("mT_d", [16, 128, NOWN], BF16); h1bf_d = dscr("h1bf_d", [NOWN, D], BF16)
    b_hmT_d, b_attT_d, b_mT_d, b_h1bf_d = (Buf() for _ in range(4))
    o_d = dscr("o_d", [3, NOWN, 512])
    lse_d = dscr("lse_d", [3, NOWN, 4])
    b_h_d, b_h1_d, b_kT_d, b_qT_d, b_v_d, b_o_d, b_lse_d = (Buf() for _ in range(7))
    dbg = {}
    if DEBUG:
        for nm, shp in (("dbg_hm", [NOWN, 1024]), ("dbg_att", [NOWN, 512]), ("dbg_h1", [NOWN, D]), ("dbg_y2", [NOWN, D])):
            dbg[nm] = nc.dram_tensor(nm, shp, F32, kind="ExternalOutput").ap()

    with ExitStack() as es:
        S = Sched(nc, es)
        cnt = [0]

        def sb(es_, shape, dt=F32):
            cnt[0] += 1
            t = es_.enter_context(nc.sbuf_tensor(f"t{cnt[0]}", list(shape), dt))
            return t, Buf()

        psT = [es.enter_context(nc.psum_tensor(f"ps{i}", [128, 512], F32)) for i in range(8)]
        psB = [Buf() for _ in range(8)]
        psi = [0]

        ps_split = [False]
        psr_i = [0]

        def ps():
            if ps_split[0]:
                i = 4 + (psi[0] % 4)
                psi[0] = (psi[0] + 1) % 4
                return psT[i], psB[i]
            i = psi[0]
            psi[0] = (i + 1) % 8
            return psT[i], psB[i]

        def psr():
            i = psr_i[0]
            psr_i[0] = (i + 1) % 4
            return psT[i], psB[i]

        rr = [0]

        def evac_eng():
            rr[0] += 1
            return "act" if rr[0] % 2 else "dve"

        def copy(eng, out, in_, reads, writes):
            if eng == "act":
                S.op("act", lambda e: e.copy(out=out, in_=in_), reads, writes)
            else:
                S.op(eng, lambda e: e.tensor_copy(out=out, in_=in_), reads, writes)

        def load_const(es_, src, shape, dt=F32, q="sp"):
            t, b = sb(es_, shape, dt)
            idx = tuple(slice(None) for _ in shape)
            S.dma(q, t[idx], src, writes=[b])
            return t, b

        idf, b_idf = load_const(es, ident_f, [128, 128])
        idh, b_idh = load_const(es, ident_h, [128, 128], BF16)
        bC = Buf()
        def lc(src, shape, dt=F32):
            t, _ = sb(es, shape, dt)
            idx = tuple(slice(None) for _ in shape)
            S.dma("sp", t[idx], src, writes=[bC])
            return t
        c_lng = lc(lng_col, [128, 16]); c_lnb = lc(lnb_col, [128, 16])

        def layer_norm_stats(x_ap, b_x, st, b_st, mvt, b_mv, sd, b_sd, nchunk=4, csz=512):
            for i in range(nchunk):
                S.op("dve", lambda e, i=i: e.bn_stats(out=st[:, 6 * i:6 * i + 6], in_=x_ap[:, csz * i:csz * i + csz]),
                     reads=[b_x], writes=[b_st])
            S.op("dve", lambda e: e.bn_aggr(out=mvt[:, 0:2], in_=st[:, 0:6 * nchunk]), reads=[b_st], writes=[b_mv])
            S.op("dve", lambda e: e.tensor_scalar(out=sd[:, 0:1], in0=mvt[:, 1:2], scalar1=EPS, scalar2=None, op0=ALU.add),
                 reads=[b_mv], writes=[b_sd])
            S.op("act", lambda e: e.activation(out=sd[:, 0:1], in_=sd[:, 0:1], func=AF.Sqrt), reads=[b_sd], writes=[b_sd])
            S.op("dve", lambda e: e.reciprocal(out=sd[:, 0:1], in_=sd[:, 0:1]), reads=[b_sd], writes=[b_sd])
            S.op("dve", lambda e: e.tensor_scalar(out=sd[:, 1:2], in0=mvt[:, 0:1], scalar1=sd[:, 0:1], scalar2=-1.0,
                                                  op0=ALU.mult, op1=ALU.mult), reads=[b_mv, b_sd], writes=[b_sd])

        Wt_all, b_Wt = sb(es, [128, 8, 32]); mk_all, b_mk = sb(es, [128, 8, 32]); cumm, b_cumm = sb(es, [128, 8, 32])

        with ExitStack() as e1:
            bC1 = Buf()
            def lc1(src, shape, dt=F32):
                t, _ = sb(e1, shape, dt)
                idx = tuple(slice(None) for _ in shape)
                S.dma("sp", t[idx], src, writes=[bC1])
                return t
            c_cw = lc1(cw, [128, 8, 4]); c_cb = lc1(cb, [128, 8]); c_ifb = lc1(ifb, [4, 2])
            c_mnw = lc1(mnw_b, [128, 1024]); c_tri = lc1(tri_neg, [128, 128]); c_sel4 = lc1(sel4, [4, 4, 128])
            xs = [sb(e1, [128, D]) for _ in range(2)]
            xn_l = [sb(e1, [128, D]) for _ in range(2)]
            ln_l = [(sb(e1, [128, 24]), sb(e1, [128, 2]), sb(e1, [128, 2])) for _ in range(2)]
            hT_st, b_hT_st = sb(e1, [128, 16, 512], BF16)
            wbuf = [sb(e1, [128, 16, 512], BF16) for _ in range(2)]
            wif, b_wif = sb(e1, [128, 16, 8], BF16)
            S.dma("pool", wif[:, :, :], w_in[:, C_IF:C_IF + 8].rearrange("(k p) n -> p k n", p=128), writes=[b_wif])
            wi = [0]
            stage_o, b_stage_o = sb(e1, [128, 4, 512], BF16)
            stage_t, b_stage_t = sb(e1, [128, 4, 512], BF16)
            halo, b_halo = sb(e1, [128, 8, 3])
            S.op("pool", lambda e: e.memset(halo[:, :, :], 0.0), writes=[b_halo])
            rw, b_rw = sb(e1, [128, 515])
            cacc, b_cacc = sb(e1, [128, 512])
            kT_st, b_kT_st = sb(e1, [128, 4, 512], BF16)
            qT_st, b_qT_st = sb(e1, [128, 4, 512], BF16)
            vext, b_vext = sb(e1, [128, 4, 4, 258], BF16)
            S.op("pool", lambda e: e.memset(vext[:, :, :, 256:257], 1.0), writes=[b_vext])
            S.op("pool", lambda e: e.memset(vext[:, :, :, 257:258], 0.0), writes=[b_vext])
            smo, b_smo = sb(e1, [128, 4, 1024], BF16)
            gI, b_gI = sb(e1, [4, 512]); gF, b_gF = sb(e1, [4, 512])
            gB, b_gB = sb(e1, [4, 512]); gMg, b_gMg = sb(e1, [4, 512])
            gm_sb, b_gm = sb(e1, [4, 2, 512])
            QS, b_QS = sb(e1, [128, 512])
            S.op("pool", lambda e: e.memset(QS[:, :], 0.0), writes=[b_QS])
            vmask, b_vmask = sb(e1, [128, 512])
            ones512, b_ones512 = sb(e1, [4, 512])
            S.op("pool", lambda e: e.memset(ones512[:, :], 1.0), writes=[b_ones512])
            carryB, b_cB = sb(e1, [4, 1]); carryM, b_cM = sb(e1, [4, 1])
            S.op("pool", lambda e: e.memset(carryB[:, :], 0.0), writes=[b_cB])
            S.op("pool", lambda e: e.memset(carryM[:, :], 0.0), writes=[b_cM])
            QT, b_QT = sb(e1, [128, 4, 128])
            NMGB, b_NMGB = sb(e1, [128, 4, 513])
            S.op("pool", lambda e: e.memset(NMGB[:, :, :], 0.0), writes=[b_NMGB])
            Cst, b_Cst = sb(e1, [128, 4, 258]); Cbf, b_Cbf = sb(e1, [128, 4, 258], BF16)
            S.op("pool", lambda e: e.memset(Cst[:, :, :], 0.0), writes=[b_Cst])
            S.op("pool", lambda e: e.memset(Cbf[:, :, :], 0.0), writes=[b_Cbf])
            sc_l = [sb(e1, [128, 16]) for _ in range(4)]
            kw_l = [sb(e1, [128, 128], BF16) for _ in range(4)]
            arg_l = [sb(e1, [128, 128]) for _ in range(4)]; Pt_l = [sb(e1, [128, 128], BF16) for _ in range(4)]
            intra_l = [sb(e1, [128, 258]) for _ in range(4)]; Rn_l = [sb(e1, [128, 258]) for _ in range(4)]
            hraw_l = [sb(e1, [128, 256]) for _ in range(4)]
            hm_t, b_hm_t = sb(e1, [128, 1024]); hm_h, b_hm_h = sb(e1, [128, 1024], BF16)
            hmT_c, b_hmT_c = sb(e1, [128, 8, 128], BF16)
            st2_l = [sb(e1, [128, 6]) for _ in range(4)]; mv2_l = [sb(e1, [128, 2]) for _ in range(4)]; sd2_l = [sb(e1, [128, 2]) for _ in range(4)]

            def wload(c0, ncols):
                t, b = wbuf[wi[0] % 2]
                wi[0] += 1
                S.dma("pool", t[:, :, 0:ncols], w_in[:, c0:c0 + ncols].rearrange("(k p) n -> p k n", p=128), writes=[b])
                return t, b

            hook_box = [None]

            def proj_fm(wt, bw, ncol_chunks, consume):
                for cc in range(ncol_chunks):
                    p_, bp = ps()
                    for kc in range(16):
                        S.op("pe", lambda e, kc=kc, cc=cc, p_=p_: e.matmul(p_[:, :], lhsT=wt[:, kc, 128 * cc:128 * cc + 128],
                                                                         rhs=hT_st[:, kc, :], start=(kc == 0), stop=(kc == 15)),
                             reads=[bw, b_hT_st], writes=[bp])
                    consume(cc, p_, bp)
                    if hook_box[0] is not None:
                        hook_box[0]()

            def proj_tm(wt, bw, ncols, consume):
                for tt in range(4):
                    p_, bp = ps()
                    for kc in range(16):
                        S.op("pe", lambda e, kc=kc, tt=tt, p_=p_: e.matmul(p_[:, 0:ncols], lhsT=hT_st[:, kc, 128 * tt:128 * tt + 128],
                                                                         rhs=wt[:, kc, 0:ncols], start=(kc == 0), stop=(kc == 15)),
                             reads=[bw, b_hT_st], writes=[bp])
                    consume(tt, p_, bp)
                    if hook_box[0] is not None:
                        hook_box[0]()

            for sti in range(8):
                own = sti >= 6
                t0 = 512 * sti
                def ln_A(tt):
                    w0 = t0 + 128 * tt
                    xt, b_xt = xs[tt % 2]
                    xn, b_xn = xn_l[tt % 2]
                    (st, b_st), (mvt, b_mv), (sd, b_sd) = ln_l[tt % 2]
                    S.dma("sp", xt[:, :], x_win[w0:w0 + 128, :], writes=[b_xt])
                    layer_norm_stats(xt, b_xt, st, b_st, mvt, b_mv, sd, b_sd)
                    S.op("act", lambda e: e.activation(out=xn[:, :], in_=xt[:, :], func=AF.Identity, scale=sd[:, 0:1], bias=sd[:, 1:2]),
                         reads=[b_xt, b_sd], writes=[b_xn])
                    if own:
                        o0 = w0 - 3072
                        S.dma("sp", h_d[o0:o0 + 128, :], xn[:, :], reads=[b_xn], writes=[b_h_d])

                def ln_B(tt):
                    xn, b_xn = xn_l[tt % 2]
                    for g in range(4):
                        p_, bp = ps()
                        for j in range(4):
                            kc = 4 * g + j
                            S.op("pe", lambda e, kc=kc, j=j, p_=p_: e.transpose(out=p_[:, 128 * j:128 * j + 128],
                                                                              in_=xn[:, 128 * kc:128 * kc + 128], identity=idf[:, :]),
                                 reads=[b_xn, b_idf], writes=[bp])
                        for j in range(4):
                            kc = 4 * g + j
                            eng = evac_eng()
                            if eng == "act":
                                S.op("act", lambda e, kc=kc, j=j, p_=p_: e.activation(
                                    out=hT_st[:, kc, 128 * tt:128 * tt + 128], in_=p_[:, 128 * j:128 * j + 128], func=AF.Identity,
                                    scale=c_lng[:, kc:kc + 1], bias=c_lnb[:, kc:kc + 1]), reads=[bp, bC], writes=[b_hT_st])
                            else:
                                S.op("dve", lambda e, kc=kc, j=j, p_=p_: e.tensor_scalar(
                                    out=hT_st[:, kc, 128 * tt:128 * tt + 128], in0=p_[:, 128 * j:128 * j + 128],
                                    scalar1=c_lng[:, kc:kc + 1], scalar2=c_lnb[:, kc:kc + 1], op0=ALU.mult, op1=ALU.add),
                                     reads=[bp, bC], writes=[b_hT_st])
                ln_A(0); ln_A(1); ln_B(0); ln_A(2); ln_B(1); ln_A(3); ln_B(2); ln_B(3)
                if own:
                    o0 = t0 - 3072
                    S.dma("sp", hT_d[:, :, o0:o0 + 512].rearrange("k p t -> p k t"), hT_st[:, :, :], reads=[b_hT_st], writes=[b_hT_d])

                S.dma("sp", gm_sb[:, :, :], gmask[:, :, t0:t0 + 512], writes=[b_gm])
                S.dma("sp", vmask[:, :], valid_b[:, t0:t0 + 512], writes=[b_vmask])
                for gi_, (dst, b_dst) in enumerate(((gI, b_gI), (gF, b_gF))):
                    p_, bp = ps()
                    for kc in range(16):
                        S.op("pe", lambda e, kc=kc, p_=p_, gi_=gi_: e.matmul(p_[0:4, :], lhsT=wif[:, kc, 4 * gi_:4 * gi_ + 4],
                                                                           rhs=hT_st[:, kc, :], start=(kc == 0), stop=(kc == 15)),
                             reads=[b_wif, b_hT_st], writes=[bp])
                    S.op("dve", lambda e, p_=p_, gi_=gi_, dst=dst: e.scalar_tensor_tensor(
                        out=dst[:, :], in0=p_[0:4, :], scalar=c_ifb[:, gi_:gi_ + 1], in1=gm_sb[:, gi_, :], op0=ALU.add, op1=ALU.add),
                         reads=[bp, bC1, b_gm], writes=[b_dst])
                S.op("act", lambda e: e.activation(out=gF[:, :], in_=gF[:, :], func=AF.Exp, scale=-1.0), reads=[b_gF], writes=[b_gF])
                S.op("dve", lambda e: e.tensor_scalar(out=gF[:, :], in0=gF[:, :], scalar1=1.0, scalar2=None, op0=ALU.add),
                     reads=[b_gF], writes=[b_gF])
                S.op("act", lambda e: e.activation(out=gF[:, :], in_=gF[:, :], func=AF.Ln), reads=[b_gF], writes=[b_gF])
                S.op("dve", lambda e: e.tensor_tensor_scan(out=gB[:, :], data0=ones512[:, :], data1=gF[:, :], initial=carryB[:, 0:1],
                                                           op0=ALU.mult, op1=ALU.subtract),
                     reads=[b_gF, b_cB, b_ones512], writes=[b_gB])
                S.op("dve", lambda e: e.tensor_copy(out=carryB[:, :], in_=gB[:, 511:512]), reads=[b_gB], writes=[b_cB])
                S.op("dve", lambda e: e.tensor_tensor(out=QS[0:4, :], in0=gI[:, :], in1=gB[:, :], op=ALU.subtract),
                     reads=[b_gI, b_gB], writes=[b_QS])
                S.op("dve", lambda e: e.tensor_tensor_scan(out=gMg[:, :], data0=QS[0:4, :], data1=QS[0:4, :], initial=carryM[:, 0:1],
                                                           op0=ALU.max, op1=ALU.max),
                     reads=[b_QS, b_cM], writes=[b_gMg])
                S.op("dve", lambda e: e.tensor_copy(out=carryM[:, :], in_=gMg[:, 511:512]), reads=[b_gMg], writes=[b_cM])
                S.op("dve", lambda e: e.tensor_scalar(out=QS[32:36, :], in0=gMg[:, :], scalar1=-1.0, scalar2=None, op0=ALU.mult),
                     reads=[b_gMg], writes=[b_QS])
                S.op("dve", lambda e: e.tensor_tensor(out=gI[:, :], in0=gB[:, :], in1=gMg[:, :], op=ALU.add),
                     reads=[b_gB, b_gMg], writes=[b_gI])
                S.op("act", lambda e: e.activation(out=QS[64:68, :], in_=gI[:, :], func=AF.Exp, scale=-1.0), reads=[b_gI], writes=[b_QS])
                S.op("dve", lambda e: e.tensor_scalar(out=gF[:, :], in0=gMg[:, :], scalar1=-1.0, scalar2=None, op0=ALU.mult),
                     reads=[b_gMg], writes=[b_gF])
                for which in ((1, 0) if sti >= 5 else (1,)):
                    wt, bw = wload(C_MK if which else C_MQ, 512)
                    dstT, b_dstT = (kT_st, b_kT_st) if which else (qT_st, b_qT_st)
                    def cons_c(cc, p_, bp, which=which, dstT=dstT, b_dstT=b_dstT):
                        ch = 4 * which + cc
                        S.op("pool", lambda e: e.tensor_copy(out=rw[:, 0:3], in_=halo[:, ch, :]), reads=[b_halo], writes=[b_rw])
                        S.op("dve", lambda e: e.tensor_tensor(out=rw[:, 3:515], in0=p_[:, :], in1=vmask[:, :], op=ALU.mult),
                             reads=[bp, b_vmask], writes=[b_rw])
                        S.op("dve", lambda e: e.tensor_scalar(out=cacc[:, :], in0=rw[:, 3:515], scalar1=c_cw[:, ch, 3:4],
                                                              scalar2=c_cb[:, ch:ch + 1], op0=ALU.mult, op1=ALU.add),
                             reads=[b_rw, bC1], writes=[b_cacc])
                        for j in range(3):
                            S.op("dve", lambda e, j=j: e.scalar_tensor_tensor(out=cacc[:, :], in0=rw[:, j:j + 512],
                                                                              scalar=c_cw[:, ch, j:j + 1], in1=cacc[:, :],
                                                                              op0=ALU.mult, op1=ALU.add),
                                 reads=[b_rw, bC1, b_cacc], writes=[b_cacc])
                        S.op("act", lambda e: e.activation(out=dstT[:, cc, :], in_=cacc[:, :], func=AF.Silu),
                             reads=[b_cacc], writes=[b_dstT])
                        S.op("pool", lambda e: e.tensor_copy(out=halo[:, ch, :], in_=rw[:, 512:515]), reads=[b_rw], writes=[b_halo])
                    proj_fm(wt, bw, 4, cons_c)
                for half in range(2):
                    wt, bw = wload(C_MV + 512 * half, 512)
                    def cons_mv(tt, p_, bp, half=half):
                        copy(evac_eng(), vext[:, tt, 2 * half:2 * half + 2, 0:256],
                             p_[:, :].rearrange("p (h c) -> p h c", c=256), [bp], [b_vext])
                    proj_tm(wt, bw, 512, cons_mv)
                if own:
                    for half in range(2):
                        wt, bw = wload(C_MO + 512 * half, 512)
                        def cons_mo(tt, p_, bp, half=half):
                            S.op("act", lambda e: e.activation(out=smo[:, tt, 512 * half:512 * half + 512], in_=p_[:, :], func=AF.Sigmoid),
                                 reads=[bp], writes=[b_smo])
                        proj_tm(wt, bw, 512, cons_mo)
                S.op("dve", lambda e: e.tensor_copy(out=NMGB[:, :, 0:1], in_=NMGB[:, :, 512:513]), reads=[b_NMGB], writes=[b_NMGB])
                for hd in range(4):
                    p_, bp = ps()
                    S.op("pe", lambda e, hd=hd, p_=p_: e.matmul(p_[:, :], lhsT=c_sel4[:, hd, :], rhs=gF[:, :], start=True, stop=True),
                         reads=[b_gF, bC1], writes=[bp])
                    copy(evac_eng(), NMGB[:, hd, 1:513], p_[:, :], [bp], [b_NMGB])
                for c in range(4):
                    p_, bp = ps()
                    S.op("pe", lambda e, c=c, p_=p_: e.transpose(out=p_[:, 0:128], in_=QS[:, 128 * c:128 * c + 128], identity=idf[:, :]),
                         reads=[b_QS, b_idf], writes=[bp])
                    copy(evac_eng(), QT[:, c, :], p_[:, 0:128], [bp], [b_QT])

                def rec_gen():
                    HD = range(4)
                    for c in range(4):
                        cs_ = slice(128 * c, 128 * c + 128)
                        def nmgL(hd): return NMGB[:, hd, 128 * c + 128:128 * c + 129]
                        def nmgS(hd): return NMGB[:, hd, 128 * c:128 * c + 1]
                        def Acol(hd): return QT[:, c, hd:hd + 1]
                        if own:
                            pS_l = {}; pI_l = {}; pX_l = {}
                            yield
                            for hd in HD:
                                pS, bpS = psr(); pS_l[hd] = (pS, bpS)
                                S.op("pe", lambda e, pS=pS, hd=hd: e.matmul(pS[:, 0:128], lhsT=kT_st[:, hd, cs_], rhs=qT_st[:, hd, cs_], start=True, stop=True),
                                     reads=[b_kT_st, b_qT_st], writes=[bpS])
                            yield
                            for hd in HD:
                                arg, b_arg = arg_l[hd]
                                S.op("dve", lambda e, hd=hd, arg=arg: e.scalar_tensor_tensor(
                                    out=arg[:, :], in0=NMGB[:, hd, 128 * c + 1:128 * c + 129], scalar=Acol(hd), in1=c_tri[:, :],
                                    op0=ALU.add, op1=ALU.add), reads=[b_NMGB, b_QT, bC1], writes=[b_arg])
                            yield
                            for hd in HD:
                                arg, b_arg = arg_l[hd]
                                S.op("act", lambda e, arg=arg: e.activation(out=arg[:, :], in_=arg[:, :], func=AF.Exp), reads=[b_arg], writes=[b_arg])
                            yield
                            for hd in HD:
                                arg, b_arg = arg_l[hd]; Pt, b_Pt = Pt_l[hd]; pS, bpS = pS_l[hd]
                                S.op("dve", lambda e, pS=pS, Pt=Pt, arg=arg: e.scalar_tensor_tensor(out=Pt[:, :], in0=pS[:, 0:128], scalar=128 ** -0.5,
                                                                                                  in1=arg[:, :], op0=ALU.mult, op1=ALU.mult),
                                     reads=[bpS, b_arg], writes=[b_Pt])
                            yield
                            for hd in HD:
                                Pt, b_Pt = Pt_l[hd]
                                pI, bpI = psr(); pI_l[hd] = (pI, bpI)
                                S.op("pe", lambda e, pI=pI, hd=hd, Pt=Pt: e.matmul(pI[:, 0:258], lhsT=Pt[:, :], rhs=vext[:, c, hd, :], start=True, stop=True),
                                     reads=[b_Pt, b_vext], writes=[bpI])
                            yield
                            for hd in HD:
                                intra, b_intra = intra_l[hd]; pI, bpI = pI_l[hd]
                                S.op("act", lambda e, pI=pI, intra=intra: e.copy(out=intra[:, :], in_=pI[:, 0:258]), reads=[bpI], writes=[b_intra])
                            yield
                            for hd in HD:
                                pX, bpX = psr(); pX_l[hd] = (pX, bpX)
                                S.op("pe", lambda e, pX=pX, hd=hd: e.matmul(pX[:, 0:258], lhsT=qT_st[:, hd, cs_], rhs=Cbf[:, hd, :], start=True, stop=True),
                                     reads=[b_qT_st, b_Cbf], writes=[bpX])
                            yield
                            for hd in HD:
                                sc, b_sc = sc_l[hd]
                                S.op("dve", lambda e, hd=hd, sc=sc: e.tensor_scalar(out=sc[:, 0:1], in0=nmgS(hd), scalar1=-1.0, scalar2=None, op0=ALU.mult),
                                     reads=[b_NMGB], writes=[b_sc])
                            yield
                            for hd in HD:
                                sc, b_sc = sc_l[hd]; intra, b_intra = intra_l[hd]; pI, bpI = pI_l[hd]
                                S.op("act", lambda e, hd=hd, sc=sc: e.activation(out=sc[:, 1:2], in_=QT[:, c, 32 + hd:33 + hd], func=AF.Exp, bias=sc[:, 0:1]),
                                     reads=[b_QT, b_sc], writes=[b_sc])
                            yield
                            for hd in HD:
                                sc, b_sc = sc_l[hd]
                                S.op("dve", lambda e, sc=sc: e.tensor_scalar(out=sc[:, 1:2], in0=sc[:, 1:2], scalar1=128 ** -0.5, scalar2=None, op0=ALU.mult),
                                     reads=[b_sc], writes=[b_sc])
                            yield
                            for hd in HD:
                                sc, b_sc = sc_l[hd]; intra, b_intra = intra_l[hd]; Rn, b_Rn = Rn_l[hd]; pX, bpX = pX_l[hd]
                                S.op("dve", lambda e, pX=pX, sc=sc, intra=intra, Rn=Rn: e.scalar_tensor_tensor(out=Rn[:, :], in0=pX[:, 0:258], scalar=sc[:, 1:2],
                                                                                                             in1=intra[:, :], op0=ALU.mult, op1=ALU.add),
                                     reads=[bpX, b_sc, b_intra], writes=[b_Rn])
                            yield
                            for hd in HD:
                                sc, b_sc = sc_l[hd]; Rn, b_Rn = Rn_l[hd]
                                S.op("dve", lambda e, sc=sc, Rn=Rn: e.tensor_scalar(out=sc[:, 3:4], in0=Rn[:, 256:257], scalar1=-1.0, scalar2=None, op0=ALU.mult),
                                     reads=[b_Rn], writes=[b_sc])
                            yield
                            for hd in HD:
                                sc, b_sc = sc_l[hd]; Rn, b_Rn = Rn_l[hd]
                                S.op("dve", lambda e, sc=sc, Rn=Rn: e.tensor_tensor(out=sc[:, 2:3], in0=sc[:, 3:4], in1=Rn[:, 256:257], op=ALU.max),
                                     reads=[b_Rn, b_sc], writes=[b_sc])
                            yield
                            for hd in HD:
                                sc, b_sc = sc_l[hd]
                                S.op("dve", lambda e, hd=hd, sc=sc: e.tensor_tensor(out=sc[:, 2:3], in0=sc[:, 2:3], in1=QT[:, c, 64 + hd:65 + hd], op=ALU.max),
                                     reads=[b_sc, b_QT], writes=[b_sc])
                            yield
                            for hd in HD:
                                sc, b_sc = sc_l[hd]
                                S.op("dve", lambda e, sc=sc: e.reciprocal(out=sc[:, 2:3], in_=sc[:, 2:3]), reads=[b_sc], writes=[b_sc])
                            yield
                            for hd in HD:
                                sc, b_sc = sc_l[hd]; Rn, b_Rn = Rn_l[hd]; hraw, b_hraw = hraw_l[hd]
                                S.op("dve", lambda e, sc=sc, Rn=Rn, hraw=hraw: e.tensor_scalar(out=hraw[:, :], in0=Rn[:, 0:256], scalar1=sc[:, 2:3], scalar2=None, op0=ALU.mult),
                                     reads=[b_Rn, b_sc], writes=[b_hraw])
                            yield
                            for hd in HD:
                                hraw, b_hraw = hraw_l[hd]; st2, b_st2 = st2_l[hd]
                                S.op("dve", lambda e, hraw=hraw, st2=st2: e.bn_stats(out=st2[:, 0:6], in_=hraw[:, :]), reads=[b_hraw], writes=[b_st2])
                            yield
                            for hd in HD:
                                st2, b_st2 = st2_l[hd]; mv2, b_mv2 = mv2_l[hd]
                                S.op("dve", lambda e, st2=st2, mv2=mv2: e.bn_aggr(out=mv2[:, 0:2], in_=st2[:, 0:6]), reads=[b_st2], writes=[b_mv2])
                            yield
                            for hd in HD:
                                mv2, b_mv2 = mv2_l[hd]; sd2, b_sd2 = sd2_l[hd]
                                S.op("dve", lambda e, mv2=mv2, sd2=sd2: e.tensor_scalar(out=sd2[:, 0:1], in0=mv2[:, 1:2], scalar1=EPS, scalar2=None, op0=ALU.add),
                                     reads=[b_mv2], writes=[b_sd2])
                            yield
                            for hd in HD:
                                sd2, b_sd2 = sd2_l[hd]
                                S.op("act", lambda e, sd2=sd2: e.activation(out=sd2[:, 0:1], in_=sd2[:, 0:1], func=AF.Sqrt), reads=[b_sd2], writes=[b_sd2])
                            yield
                            for hd in HD:
                                sd2, b_sd2 = sd2_l[hd]
                                S.op("dve", lambda e, sd2=sd2: e.reciprocal(out=sd2[:, 0:1], in_=sd2[:, 0:1]), reads=[b_sd2], writes=[b_sd2])
                            yield
                            for hd in HD:
                                mv2, b_mv2 = mv2_l[hd]; sd2, b_sd2 = sd2_l[hd]
                                S.op("dve", lambda e, mv2=mv2, sd2=sd2: e.tensor_scalar(out=sd2[:, 1:2], in0=mv2[:, 0:1], scalar1=sd2[:, 0:1], scalar2=-1.0,
                                                                                      op0=ALU.mult, op1=ALU.mult), reads=[b_mv2, b_sd2], writes=[b_sd2])
                            yield
                            for hd in HD:
                                hraw, b_hraw = hraw_l[hd]; sd2, b_sd2 = sd2_l[hd]
                                S.op("act", lambda e, hraw=hraw, sd2=sd2: e.activation(out=hraw[:, :], in_=hraw[:, :], func=AF.Identity, scale=sd2[:, 0:1], bias=sd2[:, 1:2]),
                                     reads=[b_hraw, b_sd2], writes=[b_hraw])
                            yield
                            for hd in HD:
                                hraw, b_hraw = hraw_l[hd]
                                S.op("dve", lambda e, hd=hd, hraw=hraw: e.tensor_tensor(out=hm_t[:, 256 * hd:256 * hd + 256], in0=hraw[:, :],
                                                                                      in1=c_mnw[:, 256 * hd:256 * hd + 256], op=ALU.mult),
                                     reads=[b_hraw, bC1], writes=[b_hm_t])
                        pK_l = {}; pU_l = {}
                        yield
                        for hd in HD:
                            sc, b_sc = sc_l[hd]
                            S.op("act", lambda e, hd=hd, sc=sc: e.activation(out=sc[:, 4:5], in_=Acol(hd), func=AF.Exp, bias=nmgL(hd)),
                                 reads=[b_QT, b_NMGB], writes=[b_sc])
                            S.op("act", lambda e, hd=hd, sc=sc: e.activation(out=sc[:, 5:6], in_=nmgS(hd), func=AF.Exp, scale=-1.0, bias=nmgL(hd)),
                                 reads=[b_NMGB], writes=[b_sc])
                        yield
                        for hd in HD:
                            pK, bpK = psr(); pKh = pK.bitcast(BF16); pK_l[hd] = (pKh, bpK)
                            S.op("pe", lambda e, pKh=pKh, hd=hd: e.transpose(out=pKh[:, 0:128], in_=kT_st[:, hd, cs_], identity=idh[:, :]),
                                 reads=[b_kT_st, b_idh], writes=[bpK])
                        yield
                        for hd in HD:
                            sc, b_sc = sc_l[hd]; kw, b_kw = kw_l[hd]; pKh, bpK = pK_l[hd]
                            S.op("dve", lambda e, pKh=pKh, sc=sc, kw=kw: e.tensor_scalar(out=kw[:, :], in0=pKh[:, 0:128], scalar1=sc[:, 4:5], scalar2=None, op0=ALU.mult),
                                 reads=[bpK, b_sc], writes=[b_kw])
                        yield
                        for hd in HD:
                            kw, b_kw = kw_l[hd]
                            pU, bpU = psr(); pU_l[hd] = (pU, bpU)
                            S.op("pe", lambda e, pU=pU, hd=hd, kw=kw: e.matmul(pU[:, 0:258], lhsT=kw[:, :], rhs=vext[:, c, hd, :], start=True, stop=True),
                                 reads=[b_kw, b_vext], writes=[bpU])
                        yield
                        for hd in HD:
                            sc, b_sc = sc_l[hd]; pU, bpU = pU_l[hd]
                            S.op("dve", lambda e, pU=pU, hd=hd, sc=sc: e.scalar_tensor_tensor(out=Cst[:, hd, :], in0=Cst[:, hd, :], scalar=sc[:, 5:6],
                                                                                            in1=pU[:, 0:258], op0=ALU.mult, op1=ALU.add),
                                 reads=[b_Cst, b_sc, bpU], writes=[b_Cst])
                        if sti >= 5:
                            S.op("pool", lambda e: e.tensor_copy(out=Cbf[:, :, :], in_=Cst[:, :, :]), reads=[b_Cst], writes=[b_Cbf])
                        if own:
                            S.op("dve", lambda e, c=c: e.tensor_tensor(out=hm_t[:, :], in0=hm_t[:, :], in1=smo[:, c, :], op=ALU.mult),
                                 reads=[b_hm_t, b_smo], writes=[b_hm_t])
                            o0 = t0 - 3072 + 128 * c
                            if DEBUG:
                                S.dma("sp", dbg["dbg_hm"][o0:o0 + 128, :], hm_t[:, :], reads=[b_hm_t], final=True)
                            S.op("act", lambda e: e.copy(out=hm_h[:, :], in_=hm_t[:, :]), reads=[b_hm_t], writes=[b_hm_h])
                            for half in range(2):
                                p_, bp = psr()
                                ph = p_.bitcast(BF16)
                                for j in range(4):
                                    kc = 4 * half + j
                                    S.op("pe", lambda e, ph=ph, kc=kc, j=j: e.transpose(out=ph[:, 128 * j:128 * j + 128],
                                                                                      in_=hm_h[:, 128 * kc:128 * kc + 128], identity=idh[:, :]),
                                         reads=[b_hm_h, b_idh], writes=[bp])
                                copy(evac_eng(), hmT_c[:, 4 * half:4 * half + 4, :],
                                     ph[:, 0:512].rearrange("p (k t) -> p k t", t=128), [bp], [b_hmT_c])
                            S.dma("sp", hmT_d[:, :, o0:o0 + 128].rearrange("k p t -> p k t"), hmT_c[:, :, :], reads=[b_hmT_c], writes=[b_hmT_d])
                    yield
                rec = rec_gen()
                hook_box[0] = lambda: next(rec, None)
                ps_split[0] = True; psi[0] = 0; psr_i[0] = 0
                att_groups = [g for g in range(3) if t0 >= (1024 if g == 2 else 2560)]
                for g in att_groups:
                    wt, bw = wload(C_AK + 512 * g, 512)
                    def cons_k(cc, p_, bp, g=g):
                        copy(evac_eng(), stage_o[:, cc, :], p_[:, :], [bp], [b_stage_o])
                    proj_fm(wt, bw, 4, cons_k)
                    S.dma("sp", kT_d[4 * g:4 * g + 4, :, t0:t0 + 512].rearrange("h p t -> p h t"), stage_o[:, :, :],
                          reads=[b_stage_o], writes=[b_kT_d])
                    wt, bw = wload(C_AV + 512 * g, 512)
                    def cons_v(tt, p_, bp, g=g):
                        copy(evac_eng(), stage_t[:, tt, :], p_[:, :], [bp], [b_stage_t])
                    proj_tm(wt, bw, 512, cons_v)
                    S.dma("sp", v_d[t0:t0 + 512, 512 * g:512 * g + 512].rearrange("(t p) c -> p t c", p=128), stage_t[:, :, :],
                          reads=[b_stage_t], writes=[b_v_d])
                    if own:
                        wt, bw = wload(C_AQ + 512 * g, 512)
                        proj_fm(wt, bw, 4, cons_k)
                        o0 = t0 - 3072
                        S.dma("sp", qT_d[4 * g:4 * g + 4, :, o0:o0 + 512].rearrange("h p t -> p h t"), stage_o[:, :, :],
                              reads=[b_stage_o], writes=[b_qT_d])

                hook_box[0] = None
                for _ in rec:
                    pass
                ps_split[0] = False; psi[0] = 0
        S.barrier()
        if upto >= 2:
          with ExitStack() as e2:
            qT_sb, b_qT_sb = sb(e2, [128, 4, NOWN], BF16)
            kT_sb, b_kT_sb = sb(e2, [128, 4, 3072], BF16)
            pad_sb, b_pad_sb = sb(e2, [128, 3072])
            S.dma("sp", pad_sb[:, :], padneg_b[:, 1024:4096], writes=[b_pad_sb])
            ab_sb, b_ab_sb = sb(e2, [128, 4, 256])
            Vb = [sb(e2, [128, 2, 512], BF16) for _ in range(2)]
            S2_l = [sb(e2, [128, 256]) for _ in range(4)]; Pb_l = [sb(e2, [128, 256], BF16) for _ in range(4)]
            PT_l = [sb(e2, [128, 2, 128], BF16) for _ in range(4)]
            a_sc_l = [sb(e2, [128, 8]) for _ in range(4)]
            o_sb = [sb(e2, [128, 4, 128]) for _ in range(2)]
            l_sb = [sb(e2, [128, 4]) for _ in range(2)]
            ui = 0
            for g in range(3):
                r = (1, 4, 16)[g]
                w_lo = 1024 if g == 2 else 2560
                nq = min(128, 1024 // r)
                nk = 128 + nq
                nch = (1024 // r) // nq
                S.dma("sp", qT_sb[:, :, :], qT_d[4 * g:4 * g + 4, :, :].rearrange("h p t -> p h t"), reads=[b_qT_d], writes=[b_qT_sb])
                S.dma("sp", kT_sb[:, :, 0:4096 - w_lo], kT_d[4 * g:4 * g + 4, :, w_lo:4096].rearrange("h p t -> p h t"),
                      reads=[b_kT_d], writes=[b_kT_sb])
                S.dma("sp", ab_sb[:, :, :], abias[4 * g:4 * g + 4, :, :].rearrange("h p k -> p h k"), writes=[b_ab_sb])
                for p in range(r):
                    for ci in range(nch):
                        j0 = 3072 // r + ci * nq
                        wk0 = p + r * (j0 - 128)
                        kc0 = wk0 - w_lo
                        qc0 = p + r * j0 - 3072
                        vt, b_vt = Vb[ui % 2]
                        ot, b_ot = o_sb[ui % 2]
                        lt, b_lt = l_sb[ui % 2]
                        ui += 1
                        S.dma("sp", vt[:, 0, :], v_d[ss(wk0, 128, r), 512 * g:512 * g + 512], reads=[b_v_d], writes=[b_vt])
                        S.dma("sp", vt[0:nq, 1, :], v_d[ss(wk0 + 128 * r, nq, r), 512 * g:512 * g + 512], reads=[b_v_d], writes=[b_vt])
                        HD = range(4)
                        pS_l = {}; pT_l = {}; pO_l = {}
                        for hd in HD:
                            pS, bpS = ps(); pS_l[hd] = (pS, bpS)
                            S.op("pe", lambda e, pS=pS, hd=hd: e.matmul(pS[0:nq, 0:nk], lhsT=qT_sb[:, hd, ss(qc0, nq, r)],
                                                                      rhs=kT_sb[:, hd, ss(kc0, nk, r)], start=True, stop=True),
                                 reads=[b_qT_sb, b_kT_sb], writes=[bpS])
                        for hd in HD:
                            S2, b_S2 = S2_l[hd]; pS, bpS = pS_l[hd]
                            S.op("dve", lambda e, pS=pS, hd=hd, S2=S2: e.scalar_tensor_tensor(out=S2[0:nq, 0:nk], in0=pS[0:nq, 0:nk], scalar=128 ** -0.5,
                                                                                            in1=ab_sb[0:nq, hd, 0:nk], op0=ALU.mult, op1=ALU.add),
                                 reads=[bpS, b_ab_sb], writes=[b_S2])
                            S.op("pool", lambda e, S2=S2: e.tensor_tensor(out=S2[0:nq, 0:nk], in0=S2[0:nq, 0:nk], in1=pad_sb[0:nq, ss(wk0 - 1024, nk, r)], op=ALU.add),
                                 reads=[b_S2, b_pad_sb], writes=[b_S2])
                        for hd in HD:
                            S2, b_S2 = S2_l[hd]; a_sc, b_a_sc = a_sc_l[hd]
                            S.op("dve", lambda e, S2=S2, a_sc=a_sc: e.tensor_reduce(out=a_sc[0:nq, 1:2], in_=S2[0:nq, 0:nk], axis=AX.X, op=ALU.max, negate=True),
                                 reads=[b_S2], writes=[b_a_sc])
                        for hd in HD:
                            S2, b_S2 = S2_l[hd]; a_sc, b_a_sc = a_sc_l[hd]; Pb, b_Pb = Pb_l[hd]
                            S.op("act", lambda e, S2=S2, a_sc=a_sc, Pb=Pb: e.activation(out=Pb[0:nq, 0:nk], in_=S2[0:nq, 0:nk], func=AF.Exp, bias=a_sc[0:nq, 1:2],
                                                                                       accum_out=a_sc[0:nq, 2:3]), reads=[b_S2, b_a_sc], writes=[b_Pb, b_a_sc])
                        for hd in HD:
                            Pb, b_Pb = Pb_l[hd]
                            pT_, bpT = ps(); pTh = pT_.bitcast(BF16); pT_l[hd] = (pTh, bpT)
                            S.op("pe", lambda e, pTh=pTh, Pb=Pb: e.transpose(out=pTh[:, 0:nq], in_=Pb[0:nq, 0:128], identity=idh[0:nq, 0:nq]),
                                 reads=[b_Pb, b_idh], writes=[bpT])
                            S.op("pe", lambda e, pTh=pTh, Pb=Pb: e.transpose(out=pTh[0:nq, 128:128 + nq], in_=Pb[0:nq, 128:128 + nq], identity=idh[0:nq, 0:nq]),
                                 reads=[b_Pb, b_idh], writes=[bpT])
                        for hd in HD:
                            PT, b_PT = PT_l[hd]; pTh, bpT = pT_l[hd]
                            copy("act", PT[:, 0, 0:nq], pTh[:, 0:nq], [bpT], [b_PT])
                            copy("dve", PT[0:nq, 1, 0:nq], pTh[0:nq, 128:128 + nq], [bpT], [b_PT])
                        for hd in HD:
                            PT, b_PT = PT_l[hd]
                            pO, bpO = ps(); pO_l[hd] = (pO, bpO)
                            S.op("pe", lambda e, pO=pO, hd=hd, vt=vt, PT=PT: e.matmul(pO[0:nq, 0:128], lhsT=PT[:, 0, 0:nq], rhs=vt[:, 0, 128 * hd:128 * hd + 128],
                                                                                    start=True, stop=False), reads=[b_PT, b_vt], writes=[bpO])
                            S.op("pe", lambda e, pO=pO, hd=hd, vt=vt, PT=PT: e.matmul(pO[0:nq, 0:128], lhsT=PT[0:nq, 1, 0:nq], rhs=vt[0:nq, 1, 128 * hd:128 * hd + 128],
                                                                                    start=False, stop=True), reads=[b_PT, b_vt], writes=[bpO])
                        for hd in HD:
                            a_sc, b_a_sc = a_sc_l[hd]
                            S.op("dve", lambda e, a_sc=a_sc: e.reciprocal(out=a_sc[0:nq, 3:4], in_=a_sc[0:nq, 2:3]), reads=[b_a_sc], writes=[b_a_sc])
                            S.op("act", lambda e, a_sc=a_sc: e.activation(out=a_sc[0:nq, 4:5], in_=a_sc[0:nq, 2:3], func=AF.Ln), reads=[b_a_sc], writes=[b_a_sc])
                        for hd in HD:
                            a_sc, b_a_sc = a_sc_l[hd]; pO, bpO = pO_l[hd]
                            S.op("dve", lambda e, pO=pO, hd=hd, ot=ot, a_sc=a_sc: e.tensor_scalar(out=ot[0:nq, hd, :], in0=pO[0:nq, 0:128], scalar1=a_sc[0:nq, 3:4],
                                                                                                scalar2=None, op0=ALU.mult), reads=[bpO, b_a_sc], writes=[b_ot])
                            S.op("dve", lambda e, hd=hd, lt=lt, a_sc=a_sc: e.tensor_tensor(out=lt[0:nq, hd:hd + 1], in0=a_sc[0:nq, 4:5], in1=a_sc[0:nq, 1:2], op=ALU.subtract),
                                 reads=[b_a_sc], writes=[b_lt])
                        S.dma("sp", o_d[g, ss(qc0, nq, r), :], ot[0:nq, :, :].rearrange("p h d -> p (h d)"), reads=[b_ot], writes=[b_o_d])
                        S.dma("sp", lse_d[g, ss(qc0, nq, r), :], lt[0:nq, :], reads=[b_lt], writes=[b_lse_d])
            o3 = [sb(e2, [128, 3, 512]) for _ in range(2)]
            l3, b_l3 = sb(e2, [128, 3, 4]); e3, b_e3 = sb(e2, [128, 3, 4]); m_sc, b_m_sc = sb(e2, [128, 12])
            att_t, b_att_t = sb(e2, [128, 512]); att_h, b_att_h = sb(e2, [128, 512], BF16)
            attT_c, b_attT_c = sb(e2, [128, 4, 128], BF16)
            for t in range(8):
                ot3, b_ot3 = o3[t % 2]
                S.dma("sp", ot3[:, :, :], o_d[:, 128 * t:128 * t + 128, :].rearrange("g p c -> p g c"), reads=[b_o_d], writes=[b_ot3])
                S.dma("sp", l3[:, :, :], lse_d[:, 128 * t:128 * t + 128, :].rearrange("g p c -> p g c"), reads=[b_lse_d], writes=[b_l3])
                S.op("dve", lambda e: e.tensor_tensor(out=m_sc[:, 0:4], in0=l3[:, 0, :], in1=l3[:, 1, :], op=ALU.max), reads=[b_l3], writes=[b_m_sc])
                S.op("dve", lambda e: e.tensor_tensor(out=m_sc[:, 0:4], in0=m_sc[:, 0:4], in1=l3[:, 2, :], op=ALU.max), reads=[b_l3, b_m_sc], writes=[b_m_sc])
                for g in range(3):
                    S.op("dve", lambda e, g=g: e.tensor_tensor(out=e3[:, g, :], in0=l3[:, g, :], in1=m_sc[:, 0:4], op=ALU.subtract),
                         reads=[b_l3, b_m_sc], writes=[b_e3])
                S.op("act", lambda e: e.activation(out=e3[:, :, :], in_=e3[:, :, :], func=AF.Exp), reads=[b_e3], writes=[b_e3])
                S.op("dve", lambda e: e.tensor_tensor(out=m_sc[:, 4:8], in0=e3[:, 0, :], in1=e3[:, 1, :], op=ALU.add), reads=[b_e3], writes=[b_m_sc])
                S.op("dve", lambda e: e.tensor_tensor(out=m_sc[:, 4:8], in0=m_sc[:, 4:8], in1=e3[:, 2, :], op=ALU.add), reads=[b_e3, b_m_sc], writes=[b_m_sc])
                S.op("dve", lambda e: e.reciprocal(out=m_sc[:, 8:12], in_=m_sc[:, 4:8]), reads=[b_m_sc], writes=[b_m_sc])
                for g in range(3):
                    S.op("dve", lambda e, g=g: e.tensor_tensor(out=e3[:, g, :], in0=e3[:, g, :], in1=m_sc[:, 8:12], op=ALU.mult),
                         reads=[b_e3, b_m_sc], writes=[b_e3])
                for sl in range(4):
                    S.op("dve", lambda e, sl=sl, ot3=ot3: e.tensor_scalar(out=att_t[:, 128 * sl:128 * sl + 128], in0=ot3[:, 0, 128 * sl:128 * sl + 128],
                                                                        scalar1=e3[:, 0, sl:sl + 1], scalar2=None, op0=ALU.mult),
                         reads=[b_ot3, b_e3], writes=[b_att_t])
                    for g in (1, 2):
                        S.op("dve", lambda e, sl=sl, g=g, ot3=ot3: e.scalar_tensor_tensor(
                            out=att_t[:, 128 * sl:128 * sl + 128], in0=ot3[:, g, 128 * sl:128 * sl + 128], scalar=e3[:, g, sl:sl + 1],
                            in1=att_t[:, 128 * sl:128 * sl + 128], op0=ALU.mult, op1=ALU.add), reads=[b_ot3, b_e3, b_att_t], writes=[b_att_t])
                if DEBUG:
                    S.dma("sp", dbg["dbg_att"][128 * t:128 * t + 128, :], att_t[:, :], reads=[b_att_t], final=True)
                S.op("act", lambda e: e.copy(out=att_h[:, :], in_=att_t[:, :]), reads=[b_att_t], writes=[b_att_h])
                p_, bp = ps()
                ph = p_.bitcast(BF16)
                for j in range(4):
                    S.op("pe", lambda e, ph=ph, j=j: e.transpose(out=ph[:, 128 * j:128 * j + 128], in_=att_h[:, 128 * j:128 * j + 128], identity=idh[:, :]),
                         reads=[b_att_h, b_idh], writes=[bp])
                copy(evac_eng(), attT_c[:, :, :], ph[:, 0:512].rearrange("p (k t) -> p k t", t=128), [bp], [b_attT_c])
                S.dma("sp", attT_d[:, :, 128 * t:128 * t + 128].rearrange("k p t -> p k t"), attT_c[:, :, :], reads=[b_attT_c], writes=[b_attT_d])
          S.barrier()

        if upto >= 4:
          if True:
            with ExitStack() as e4a:
                mergedT, b_mergedT = sb(e4a, [128, 16, NOWN], BF16)
                hmT, b_hmT = sb(e4a, [128, 8, NOWN], BF16); attT, b_attT = sb(e4a, [128, 4, NOWN], BF16)
                S.dma("sp", hmT[:, :, :], hmT_d[:, :, :].rearrange("k p t -> p k t"), reads=[b_hmT_d], writes=[b_hmT])
                S.dma("sp", attT[:, :, :], attT_d[:, :, :].rearrange("k p t -> p k t"), reads=[b_attT_d], writes=[b_attT])
                hTo, b_hTo = sb(e4a, [128, 16, NOWN], BF16)
                S.dma("sp", hTo[:, :, :], hT_d[:, :, :].rearrange("k p t -> p k t"), reads=[b_hT_d], writes=[b_hTo])
                wpa, b_wpa = sb(e4a, [128, 4, D], BF16); wpm, b_wpm = sb(e4a, [128, 8, D], BF16)
                for kc in range(4):
                    S.dma("pool", wpa[:, kc, :], w_pa[128 * kc:128 * kc + 128, :], writes=[b_wpa])
                for kc in range(8):
                    S.dma("pool", wpm[:, kc, :], w_pm[128 * kc:128 * kc + 128, :], writes=[b_wpm])
                wg = [sb(e4a, [128, 2, 16, 256], BF16) for _ in range(2)]
                sg_l = [(sb(e4a, [128, 512]), sb(e4a, [128, 512])) for _ in range(2)]
                sgi = [0]
                for blk in range(8):
                    wgt, b_wgt = wg[blk % 2]
                    for a_, c0 in enumerate((C_GA, C_GM)):
                        S.dma("pool", wgt[:, a_, :, :], w_in[:, c0 + 256 * blk:c0 + 256 * blk + 256].rearrange("(k p) n -> p k n", p=128), writes=[b_wgt])
                    for sub in range(2):
                        cc = 2 * blk + sub
                        for th in range(2):
                            tk = slice(512 * th, 512 * th + 512)
                            pA, bpA = ps(); pM, bpM = ps(); pGA, bpGA = ps(); pGM, bpGM = ps()
                            (sga, b_sga), (sgm, b_sgm) = sg_l[sgi[0] % 2]; sgi[0] += 1
                            for kc in range(4):
                                S.op("pe", lambda e, kc=kc, pA=pA, cc=cc, tk=tk: e.matmul(pA[:, :], lhsT=wpa[:, kc, 128 * cc:128 * cc + 128], rhs=attT[:, kc, tk],
                                                                                       start=(kc == 0), stop=(kc == 3)), reads=[b_wpa, b_attT], writes=[bpA])
                            for kc in range(8):
                                S.op("pe", lambda e, kc=kc, pM=pM, cc=cc, tk=tk: e.matmul(pM[:, :], lhsT=wpm[:, kc, 128 * cc:128 * cc + 128], rhs=hmT[:, kc, tk],
                                                                                       start=(kc == 0), stop=(kc == 7)), reads=[b_wpm, b_hmT], writes=[bpM])
                            for a_, (pG, bpG) in enumerate(((pGA, bpGA), (pGM, bpGM))):
                                for kc in range(16):
                                    S.op("pe", lambda e, kc=kc, pG=pG, a_=a_, sub=sub, tk=tk, wgt=wgt: e.matmul(
                                        pG[:, :], lhsT=wgt[:, a_, kc, 128 * sub:128 * sub + 128], rhs=hTo[:, kc, tk], start=(kc == 0), stop=(kc == 15)),
                                         reads=[b_wgt, b_hTo], writes=[bpG])
                            S.op("act", lambda e, pGA=pGA: e.activation(out=sga[:, :], in_=pGA[:, :], func=AF.Sigmoid), reads=[bpGA], writes=[b_sga])
                            S.op("act", lambda e, pGM=pGM: e.activation(out=sgm[:, :], in_=pGM[:, :], func=AF.Sigmoid), reads=[bpGM], writes=[b_sgm])
                            S.op("dve", lambda e, pA=pA: e.tensor_tensor(out=sga[:, :], in0=sga[:, :], in1=pA[:, :], op=ALU.mult), reads=[b_sga, bpA], writes=[b_sga])
                            S.op("dve", lambda e, pM=pM: e.tensor_tensor(out=sgm[:, :], in0=sgm[:, :], in1=pM[:, :], op=ALU.mult), reads=[b_sgm, bpM], writes=[b_sgm])
                            S.op("dve", lambda e, cc=cc, tk=tk: e.tensor_tensor(out=mergedT[:, cc, tk], in0=sga[:, :], in1=sgm[:, :], op=ALU.add),
                                 reads=[b_sga, b_sgm], writes=[b_mergedT])
                S.dma("sp", mT_d[:, :, :].rearrange("k p t -> p k t"), mergedT[:, :, :], reads=[b_mergedT], writes=[b_mT_d])
            S.barrier()
            with ExitStack() as e5:
                mergedT, b_mergedT = sb(e5, [128, 16, NOWN], BF16)
                S.dma("sp", mergedT[:, :, :], mT_d[:, :, :].rearrange("k p t -> p k t"), reads=[b_mT_d], writes=[b_mergedT])
                h1c, b_h1c = sb(e5, [128, D], BF16)
                wo, b_wo = sb(e5, [128, 16, D], BF16)
                for kc in range(16):
                    S.dma("pool", wo[:, kc, :], w_out[128 * kc:128 * kc + 128, :], writes=[b_wo])
                cA, b_cA = sb(e5, [128, D]); cB, b_cB = sb(e5, [128, D])
                cG1, b_cG1 = sb(e5, [128, D]); cB1, b_cB1 = sb(e5, [128, D])
                S.dma("sp", cA[:, :], lng_b, writes=[b_cA]); S.dma("sp", cB[:, :], lnb_b, writes=[b_cB])
                S.dma("sp", cG1[:, :], ln1g_b, writes=[b_cG1]); S.dma("sp", cB1[:, :], ln1b_b, writes=[b_cB1])
                h1t, b_h1t = None, None
                c_wr, b_c_wr = sb(e5, [128, 16, 36]); c_br, b_c_br = sb(e5, [128, 36])
                S.dma("sp", c_wr[:, :, :], wr, writes=[b_c_wr]); S.dma("sp", c_br[:, :], br_b, writes=[b_c_br])
                xz_l = [(sb(e5, [128, D]), sb(e5, [128, D])) for _ in range(2)]
                h1T, b_h1T = sb(e5, [128, 16, 128])
                st, b_st = sb(e5, [128, 24]); mvt, b_mv = sb(e5, [128, 2]); sd, b_sd = sb(e5, [128, 2])
                lg, b_lg = sb(e5, [128, 36]); r_sc, b_r_sc = sb(e5, [128, 16]); goh, b_goh = sb(e5, [128, 4])
                esel, b_esel = sb(e5, [128, 8]); top8, b_top8 = sb(e5, [128, 8]); oh, b_oh = sb(e5, [128, 2, 8]); wsel, b_wsel = sb(e5, [128, 8])
                def s5_mm(t):
                        tk = slice(128 * t, 128 * t + 128)
                        (xnt, b_xnt), (z, b_z) = xz_l[t % 2]
                        h1t, b_h1t = z, b_z
                        S.dma("sp", xnt[:, :], h_d[tk, :], reads=[b_h_d], writes=[b_xnt])
                        S.op("dve", lambda e: e.tensor_tensor(out=xnt[:, :], in0=xnt[:, :], in1=cA[:, :], op=ALU.mult), reads=[b_xnt, b_cA], writes=[b_xnt])
                        S.op("dve", lambda e: e.tensor_tensor(out=xnt[:, :], in0=xnt[:, :], in1=cB[:, :], op=ALU.add), reads=[b_xnt, b_cB], writes=[b_xnt])
                        for cb_ in range(4):
                            cs = slice(512 * cb_, 512 * cb_ + 512)
                            pY, bpY = ps()
                            for kc in range(16):
                                S.op("pe", lambda e, kc=kc, pY=pY, cs=cs, tk=tk: e.matmul(pY[:, :], lhsT=mergedT[:, kc, tk], rhs=wo[:, kc, cs],
                                                                                       start=(kc == 0), stop=(kc == 15)), reads=[b_mergedT, b_wo], writes=[bpY])
                            S.op("dve", lambda e, pY=pY, cs=cs: e.scalar_tensor_tensor(out=z[:, cs], in0=xnt[:, cs], scalar=ALPHA, in1=pY[:, :],
                                                                                     op0=ALU.mult, op1=ALU.add), reads=[b_xnt, bpY], writes=[b_z])

                def s5_rest(t):
                        tk = slice(128 * t, 128 * t + 128)
                        (xnt, b_xnt), (z, b_z) = xz_l[t % 2]
                        h1t, b_h1t = z, b_z
                        layer_norm_stats(z, b_z, st, b_st, mvt, b_mv, sd, b_sd)
                        S.op("act", lambda e: e.activation(out=z[:, :], in_=z[:, :], func=AF.Identity, scale=sd[:, 0:1], bias=sd[:, 1:2]),
                             reads=[b_z, b_sd], writes=[b_z])
                        S.op("dve", lambda e: e.tensor_tensor(out=z[:, :], in0=z[:, :], in1=cG1[:, :], op=ALU.mult), reads=[b_z, b_cG1], writes=[b_z])
                        S.op("dve", lambda e: e.tensor_tensor(out=h1t[:, :], in0=h1t[:, :], in1=cB1[:, :], op=ALU.add), reads=[b_h1t, b_cB1], writes=[b_h1t])
                        S.dma("sp", h1_d[tk, :], h1t[:, :], reads=[b_h1t], writes=[b_h1_d])
                        if DEBUG:
                            S.dma("sp", dbg["dbg_h1"][tk, :], h1t[:, :], reads=[b_h1t], final=True)
                        S.op("act", lambda e: e.copy(out=h1c[:, :], in_=h1t[:, :]), reads=[b_h1t], writes=[b_h1c])
                        S.dma("sp", h1bf_d[tk, :], h1c[:, :], reads=[b_h1c], writes=[b_h1bf_d])
                        for g4 in range(4):
                            p_, bp = ps()
                            for j in range(4):
                                kc = 4 * g4 + j
                                S.op("pe", lambda e, p_=p_, kc=kc, j=j: e.transpose(out=p_[:, 128 * j:128 * j + 128], in_=h1t[:, 128 * kc:128 * kc + 128], identity=idf[:, :]),
                                     reads=[b_h1t, b_idf], writes=[bp])
                            copy(evac_eng(), h1T[:, 4 * g4:4 * g4 + 4, :], p_[:, :].rearrange("p (k t) -> p k t", t=128), [bp], [b_h1T])
                        pL, bpL = ps()
                        for kc in range(16):
                            S.op("pe", lambda e, kc=kc, pL=pL: e.matmul(pL[:, 0:36], lhsT=h1T[:, kc, :], rhs=c_wr[:, kc, :], start=(kc == 0), stop=(kc == 15)),
                                 reads=[b_h1T, b_c_wr], writes=[bpL])
                        S.op("dve", lambda e, pL=pL: e.tensor_tensor(out=lg[:, :], in0=pL[:, 0:36], in1=c_br[:, :], op=ALU.add), reads=[bpL, b_c_br], writes=[b_lg])
                        S.op("dve", lambda e: e.tensor_reduce(out=r_sc[:, 0:1], in_=lg[:, 0:4], axis=AX.X, op=ALU.max), reads=[b_lg], writes=[b_r_sc])
                        S.op("dve", lambda e: e.tensor_scalar(out=goh[:, :], in0=lg[:, 0:4], scalar1=r_sc[:, 0:1], scalar2=None, op0=ALU.is_equal),
                             reads=[b_lg, b_r_sc], writes=[b_goh])
                        S.op("dve", lambda e: e.tensor_scalar(out=r_sc[:, 1:2], in0=r_sc[:, 0:1], scalar1=-1.0, scalar2=None, op0=ALU.mult), reads=[b_r_sc], writes=[b_r_sc])
                        S.op("act", lambda e: e.activation(out=wsel[:, 0:4], in_=lg[:, 0:4], func=AF.Exp, bias=r_sc[:, 1:2], accum_out=r_sc[:, 2:3]),
                             reads=[b_lg, b_r_sc], writes=[b_wsel, b_r_sc])
                        S.op("dve", lambda e: e.reciprocal(out=r_sc[:, 3:4], in_=r_sc[:, 2:3]), reads=[b_r_sc], writes=[b_r_sc])
                        S.op("dve", lambda e: e.tensor_scalar(out=esel[:, :], in0=lg[:, 4:12], scalar1=goh[:, 0:1], scalar2=None, op0=ALU.mult),
                             reads=[b_lg, b_goh], writes=[b_esel])
                        for g in (1, 2, 3):
                            S.op("dve", lambda e, g=g: e.scalar_tensor_tensor(out=esel[:, :], in0=lg[:, 4 + 8 * g:12 + 8 * g], scalar=goh[:, g:g + 1], in1=esel[:, :],
                                                                              op0=ALU.mult, op1=ALU.add), reads=[b_lg, b_goh, b_esel], writes=[b_esel])
                        S.op("dve", lambda e: e.max(out=top8[:, :], in_=esel[:, :]), reads=[b_esel], writes=[b_top8])
                        S.op("dve", lambda e: e.tensor_tensor(out=r_sc[:, 4:5], in0=top8[:, 1:2], in1=top8[:, 0:1], op=ALU.subtract), reads=[b_top8], writes=[b_r_sc])
                        S.op("act", lambda e: e.activation(out=r_sc[:, 5:6], in_=r_sc[:, 4:5], func=AF.Exp), reads=[b_r_sc], writes=[b_r_sc])
                        S.op("dve", lambda e: e.tensor_scalar(out=r_sc[:, 6:7], in0=r_sc[:, 5:6], scalar1=1.0, scalar2=None, op0=ALU.add), reads=[b_r_sc], writes=[b_r_sc])
                        S.op("dve", lambda e: e.reciprocal(out=r_sc[:, 6:7], in_=r_sc[:, 6:7]), reads=[b_r_sc], writes=[b_r_sc])
                        S.op("dve", lambda e: e.tensor_tensor(out=r_sc[:, 7:8], in0=r_sc[:, 5:6], in1=r_sc[:, 6:7], op=ALU.mult), reads=[b_r_sc], writes=[b_r_sc])
                        S.op("dve", lambda e: e.tensor_scalar(out=r_sc[:, 6:8], in0=r_sc[:, 6:8], scalar1=r_sc[:, 3:4], scalar2=None, op0=ALU.mult),
                             reads=[b_r_sc], writes=[b_r_sc])
                        for j in range(2):
                            S.op("dve", lambda e, j=j: e.tensor_scalar(out=oh[:, j, :], in0=esel[:, :], scalar1=top8[:, j:j + 1], scalar2=None, op0=ALU.is_equal),
                                 reads=[b_esel, b_top8], writes=[b_oh])
                        S.op("dve", lambda e: e.tensor_scalar(out=wsel[:, :], in0=oh[:, 0, :], scalar1=r_sc[:, 6:7], scalar2=None, op0=ALU.mult),
                             reads=[b_oh, b_r_sc], writes=[b_wsel])
                        S.op("dve", lambda e: e.scalar_tensor_tensor(out=wsel[:, :], in0=oh[:, 1, :], scalar=r_sc[:, 7:8], in1=wsel[:, :], op0=ALU.mult, op1=ALU.add),
                             reads=[b_oh, b_r_sc, b_wsel], writes=[b_wsel])
                        for g in range(4):
                            S.op("dve", lambda e, g=g, t=t: e.tensor_scalar(out=Wt_all[:, t, 8 * g:8 * g + 8], in0=wsel[:, :], scalar1=goh[:, g:g + 1], scalar2=None, op0=ALU.mult),
                                 reads=[b_wsel, b_goh], writes=[b_Wt])
                        S.op("dve", lambda e, t=t: e.tensor_scalar(out=mk_all[:, t, :], in0=Wt_all[:, t, :], scalar1=0.0, scalar2=None, op0=ALU.is_gt),
                             reads=[b_Wt], writes=[b_mk])

                s5_mm(0)
                for t in range(8):
                    if t + 1 < 8:
                        s5_mm(t + 1)
                    s5_rest(t)
                c_ones, b_c_ones = sb(e5, [128, 128]); c_ltri, b_c_ltri = sb(e5, [128, 128])
                S.dma("sp", c_ones[:, :], ones_f, writes=[b_c_ones]); S.dma("sp", c_ltri[:, :], ltri, writes=[b_c_ltri])
                for t in range(8):
                    pC, bpC = ps()
                    for t2 in range(t):
                        S.op("pe", lambda e, pC=pC, t2=t2: e.matmul(pC[:, 0:32], lhsT=c_ones[:, :], rhs=mk_all[:, t2, :], start=(t2 == 0), stop=False),
                             reads=[b_c_ones, b_mk], writes=[bpC])
                    S.op("pe", lambda e, pC=pC, t=t: e.matmul(pC[:, 0:32], lhsT=c_ltri[:, :], rhs=mk_all[:, t, :], start=(t == 0), stop=True),
                         reads=[b_c_ltri, b_mk], writes=[bpC])
                    S.op("dve", lambda e, pC=pC, t=t: e.tensor_tensor(out=cumm[:, t, :], in0=pC[:, 0:32], in1=mk_all[:, t, :], op=ALU.mult),
                         reads=[bpC, b_mk], writes=[b_cumm])
          S.barrier()

        if upto >= 6:
          with ExitStack() as e6:
            y_acc, b_y_acc = sb(e6, [128, 8, D])
            S.op("pool", lambda e: e.memset(y_acc[:, :, :], 0.0), writes=[b_y_acc])
            e6a = ExitStack()
            e6_outer = e6
            e6 = e6a
            h1_bf, b_h1_bf = sb(e6, [128, 8, D], BF16)
            S.dma("sp", h1_bf[:, :, :], h1bf_d.rearrange("(t p) c -> p t c", p=128), reads=[b_h1bf_d], writes=[b_h1_bf])
            c_iota, b_c_iota = sb(e6, [128, 128])
            S.dma("sp", c_iota[:, :], iota1, writes=[b_c_iota])
            wgu = [sb(e6, [128, 16, 512], BF16) for _ in range(3)]
            wdn = [sb(e6, [128, 11, 512], BF16) for _ in range(2)]
            Sel, b_Sel = sb(e6, [128, 8, 128], BF16); SelT_l = [sb(e6, [128, NOWN], BF16) for _ in range(2)]
            XeT, b_XeT = sb(e6, [128, 16, 128], BF16)
            sg, b_sg = sb(e6, [128, DFF]); Hs, b_Hs = sb(e6, [128, DFF], BF16)
            HT, b_HT = sb(e6, [128, 11, 128], BF16); Yb_l = [sb(e6, [128, D], BF16) for _ in range(2)]
            pending = []
            gi_ = [0]; di_ = [0]
            for ex in range(NEXP):
                SelT, b_SelT = SelT_l[ex % 2]; Yb, b_Yb = Yb_l[ex % 2]
                for t in range(8):
                    S.op("dve", lambda e, t=t, ex=ex: e.tensor_scalar(out=Sel[:, t, :], in0=c_iota[:, :], scalar1=cumm[:, t, ex:ex + 1], scalar2=None, op0=ALU.is_equal),
                         reads=[b_c_iota, b_cumm], writes=[b_Sel])
                pB, bpB = ps()
                pBh = pB.bitcast(BF16)
                for t in range(8):
                    S.op("pe", lambda e, pBh=pBh, t=t: e.transpose(out=pBh[:, 128 * t:128 * t + 128], in_=Sel[:, t, :], identity=idh[:, :]),
                         reads=[b_Sel, b_idh], writes=[bpB])
                copy(evac_eng(), SelT[:, :], pBh[:, :], [bpB], [b_SelT])
                for g4 in range(4):
                    pX_, bpX_ = ps()
                    for j in range(4):
                        kc = 4 * g4 + j
                        for t in range(8):
                            S.op("pe", lambda e, pX_=pX_, kc=kc, j=j, t=t: e.matmul(pX_[:, 128 * j:128 * j + 128], lhsT=h1_bf[:, t, 128 * kc:128 * kc + 128], rhs=Sel[:, t, :],
                                                                                 start=(t == 0), stop=(t == 7)), reads=[b_h1_bf, b_Sel], writes=[bpX_])
                    copy(evac_eng(), XeT[:, 4 * g4:4 * g4 + 4, :], pX_[:, :].rearrange("p (k t) -> p k t", t=128), [bpX_], [b_XeT])
                for which, wsrc in enumerate((w_gate, w_up)):
                    for blk in range(3):
                        c0 = 512 * blk
                        ncol = min(512, DFF - c0)
                        wt_, bw_ = wgu[gi_[0] % 3]; gi_[0] += 1
                        S.dma("pool", wt_[:, :, 0:ncol], wsrc[ex, :, c0:c0 + ncol].rearrange("(k p) n -> p k n", p=128), writes=[bw_])
                        pG, bpG = ps()
                        for kc in range(16):
                            S.op("pe", lambda e, pG=pG, wt_=wt_, kc=kc: e.matmul(pG[:, 0:ncol], lhsT=XeT[:, kc, :], rhs=wt_[:, kc, 0:ncol], start=(kc == 0), stop=(kc == 15)),
                                 reads=[b_XeT, bw_], writes=[bpG])
                        if which == 0:
                            S.op("act", lambda e, pG=pG, c0=c0: e.activation(out=sg[:, c0:c0 + ncol], in_=pG[:, 0:ncol], func=AF.Silu), reads=[bpG], writes=[b_sg])
                            if blk == 2:
                                while pending:
                                    pending.pop(0)()
                        else:
                            S.op("dve", lambda e, pG=pG, c0=c0: e.tensor_tensor(out=Hs[:, c0:c0 + ncol], in0=sg[:, c0:c0 + ncol], in1=pG[:, 0:ncol], op=ALU.mult),
                                 reads=[b_sg, bpG], writes=[b_Hs])
                for g3 in range(3):
                    pH, bpH = ps()
                    pHh = pH.bitcast(BF16)
                    nj = min(4, 11 - 4 * g3)
                    for j in range(nj):
                        fc = 4 * g3 + j
                        S.op("pe", lambda e, pHh=pHh, j=j, fc=fc: e.transpose(out=pHh[:, 128 * j:128 * j + 128], in_=Hs[:, 128 * fc:128 * fc + 128], identity=idh[:, :]),
                             reads=[b_Hs, b_idh], writes=[bpH])
                    copy(evac_eng(), HT[:, 4 * g3:4 * g3 + nj, :], pHh[:, 0:128 * nj].rearrange("p (k t) -> p k t", t=128), [bpH], [b_HT])
                for cb_ in range(4):
                    cs = slice(512 * cb_, 512 * cb_ + 512)
                    wd_, bwd_ = wdn[di_[0] % 2]; di_[0] += 1
                    S.dma("pool", wd_[:, :, :], w_down[ex, :, cs].rearrange("(k p) n -> p k n", p=128), writes=[bwd_])
                    pY, bpY = ps()
                    for fc in range(11):
                        S.op("pe", lambda e, pY=pY, fc=fc, wd_=wd_: e.matmul(pY[:, :], lhsT=HT[:, fc, :], rhs=wd_[:, fc, :], start=(fc == 0), stop=(fc == 10)),
                             reads=[b_HT, bwd_], writes=[bpY])
                    copy(evac_eng(), Yb[:, cs], pY[:, :], [bpY], [b_Yb])
                def combine(ex=ex, SelT=SelT, b_SelT=b_SelT, Yb=Yb, b_Yb=b_Yb):
                    for t in range(8):
                        for cb_ in range(4):
                            cs = slice(512 * cb_, 512 * cb_ + 512)
                            pZ, bpZ = ps()
                            S.op("pe", lambda e, pZ=pZ, t=t, cs=cs: e.matmul(pZ[:, :], lhsT=SelT[:, 128 * t:128 * t + 128], rhs=Yb[:, cs], start=True, stop=True),
                                 reads=[b_SelT, b_Yb], writes=[bpZ])
                            S.op("dve", lambda e, pZ=pZ, t=t, cs=cs: e.scalar_tensor_tensor(out=y_acc[:, t, cs], in0=pZ[:, :], scalar=Wt_all[:, t, ex:ex + 1],
                                                                                          in1=y_acc[:, t, cs], op0=ALU.mult, op1=ALU.add),
                                 reads=[bpZ, b_Wt, b_y_acc], writes=[b_y_acc])
                pending.append(combine)
            while pending:
                pending.pop(0)()
            e6a.close()
            e6 = e6_outer
            S.barrier()
            cG2, b_cG2 = sb(e6, [128, D]); cB2, b_cB2 = sb(e6, [128, D])
            S.dma("sp", cG2[:, :], ln2g_b, writes=[b_cG2]); S.dma("sp", cB2[:, :], ln2b_b, writes=[b_cB2])
            h1r = [sb(e6, [128, D]) for _ in range(2)]
            st, b_st = sb(e6, [128, 24]); mvt, b_mv = sb(e6, [128, 2]); sd, b_sd = sb(e6, [128, 2])
            for t in range(8):
                tk = slice(128 * t, 128 * t + 128)
                ht_, b_ht_ = h1r[t % 2]
                if DEBUG:
                    S.dma("sp", dbg["dbg_y2"][tk, :], y_acc[:, t, :], reads=[b_y_acc], final=True)
                S.dma("sp", ht_[:, :], h1_d[tk, :], reads=[b_h1_d], writes=[b_ht_])
                S.op("dve", lambda e, t=t, ht_=ht_: e.scalar_tensor_tensor(out=ht_[:, :], in0=ht_[:, :], scalar=ALPHA, in1=y_acc[:, t, :], op0=ALU.mult, op1=ALU.add),
                     reads=[b_ht_, b_y_acc], writes=[b_ht_])
                layer_norm_stats(ht_, b_ht_, st, b_st, mvt, b_mv, sd, b_sd)
                S.op("act", lambda e, ht_=ht_: e.activation(out=ht_[:, :], in_=ht_[:, :], func=AF.Identity, scale=sd[:, 0:1], bias=sd[:, 1:2]),
                     reads=[b_ht_, b_sd], writes=[b_ht_])
                S.op("dve", lambda e, ht_=ht_: e.tensor_tensor(out=ht_[:, :], in0=ht_[:, :], in1=cG2[:, :], op=ALU.mult), reads=[b_ht_, b_cG2], writes=[b_ht_])
                S.op("dve", lambda e, ht_=ht_: e.tensor_tensor(out=ht_[:, :], in0=ht_[:, :], in1=cB2[:, :], op=ALU.add), reads=[b_ht_, b_cB2], writes=[b_ht_])
                S.dma("sp", out_d[tk, :], ht_[:, :], reads=[b_ht_], final=True)
        S.barrier()
        S.finish()
    return nc


def _bcast(v, n=128):
    v = np.asarray(v, np.float32).reshape(1, -1)
    return np.ascontiguousarray(np.broadcast_to(v, (n, v.shape[1])))


def prep_inputs(inp):
    f32 = np.float32
    x = np.asarray(inp["x"], f32)
    shared = {}
    shared["w_in"] = np.ascontiguousarray(np.asarray(inp["w_in"], f32)[0])
    g = np.asarray(inp["ln_in_g"], f32); b = np.asarray(inp["ln_in_b"], f32)
    shared["lng_col"] = np.ascontiguousarray(g.reshape(16, 128).T); shared["lnb_col"] = np.ascontiguousarray(b.reshape(16, 128).T)
    shared["lng_b"] = _bcast(g); shared["lnb_b"] = _bcast(b)
    cwv = np.asarray(inp["m_conv_w"], f32)[0]
    shared["cw"] = np.ascontiguousarray(cwv.reshape(4, 8, 128).transpose(2, 1, 0))
    shared["cb"] = np.ascontiguousarray(np.asarray(inp["m_conv_b"], f32)[0].reshape(8, 128).T)
    shared["ifb"] = np.ascontiguousarray(np.asarray(inp["m_if_bias"], f32)[0].reshape(2, 4).T)
    shared["mnw_b"] = _bcast(np.asarray(inp["m_norm_w"], f32)[0].reshape(-1))
    shared["w_pa"] = np.ascontiguousarray(np.asarray(inp["w_proj_att"], f32)[0])
    shared["w_pm"] = np.ascontiguousarray(np.asarray(inp["w_proj_mlstm"], f32)[0])
    shared["w_out"] = np.ascontiguousarray(np.asarray(inp["w_out"], f32)[0])
    shared["ln1g_b"] = _bcast(np.asarray(inp["ln1_g"], f32)[0]); shared["ln1b_b"] = _bcast(np.asarray(inp["ln1_b"], f32)[0])
    shared["ln2g_b"] = _bcast(np.asarray(inp["ln2_g"], f32)[0]); shared["ln2b_b"] = _bcast(np.asarray(inp["ln2_b"], f32)[0])
    wrc = np.concatenate([np.asarray(inp["w_router_group"], f32)[0], np.asarray(inp["w_router_expert"], f32)[0]], axis=1)
    shared["wr"] = np.ascontiguousarray(wrc.reshape(16, 128, 36).transpose(1, 0, 2))
    shared["br_b"] = _bcast(np.concatenate([np.asarray(inp["b_router_group"], f32)[0], np.asarray(inp["b_router_expert"], f32)[0]]))
    shared["w_gate"] = np.ascontiguousarray(np.asarray(inp["w_gate"], f32)[0])
    shared["w_up"] = np.ascontiguousarray(np.asarray(inp["w_up"], f32)[0])
    shared["w_down"] = np.ascontiguousarray(np.asarray(inp["w_down"], f32)[0])
    slopes = alibi_slopes(12)
    qi = np.arange(128)[:, None]; ki = np.arange(256)[None, :]
    delta = 128 + qi - ki
    ab = np.zeros((12, 128, 256), f32)
    for h in range(12):
        r = (1, 4, 16)[h // 4]
        ab[h] = np.where((delta >= 0) & (delta <= 128), -slopes[h] * (delta * r).astype(f32), NEG)
    shared["abias"] = ab
    shared["ident_f"] = np.eye(128, dtype=f32)
    shared["ident_h"] = np.eye(128, dtype=f32).astype(ml_dtypes.bfloat16)
    s_ = np.arange(128)[:, None]; l_ = np.arange(128)[None, :]
    shared["tri_neg"] = np.where(s_ <= l_, 0.0, NEG).astype(f32)
    shared["ltri"] = (s_ <= l_).astype(f32)
    shared["ones_f"] = np.ones((128, 128), f32)
    shared["iota1"] = np.ascontiguousarray(np.broadcast_to(np.arange(1, 129, dtype=f32)[None, :], (128, 128)))
    shared["iota1c"] = np.arange(1, 129, dtype=f32).reshape(128, 1)
    sel4 = np.zeros((4, 4, 128), f32)
    for k in range(4):
        sel4[k, k, :] = 1.0
    shared["sel4"] = sel4
    sel32 = np.zeros((32, 32, 128), f32)
    for k in range(32):
        sel32[k, k, :] = 1.0
    shared["sel32"] = sel32
    in_maps = []
    for c in range(8):
        b_, k_ = c // 4, c % 4
        base = 1024 * k_ - 3072
        xw = np.zeros((NW, D), f32)
        lo = max(0, -base)
        xw[lo:] = x[b_, base + lo: base + NW]
        valid = (np.arange(NW) + base >= 0).astype(f32)
        m = dict(shared)
        m["x_win"] = xw
        m["padneg_b"] = _bcast((valid - 1.0) * (-NEG))
        m["valid_b"] = _bcast(valid)
        gm = np.zeros((4, 2, NW), f32)
        gm[:, 0, :] = (1.0 - valid) * NEG
        gm[:, 1, :] = (1.0 - valid) * (-NEG)
        m["gmask"] = gm
        in_maps.append(m)
    return in_maps


_NC = None


def kernel(**inputs):
    global _NC
    if _NC is None:
        _NC = build_program()
    in_maps = prep_inputs(inputs)
    res = run_bass_kernel_spmd(_NC, in_maps, core_ids=list(range(8)))
    out = np.zeros((2, 4096, D), np.float32)
    for c in range(8):
        b_, k_ = c // 4, c % 4
        out[b_, 1024 * k_:1024 * k_ + 1024] = res.results[c]["out"]
    return out
```

```python
import math
from contextlib import ExitStack
import numpy as np
import ml_dtypes
import concourse.bass as bass
import concourse.mybir as mybir
from concourse.bass_utils import run_bass_kernel_spmd

F32 = mybir.dt.float32
BF16 = mybir.dt.bfloat16
AF = mybir.ActivationFunctionType
ALU = mybir.AluOpType
AX = mybir.AxisListType

D = 2048
NW = 4096
NOWN = 1024
NPROJ = 11784
ALPHA = 2 ** 0.25
EPS = 1e-5
NEG = -30000.0
DFF = 1408
NEXP = 32
DEBUG = False


class Buf:
    __slots__ = ("w", "r")

    def __init__(self):
        self.w = None
        self.r = []


class Sched:
    ENG = ("pe", "act", "dve", "pool", "sp")

    def __init__(self, nc, es, n_dma_sems=20):
        self.nc = nc
        self.eobj = {"pe": nc.tensor, "act": nc.scalar, "dve": nc.vector, "pool": nc.gpsimd, "sp": nc.sync}
        self.cnt = {e: 0 for e in self.ENG}
        self.sem = {e: es.enter_context(nc.semaphore("c_" + e)) for e in self.ENG}
        self.seen = {e: {} for e in self.ENG}
        self.pending = {e: [] for e in self.ENG}
        self.dsems, self.dtot, self.dnext = {}, {}, {}
        for q in ("sp", "pool", "act"):
            self.dsems[q] = [es.enter_context(nc.semaphore(f"d_{q}_{i}")) for i in range(n_dma_sems)]
            self.dnext[q] = 0
        self.final_events = []

    def _deps(self, reads, writes):
        deps = []
        for b in reads:
            if b.w is not None:
                deps.append(b.w)
        for b in writes:
            if b.w is not None:
                deps.append(b.w)
            deps.extend(b.r)
        return deps

    def _filter(self, eng, deps):
        seen = self.seen[eng]
        own = self.sem[eng]
        best = {}
        for (sem, val) in deps + self.pending[eng]:
            if sem is own and eng in ("pe", "sp"):
                continue
            k = id(sem)
            if seen.get(k, 0) >= val:
                continue
            seen[k] = val
            best[k] = (sem, val)
        self.pending[eng] = []
        return list(best.values())

    def _emit(self, ename, waits, fn, sem, inc):
        eng = self.eobj[ename]
        for (s_, v) in waits:
            eng.wait_ge(s_, v)
        fn(eng).then_inc(sem, inc)

    def op(self, eng, fn, reads=(), writes=()):
        waits = self._filter(eng, self._deps(reads, writes))
        self.cnt[eng] += 1
        ev = (self.sem[eng], self.cnt[eng])
        for b in reads:
            b.r.append(ev)
        for b in writes:
            b.w = ev
            b.r = []
        self._emit(eng, waits, fn, self.sem[eng], 1)
        return ev

    def dma(self, q, out, in_, reads=(), writes=(), final=False):
        deps = self._deps(reads, writes)
        i = self.dnext[q]
        self.dnext[q] = (i + 1) % len(self.dsems[q])
        dsem = self.dsems[q][i]
        prev = self.dtot.get(id(dsem), 0)
        if prev:
            deps.append((dsem, prev))
        waits = self._filter(q, deps)
        tot = prev + 16
        self.dtot[id(dsem)] = tot
        ev = (dsem, tot)
        for b in reads:
            b.r.append(ev)
        for b in writes:
            b.w = ev
            b.r = []
        self._emit(q, waits, (lambda e: e.dma_start(out=out, in_=in_)), dsem, 16)
        if final:
            self.final_events.append(ev)
        return ev

    def barrier(self):
        evs = [(self.sem[e], self.cnt[e]) for e in self.ENG if self.cnt[e] > 0]
        for q in self.dsems:
            for s in self.dsems[q]:
                t = self.dtot.get(id(s), 0)
                if t:
                    evs.append((s, t))
        for e in self.ENG:
            self.pending[e].extend(evs)

    def finish(self):
        for (s_, v) in self._filter("sp", list(self.final_events)):
            self.nc.sync.wait_ge(s_, v)


def ss(start, n, r):
    return slice(start, start + (n - 1) * r + 1, r)


def alibi_slopes(n):
    def geometric(k):
        start = 2.0 ** (-8.0 / k)
        return [start ** (i + 1) for i in range(k)]
    c = 2 ** int(math.floor(math.log2(n)))
    s = geometric(c) if c == n else geometric(c) + geometric(2 * c)[0::2][: n - c]
    return np.array(sorted(s, reverse=True), dtype=np.float32)


C_AQ, C_AK, C_AV = 0, 1536, 3072
C_MQ, C_MK = 4608, 5120
C_MV, C_MO = 5632, 6656
C_IF = 7680
C_GA, C_GM = 7688, 9736


def build_program(upto=9):
    nc = bass.Bass("TRN2", target_bir_lowering=False)

    def din(name, shape, dt=F32):
        return nc.dram_tensor(name, list(shape), dt, kind="ExternalInput").ap()

    def dscr(name, shape, dt=F32):
        return nc.dram_tensor(name, list(shape), dt, kind="Internal").ap()

    x_win = din("x_win", [NW, D])
    w_in = din("w_in", [D, NPROJ])
    padneg_b = din("padneg_b", [128, NW])
    valid_b = din("valid_b", [128, NW])
    gmask = din("gmask", [4, 2, NW])
    lng_col = din("lng_col", [128, 16]); lnb_col = din("lnb_col", [128, 16])
    lng_b = din("lng_b", [128, D]); lnb_b = din("lnb_b", [128, D])
    cw = din("cw", [128, 8, 4]); cb = din("cb", [128, 8])
    ifb = din("ifb", [4, 2])
    mnw_b = din("mnw_b", [128, 1024])
    w_pa = din("w_pa", [512, D]); w_pm = din("w_pm", [1024, D]); w_out = din("w_out", [D, D])
    ln1g_b = din("ln1g_b", [128, D]); ln1b_b = din("ln1b_b", [128, D])
    ln2g_b = din("ln2g_b", [128, D]); ln2b_b = din("ln2b_b", [128, D])
    wr = din("wr", [128, 16, 36]); br_b = din("br_b", [128, 36])
    if upto >= 6:
        w_gate = din("w_gate", [NEXP, D, DFF]); w_up = din("w_up", [NEXP, D, DFF]); w_down = din("w_down", [NEXP, DFF, D])
    abias = din("abias", [12, 128, 256])
    ident_f = din("ident_f", [128, 128]); ident_h = din("ident_h", [128, 128], BF16)
    tri_neg = din("tri_neg", [128, 128])
    ltri = din("ltri", [128, 128])
    ones_f = din("ones_f", [128, 128])
    iota1 = din("iota1", [128, 128])
    iota1c = din("iota1c", [128, 1])
    sel4 = din("sel4", [4, 4, 128])
    sel32 = din("sel32", [32, 32, 128])
    out_d = nc.dram_tensor("out", [NOWN, D], F32, kind="ExternalOutput").ap()

    h_d = dscr("h_d", [NOWN, D])
    h1_d = dscr("h1_d", [NOWN, D])
    kT_d = dscr("kT_d", [12, 128, NW], BF16)
    qT_d = dscr("qT_d", [12, 128, NOWN], BF16)
    v_d = dscr("v_d", [NW, 1536], BF16)
    hT_d = dscr("hT_d", [16, 128, NOWN], BF16)
    b_hT_d = Buf()
    hmT_d = dscr("hmT_d", [8, 128, NOWN], BF16); attT_d = dscr("attT_d", [4, 128, NOWN], BF16)
    mT_d = dscr("mT_d", [16, 128, NOWN], BF16); h1bf_d = dscr("h1bf_d", [NOWN, D], BF16)
    b_hmT_d, b_attT_d, b_mT_d, b_h1bf_d = (Buf() for _ in range(4))
    o_d = dscr("o_d", [3, NOWN, 512])
    lse_d = dscr("lse_d", [3, NOWN, 4])
    b_h_d, b_h1_d, b_kT_d, b_qT_d, b_v_d, b_o_d, b_lse_d = (Buf() for _ in range(7))
    dbg = {}
    if DEBUG:
        for nm, shp in (("dbg_hm", [NOWN, 1024]), ("dbg_att", [NOWN, 512]), ("dbg_h1", [NOWN, D]), ("dbg_y2", [NOWN, D])):
            dbg[nm] = nc.dram_tensor(nm, shp, F32, kind="ExternalOutput").ap()

    with ExitStack() as es:
        S = Sched(nc, es)
        cnt = [0]

        def sb(es_, shape, dt=F32):
            cnt[0] += 1
            t = es_.enter_context(nc.sbuf_tensor(f"t{cnt[0]}", list(shape), dt))
            return t, Buf()

        psT = [es.enter_context(nc.psum_tensor(f"ps{i}", [128, 512], F32)) for i in range(8)]
        psB = [Buf() for _ in range(8)]
        psi = [0]

        ps_split = [False]
        psr_i = [0]

        def ps():
            if ps_split[0]:
                i = 4 + (psi[0] % 4)
                psi[0] = (psi[0] + 1) % 4
                return psT[i], psB[i]
            i = psi[0]
            psi[0] = (i + 1) % 8
            return psT[i], psB[i]

        def psr():
            i = psr_i[0]
            psr_i[0] = (i + 1) % 4
            return psT[i], psB[i]

        rr = [0]

        def evac_eng():
            rr[0] += 1
            return "act" if rr[0] % 2 else "dve"

        def copy(eng, out, in_, reads, writes):
            if eng == "act":
                S.op("act", lambda e: e.copy(out=out, in_=in_), reads, writes)
            else:
                S.op(eng, lambda e: e.tensor_copy(out=out, in_=in_), reads, writes)

        def load_const(es_, src, shape, dt=F32, q="sp"):
            t, b = sb(es_, shape, dt)
            idx = tuple(slice(None) for _ in shape)
            S.dma(q, t[idx], src, writes=[b])
            return t, b

        idf, b_idf = load_const(es, ident_f, [128, 128])
        idh, b_idh = load_const(es, ident_h, [128, 128], BF16)
        bC = Buf()
        def lc(src, shape, dt=F32):
            t, _ = sb(es, shape, dt)
            idx = tuple(slice(None) for _ in shape)
            S.dma("sp", t[idx], src, writes=[bC])
            return t
        c_lng = lc(lng_col, [128, 16]); c_lnb = lc(lnb_col, [128, 16])

        def layer_norm_stats(x_ap, b_x, st, b_st, mvt, b_mv, sd, b_sd, nchunk=4, csz=512):
            for i in range(nchunk):
                S.op("dve", lambda e, i=i: e.bn_stats(out=st[:, 6 * i:6 * i + 6], in_=x_ap[:, csz * i:csz * i + csz]),
                     reads=[b_x], writes=[b_st])
            S.op("dve", lambda e: e.bn_aggr(out=mvt[:, 0:2], in_=st[:, 0:6 * nchunk]), reads=[b_st], writes=[b_mv])
            S.op("dve", lambda e: e.tensor_scalar(out=sd[:, 0:1], in0=mvt[:, 1:2], scalar1=EPS, scalar2=None, op0=ALU.add),
                 reads=[b_mv], writes=[b_sd])
            S.op("act", lambda e: e.activation(out=sd[:, 0:1], in_=sd[:, 0:1], func=AF.Sqrt), reads=[b_sd], writes=[b_sd])
            S.op("dve", lambda e: e.reciprocal(out=sd[:, 0:1], in_=sd[:, 0:1]), reads=[b_sd], writes=[b_sd])
            S.op("dve", lambda e: e.tensor_scalar(out=sd[:, 1:2], in0=mvt[:, 0:1], scalar1=sd[:, 0:1], scalar2=-1.0,
                                                  op0=ALU.mult, op1=ALU.mult), reads=[b_mv, b_sd], writes=[b_sd])

        Wt_all, b_Wt = sb(es, [128, 8, 32]); mk_all, b_mk = sb(es, [128, 8, 32]); cumm, b_cumm = sb(es, [128, 8, 32])

        with ExitStack() as e1:
            bC1 = Buf()
            def lc1(src, shape, dt=F32):
                t, _ = sb(e1, shape, dt)
                idx = tuple(slice(None) for _ in shape)
                S.dma("sp", t[idx], src, writes=[bC1])
                return t
            c_cw = lc1(cw, [128, 8, 4]); c_cb = lc1(cb, [128, 8]); c_ifb = lc1(ifb, [4, 2])
            c_mnw = lc1(mnw_b, [128, 1024]); c_tri = lc1(tri_neg, [128, 128]); c_sel4 = lc1(sel4, [4, 4, 128])
            xs = [sb(e1, [128, D]) for _ in range(2)]
            xn_l = [sb(e1, [128, D]) for _ in range(2)]
            ln_l = [(sb(e1, [128, 24]), sb(e1, [128, 2]), sb(e1, [128, 2])) for _ in range(2)]
            hT_st, b_hT_st = sb(e1, [128, 16, 512], BF16)
            wbuf = [sb(e1, [128, 16, 512], BF16) for _ in range(2)]
            wif, b_wif = sb(e1, [128, 16, 8], BF16)
            S.dma("pool", wif[:, :, :], w_in[:, C_IF:C_IF + 8].rearrange("(k p) n -> p k n", p=128), writes=[b_wif])
            wi = [0]
            stage_o, b_stage_o = sb(e1, [128, 4, 512], BF16)
            stage_t, b_stage_t = sb(e1, [128, 4, 512], BF16)
            halo, b_halo = sb(e1, [128, 8, 3])
            S.op("pool", lambda e: e.memset(halo[:, :, :], 0.0), writes=[b_halo])
            rw, b_rw = sb(e1, [128, 515])
            cacc, b_cacc = sb(e1, [128, 512])
            kT_st, b_kT_st = sb(e1, [128, 4, 512], BF16)
            qT_st, b_qT_st = sb(e1, [128, 4, 512], BF16)
            vext, b_vext = sb(e1, [128, 4, 4, 258], BF16)
            S.op("pool", lambda e: e.memset(vext[:, :, :, 256:257], 1.0), writes=[b_vext])
            S.op("pool", lambda e: e.memset(vext[:, :, :, 257:258], 0.0), writes=[b_vext])
            smo, b_smo = sb(e1, [128, 4, 1024], BF16)
            gI, b_gI = sb(e1, [4, 512]); gF, b_gF = sb(e1, [4, 512])
            gB, b_gB = sb(e1, [4, 512]); gMg, b_gMg = sb(e1, [4, 512])
            gm_sb, b_gm = sb(e1, [4, 2, 512])
            QS, b_QS = sb(e1, [128, 512])
            S.op("pool", lambda e: e.memset(QS[:, :], 0.0), writes=[b_QS])
            vmask, b_vmask = sb(e1, [128, 512])
            ones512, b_ones512 = sb(e1, [4, 512])
            S.op("pool", lambda e: e.memset(ones512[:, :], 1.0), writes=[b_ones512])
            carryB, b_cB = sb(e1, [4, 1]); carryM, b_cM = sb(e1, [4, 1])
            S.op("pool", lambda e: e.memset(carryB[:, :], 0.0), writes=[b_cB])
            S.op("pool", lambda e: e.memset(carryM[:, :], 0.0), writes=[b_cM])
            QT, b_QT = sb(e1, [128, 4, 128])
            NMGB, b_NMGB = sb(e1, [128, 4, 513])
            S.op("pool", lambda e: e.memset(NMGB[:, :, :], 0.0), writes=[b_NMGB])
            Cst, b_Cst = sb(e1, [128, 4, 258]); Cbf, b_Cbf = sb(e1, [128, 4, 258], BF16)
            S.op("pool", lambda e: e.memset(Cst[:, :, :], 0.0), writes=[b_Cst])
            S.op("pool", lambda e: e.memset(Cbf[:, :, :], 0.0), writes=[b_Cbf])
            sc_l = [sb(e1, [128, 16]) for _ in range(4)]
            kw_l = [sb(e1, [128, 128], BF16) for _ in range(4)]
            arg_l = [sb(e1, [128, 128]) for _ in range(4)]; Pt_l = [sb(e1, [128, 128], BF16) for _ in range(4)]
            intra_l = [sb(e1, [128, 258]) for _ in range(4)]; Rn_l = [sb(e1, [128, 258]) for _ in range(4)]
            hraw_l = [sb(e1, [128, 256]) for _ in range(4)]
            hm_t, b_hm_t = sb(e1, [128, 1024]); hm_h, b_hm_h = sb(e1, [128, 1024], BF16)
            hmT_c, b_hmT_c = sb(e1, [128, 8, 128], BF16)
            st2_l = [sb(e1, [128, 6]) for _ in range(4)]; mv2_l = [sb(e1, [128, 2]) for _ in range(4)]; sd2_l = [sb(e1, [128, 2]) for _ in range(4)]

            def wload(c0, ncols):
                t, b = wbuf[wi[0] % 2]
                wi[0] += 1
                S.dma("pool", t[:, :, 0:ncols], w_in[:, c0:c0 + ncols].rearrange("(k p) n -> p k n", p=128), writes=[b])
                return t, b

            hook_box = [None]

            def proj_fm(wt, bw, ncol_chunks, consume):
                for cc in range(ncol_chunks):
                    p_, bp = ps()
                    for kc in range(16):
                        S.op("pe", lambda e, kc=kc, cc=cc, p_=p_: e.matmul(p_[:, :], lhsT=wt[:, kc, 128 * cc:128 * cc + 128],
                                                                         rhs=hT_st[:, kc, :], start=(kc == 0), stop=(kc == 15)),
                             reads=[bw, b_hT_st], writes=[bp])
                    consume(cc, p_, bp)
                    if hook_box[0] is not None:
                        hook_box[0]()

            def proj_tm(wt, bw, ncols, consume):
                for tt in range(4):
                    p_, bp = ps()
                    for kc in range(16):
                        S.op("pe", lambda e, kc=kc, tt=tt, p_=p_: e.matmul(p_[:, 0:ncols], lhsT=hT_st[:, kc, 128 * tt:128 * tt + 128],
                                                                         rhs=wt[:, kc, 0:ncols], start=(kc == 0), stop=(kc == 15)),
                             reads=[bw, b_hT_st], writes=[bp])
                    consume(tt, p_, bp)
                    if hook_box[0] is not None:
                        hook_box[0]()

            for sti in range(8):
                own = sti >= 6
                t0 = 512 * sti
                def x_load(n):
                    if n < 32:
                        xt_, b_xt_ = xs[n % 2]
                        S.dma("sp", xt_[:, :], x_win[128 * n:128 * n + 128, :], writes=[b_xt_])

                def ln_A(tt):
                    w0 = t0 + 128 * tt
                    xt, b_xt = xs[tt % 2]
                    xn, b_xn = xn_l[tt % 2]
                    (st, b_st), (mvt, b_mv), (sd, b_sd) = ln_l[tt % 2]
                    if sti == 0 and tt == 0:
                        x_load(0); x_load(1)
                    layer_norm_stats(xt, b_xt, st, b_st, mvt, b_mv, sd, b_sd)
                    S.op("act", lambda e: e.activation(out=xn[:, :], in_=xt[:, :], func=AF.Identity, scale=sd[:, 0:1], bias=sd[:, 1:2]),
                         reads=[b_xt, b_sd], writes=[b_xn])
                    x_load(4 * sti + tt + 2)
                    if own:
                        o0 = w0 - 3072
                        S.dma("sp", h_d[o0:o0 + 128, :], xn[:, :], reads=[b_xn], writes=[b_h_d])

                def ln_B(tt):
                    xn, b_xn = xn_l[tt % 2]
                    for g in range(4):
                        p_, bp = ps()
                        for j in range(4):
                            kc = 4 * g + j
                            S.op("pe", lambda e, kc=kc, j=j, p_=p_: e.transpose(out=p_[:, 128 * j:128 * j + 128],
                                                                              in_=xn[:, 128 * kc:128 * kc + 128], identity=idf[:, :]),
                                 reads=[b_xn, b_idf], writes=[bp])
                        for j in range(4):
                            kc = 4 * g + j
                            eng = evac_eng()
                            if eng == "act":
                                S.op("act", lambda e, kc=kc, j=j, p_=p_: e.activation(
                                    out=hT_st[:, kc, 128 * tt:128 * tt + 128], in_=p_[:, 128 * j:128 * j + 128], func=AF.Identity,
                                    scale=c_lng[:, kc:kc + 1], bias=c_lnb[:, kc:kc + 1]), reads=[bp, bC], writes=[b_hT_st])
                            else:
                                S.op("dve", lambda e, kc=kc, j=j, p_=p_: e.tensor_scalar(
                                    out=hT_st[:, kc, 128 * tt:128 * tt + 128], in0=p_[:, 128 * j:128 * j + 128],
                                    scalar1=c_lng[:, kc:kc + 1], scalar2=c_lnb[:, kc:kc + 1], op0=ALU.mult, op1=ALU.add),
                                     reads=[bp, bC], writes=[b_hT_st])
                ln_A(0); ln_A(1); ln_B(0); ln_A(2); ln_B(1); ln_A(3); ln_B(2); ln_B(3)
                if own:
                    o0 = t0 - 3072
                    S.dma("sp", hT_d[:, :, o0:o0 + 512].rearrange("k p t -> p k t"), hT_st[:, :, :], reads=[b_hT_st], writes=[b_hT_d])

                S.dma("sp", gm_sb[:, :, :], gmask[:, :, t0:t0 + 512], writes=[b_gm])
                S.dma("sp", vmask[:, :], valid_b[:, t0:t0 + 512], writes=[b_vmask])
                for gi_, (dst, b_dst) in enumerate(((gI, b_gI), (gF, b_gF))):
                    p_, bp = ps()
                    for kc in range(16):
                        S.op("pe", lambda e, kc=kc, p_=p_, gi_=gi_: e.matmul(p_[0:4, :], lhsT=wif[:, kc, 4 * gi_:4 * gi_ + 4],
                                                                           rhs=hT_st[:, kc, :], start=(kc == 0), stop=(kc == 15)),
                             reads=[b_wif, b_hT_st], writes=[bp])
                    S.op("dve", lambda e, p_=p_, gi_=gi_, dst=dst: e.scalar_tensor_tensor(
                        out=dst[:, :], in0=p_[0:4, :], scalar=c_ifb[:, gi_:gi_ + 1], in1=gm_sb[:, gi_, :], op0=ALU.add, op1=ALU.add),
                         reads=[bp, bC1, b_gm], writes=[b_dst])
                S.op("act", lambda e: e.activation(out=gF[:, :], in_=gF[:, :], func=AF.Exp, scale=-1.0), reads=[b_gF], writes=[b_gF])
                S.op("dve", lambda e: e.tensor_scalar(out=gF[:, :], in0=gF[:, :], scalar1=1.0, scalar2=None, op0=ALU.add),
                     reads=[b_gF], writes=[b_gF])
                S.op("act", lambda e: e.activation(out=gF[:, :], in_=gF[:, :], func=AF.Ln), reads=[b_gF], writes=[b_gF])
                S.op("dve", lambda e: e.tensor_tensor_scan(out=gB[:, :], data0=ones512[:, :], data1=gF[:, :], initial=carryB[:, 0:1],
                                                           op0=ALU.mult, op1=ALU.subtract),
                     reads=[b_gF, b_cB, b_ones512], writes=[b_gB])
                S.op("dve", lambda e: e.tensor_copy(out=carryB[:, :], in_=gB[:, 511:512]), reads=[b_gB], writes=[b_cB])
                S.op("dve", lambda e: e.tensor_tensor(out=QS[0:4, :], in0=gI[:, :], in1=gB[:, :], op=ALU.subtract),
                     reads=[b_gI, b_gB], writes=[b_QS])
                S.op("dve", lambda e: e.tensor_tensor_scan(out=gMg[:, :], data0=QS[0:4, :], data1=QS[0:4, :], initial=carryM[:, 0:1],
                                                           op0=ALU.max, op1=ALU.max),
                     reads=[b_QS, b_cM], writes=[b_gMg])
                S.op("dve", lambda e: e.tensor_copy(out=carryM[:, :], in_=gMg[:, 511:512]), reads=[b_gMg], writes=[b_cM])
                S.op("dve", lambda e: e.tensor_scalar(out=QS[32:36, :], in0=gMg[:, :], scalar1=-1.0, scalar2=None, op0=ALU.mult),
                     reads=[b_gMg], writes=[b_QS])
                S.op("dve", lambda e: e.tensor_tensor(out=gI[:, :], in0=gB[:, :], in1=gMg[:, :], op=ALU.add),
                     reads=[b_gB, b_gMg], writes=[b_gI])
                S.op("act", lambda e: e.activation(out=QS[64:68, :], in_=gI[:, :], func=AF.Exp, scale=-1.0), reads=[b_gI], writes=[b_QS])
                S.op("dve", lambda e: e.tensor_scalar(out=gF[:, :], in0=gMg[:, :], scalar1=-1.0, scalar2=None, op0=ALU.mult),
                     reads=[b_gMg], writes=[b_gF])
                for which in ((1, 0) if sti >= 5 else (1,)):
                    wt, bw = wload(C_MK if which else C_MQ, 512)
                    dstT, b_dstT = (kT_st, b_kT_st) if which else (qT_st, b_qT_st)
                    def cons_c(cc, p_, bp, which=which, dstT=dstT, b_dstT=b_dstT):
                        ch = 4 * which + cc
                        S.op("act", lambda e: e.copy(out=rw[:, 0:3], in_=halo[:, ch, :]), reads=[b_halo], writes=[b_rw])
                        S.op("dve", lambda e: e.tensor_tensor(out=rw[:, 3:515], in0=p_[:, :], in1=vmask[:, :], op=ALU.mult),
                             reads=[bp, b_vmask], writes=[b_rw])
                        S.op("dve", lambda e: e.tensor_scalar(out=cacc[:, :], in0=rw[:, 3:515], scalar1=c_cw[:, ch, 3:4],
                                                              scalar2=c_cb[:, ch:ch + 1], op0=ALU.mult, op1=ALU.add),
                             reads=[b_rw, bC1], writes=[b_cacc])
                        for j in range(3):
                            S.op("dve", lambda e, j=j: e.scalar_tensor_tensor(out=cacc[:, :], in0=rw[:, j:j + 512],
                                                                              scalar=c_cw[:, ch, j:j + 1], in1=cacc[:, :],
                                                                              op0=ALU.mult, op1=ALU.add),
                                 reads=[b_rw, bC1, b_cacc], writes=[b_cacc])
                        S.op("act", lambda e: e.activation(out=dstT[:, cc, :], in_=cacc[:, :], func=AF.Silu),
                             reads=[b_cacc], writes=[b_dstT])
                        S.op("act", lambda e: e.copy(out=halo[:, ch, :], in_=rw[:, 512:515]), reads=[b_rw], writes=[b_halo])
                    proj_fm(wt, bw, 4, cons_c)
                for half in range(2):
                    wt, bw = wload(C_MV + 512 * half, 512)
                    def cons_mv(tt, p_, bp, half=half):
                        copy(evac_eng(), vext[:, tt, 2 * half:2 * half + 2, 0:256],
                             p_[:, :].rearrange("p (h c) -> p h c", c=256), [bp], [b_vext])
                    proj_tm(wt, bw, 512, cons_mv)
                if own:
                    for half in range(2):
                        wt, bw = wload(C_MO + 512 * half, 512)
                        def cons_mo(tt, p_, bp, half=half):
                            S.op("act", lambda e: e.activation(out=smo[:, tt, 512 * half:512 * half + 512], in_=p_[:, :], func=AF.Sigmoid),
                                 reads=[bp], writes=[b_smo])
                        proj_tm(wt, bw, 512, cons_mo)
                S.op("dve", lambda e: e.tensor_copy(out=NMGB[:, :, 0:1], in_=NMGB[:, :, 512:513]), reads=[b_NMGB], writes=[b_NMGB])
                for hd in range(4):
                    p_, bp = ps()
                    S.op("pe", lambda e, hd=hd, p_=p_: e.matmul(p_[:, :], lhsT=c_sel4[:, hd, :], rhs=gF[:, :], start=True, stop=True),
                         reads=[b_gF, bC1], writes=[bp])
                    copy(evac_eng(), NMGB[:, hd, 1:513], p_[:, :], [bp], [b_NMGB])
                for c in range(4):
                    p_, bp = ps()
                    S.op("pe", lambda e, c=c, p_=p_: e.transpose(out=p_[:, 0:128], in_=QS[:, 128 * c:128 * c + 128], identity=idf[:, :]),
                         reads=[b_QS, b_idf], writes=[bp])
                    copy(evac_eng(), QT[:, c, :], p_[:, 0:128], [bp], [b_QT])

                def rec_gen():
                    HD = range(4)
                    for c in range(4):
                        cs_ = slice(128 * c, 128 * c + 128)
                        def nmgL(hd): return NMGB[:, hd, 128 * c + 128:128 * c + 129]
                        def nmgS(hd): return NMGB[:, hd, 128 * c:128 * c + 1]
                        def Acol(hd): return QT[:, c, hd:hd + 1]
                        if own:
                            pS_l = {}; pI_l = {}; pX_l = {}
                            yield
                            for hd in HD:
                                pS, bpS = psr(); pS_l[hd] = (pS, bpS)
                                S.op("pe", lambda e, pS=pS, hd=hd: e.matmul(pS[:, 0:128], lhsT=kT_st[:, hd, cs_], rhs=qT_st[:, hd, cs_], start=True, stop=True),
                                     reads=[b_kT_st, b_qT_st], writes=[bpS])
                            yield
                            for hd in HD:
                                arg, b_arg = arg_l[hd]
                                S.op("dve", lambda e, hd=hd, arg=arg: e.scalar_tensor_tensor(
                                    out=arg[:, :], in0=NMGB[:, hd, 128 * c + 1:128 * c + 129], scalar=Acol(hd), in1=c_tri[:, :],
                                    op0=ALU.add, op1=ALU.add), reads=[b_NMGB, b_QT, bC1], writes=[b_arg])
                            yield
                            for hd in HD:
                                arg, b_arg = arg_l[hd]
                                S.op("act", lambda e, arg=arg: e.activation(out=arg[:, :], in_=arg[:, :], func=AF.Exp), reads=[b_arg], writes=[b_arg])
                            yield
                            for hd in HD:
                                arg, b_arg = arg_l[hd]; Pt, b_Pt = Pt_l[hd]; pS, bpS = pS_l[hd]
                                S.op("dve", lambda e, pS=pS, Pt=Pt, arg=arg: e.scalar_tensor_tensor(out=Pt[:, :], in0=pS[:, 0:128], scalar=128 ** -0.5,
                                                                                                  in1=arg[:, :], op0=ALU.mult, op1=ALU.mult),
                                     reads=[bpS, b_arg], writes=[b_Pt])
                            yield
                            for hd in HD:
                                Pt, b_Pt = Pt_l[hd]
                                pI, bpI = psr(); pI_l[hd] = (pI, bpI)
                                S.op("pe", lambda e, pI=pI, hd=hd, Pt=Pt: e.matmul(pI[:, 0:258], lhsT=Pt[:, :], rhs=vext[:, c, hd, :], start=True, stop=True),
                                     reads=[b_Pt, b_vext], writes=[bpI])
                            yield
                            for hd in HD:
                                intra, b_intra = intra_l[hd]; pI, bpI = pI_l[hd]
                                S.op("act", lambda e, pI=pI, intra=intra: e.copy(out=intra[:, :], in_=pI[:, 0:258]), reads=[bpI], writes=[b_intra])
                            yield
                            for hd in HD:
                                pX, bpX = psr(); pX_l[hd] = (pX, bpX)
                                S.op("pe", lambda e, pX=pX, hd=hd: e.matmul(pX[:, 0:258], lhsT=qT_st[:, hd, cs_], rhs=Cbf[:, hd, :], start=True, stop=True),
                                     reads=[b_qT_st, b_Cbf], writes=[bpX])
                            yield
                            for hd in HD:
                                sc, b_sc = sc_l[hd]
                                S.op("dve", lambda e, hd=hd, sc=sc: e.tensor_scalar(out=sc[:, 0:1], in0=nmgS(hd), scalar1=-1.0, scalar2=None, op0=ALU.mult),
                                     reads=[b_NMGB], writes=[b_sc])
                            yield
                            for hd in HD:
                                sc, b_sc = sc_l[hd]; intra, b_intra = intra_l[hd]; pI, bpI = pI_l[hd]
                                S.op("act", lambda e, hd=hd, sc=sc: e.activation(out=sc[:, 1:2], in_=QT[:, c, 32 + hd:33 + hd], func=AF.Exp, bias=sc[:, 0:1]),
                                     reads=[b_QT, b_sc], writes=[b_sc])
                            yield
                            for hd in HD:
                                sc, b_sc = sc_l[hd]
                                S.op("dve", lambda e, sc=sc: e.tensor_scalar(out=sc[:, 1:2], in0=sc[:, 1:2], scalar1=128 ** -0.5, scalar2=None, op0=ALU.mult),
                                     reads=[b_sc], writes=[b_sc])
                            yield
                            for hd in HD:
                                sc, b_sc = sc_l[hd]; intra, b_intra = intra_l[hd]; Rn, b_Rn = Rn_l[hd]; pX, bpX = pX_l[hd]
                                S.op("dve", lambda e, pX=pX, sc=sc, intra=intra, Rn=Rn: e.scalar_tensor_tensor(out=Rn[:, :], in0=pX[:, 0:258], scalar=sc[:, 1:2],
                                                                                                             in1=intra[:, :], op0=ALU.mult, op1=ALU.add),
                                     reads=[bpX, b_sc, b_intra], writes=[b_Rn])
                            yield
                            for hd in HD:
                                sc, b_sc = sc_l[hd]; Rn, b_Rn = Rn_l[hd]
                                S.op("dve", lambda e, sc=sc, Rn=Rn: e.tensor_scalar(out=sc[:, 3:4], in0=Rn[:, 256:257], scalar1=-1.0, scalar2=None, op0=ALU.mult),
                                     reads=[b_Rn], writes=[b_sc])
                            yield
                            for hd in HD:
                                sc, b_sc = sc_l[hd]; Rn, b_Rn = Rn_l[hd]
                                S.op("dve", lambda e, sc=sc, Rn=Rn: e.tensor_tensor(out=sc[:, 2:3], in0=sc[:, 3:4], in1=Rn[:, 256:257], op=ALU.max),
                                     reads=[b_Rn, b_sc], writes=[b_sc])
                            yield
                            for hd in HD:
                                sc, b_sc = sc_l[hd]
                                S.op("dve", lambda e, hd=hd, sc=sc: e.tensor_tensor(out=sc[:, 2:3], in0=sc[:, 2:3], in1=QT[:, c, 64 + hd:65 + hd], op=ALU.max),
                                     reads=[b_sc, b_QT], writes=[b_sc])
                            yield
                            for hd in HD:
                                sc, b_sc = sc_l[hd]
                                S.op("dve", lambda e, sc=sc: e.reciprocal(out=sc[:, 2:3], in_=sc[:, 2:3]), reads=[b_sc], writes=[b_sc])
                            yield
                            for hd in HD:
                                sc, b_sc = sc_l[hd]; Rn, b_Rn = Rn_l[hd]; hraw, b_hraw = hraw_l[hd]
                                S.op("dve", lambda e, sc=sc, Rn=Rn, hraw=hraw: e.tensor_scalar(out=hraw[:, :], in0=Rn[:, 0:256], scalar1=sc[:, 2:3], scalar2=None, op0=ALU.mult),
                                     reads=[b_Rn, b_sc], writes=[b_hraw])
                            yield
                            for hd in HD:
                                hraw, b_hraw = hraw_l[hd]; st2, b_st2 = st2_l[hd]
                                S.op("dve", lambda e, hraw=hraw, st2=st2: e.bn_stats(out=st2[:, 0:6], in_=hraw[:, :]), reads=[b_hraw], writes=[b_st2])
                            yield
                            for hd in HD:
                                st2, b_st2 = st2_l[hd]; mv2, b_mv2 = mv2_l[hd]
                                S.op("dve", lambda e, st2=st2, mv2=mv2: e.bn_aggr(out=mv2[:, 0:2], in_=st2[:, 0:6]), reads=[b_st2], writes=[b_mv2])
                            yield
                            for hd in HD:
                                mv2, b_mv2 = mv2_l[hd]; sd2, b_sd2 = sd2_l[hd]
                                S.op("dve", lambda e, mv2=mv2, sd2=sd2: e.tensor_scalar(out=sd2[:, 0:1], in0=mv2[:, 1:2], scalar1=EPS, scalar2=None, op0=ALU.add),
                                     reads=[b_mv2], writes=[b_sd2])
                            yield
                            for hd in HD:
                                sd2, b_sd2 = sd2_l[hd]
                                S.op("act", lambda e, sd2=sd2: e.activation(out=sd2[:, 0:1], in_=sd2[:, 0:1], func=AF.Sqrt), reads=[b_sd2], writes=[b_sd2])
                            yield
                            for hd in HD:
                                sd2, b_sd2 = sd2_l[hd]
                                S.op("dve", lambda e, sd2=sd2: e.reciprocal(out=sd2[:, 0:1], in_=sd2[:, 0:1]), reads=[b_sd2], writes=[b_sd2])
                            yield
                            for hd in HD:
                                mv2, b_mv2 = mv2_l[hd]; sd2, b_sd2 = sd2_l[hd]
                                S.op("dve", lambda e, mv2=mv2, sd2=sd2: e.tensor_scalar(out=sd2[:, 1:2], in0=mv2[:, 0:1], scalar1=sd2[:, 0:1], scalar2=-1.0,
                                                                                      op0=ALU.mult, op1=ALU.mult), reads=[b_mv2, b_sd2], writes=[b_sd2])
                            yield
                            for hd in HD:
                                hraw, b_hraw = hraw_l[hd]; sd2, b_sd2 = sd2_l[hd]
                                S.op("act", lambda e, hraw=hraw, sd2=sd2: e.activation(out=hraw[:, :], in_=hraw[:, :], func=AF.Identity, scale=sd2[:, 0:1], bias=sd2[:, 1:2]),
                                     reads=[b_hraw, b_sd2], writes=[b_hraw])
                            yield
                            for hd in HD:
                                hraw, b_hraw = hraw_l[hd]
                                S.op("dve", lambda e, hd=hd, hraw=hraw: e.tensor_tensor(out=hm_t[:, 256 * hd:256 * hd + 256], in0=hraw[:, :],
                                                                                      in1=c_mnw[:, 256 * hd:256 * hd + 256], op=ALU.mult),
                                     reads=[b_hraw, bC1], writes=[b_hm_t])
                        pK_l = {}; pU_l = {}
                        yield
                        for hd in HD:
                            sc, b_sc = sc_l[hd]
                            S.op("act", lambda e, hd=hd, sc=sc: e.activation(out=sc[:, 4:5], in_=Acol(hd), func=AF.Exp, bias=nmgL(hd)),
                                 reads=[b_QT, b_NMGB], writes=[b_sc])
                            S.op("act", lambda e, hd=hd, sc=sc: e.activation(out=sc[:, 5:6], in_=nmgS(hd), func=AF.Exp, scale=-1.0, bias=nmgL(hd)),
                                 reads=[b_NMGB], writes=[b_sc])
                        yield
                        for hd in HD:
                            pK, bpK = psr(); pKh = pK.bitcast(BF16); pK_l[hd] = (pKh, bpK)
                            S.op("pe", lambda e, pKh=pKh, hd=hd: e.transpose(out=pKh[:, 0:128], in_=kT_st[:, hd, cs_], identity=idh[:, :]),
                                 reads=[b_kT_st, b_idh], writes=[bpK])
                        yield
                        for hd in HD:
                            sc, b_sc = sc_l[hd]; kw, b_kw = kw_l[hd]; pKh, bpK = pK_l[hd]
                            S.op("dve", lambda e, pKh=pKh, sc=sc, kw=kw: e.tensor_scalar(out=kw[:, :], in0=pKh[:, 0:128], scalar1=sc[:, 4:5], scalar2=None, op0=ALU.mult),
                                 reads=[bpK, b_sc], writes=[b_kw])
                        yield
                        for hd in HD:
                            kw, b_kw = kw_l[hd]
                            pU, bpU = psr(); pU_l[hd] = (pU, bpU)
                            S.op("pe", lambda e, pU=pU, hd=hd, kw=kw: e.matmul(pU[:, 0:258], lhsT=kw[:, :], rhs=vext[:, c, hd, :], start=True, stop=True),
                                 reads=[b_kw, b_vext], writes=[bpU])
                        yield
                        for hd in HD:
                            sc, b_sc = sc_l[hd]; pU, bpU = pU_l[hd]
                            S.op("dve", lambda e, pU=pU, hd=hd, sc=sc: e.scalar_tensor_tensor(out=Cst[:, hd, :], in0=Cst[:, hd, :], scalar=sc[:, 5:6],
                                                                                            in1=pU[:, 0:258], op0=ALU.mult, op1=ALU.add),
                                 reads=[b_Cst, b_sc, bpU], writes=[b_Cst])
                        if sti >= 5:
                            S.op("act", lambda e: e.copy(out=Cbf[:, :, :], in_=Cst[:, :, :]), reads=[b_Cst], writes=[b_Cbf])
                        if own:
                            S.op("dve", lambda e, c=c: e.tensor_tensor(out=hm_t[:, :], in0=hm_t[:, :], in1=smo[:, c, :], op=ALU.mult),
                                 reads=[b_hm_t, b_smo], writes=[b_hm_t])
                            o0 = t0 - 3072 + 128 * c
                            if DEBUG:
                                S.dma("sp", dbg["dbg_hm"][o0:o0 + 128, :], hm_t[:, :], reads=[b_hm_t], final=True)
                            S.op("act", lambda e: e.copy(out=hm_h[:, :], in_=hm_t[:, :]), reads=[b_hm_t], writes=[b_hm_h])
                            for half in range(2):
                                p_, bp = psr()
                                ph = p_.bitcast(BF16)
                                for j in range(4):
                                    kc = 4 * half + j
                                    S.op("pe", lambda e, ph=ph, kc=kc, j=j: e.transpose(out=ph[:, 128 * j:128 * j + 128],
                                                                                      in_=hm_h[:, 128 * kc:128 * kc + 128], identity=idh[:, :]),
                                         reads=[b_hm_h, b_idh], writes=[bp])
                                copy(evac_eng(), hmT_c[:, 4 * half:4 * half + 4, :],
                                     ph[:, 0:512].rearrange("p (k t) -> p k t", t=128), [bp], [b_hmT_c])
                            S.dma("sp", hmT_d[:, :, o0:o0 + 128].rearrange("k p t -> p k t"), hmT_c[:, :, :], reads=[b_hmT_c], writes=[b_hmT_d])
                    yield
                rec = rec_gen()
                hook_box[0] = lambda: next(rec, None)
                ps_split[0] = True; psi[0] = 0; psr_i[0] = 0
                att_groups = [g for g in range(3) if t0 >= (1024 if g == 2 else 2560)]
                for g in att_groups:
                    wt, bw = wload(C_AK + 512 * g, 512)
                    def cons_k(cc, p_, bp, g=g):
                        copy(evac_eng(), stage_o[:, cc, :], p_[:, :], [bp], [b_stage_o])
                    proj_fm(wt, bw, 4, cons_k)
                    S.dma("sp", kT_d[4 * g:4 * g + 4, :, t0:t0 + 512].rearrange("h p t -> p h t"), stage_o[:, :, :],
                          reads=[b_stage_o], writes=[b_kT_d])
                    wt, bw = wload(C_AV + 512 * g, 512)
                    def cons_v(tt, p_, bp, g=g):
                        copy(evac_eng(), stage_t[:, tt, :], p_[:, :], [bp], [b_stage_t])
                    proj_tm(wt, bw, 512, cons_v)
                    S.dma("sp", v_d[t0:t0 + 512, 512 * g:512 * g + 512].rearrange("(t p) c -> p t c", p=128), stage_t[:, :, :],
                          reads=[b_stage_t], writes=[b_v_d])
                    if own:
                        wt, bw = wload(C_AQ + 512 * g, 512)
                        proj_fm(wt, bw, 4, cons_k)
                        o0 = t0 - 3072
                        S.dma("sp", qT_d[4 * g:4 * g + 4, :, o0:o0 + 512].rearrange("h p t -> p h t"), stage_o[:, :, :],
                              reads=[b_stage_o], writes=[b_qT_d])

                hook_box[0] = None
                for _ in rec:
                    pass
                ps_split[0] = False; psi[0] = 0
        S.barrier()
        if upto >= 2:
          with ExitStack() as e2:
            qT_sb, b_qT_sb = sb(e2, [128, 4, NOWN], BF16)
            kT_sb, b_kT_sb = sb(e2, [128, 4, 3072], BF16)
            pad_sb, b_pad_sb = sb(e2, [128, 3072])
            S.dma("sp", pad_sb[:, :], padneg_b[:, 1024:4096], writes=[b_pad_sb])
            ab_sb, b_ab_sb = sb(e2, [128, 4, 256])
            Vb = [sb(e2, [128, 2, 512], BF16) for _ in range(2)]
            S2_l = [sb(e2, [128, 256]) for _ in range(4)]; Pb_l = [sb(e2, [128, 256], BF16) for _ in range(4)]
            PT_l = [sb(e2, [128, 2, 128], BF16) for _ in range(4)]
            a_sc_l = [sb(e2, [128, 8]) for _ in range(4)]
            o_sb = [sb(e2, [128, 4, 128]) for _ in range(2)]
            l_sb = [sb(e2, [128, 4]) for _ in range(2)]
            ui = 0
            for g in range(3):
                r = (1, 4, 16)[g]
                w_lo = 1024 if g == 2 else 2560
                nq = min(128, 1024 // r)
                nk = 128 + nq
                nch = (1024 // r) // nq
                S.dma("sp", qT_sb[:, :, :], qT_d[4 * g:4 * g + 4, :, :].rearrange("h p t -> p h t"), reads=[b_qT_d], writes=[b_qT_sb])
                S.dma("sp", kT_sb[:, :, 0:4096 - w_lo], kT_d[4 * g:4 * g + 4, :, w_lo:4096].rearrange("h p t -> p h t"),
                      reads=[b_kT_d], writes=[b_kT_sb])
                S.dma("sp", ab_sb[:, :, :], abias[4 * g:4 * g + 4, :, :].rearrange("h p k -> p h k"), writes=[b_ab_sb])
                for p in range(r):
                    for ci in range(nch):
                        j0 = 3072 // r + ci * nq
                        wk0 = p + r * (j0 - 128)
                        kc0 = wk0 - w_lo
                        qc0 = p + r * j0 - 3072
                        vt, b_vt = Vb[ui % 2]
                        ot, b_ot = o_sb[ui % 2]
                        lt, b_lt = l_sb[ui % 2]
                        ui += 1
                        S.dma("sp", vt[:, 0, :], v_d[ss(wk0, 128, r), 512 * g:512 * g + 512], reads=[b_v_d], writes=[b_vt])
                        S.dma("sp", vt[0:nq, 1, :], v_d[ss(wk0 + 128 * r, nq, r), 512 * g:512 * g + 512], reads=[b_v_d], writes=[b_vt])
                        HD = range(4)
                        pS_l = {}; pT_l = {}; pO_l = {}
                        for hd in HD:
                            pS, bpS = ps(); pS_l[hd] = (pS, bpS)
                            S.op("pe", lambda e, pS=pS, hd=hd: e.matmul(pS[0:nq, 0:nk], lhsT=qT_sb[:, hd, ss(qc0, nq, r)],
                                                                      rhs=kT_sb[:, hd, ss(kc0, nk, r)], start=True, stop=True),
                                 reads=[b_qT_sb, b_kT_sb], writes=[bpS])
                        for hd in HD:
                            S2, b_S2 = S2_l[hd]; pS, bpS = pS_l[hd]
                            S.op("dve", lambda e, pS=pS, hd=hd, S2=S2: e.scalar_tensor_tensor(out=S2[0:nq, 0:nk], in0=pS[0:nq, 0:nk], scalar=128 ** -0.5,
                                                                                            in1=ab_sb[0:nq, hd, 0:nk], op0=ALU.mult, op1=ALU.add),
                                 reads=[bpS, b_ab_sb], writes=[b_S2])
                            S.op("pool", lambda e, S2=S2: e.tensor_tensor(out=S2[0:nq, 0:nk], in0=S2[0:nq, 0:nk], in1=pad_sb[0:nq, ss(wk0 - 1024, nk, r)], op=ALU.add),
                                 reads=[b_S2, b_pad_sb], writes=[b_S2])
                        for hd in HD:
                            S2, b_S2 = S2_l[hd]; a_sc, b_a_sc = a_sc_l[hd]
                            S.op("dve", lambda e, S2=S2, a_sc=a_sc: e.tensor_reduce(out=a_sc[0:nq, 1:2], in_=S2[0:nq, 0:nk], axis=AX.X, op=ALU.max, negate=True),
                                 reads=[b_S2], writes=[b_a_sc])
                        for hd in HD:
                            S2, b_S2 = S2_l[hd]; a_sc, b_a_sc = a_sc_l[hd]; Pb, b_Pb = Pb_l[hd]
                            S.op("act", lambda e, S2=S2, a_sc=a_sc, Pb=Pb: e.activation(out=Pb[0:nq, 0:nk], in_=S2[0:nq, 0:nk], func=AF.Exp, bias=a_sc[0:nq, 1:2],
                                                                                       accum_out=a_sc[0:nq, 2:3]), reads=[b_S2, b_a_sc], writes=[b_Pb, b_a_sc])
                        for hd in HD:
                            Pb, b_Pb = Pb_l[hd]
                            pT_, bpT = ps(); pTh = pT_.bitcast(BF16); pT_l[hd] = (pTh, bpT)
                            S.op("pe", lambda e, pTh=pTh, Pb=Pb: e.transpose(out=pTh[:, 0:nq], in_=Pb[0:nq, 0:128], identity=idh[0:nq, 0:nq]),
                                 reads=[b_Pb, b_idh], writes=[bpT])
                            S.op("pe", lambda e, pTh=pTh, Pb=Pb: e.transpose(out=pTh[0:nq, 128:128 + nq], in_=Pb[0:nq, 128:128 + nq], identity=idh[0:nq, 0:nq]),
                                 reads=[b_Pb, b_idh], writes=[bpT])
                        for hd in HD:
                            PT, b_PT = PT_l[hd]; pTh, bpT = pT_l[hd]
                            copy("act", PT[:, 0, 0:nq], pTh[:, 0:nq], [bpT], [b_PT])
                            copy("dve", PT[0:nq, 1, 0:nq], pTh[0:nq, 128:128 + nq], [bpT], [b_PT])
                        for hd in HD:
                            PT, b_PT = PT_l[hd]
                            pO, bpO = ps(); pO_l[hd] = (pO, bpO)
                            S.op("pe", lambda e, pO=pO, hd=hd, vt=vt, PT=PT: e.matmul(pO[0:nq, 0:128], lhsT=PT[:, 0, 0:nq], rhs=vt[:, 0, 128 * hd:128 * hd + 128],
                                                                                    start=True, stop=False), reads=[b_PT, b_vt], writes=[bpO])
                            S.op("pe", lambda e, pO=pO, hd=hd, vt=vt, PT=PT: e.matmul(pO[0:nq, 0:128], lhsT=PT[0:nq, 1, 0:nq], rhs=vt[0:nq, 1, 128 * hd:128 * hd + 128],
                                                                                    start=False, stop=True), reads=[b_PT, b_vt], writes=[bpO])
                        for hd in HD:
                            a_sc, b_a_sc = a_sc_l[hd]
                            S.op("dve", lambda e, a_sc=a_sc: e.reciprocal(out=a_sc[0:nq, 3:4], in_=a_sc[0:nq, 2:3]), reads=[b_a_sc], writes=[b_a_sc])
                            S.op("act", lambda e, a_sc=a_sc: e.activation(out=a_sc[0:nq, 4:5], in_=a_sc[0:nq, 2:3], func=AF.Ln), reads=[b_a_sc], writes=[b_a_sc])
                        for hd in HD:
                            a_sc, b_a_sc = a_sc_l[hd]; pO, bpO = pO_l[hd]
                            S.op("dve", lambda e, pO=pO, hd=hd, ot=ot, a_sc=a_sc: e.tensor_scalar(out=ot[0:nq, hd, :], in0=pO[0:nq, 0:128], scalar1=a_sc[0:nq, 3:4],
                                                                                                scalar2=None, op0=ALU.mult), reads=[bpO, b_a_sc], writes=[b_ot])
                            S.op("dve", lambda e, hd=hd, lt=lt, a_sc=a_sc: e.tensor_tensor(out=lt[0:nq, hd:hd + 1], in0=a_sc[0:nq, 4:5], in1=a_sc[0:nq, 1:2], op=ALU.subtract),
                                 reads=[b_a_sc], writes=[b_lt])
                        S.dma("sp", o_d[g, ss(qc0, nq, r), :], ot[0:nq, :, :].rearrange("p h d -> p (h d)"), reads=[b_ot], writes=[b_o_d])
                        S.dma("sp", lse_d[g, ss(qc0, nq, r), :], lt[0:nq, :], reads=[b_lt], writes=[b_lse_d])
            o3 = [sb(e2, [128, 3, 512]) for _ in range(2)]
            l3, b_l3 = sb(e2, [128, 3, 4]); e3, b_e3 = sb(e2, [128, 3, 4]); m_sc, b_m_sc = sb(e2, [128, 12])
            att_t, b_att_t = sb(e2, [128, 512]); att_h, b_att_h = sb(e2, [128, 512], BF16)
            attT_c, b_attT_c = sb(e2, [128, 4, 128], BF16)
            for t in range(8):
                ot3, b_ot3 = o3[t % 2]
                S.dma("sp", ot3[:, :, :], o_d[:, 128 * t:128 * t + 128, :].rearrange("g p c -> p g c"), reads=[b_o_d], writes=[b_ot3])
                S.dma("sp", l3[:, :, :], lse_d[:, 128 * t:128 * t + 128, :].rearrange("g p c -> p g c"), reads=[b_lse_d], writes=[b_l3])
                S.op("dve", lambda e: e.tensor_tensor(out=m_sc[:, 0:4], in0=l3[:, 0, :], in1=l3[:, 1, :], op=ALU.max), reads=[b_l3], writes=[b_m_sc])
                S.op("dve", lambda e: e.tensor_tensor(out=m_sc[:, 0:4], in0=m_sc[:, 0:4], in1=l3[:, 2, :], op=ALU.max), reads=[b_l3, b_m_sc], writes=[b_m_sc])
                for g in range(3):
                    S.op("dve", lambda e, g=g: e.tensor_tensor(out=e3[:, g, :], in0=l3[:, g, :], in1=m_sc[:, 0:4], op=ALU.subtract),
                         reads=[b_l3, b_m_sc], writes=[b_e3])
                S.op("act", lambda e: e.activation(out=e3[:, :, :], in_=e3[:, :, :], func=AF.Exp), reads=[b_e3], writes=[b_e3])
                S.op("dve", lambda e: e.tensor_tensor(out=m_sc[:, 4:8], in0=e3[:, 0, :], in1=e3[:, 1, :], op=ALU.add), reads=[b_e3], writes=[b_m_sc])
                S.op("dve", lambda e: e.tensor_tensor(out=m_sc[:, 4:8], in0=m_sc[:, 4:8], in1=e3[:, 2, :], op=ALU.add), reads=[b_e3, b_m_sc], writes=[b_m_sc])
                S.op("dve", lambda e: e.reciprocal(out=m_sc[:, 8:12], in_=m_sc[:, 4:8]), reads=[b_m_sc], writes=[b_m_sc])
                for g in range(3):
                    S.op("dve", lambda e, g=g: e.tensor_tensor(out=e3[:, g, :], in0=e3[:, g, :], in1=m_sc[:, 8:12], op=ALU.mult),
                         reads=[b_e3, b_m_sc], writes=[b_e3])
                for sl in range(4):
                    S.op("dve", lambda e, sl=sl, ot3=ot3: e.tensor_scalar(out=att_t[:, 128 * sl:128 * sl + 128], in0=ot3[:, 0, 128 * sl:128 * sl + 128],
                                                                        scalar1=e3[:, 0, sl:sl + 1], scalar2=None, op0=ALU.mult),
                         reads=[b_ot3, b_e3], writes=[b_att_t])
                    for g in (1, 2):
                        S.op("dve", lambda e, sl=sl, g=g, ot3=ot3: e.scalar_tensor_tensor(
                            out=att_t[:, 128 * sl:128 * sl + 128], in0=ot3[:, g, 128 * sl:128 * sl + 128], scalar=e3[:, g, sl:sl + 1],
                            in1=att_t[:, 128 * sl:128 * sl + 128], op0=ALU.mult, op1=ALU.add), reads=[b_ot3, b_e3, b_att_t], writes=[b_att_t])
                if DEBUG:
                    S.dma("sp", dbg["dbg_att"][128 * t:128 * t + 128, :], att_t[:, :], reads=[b_att_t], final=True)
                S.op("act", lambda e: e.copy(out=att_h[:, :], in_=att_t[:, :]), reads=[b_att_t], writes=[b_att_h])
                p_, bp = ps()
                ph = p_.bitcast(BF16)
                for j in range(4):
                    S.op("pe", lambda e, ph=ph, j=j: e.transpose(out=ph[:, 128 * j:128 * j + 128], in_=att_h[:, 128 * j:128 * j + 128], identity=idh[:, :]),
                         reads=[b_att_h, b_idh], writes=[bp])
                copy(evac_eng(), attT_c[:, :, :], ph[:, 0:512].rearrange("p (k t) -> p k t", t=128), [bp], [b_attT_c])
                S.dma("sp", attT_d[:, :, 128 * t:128 * t + 128].rearrange("k p t -> p k t"), attT_c[:, :, :], reads=[b_attT_c], writes=[b_attT_d])
          S.barrier()

        if upto >= 4:
          if True:
            with ExitStack() as e4a:
                mergedT, b_mergedT = sb(e4a, [128, 16, NOWN], BF16)
                hmT, b_hmT = sb(e4a, [128, 8, NOWN], BF16); attT, b_attT = sb(e4a, [128, 4, NOWN], BF16)
                S.dma("sp", hmT[:, :, :], hmT_d[:, :, :].rearrange("k p t -> p k t"), reads=[b_hmT_d], writes=[b_hmT])
                S.dma("sp", attT[:, :, :], attT_d[:, :, :].rearrange("k p t -> p k t"), reads=[b_attT_d], writes=[b_attT])
                hTo, b_hTo = sb(e4a, [128, 16, NOWN], BF16)
                S.dma("sp", hTo[:, :, :], hT_d[:, :, :].rearrange("k p t -> p k t"), reads=[b_hT_d], writes=[b_hTo])
                wpa, b_wpa = sb(e4a, [128, 4, D], BF16); wpm, b_wpm = sb(e4a, [128, 8, D], BF16)
                for kc in range(4):
                    S.dma("pool", wpa[:, kc, :], w_pa[128 * kc:128 * kc + 128, :], writes=[b_wpa])
                for kc in range(8):
                    S.dma("pool", wpm[:, kc, :], w_pm[128 * kc:128 * kc + 128, :], writes=[b_wpm])
                wg = [sb(e4a, [128, 2, 16, 256], BF16) for _ in range(2)]
                sg_l = [(sb(e4a, [128, 512]), sb(e4a, [128, 512])) for _ in range(2)]
                sgi = [0]
                for blk in range(8):
                    wgt, b_wgt = wg[blk % 2]
                    for a_, c0 in enumerate((C_GA, C_GM)):
                        S.dma("pool", wgt[:, a_, :, :], w_in[:, c0 + 256 * blk:c0 + 256 * blk + 256].rearrange("(k p) n -> p k n", p=128), writes=[b_wgt])
                    for sub in range(2):
                        cc = 2 * blk + sub
                        for th in range(2):
                            tk = slice(512 * th, 512 * th + 512)
                            pA, bpA = ps(); pM, bpM = ps(); pGA, bpGA = ps(); pGM, bpGM = ps()
                            (sga, b_sga), (sgm, b_sgm) = sg_l[sgi[0] % 2]; sgi[0] += 1
                            for kc in range(4):
                                S.op("pe", lambda e, kc=kc, pA=pA, cc=cc, tk=tk: e.matmul(pA[:, :], lhsT=wpa[:, kc, 128 * cc:128 * cc + 128], rhs=attT[:, kc, tk],
                                                                                       start=(kc == 0), stop=(kc == 3)), reads=[b_wpa, b_attT], writes=[bpA])
                            for kc in range(8):
                                S.op("pe", lambda e, kc=kc, pM=pM, cc=cc, tk=tk: e.matmul(pM[:, :], lhsT=wpm[:, kc, 128 * cc:128 * cc + 128], rhs=hmT[:, kc, tk],
                                                                                       start=(kc == 0), stop=(kc == 7)), reads=[b_wpm, b_hmT], writes=[bpM])
                            for a_, (pG, bpG) in enumerate(((pGA, bpGA), (pGM, bpGM))):
                                for kc in range(16):
                                    S.op("pe", lambda e, kc=kc, pG=pG, a_=a_, sub=sub, tk=tk, wgt=wgt: e.matmul(
                                        pG[:, :], lhsT=wgt[:, a_, kc, 128 * sub:128 * sub + 128], rhs=hTo[:, kc, tk], start=(kc == 0), stop=(kc == 15)),
                                         reads=[b_wgt, b_hTo], writes=[bpG])
                            S.op("act", lambda e, pGA=pGA: e.activation(out=sga[:, :], in_=pGA[:, :], func=AF.Sigmoid), reads=[bpGA], writes=[b_sga])
                            S.op("act", lambda e, pGM=pGM: e.activation(out=sgm[:, :], in_=pGM[:, :], func=AF.Sigmoid), reads=[bpGM], writes=[b_sgm])
                            S.op("dve", lambda e, pA=pA: e.tensor_tensor(out=sga[:, :], in0=sga[:, :], in1=pA[:, :], op=ALU.mult), reads=[b_sga, bpA], writes=[b_sga])
                            S.op("dve", lambda e, pM=pM: e.tensor_tensor(out=sgm[:, :], in0=sgm[:, :], in1=pM[:, :], op=ALU.mult), reads=[b_sgm, bpM], writes=[b_sgm])
                            S.op("dve", lambda e, cc=cc, tk=tk: e.tensor_tensor(out=mergedT[:, cc, tk], in0=sga[:, :], in1=sgm[:, :], op=ALU.add),
                                 reads=[b_sga, b_sgm], writes=[b_mergedT])
                S.dma("sp", mT_d[:, :, :].rearrange("k p t -> p k t"), mergedT[:, :, :], reads=[b_mergedT], writes=[b_mT_d])
            S.barrier()
            with ExitStack() as e5:
                mergedT, b_mergedT = sb(e5, [128, 16, NOWN], BF16)
                S.dma("sp", mergedT[:, :, :], mT_d[:, :, :].rearrange("k p t -> p k t"), reads=[b_mT_d], writes=[b_mergedT])
                h1c, b_h1c = sb(e5, [128, D], BF16)
                wo, b_wo = sb(e5, [128, 16, D], BF16)
                for kc in range(16):
                    S.dma("pool", wo[:, kc, :], w_out[128 * kc:128 * kc + 128, :], writes=[b_wo])
                cA, b_cA = sb(e5, [128, D]); cB, b_cB = sb(e5, [128, D])
                cG1, b_cG1 = sb(e5, [128, D]); cB1, b_cB1 = sb(e5, [128, D])
                S.dma("sp", cA[:, :], lng_b, writes=[b_cA]); S.dma("sp", cB[:, :], lnb_b, writes=[b_cB])
                S.dma("sp", cG1[:, :], ln1g_b, writes=[b_cG1]); S.dma("sp", cB1[:, :], ln1b_b, writes=[b_cB1])
                h1t, b_h1t = None, None
                c_wr, b_c_wr = sb(e5, [128, 16, 36]); c_br, b_c_br = sb(e5, [128, 36])
                S.dma("sp", c_wr[:, :, :], wr, writes=[b_c_wr]); S.dma("sp", c_br[:, :], br_b, writes=[b_c_br])
                xz_l = [(sb(e5, [128, D]), sb(e5, [128, D])) for _ in range(2)]
                h1T, b_h1T = sb(e5, [128, 16, 128])
                st, b_st = sb(e5, [128, 24]); mvt, b_mv = sb(e5, [128, 2]); sd, b_sd = sb(e5, [128, 2])
                lg, b_lg = sb(e5, [128, 36]); r_sc, b_r_sc = sb(e5, [128, 16]); goh, b_goh = sb(e5, [128, 4])
                esel, b_esel = sb(e5, [128, 8]); top8, b_top8 = sb(e5, [128, 8]); oh, b_oh = sb(e5, [128, 2, 8]); wsel, b_wsel = sb(e5, [128, 8])
                def s5_mm(t):
                        tk = slice(128 * t, 128 * t + 128)
                        (xnt, b_xnt), (z, b_z) = xz_l[t % 2]
                        h1t, b_h1t = z, b_z
                        S.dma("sp", xnt[:, :], h_d[tk, :], reads=[b_h_d], writes=[b_xnt])
                        S.op("dve", lambda e: e.tensor_tensor(out=xnt[:, :], in0=xnt[:, :], in1=cA[:, :], op=ALU.mult), reads=[b_xnt, b_cA], writes=[b_xnt])
                        S.op("dve", lambda e: e.tensor_tensor(out=xnt[:, :], in0=xnt[:, :], in1=cB[:, :], op=ALU.add), reads=[b_xnt, b_cB], writes=[b_xnt])
                        for cb_ in range(4):
                            cs = slice(512 * cb_, 512 * cb_ + 512)
                            pY, bpY = ps()
                            for kc in range(16):
                                S.op("pe", lambda e, kc=kc, pY=pY, cs=cs, tk=tk: e.matmul(pY[:, :], lhsT=mergedT[:, kc, tk], rhs=wo[:, kc, cs],
                                                                                       start=(kc == 0), stop=(kc == 15)), reads=[b_mergedT, b_wo], writes=[bpY])
                            S.op("dve", lambda e, pY=pY, cs=cs: e.scalar_tensor_tensor(out=z[:, cs], in0=xnt[:, cs], scalar=ALPHA, in1=pY[:, :],
                                                                                     op0=ALU.mult, op1=ALU.add), reads=[b_xnt, bpY], writes=[b_z])

                def s5_rest(t):
                        tk = slice(128 * t, 128 * t + 128)
                        (xnt, b_xnt), (z, b_z) = xz_l[t % 2]
                        h1t, b_h1t = z, b_z
                        layer_norm_stats(z, b_z, st, b_st, mvt, b_mv, sd, b_sd)
                        S.op("act", lambda e: e.activation(out=z[:, :], in_=z[:, :], func=AF.Identity, scale=sd[:, 0:1], bias=sd[:, 1:2]),
                             reads=[b_z, b_sd], writes=[b_z])
                        S.op("dve", lambda e: e.tensor_tensor(out=z[:, :], in0=z[:, :], in1=cG1[:, :], op=ALU.mult), reads=[b_z, b_cG1], writes=[b_z])
                        S.op("dve", lambda e: e.tensor_tensor(out=h1t[:, :], in0=h1t[:, :], in1=cB1[:, :], op=ALU.add), reads=[b_h1t, b_cB1], writes=[b_h1t])
                        S.dma("sp", h1_d[tk, :], h1t[:, :], reads=[b_h1t], writes=[b_h1_d])
                        if DEBUG:
                            S.dma("sp", dbg["dbg_h1"][tk, :], h1t[:, :], reads=[b_h1t], final=True)
                        S.op("act", lambda e: e.copy(out=h1c[:, :], in_=h1t[:, :]), reads=[b_h1t], writes=[b_h1c])
                        S.dma("sp", h1bf_d[tk, :], h1c[:, :], reads=[b_h1c], writes=[b_h1bf_d])
                        for g4 in range(4):
                            p_, bp = ps()
                            for j in range(4):
                                kc = 4 * g4 + j
                                S.op("pe", lambda e, p_=p_, kc=kc, j=j: e.transpose(out=p_[:, 128 * j:128 * j + 128], in_=h1t[:, 128 * kc:128 * kc + 128], identity=idf[:, :]),
                                     reads=[b_h1t, b_idf], writes=[bp])
                            copy(evac_eng(), h1T[:, 4 * g4:4 * g4 + 4, :], p_[:, :].rearrange("p (k t) -> p k t", t=128), [bp], [b_h1T])
                        pL, bpL = ps()
                        for kc in range(16):
                            S.op("pe", lambda e, kc=kc, pL=pL: e.matmul(pL[:, 0:36], lhsT=h1T[:, kc, :], rhs=c_wr[:, kc, :], start=(kc == 0), stop=(kc == 15)),
                                 reads=[b_h1T, b_c_wr], writes=[bpL])
                        S.op("dve", lambda e, pL=pL: e.tensor_tensor(out=lg[:, :], in0=pL[:, 0:36], in1=c_br[:, :], op=ALU.add), reads=[bpL, b_c_br], writes=[b_lg])
                        S.op("dve", lambda e: e.tensor_reduce(out=r_sc[:, 0:1], in_=lg[:, 0:4], axis=AX.X, op=ALU.max), reads=[b_lg], writes=[b_r_sc])
                        S.op("dve", lambda e: e.tensor_scalar(out=goh[:, :], in0=lg[:, 0:4], scalar1=r_sc[:, 0:1], scalar2=None, op0=ALU.is_equal),
                             reads=[b_lg, b_r_sc], writes=[b_goh])
                        S.op("dve", lambda e: e.tensor_scalar(out=r_sc[:, 1:2], in0=r_sc[:, 0:1], scalar1=-1.0, scalar2=None, op0=ALU.mult), reads=[b_r_sc], writes=[b_r_sc])
                        S.op("act", lambda e: e.activation(out=wsel[:, 0:4], in_=lg[:, 0:4], func=AF.Exp, bias=r_sc[:, 1:2], accum_out=r_sc[:, 2:3]),
                             reads=[b_lg, b_r_sc], writes=[b_wsel, b_r_sc])
                        S.op("dve", lambda e: e.reciprocal(out=r_sc[:, 3:4], in_=r_sc[:, 2:3]), reads=[b_r_sc], writes=[b_r_sc])
                        S.op("dve", lambda e: e.tensor_scalar(out=esel[:, :], in0=lg[:, 4:12], scalar1=goh[:, 0:1], scalar2=None, op0=ALU.mult),
                             reads=[b_lg, b_goh], writes=[b_esel])
                        for g in (1, 2, 3):
                            S.op("dve", lambda e, g=g: e.scalar_tensor_tensor(out=esel[:, :], in0=lg[:, 4 + 8 * g:12 + 8 * g], scalar=goh[:, g:g + 1], in1=esel[:, :],
                                                                              op0=ALU.mult, op1=ALU.add), reads=[b_lg, b_goh, b_esel], writes=[b_esel])
                        S.op("dve", lambda e: e.max(out=top8[:, :], in_=esel[:, :]), reads=[b_esel], writes=[b_top8])
                        S.op("dve", lambda e: e.tensor_tensor(out=r_sc[:, 4:5], in0=top8[:, 1:2], in1=top8[:, 0:1], op=ALU.subtract), reads=[b_top8], writes=[b_r_sc])
                        S.op("act", lambda e: e.activation(out=r_sc[:, 5:6], in_=r_sc[:, 4:5], func=AF.Exp), reads=[b_r_sc], writes=[b_r_sc])
                        S.op("dve", lambda e: e.tensor_scalar(out=r_sc[:, 6:7], in0=r_sc[:, 5:6], scalar1=1.0, scalar2=None, op0=ALU.add), reads=[b_r_sc], writes=[b_r_sc])
                        S.op("dve", lambda e: e.reciprocal(out=r_sc[:, 6:7], in_=r_sc[:, 6:7]), reads=[b_r_sc], writes=[b_r_sc])
                        S.op("dve", lambda e: e.tensor_tensor(out=r_sc[:, 7:8], in0=r_sc[:, 5:6], in1=r_sc[:, 6:7], op=ALU.mult), reads=[b_r_sc], writes=[b_r_sc])
                        S.op("dve", lambda e: e.tensor_scalar(out=r_sc[:, 6:8], in0=r_sc[:, 6:8], scalar1=r_sc[:, 3:4], scalar2=None, op0=ALU.mult),
                             reads=[b_r_sc], writes=[b_r_sc])
                        for j in range(2):
                            S.op("dve", lambda e, j=j: e.tensor_scalar(out=oh[:, j, :], in0=esel[:, :], scalar1=top8[:, j:j + 1], scalar2=None, op0=ALU.is_equal),
                                 reads=[b_esel, b_top8], writes=[b_oh])
                        S.op("dve", lambda e: e.tensor_scalar(out=wsel[:, :], in0=oh[:, 0, :], scalar1=r_sc[:, 6:7], scalar2=None, op0=ALU.mult),
                             reads=[b_oh, b_r_sc], writes=[b_wsel])
                        S.op("dve", lambda e: e.scalar_tensor_tensor(out=wsel[:, :], in0=oh[:, 1, :], scalar=r_sc[:, 7:8], in1=wsel[:, :], op0=ALU.mult, op1=ALU.add),
                             reads=[b_oh, b_r_sc, b_wsel], writes=[b_wsel])
                        for g in range(4):
                            S.op("dve", lambda e, g=g, t=t: e.tensor_scalar(out=Wt_all[:, t, 8 * g:8 * g + 8], in0=wsel[:, :], scalar1=goh[:, g:g + 1], scalar2=None, op0=ALU.mult),
                                 reads=[b_wsel, b_goh], writes=[b_Wt])
                        S.op("dve", lambda e, t=t: e.tensor_scalar(out=mk_all[:, t, :], in0=Wt_all[:, t, :], scalar1=0.0, scalar2=None, op0=ALU.is_gt),
                             reads=[b_Wt], writes=[b_mk])

                s5_mm(0)
                for t in range(8):
                    if t + 1 < 8:
                        s5_mm(t + 1)
                    s5_rest(t)
                c_ones, b_c_ones = sb(e5, [128, 128]); c_ltri, b_c_ltri = sb(e5, [128, 128])
                S.dma("sp", c_ones[:, :], ones_f, writes=[b_c_ones]); S.dma("sp", c_ltri[:, :], ltri, writes=[b_c_ltri])
                for t in range(8):
                    pC, bpC = ps()
                    for t2 in range(t):
                        S.op("pe", lambda e, pC=pC, t2=t2: e.matmul(pC[:, 0:32], lhsT=c_ones[:, :], rhs=mk_all[:, t2, :], start=(t2 == 0), stop=False),
                             reads=[b_c_ones, b_mk], writes=[bpC])
                    S.op("pe", lambda e, pC=pC, t=t: e.matmul(pC[:, 0:32], lhsT=c_ltri[:, :], rhs=mk_all[:, t, :], start=(t == 0), stop=True),
                         reads=[b_c_ltri, b_mk], writes=[bpC])
                    S.op("dve", lambda e, pC=pC, t=t: e.tensor_tensor(out=cumm[:, t, :], in0=pC[:, 0:32], in1=mk_all[:, t, :], op=ALU.mult),
                         reads=[bpC, b_mk], writes=[b_cumm])
          S.barrier()

        if upto >= 6:
          with ExitStack() as e6:
            y_acc, b_y_acc = sb(e6, [128, 8, D])
            S.op("pool", lambda e: e.memset(y_acc[:, :, :], 0.0), writes=[b_y_acc])
            e6a = ExitStack()
            e6_outer = e6
            e6 = e6a
            h1_bf, b_h1_bf = sb(e6, [128, 8, D], BF16)
            S.dma("sp", h1_bf[:, :, :], h1bf_d.rearrange("(t p) c -> p t c", p=128), reads=[b_h1bf_d], writes=[b_h1_bf])
            c_iota, b_c_iota = sb(e6, [128, 128])
            S.dma("sp", c_iota[:, :], iota1, writes=[b_c_iota])
            wgu = [sb(e6, [128, 16, 512], BF16) for _ in range(3)]
            wdn = [sb(e6, [128, 11, 512], BF16) for _ in range(2)]
            Sel, b_Sel = sb(e6, [128, 8, 128], BF16); SelT_l = [sb(e6, [128, NOWN], BF16) for _ in range(2)]
            XeT, b_XeT = sb(e6, [128, 16, 128], BF16)
            sg, b_sg = sb(e6, [128, DFF]); Hs, b_Hs = sb(e6, [128, DFF], BF16)
            HT, b_HT = sb(e6, [128, 11, 128], BF16); Yb_l = [sb(e6, [128, D], BF16) for _ in range(2)]
            pending = []
            gi_ = [0]; di_ = [0]
            for ex in range(NEXP):
                SelT, b_SelT = SelT_l[ex % 2]; Yb, b_Yb = Yb_l[ex % 2]
                for t in range(8):
                    S.op("dve", lambda e, t=t, ex=ex: e.tensor_scalar(out=Sel[:, t, :], in0=c_iota[:, :], scalar1=cumm[:, t, ex:ex + 1], scalar2=None, op0=ALU.is_equal),
                         reads=[b_c_iota, b_cumm], writes=[b_Sel])
                pB, bpB = ps()
                pBh = pB.bitcast(BF16)
                for t in range(8):
                    S.op("pe", lambda e, pBh=pBh, t=t: e.transpose(out=pBh[:, 128 * t:128 * t + 128], in_=Sel[:, t, :], identity=idh[:, :]),
                         reads=[b_Sel, b_idh], writes=[bpB])
                copy(evac_eng(), SelT[:, :], pBh[:, :], [bpB], [b_SelT])
                for g4 in range(4):
                    pX_, bpX_ = ps()
                    for j in range(4):
                        kc = 4 * g4 + j
                        for t in range(8):
                            S.op("pe", lambda e, pX_=pX_, kc=kc, j=j, t=t: e.matmul(pX_[:, 128 * j:128 * j + 128], lhsT=h1_bf[:, t, 128 * kc:128 * kc + 128], rhs=Sel[:, t, :],
                                                                                 start=(t == 0), stop=(t == 7)), reads=[b_h1_bf, b_Sel], writes=[bpX_])
                    copy(evac_eng(), XeT[:, 4 * g4:4 * g4 + 4, :], pX_[:, :].rearrange("p (k t) -> p k t", t=128), [bpX_], [b_XeT])
                for which, wsrc in enumerate((w_gate, w_up)):
                    for blk in range(3):
                        c0 = 512 * blk
                        ncol = min(512, DFF - c0)
                        wt_, bw_ = wgu[gi_[0] % 3]; gi_[0] += 1
                        S.dma("pool", wt_[:, :, 0:ncol], wsrc[ex, :, c0:c0 + ncol].rearrange("(k p) n -> p k n", p=128), writes=[bw_])
                        pG, bpG = ps()
                        for kc in range(16):
                            S.op("pe", lambda e, pG=pG, wt_=wt_, kc=kc: e.matmul(pG[:, 0:ncol], lhsT=XeT[:, kc, :], rhs=wt_[:, kc, 0:ncol], start=(kc == 0), stop=(kc == 15)),
                                 reads=[b_XeT, bw_], writes=[bpG])
                        if which == 0:
                            S.op("act", lambda e, pG=pG, c0=c0: e.activation(out=sg[:, c0:c0 + ncol], in_=pG[:, 0:ncol], func=AF.Silu), reads=[bpG], writes=[b_sg])
                            if blk == 2:
                                while pending:
                                    pending.pop(0)()
                        else:
                            S.op("dve", lambda e, pG=pG, c0=c0: e.tensor_tensor(out=Hs[:, c0:c0 + ncol], in0=sg[:, c0:c0 + ncol], in1=pG[:, 0:ncol], op=ALU.mult),
                                 reads=[b_sg, bpG], writes=[b_Hs])
                for g3 in range(3):
                    pH, bpH = ps()
                    pHh = pH.bitcast(BF16)
                    nj = min(4, 11 - 4 * g3)
                    for j in range(nj):
                        fc = 4 * g3 + j
                        S.op("pe", lambda e, pHh=pHh, j=j, fc=fc: e.transpose(out=pHh[:, 128 * j:128 * j + 128], in_=Hs[:, 128 * fc:128 * fc + 128], identity=idh[:, :]),
                             reads=[b_Hs, b_idh], writes=[bpH])
                    copy(evac_eng(), HT[:, 4 * g3:4 * g3 + nj, :], pHh[:, 0:128 * nj].rearrange("p (k t) -> p k t", t=128), [bpH], [b_HT])
                for cb_ in range(4):
                    cs = slice(512 * cb_, 512 * cb_ + 512)
                    wd_, bwd_ = wdn[di_[0] % 2]; di_[0] += 1
                    S.dma("pool", wd_[:, :, :], w_down[ex, :, cs].rearrange("(k p) n -> p k n", p=128), writes=[bwd_])
                    pY, bpY = ps()
                    for fc in range(11):
                        S.op("pe", lambda e, pY=pY, fc=fc, wd_=wd_: e.matmul(pY[:, :], lhsT=HT[:, fc, :], rhs=wd_[:, fc, :], start=(fc == 0), stop=(fc == 10)),
                             reads=[b_HT, bwd_], writes=[bpY])
                    copy(evac_eng(), Yb[:, cs], pY[:, :], [bpY], [b_Yb])
                def combine(ex=ex, SelT=SelT, b_SelT=b_SelT, Yb=Yb, b_Yb=b_Yb):
                    for t in range(8):
                        for cb_ in range(4):
                            cs = slice(512 * cb_, 512 * cb_ + 512)
                            pZ, bpZ = ps()
                            S.op("pe", lambda e, pZ=pZ, t=t, cs=cs: e.matmul(pZ[:, :], lhsT=SelT[:, 128 * t:128 * t + 128], rhs=Yb[:, cs], start=True, stop=True),
                                 reads=[b_SelT, b_Yb], writes=[bpZ])
                            S.op("dve", lambda e, pZ=pZ, t=t, cs=cs: e.scalar_tensor_tensor(out=y_acc[:, t, cs], in0=pZ[:, :], scalar=Wt_all[:, t, ex:ex + 1],
                                                                                          in1=y_acc[:, t, cs], op0=ALU.mult, op1=ALU.add),
                                 reads=[bpZ, b_Wt, b_y_acc], writes=[b_y_acc])
                pending.append(combine)
            while pending:
                pending.pop(0)()
            e6a.close()
            e6 = e6_outer
            S.barrier()
            cG2, b_cG2 = sb(e6, [128, D]); cB2, b_cB2 = sb(e6, [128, D])
            S.dma("sp", cG2[:, :], ln2g_b, writes=[b_cG2]); S.dma("sp", cB2[:, :], ln2b_b, writes=[b_cB2])
            h1r = [sb(e6, [128, D]) for _ in range(2)]
            st, b_st = sb(e6, [128, 24]); mvt, b_mv = sb(e6, [128, 2]); sd, b_sd = sb(e6, [128, 2])
            for t in range(8):
                tk = slice(128 * t, 128 * t + 128)
                ht_, b_ht_ = h1r[t % 2]
                if DEBUG:
                    S.dma("sp", dbg["dbg_y2"][tk, :], y_acc[:, t, :], reads=[b_y_acc], final=True)
                S.dma("sp", ht_[:, :], h1_d[tk, :], reads=[b_h1_d], writes=[b_ht_])
                S.op("dve", lambda e, t=t, ht_=ht_: e.scalar_tensor_tensor(out=ht_[:, :], in0=ht_[:, :], scalar=ALPHA, in1=y_acc[:, t, :], op0=ALU.mult, op1=ALU.add),
                     reads=[b_ht_, b_y_acc], writes=[b_ht_])
                layer_norm_stats(ht_, b_ht_, st, b_st, mvt, b_mv, sd, b_sd)
                S.op("act", lambda e, ht_=ht_: e.activation(out=ht_[:, :], in_=ht_[:, :], func=AF.Identity, scale=sd[:, 0:1], bias=sd[:, 1:2]),
                     reads=[b_ht_, b_sd], writes=[b_ht_])
                S.op("dve", lambda e, ht_=ht_: e.tensor_tensor(out=ht_[:, :], in0=ht_[:, :], in1=cG2[:, :], op=ALU.mult), reads=[b_ht_, b_cG2], writes=[b_ht_])
                S.op("dve", lambda e, ht_=ht_: e.tensor_tensor(out=ht_[:, :], in0=ht_[:, :], in1=cB2[:, :], op=ALU.add), reads=[b_ht_, b_cB2], writes=[b_ht_])
                S.dma("sp", out_d[tk, :], ht_[:, :], reads=[b_ht_], final=True)
        S.barrier()
        S.finish()
    return nc


def _bcast(v, n=128):
    v = np.asarray(v, np.float32).reshape(1, -1)
    return np.ascontiguousarray(np.broadcast_to(v, (n, v.shape[1])))


def prep_inputs(inp):
    f32 = np.float32
    x = np.asarray(inp["x"], f32)
    shared = {}
    shared["w_in"] = np.ascontiguousarray(np.asarray(inp["w_in"], f32)[0])
    g = np.asarray(inp["ln_in_g"], f32); b = np.asarray(inp["ln_in_b"], f32)
    shared["lng_col"] = np.ascontiguousarray(g.reshape(16, 128).T); shared["lnb_col"] = np.ascontiguousarray(b.reshape(16, 128).T)
    shared["lng_b"] = _bcast(g); shared["lnb_b"] = _bcast(b)
    cwv = np.asarray(inp["m_conv_w"], f32)[0]
    shared["cw"] = np.ascontiguousarray(cwv.reshape(4, 8, 128).transpose(2, 1, 0))
    shared["cb"] = np.ascontiguousarray(np.asarray(inp["m_conv_b"], f32)[0].reshape(8, 128).T)
    shared["ifb"] = np.ascontiguousarray(np.asarray(inp["m_if_bias"], f32)[0].reshape(2, 4).T)
    shared["mnw_b"] = _bcast(np.asarray(inp["m_norm_w"], f32)[0].reshape(-1))
    shared["w_pa"] = np.ascontiguousarray(np.asarray(inp["w_proj_att"], f32)[0])
    shared["w_pm"] = np.ascontiguousarray(np.asarray(inp["w_proj_mlstm"], f32)[0])
    shared["w_out"] = np.ascontiguousarray(np.asarray(inp["w_out"], f32)[0])
    shared["ln1g_b"] = _bcast(np.asarray(inp["ln1_g"], f32)[0]); shared["ln1b_b"] = _bcast(np.asarray(inp["ln1_b"], f32)[0])
    shared["ln2g_b"] = _bcast(np.asarray(inp["ln2_g"], f32)[0]); shared["ln2b_b"] = _bcast(np.asarray(inp["ln2_b"], f32)[0])
    wrc = np.concatenate([np.asarray(inp["w_router_group"], f32)[0], np.asarray(inp["w_router_expert"], f32)[0]], axis=1)
    shared["wr"] = np.ascontiguousarray(wrc.reshape(16, 128, 36).transpose(1, 0, 2))
    shared["br_b"] = _bcast(np.concatenate([np.asarray(inp["b_router_group"], f32)[0], np.asarray(inp["b_router_expert"], f32)[0]]))
    shared["w_gate"] = np.ascontiguousarray(np.asarray(inp["w_gate"], f32)[0])
    shared["w_up"] = np.ascontiguousarray(np.asarray(inp["w_up"], f32)[0])
    shared["w_down"] = np.ascontiguousarray(np.asarray(inp["w_down"], f32)[0])
    slopes = alibi_slopes(12)
    qi = np.arange(128)[:, None]; ki = np.arange(256)[None, :]
    delta = 128 + qi - ki
    ab = np.zeros((12, 128, 256), f32)
    for h in range(12):
        r = (1, 4, 16)[h // 4]
        ab[h] = np.where((delta >= 0) & (delta <= 128), -slopes[h] * (delta * r).astype(f32), NEG)
    shared["abias"] = ab
    shared["ident_f"] = np.eye(128, dtype=f32)
    shared["ident_h"] = np.eye(128, dtype=f32).astype(ml_dtypes.bfloat16)
    s_ = np.arange(128)[:, None]; l_ = np.arange(128)[None, :]
    shared["tri_neg"] = np.where(s_ <= l_, 0.0, NEG).astype(f32)
    shared["ltri"] = (s_ <= l_).astype(f32)
    shared["ones_f"] = np.ones((128, 128), f32)
    shared["iota1"] = np.ascontiguousarray(np.broadcast_to(np.arange(1, 129, dtype=f32)[None, :], (128, 128)))
    shared["iota1c"] = np.arange(1, 129, dtype=f32).reshape(128, 1)
    sel4 = np.zeros((4, 4, 128), f32)
    for k in range(4):
        sel4[k, k, :] = 1.0
    shared["sel4"] = sel4
    sel32 = np.zeros((32, 32, 128), f32)
    for k in range(32):
        sel32[k, k, :] = 1.0
    shared["sel32"] = sel32
    in_maps = []
    for c in range(8):
        b_, k_ = c // 4, c % 4
        base = 1024 * k_ - 3072
        xw = np.zeros((NW, D), f32)
        lo = max(0, -base)
        xw[lo:] = x[b_, base + lo: base + NW]
        valid = (np.arange(NW) + base >= 0).astype(f32)
        m = dict(shared)
        m["x_win"] = xw
        m["padneg_b"] = _bcast((valid - 1.0) * (-NEG))
        m["valid_b"] = _bcast(valid)
        gm = np.zeros((4, 2, NW), f32)
        gm[:, 0, :] = (1.0 - valid) * NEG
        gm[:, 1, :] = (1.0 - valid) * (-NEG)
        m["gmask"] = gm
        in_maps.append(m)
    return in_maps


_NC = None


def kernel(**inputs):
    global _NC
    if _NC is None:
        _NC = build_program()
    in_maps = prep_inputs(inputs)
    res = run_bass_kernel_spmd(_NC, in_maps, core_ids=list(range(8)))
    out = np.zeros((2, 4096, D), np.float32)
    for c in range(8):
        b_, k_ = c // 4, c % 4
        out[b_, 1024 * k_:1024 * k_ + 1024] = res.results[c]["out"]
    return out
```

```python
import math
from contextlib import ExitStack
import numpy as np
import ml_dtypes
import concourse.bass as bass
import concourse.mybir as mybir
from concourse.bass_utils import run_bass_kernel_spmd

F32 = mybir.dt.float32
BF16 = mybir.dt.bfloat16
AF = mybir.ActivationFunctionType
ALU = mybir.AluOpType
AX = mybir.AxisListType

D = 2048
NW = 4096
NOWN = 1024
NPROJ = 11784
ALPHA = 2 ** 0.25
EPS = 1e-5
NEG = -30000.0
DFF = 1408
NEXP = 32
DEBUG = False
LN_SLACK = 5


class Buf:
    __slots__ = ("w", "r")

    def __init__(self):
        self.w = None
        self.r = []


class Sched:
    ENG = ("pe", "act", "dve", "pool", "sp")

    def __init__(self, nc, es, n_dma_sems=20):
        self.nc = nc
        self.eobj = {"pe": nc.tensor, "act": nc.scalar, "dve": nc.vector, "pool": nc.gpsimd, "sp": nc.sync}
        self.cnt = {e: 0 for e in self.ENG}
        self.sem = {e: es.enter_context(nc.semaphore("c_" + e)) for e in self.ENG}
        self.seen = {e: {} for e in self.ENG}
        self.pending = {e: [] for e in self.ENG}
        self.dsems, self.dtot, self.dnext = {}, {}, {}
        for q in ("sp", "pool", "act"):
            self.dsems[q] = [es.enter_context(nc.semaphore(f"d_{q}_{i}")) for i in range(n_dma_sems)]
            self.dnext[q] = 0
        self.final_events = []

    def _deps(self, reads, writes):
        deps = []
        for b in reads:
            if b.w is not None:
                deps.append(b.w)
        for b in writes:
            if b.w is not None:
                deps.append(b.w)
            deps.extend(b.r)
        return deps

    def _filter(self, eng, deps):
        seen = self.seen[eng]
        own = self.sem[eng]
        best = {}
        for (sem, val) in deps + self.pending[eng]:
            if sem is own and eng in ("pe", "sp"):
                continue
            k = id(sem)
            if seen.get(k, 0) >= val:
                continue
            seen[k] = val
            best[k] = (sem, val)
        self.pending[eng] = []
        return list(best.values())

    def _emit(self, ename, waits, fn, sem, inc):
        eng = self.eobj[ename]
        for (s_, v) in waits:
            eng.wait_ge(s_, v)
        fn(eng).then_inc(sem, inc)

    def op(self, eng, fn, reads=(), writes=()):
        waits = self._filter(eng, self._deps(reads, writes))
        self.cnt[eng] += 1
        ev = (self.sem[eng], self.cnt[eng])
        for b in reads:
            b.r.append(ev)
        for b in writes:
            b.w = ev
            b.r = []
        self._emit(eng, waits, fn, self.sem[eng], 1)
        return ev

    def dma(self, q, out, in_, reads=(), writes=(), final=False):
        deps = self._deps(reads, writes)
        i = self.dnext[q]
        self.dnext[q] = (i + 1) % len(self.dsems[q])
        dsem = self.dsems[q][i]
        prev = self.dtot.get(id(dsem), 0)
        if prev:
            deps.append((dsem, prev))
        waits = self._filter(q, deps)
        tot = prev + 16
        self.dtot[id(dsem)] = tot
        ev = (dsem, tot)
        for b in reads:
            b.r.append(ev)
        for b in writes:
            b.w = ev
            b.r = []
        self._emit(q, waits, (lambda e: e.dma_start(out=out, in_=in_)), dsem, 16)
        if final:
            self.final_events.append(ev)
        return ev

    def barrier(self):
        evs = [(self.sem[e], self.cnt[e]) for e in self.ENG if self.cnt[e] > 0]
        for q in self.dsems:
            for s in self.dsems[q]:
                t = self.dtot.get(id(s), 0)
                if t:
                    evs.append((s, t))
        for e in self.ENG:
            self.pending[e].extend(evs)

    def finish(self):
        for (s_, v) in self._filter("sp", list(self.final_events)):
            self.nc.sync.wait_ge(s_, v)


def ss(start, n, r):
    return slice(start, start + (n - 1) * r + 1, r)


def alibi_slopes(n):
    def geometric(k):
        start = 2.0 ** (-8.0 / k)
        return [start ** (i + 1) for i in range(k)]
    c = 2 ** int(math.floor(math.log2(n)))
    s = geometric(c) if c == n else geometric(c) + geometric(2 * c)[0::2][: n - c]
    return np.array(sorted(s, reverse=True), dtype=np.float32)


C_AQ, C_AK, C_AV = 0, 1536, 3072
C_MQ, C_MK = 4608, 5120
C_MV, C_MO = 5632, 6656
C_IF = 7680
C_GA, C_GM = 7688, 9736


def build_program(upto=9):
    nc = bass.Bass("TRN2", target_bir_lowering=False)

    def din(name, shape, dt=F32):
        return nc.dram_tensor(name, list(shape), dt, kind="ExternalInput").ap()

    def dscr(name, shape, dt=F32):
        return nc.dram_tensor(name, list(shape), dt, kind="Internal").ap()

    x_win = din("x_win", [NW, D])
    w_in = din("w_in", [D, NPROJ])
    padneg_b = din("padneg_b", [128, NW])
    valid_b = din("valid_b", [128, NW])
    gmask = din("gmask", [4, 2, NW])
    lng_col = din("lng_col", [128, 16]); lnb_col = din("lnb_col", [128, 16])
    lng_b = din("lng_b", [128, D]); lnb_b = din("lnb_b", [128, D])
    cw = din("cw", [128, 8, 4]); cb = din("cb", [128, 8])
    ifb = din("ifb", [4, 2])
    mnw_b = din("mnw_b", [128, 1024])
    w_pa = din("w_pa", [512, D]); w_pm = din("w_pm", [1024, D]); w_out = din("w_out", [D, D])
    ln1g_b = din("ln1g_b", [128, D]); ln1b_b = din("ln1b_b", [128, D])
    ln2g_b = din("ln2g_b", [128, D]); ln2b_b = din("ln2b_b", [128, D])
    wr = din("wr", [128, 16, 36]); br_b = din("br_b", [128, 36])
    if upto >= 6:
        w_gate = din("w_gate", [NEXP, D, DFF]); w_up = din("w_up", [NEXP, D, DFF]); w_down = din("w_down", [NEXP, DFF, D])
    abias = din("abias", [12, 128, 256])
    ident_f = din("ident_f", [128, 128]); ident_h = din("ident_h", [128, 128], BF16)
    tri_neg = din("tri_neg", [128, 128])
    ltri = din("ltri", [128, 128])
    ones_f = din("ones_f", [128, 128])
    iota1 = din("iota1", [128, 128])
    iota1c = din("iota1c", [128, 1])
    sel4 = din("sel4", [4, 4, 128])
    sel32 = din("sel32", [32, 32, 128])
    out_d = nc.dram_tensor("out", [NOWN, D], F32, kind="ExternalOutput").ap()

    h_d = dscr("h_d", [NOWN, D])
    h1_d = dscr("h1_d", [NOWN, D])
    kT_d = dscr("kT_d", [12, 128, NW], BF16)
    qT_d = dscr("qT_d", [12, 128, NOWN], BF16)
    v_d = dscr("v_d", [NW, 1536], BF16)
    hT_d = dscr("hT_d", [16, 128, NOWN], BF16)
    b_hT_d = Buf()
    hmT_d = dscr("hmT_d", [8, 128, NOWN], BF16); attT_d = dscr("attT_d", [4, 128, NOWN], BF16)
    mT_d = dscr("mT_d", [16, 128, NOWN], BF16); h1bf_d = dscr("h1bf_d", [NOWN, D], BF16)
    b_hmT_d, b_attT_d, b_mT_d, b_h1bf_d = (Buf() for _ in range(4))
    o_d = dscr("o_d", [3, NOWN, 512])
    lse_d = dscr("lse_d", [3, NOWN, 4])
    b_h_d, b_h1_d, b_kT_d, b_qT_d, b_v_d, b_o_d, b_lse_d = (Buf() for _ in range(7))
    dbg = {}
    if DEBUG:
        for nm, shp in (("dbg_hm", [NOWN, 1024]), ("dbg_att", [NOWN, 512]), ("dbg_h1", [NOWN, D]), ("dbg_y2", [NOWN, D])):
            dbg[nm] = nc.dram_tensor(nm, shp, F32, kind="ExternalOutput").ap()

    with ExitStack() as es:
        S = Sched(nc, es)
        cnt = [0]

        def sb(es_, shape, dt=F32):
            cnt[0] += 1
            t = es_.enter_context(nc.sbuf_tensor(f"t{cnt[0]}", list(shape), dt))
            return t, Buf()

        psT = [es.enter_context(nc.psum_tensor(f"ps{i}", [128, 512], F32)) for i in range(8)]
        psB = [Buf() for _ in range(8)]
        psi = [0]

        ps_split = [False]
        psr_i = [0]

        def ps():
            if ps_split[0]:
                i = 4 + (psi[0] % 4)
                psi[0] = (psi[0] + 1) % 4
                return psT[i], psB[i]
            i = psi[0]
            psi[0] = (i + 1) % 8
            return psT[i], psB[i]

        def psr():
            i = psr_i[0]
            psr_i[0] = (i + 1) % 4
            return psT[i], psB[i]

        rr = [0]

        def evac_eng():
            rr[0] += 1
            return "act" if rr[0] % 2 else "dve"

        def copy(eng, out, in_, reads, writes):
            if eng == "act":
                S.op("act", lambda e: e.copy(out=out, in_=in_), reads, writes)
            else:
                S.op(eng, lambda e: e.tensor_copy(out=out, in_=in_), reads, writes)

        def load_const(es_, src, shape, dt=F32, q="sp"):
            t, b = sb(es_, shape, dt)
            idx = tuple(slice(None) for _ in shape)
            S.dma(q, t[idx], src, writes=[b])
            return t, b

        idf, b_idf = load_const(es, ident_f, [128, 128])
        idh, b_idh = load_const(es, ident_h, [128, 128], BF16)
        bC = Buf()
        def lc(src, shape, dt=F32):
            t, _ = sb(es, shape, dt)
            idx = tuple(slice(None) for _ in shape)
            S.dma("sp", t[idx], src, writes=[bC])
            return t
        c_lng = lc(lng_col, [128, 16]); c_lnb = lc(lnb_col, [128, 16])

        def layer_norm_stats(x_ap, b_x, st, b_st, mvt, b_mv, sd, b_sd, nchunk=4, csz=512):
            for i in range(nchunk):
                S.op("dve", lambda e, i=i: e.bn_stats(out=st[:, 6 * i:6 * i + 6], in_=x_ap[:, csz * i:csz * i + csz]),
                     reads=[b_x], writes=[b_st])
            S.op("dve", lambda e: e.bn_aggr(out=mvt[:, 0:2], in_=st[:, 0:6 * nchunk]), reads=[b_st], writes=[b_mv])
            S.op("dve", lambda e: e.tensor_scalar(out=sd[:, 0:1], in0=mvt[:, 1:2], scalar1=EPS, scalar2=None, op0=ALU.add),
                 reads=[b_mv], writes=[b_sd])
            S.op("act", lambda e: e.activation(out=sd[:, 0:1], in_=sd[:, 0:1], func=AF.Sqrt), reads=[b_sd], writes=[b_sd])
            S.op("dve", lambda e: e.reciprocal(out=sd[:, 0:1], in_=sd[:, 0:1]), reads=[b_sd], writes=[b_sd])
            S.op("dve", lambda e: e.tensor_scalar(out=sd[:, 1:2], in0=mvt[:, 0:1], scalar1=sd[:, 0:1], scalar2=-1.0,
                                                  op0=ALU.mult, op1=ALU.mult), reads=[b_mv, b_sd], writes=[b_sd])

        Wt_all, b_Wt = sb(es, [128, 8, 32]); mk_all, b_mk = sb(es, [128, 8, 32]); cumm, b_cumm = sb(es, [128, 8, 32])

        with ExitStack() as e1:
            bC1 = Buf()
            def lc1(src, shape, dt=F32):
                t, _ = sb(e1, shape, dt)
                idx = tuple(slice(None) for _ in shape)
                S.dma("sp", t[idx], src, writes=[bC1])
                return t
            c_cw = lc1(cw, [128, 8, 4]); c_cb = lc1(cb, [128, 8]); c_ifb = lc1(ifb, [4, 2])
            c_mnw = lc1(mnw_b, [128, 1024]); c_tri = lc1(tri_neg, [128, 128]); c_sel4 = lc1(sel4, [4, 4, 128])
            xs = [sb(e1, [128, D]) for _ in range(2)]
            xn_l = [sb(e1, [128, D])] * 2
            ln_l = [(sb(e1, [128, 24]), sb(e1, [128, 2]), sb(e1, [128, 2])) for _ in range(2)]
            hT_l = [sb(e1, [128, 16, 512], BF16) for _ in range(2)]
            hT_st, b_hT_st = hT_l[0]
            wbuf = [sb(e1, [128, 16, 512], BF16) for _ in range(2)]
            wif, b_wif = sb(e1, [128, 16, 8], BF16)
            S.dma("pool", wif[:, :, :], w_in[:, C_IF:C_IF + 8].rearrange("(k p) n -> p k n", p=128), writes=[b_wif])
            wi = [0]
            stage_o, b_stage_o = sb(e1, [128, 4, 512], BF16)
            stage_t, b_stage_t = sb(e1, [128, 4, 512], BF16)
            halo, b_halo = sb(e1, [128, 8, 3])
            S.op("pool", lambda e: e.memset(halo[:, :, :], 0.0), writes=[b_halo])
            rw, b_rw = sb(e1, [128, 515])
            cacc, b_cacc = sb(e1, [128, 512])
            kT_st, b_kT_st = sb(e1, [128, 4, 512], BF16)
            qT_st, b_qT_st = sb(e1, [128, 4, 512], BF16)
            vext, b_vext = sb(e1, [128, 4, 4, 258], BF16)
            S.op("pool", lambda e: e.memset(vext[:, :, :, 256:257], 1.0), writes=[b_vext])
            S.op("pool", lambda e: e.memset(vext[:, :, :, 257:258], 0.0), writes=[b_vext])
            smo, b_smo = sb(e1, [128, 4, 1024], BF16)
            gI, b_gI = sb(e1, [4, 512]); gF, b_gF = sb(e1, [4, 512])
            gB, b_gB = sb(e1, [4, 512]); gMg, b_gMg = sb(e1, [4, 512])
            gm_sb, b_gm = sb(e1, [4, 2, 512])
            QS, b_QS = sb(e1, [128, 512])
            S.op("pool", lambda e: e.memset(QS[:, :], 0.0), writes=[b_QS])
            vmask, b_vmask = sb(e1, [128, 512])
            ones512, b_ones512 = sb(e1, [4, 512])
            S.op("pool", lambda e: e.memset(ones512[:, :], 1.0), writes=[b_ones512])
            carryB, b_cB = sb(e1, [4, 1]); carryM, b_cM = sb(e1, [4, 1])
            S.op("pool", lambda e: e.memset(carryB[:, :], 0.0), writes=[b_cB])
            S.op("pool", lambda e: e.memset(carryM[:, :], 0.0), writes=[b_cM])
            QT, b_QT = sb(e1, [128, 4, 128])
            NMGB, b_NMGB = sb(e1, [128, 4, 513])
            S.op("pool", lambda e: e.memset(NMGB[:, :, :], 0.0), writes=[b_NMGB])
            Cst, b_Cst = sb(e1, [128, 4, 258]); Cbf, b_Cbf = sb(e1, [128, 4, 258], BF16)
            S.op("pool", lambda e: e.memset(Cst[:, :, :], 0.0), writes=[b_Cst])
            S.op("pool", lambda e: e.memset(Cbf[:, :, :], 0.0), writes=[b_Cbf])
            sc_l = [sb(e1, [128, 16]) for _ in range(4)]
            kw_l = [sb(e1, [128, 128], BF16) for _ in range(4)]
            arg_l = [sb(e1, [128, 128]) for _ in range(4)]; Pt_l = [sb(e1, [128, 128], BF16) for _ in range(4)]
            intra_l = [sb(e1, [128, 258]) for _ in range(4)]; Rn_l = [sb(e1, [128, 258]) for _ in range(4)]
            hraw_l = [sb(e1, [128, 256]) for _ in range(4)]
            hm_t, b_hm_t = sb(e1, [128, 1024]); hm_h, b_hm_h = sb(e1, [128, 1024], BF16)
            hmT_c, b_hmT_c = sb(e1, [128, 8, 128], BF16)
            st2_l = [sb(e1, [128, 6]) for _ in range(4)]; mv2_l = [sb(e1, [128, 2]) for _ in range(4)]; sd2_l = [sb(e1, [128, 2]) for _ in range(4)]

            def wload(c0, ncols):
                t, b = wbuf[wi[0] % 2]
                wi[0] += 1
                S.dma("pool", t[:, :, 0:ncols], w_in[:, c0:c0 + ncols].rearrange("(k p) n -> p k n", p=128), writes=[b])
                return t, b

            hook_box = [None]

            def proj_fm(wt, bw, ncol_chunks, consume):
                for cc in range(ncol_chunks):
                    p_, bp = ps()
                    for kc in range(16):
                        S.op("pe", lambda e, kc=kc, cc=cc, p_=p_: e.matmul(p_[:, :], lhsT=wt[:, kc, 128 * cc:128 * cc + 128],
                                                                         rhs=hT_st[:, kc, :], start=(kc == 0), stop=(kc == 15)),
                             reads=[bw, b_hT_st], writes=[bp])
                    consume(cc, p_, bp)
                    if hook_box[0] is not None:
                        hook_box[0]()

            def proj_tm(wt, bw, ncols, consume):
                for tt in range(4):
                    p_, bp = ps()
                    for kc in range(16):
                        S.op("pe", lambda e, kc=kc, tt=tt, p_=p_: e.matmul(p_[:, 0:ncols], lhsT=hT_st[:, kc, 128 * tt:128 * tt + 128],
                                                                         rhs=wt[:, kc, 0:ncols], start=(kc == 0), stop=(kc == 15)),
                             reads=[bw, b_hT_st], writes=[bp])
                    consume(tt, p_, bp)
                    if hook_box[0] is not None:
                        hook_box[0]()

            for sti in range(8):
                own = sti >= 6
                t0 = 512 * sti
                def x_load(n):
                    if n < 32:
                        xt_, b_xt_ = xs[n % 2]
                        S.dma("sp", xt_[:, :], x_win[128 * n:128 * n + 128, :], writes=[b_xt_])

                def ln_gen(sn):
                    hT_n, b_hT_n = hT_l[sn % 2]
                    own_n = sn >= 6
                    for tt in range(4):
                        w0 = 512 * sn + 128 * tt
                        xt, b_xt = xs[tt % 2]
                        xn, b_xn = xn_l[tt % 2]
                        (st, b_st), (mvt, b_mv), (sd, b_sd) = ln_l[tt % 2]
                        if sn == 0 and tt == 0:
                            x_load(0); x_load(1)
                        layer_norm_stats(xt, b_xt, st, b_st, mvt, b_mv, sd, b_sd)
                        S.op("act", lambda e: e.activation(out=xn[:, :], in_=xt[:, :], func=AF.Identity, scale=sd[:, 0:1], bias=sd[:, 1:2]),
                             reads=[b_xt, b_sd], writes=[b_xn])
                        x_load(4 * sn + tt + 2)
                        if own_n:
                            o0 = w0 - 3072
                            S.dma("sp", h_d[o0:o0 + 128, :], xn[:, :], reads=[b_xn], writes=[b_h_d])
                        for _ in range(LN_SLACK):
                            yield
                        for g in range(4):
                            p_, bp = ps()
                            for j in range(4):
                                kc = 4 * g + j
                                S.op("pe", lambda e, kc=kc, j=j, p_=p_: e.transpose(out=p_[:, 128 * j:128 * j + 128],
                                                                                  in_=xn[:, 128 * kc:128 * kc + 128], identity=idf[:, :]),
                                     reads=[b_xn, b_idf], writes=[bp])
                            for j in range(4):
                                kc = 4 * g + j
                                eng = evac_eng()
                                if eng == "act":
                                    S.op("act", lambda e, kc=kc, j=j, p_=p_: e.activation(
                                        out=hT_n[:, kc, 128 * tt:128 * tt + 128], in_=p_[:, 128 * j:128 * j + 128], func=AF.Identity,
                                        scale=c_lng[:, kc:kc + 1], bias=c_lnb[:, kc:kc + 1]), reads=[bp, bC], writes=[b_hT_n])
                                else:
                                    S.op("dve", lambda e, kc=kc, j=j, p_=p_: e.tensor_scalar(
                                        out=hT_n[:, kc, 128 * tt:128 * tt + 128], in0=p_[:, 128 * j:128 * j + 128],
                                        scalar1=c_lng[:, kc:kc + 1], scalar2=c_lnb[:, kc:kc + 1], op0=ALU.mult, op1=ALU.add),
                                         reads=[bp, bC], writes=[b_hT_n])
                            if g % 2 == 1:
                                yield

                if sti == 0:
                    for _ in ln_gen(0):
                        pass
                hT_st, b_hT_st = hT_l[sti % 2]
                if own:
                    o0 = t0 - 3072
                    S.dma("sp", hT_d[:, :, o0:o0 + 512].rearrange("k p t -> p k t"), hT_st[:, :, :], reads=[b_hT_st], writes=[b_hT_d])
                ln_next = ln_gen(sti + 1) if sti < 7 else iter(())
                rec_box = [None]

                def run_hooks():
                    next(ln_next, None)
                    if rec_box[0] is not None:
                        next(rec_box[0], None)

                S.dma("sp", gm_sb[:, :, :], gmask[:, :, t0:t0 + 512], writes=[b_gm])
                S.dma("sp", vmask[:, :], valid_b[:, t0:t0 + 512], writes=[b_vmask])
                for gi_, (dst, b_dst) in enumerate(((gI, b_gI), (gF, b_gF))):
                    p_, bp = ps()
                    for kc in range(16):
                        S.op("pe", lambda e, kc=kc, p_=p_, gi_=gi_: e.matmul(p_[0:4, :], lhsT=wif[:, kc, 4 * gi_:4 * gi_ + 4],
                                                                           rhs=hT_st[:, kc, :], start=(kc == 0), stop=(kc == 15)),
                             reads=[b_wif, b_hT_st], writes=[bp])
                    S.op("dve", lambda e, p_=p_, gi_=gi_, dst=dst: e.scalar_tensor_tensor(
                        out=dst[:, :], in0=p_[0:4, :], scalar=c_ifb[:, gi_:gi_ + 1], in1=gm_sb[:, gi_, :], op0=ALU.add, op1=ALU.add),
                         reads=[bp, bC1, b_gm], writes=[b_dst])
                S.op("act", lambda e: e.activation(out=gF[:, :], in_=gF[:, :], func=AF.Exp, scale=-1.0), reads=[b_gF], writes=[b_gF])
                S.op("dve", lambda e: e.tensor_scalar(out=gF[:, :], in0=gF[:, :], scalar1=1.0, scalar2=None, op0=ALU.add),
                     reads=[b_gF], writes=[b_gF])
                S.op("act", lambda e: e.activation(out=gF[:, :], in_=gF[:, :], func=AF.Ln), reads=[b_gF], writes=[b_gF])
                S.op("dve", lambda e: e.tensor_tensor_scan(out=gB[:, :], data0=ones512[:, :], data1=gF[:, :], initial=carryB[:, 0:1],
                                                           op0=ALU.mult, op1=ALU.subtract),
                     reads=[b_gF, b_cB, b_ones512], writes=[b_gB])
                S.op("dve", lambda e: e.tensor_copy(out=carryB[:, :], in_=gB[:, 511:512]), reads=[b_gB], writes=[b_cB])
                S.op("dve", lambda e: e.tensor_tensor(out=QS[0:4, :], in0=gI[:, :], in1=gB[:, :], op=ALU.subtract),
                     reads=[b_gI, b_gB], writes=[b_QS])
                S.op("dve", lambda e: e.tensor_tensor_scan(out=gMg[:, :], data0=QS[0:4, :], data1=QS[0:4, :], initial=carryM[:, 0:1],
                                                           op0=ALU.max, op1=ALU.max),
                     reads=[b_QS, b_cM], writes=[b_gMg])
                S.op("dve", lambda e: e.tensor_copy(out=carryM[:, :], in_=gMg[:, 511:512]), reads=[b_gMg], writes=[b_cM])
                S.op("dve", lambda e: e.tensor_scalar(out=QS[32:36, :], in0=gMg[:, :], scalar1=-1.0, scalar2=None, op0=ALU.mult),
                     reads=[b_gMg], writes=[b_QS])
                S.op("dve", lambda e: e.tensor_tensor(out=gI[:, :], in0=gB[:, :], in1=gMg[:, :], op=ALU.add),
                     reads=[b_gB, b_gMg], writes=[b_gI])
                S.op("act", lambda e: e.activation(out=QS[64:68, :], in_=gI[:, :], func=AF.Exp, scale=-1.0), reads=[b_gI], writes=[b_QS])
                S.op("dve", lambda e: e.tensor_scalar(out=gF[:, :], in0=gMg[:, :], scalar1=-1.0, scalar2=None, op0=ALU.mult),
                     reads=[b_gMg], writes=[b_gF])
                hook_box[0] = run_hooks
                for which in ((1, 0) if sti >= 5 else (1,)):
                    wt, bw = wload(C_MK if which else C_MQ, 512)
                    dstT, b_dstT = (kT_st, b_kT_st) if which else (qT_st, b_qT_st)
                    def cons_c(cc, p_, bp, which=which, dstT=dstT, b_dstT=b_dstT):
                        ch = 4 * which + cc
                        S.op("act", lambda e: e.copy(out=rw[:, 0:3], in_=halo[:, ch, :]), reads=[b_halo], writes=[b_rw])
                        S.op("dve", lambda e: e.tensor_tensor(out=rw[:, 3:515], in0=p_[:, :], in1=vmask[:, :], op=ALU.mult),
                             reads=[bp, b_vmask], writes=[b_rw])
                        S.op("dve", lambda e: e.tensor_scalar(out=cacc[:, :], in0=rw[:, 3:515], scalar1=c_cw[:, ch, 3:4],
                                                              scalar2=c_cb[:, ch:ch + 1], op0=ALU.mult, op1=ALU.add),
                             reads=[b_rw, bC1], writes=[b_cacc])
                        for j in range(3):
                            S.op("dve", lambda e, j=j: e.scalar_tensor_tensor(out=cacc[:, :], in0=rw[:, j:j + 512],
                                                                              scalar=c_cw[:, ch, j:j + 1], in1=cacc[:, :],
                                                                              op0=ALU.mult, op1=ALU.add),
                                 reads=[b_rw, bC1, b_cacc], writes=[b_cacc])
                        S.op("act", lambda e: e.activation(out=dstT[:, cc, :], in_=cacc[:, :], func=AF.Silu),
                             reads=[b_cacc], writes=[b_dstT])
                        S.op("act", lambda e: e.copy(out=halo[:, ch, :], in_=rw[:, 512:515]), reads=[b_rw], writes=[b_halo])
                    proj_fm(wt, bw, 4, cons_c)
                for half in range(2):
                    wt, bw = wload(C_MV + 512 * half, 512)
                    def cons_mv(tt, p_, bp, half=half):
                        copy(evac_eng(), vext[:, tt, 2 * half:2 * half + 2, 0:256],
                             p_[:, :].rearrange("p (h c) -> p h c", c=256), [bp], [b_vext])
                    proj_tm(wt, bw, 512, cons_mv)
                if own:
                    for half in range(2):
                        wt, bw = wload(C_MO + 512 * half, 512)
                        def cons_mo(tt, p_, bp, half=half):
                            S.op("act", lambda e: e.activation(out=smo[:, tt, 512 * half:512 * half + 512], in_=p_[:, :], func=AF.Sigmoid),
                                 reads=[bp], writes=[b_smo])
                        proj_tm(wt, bw, 512, cons_mo)
                S.op("dve", lambda e: e.tensor_copy(out=NMGB[:, :, 0:1], in_=NMGB[:, :, 512:513]), reads=[b_NMGB], writes=[b_NMGB])
                for hd in range(4):
                    p_, bp = ps()
                    S.op("pe", lambda e, hd=hd, p_=p_: e.matmul(p_[:, :], lhsT=c_sel4[:, hd, :], rhs=gF[:, :], start=True, stop=True),
                         reads=[b_gF, bC1], writes=[bp])
                    copy(evac_eng(), NMGB[:, hd, 1:513], p_[:, :], [bp], [b_NMGB])
                for c in range(4):
                    p_, bp = ps()
                    S.op("pe", lambda e, c=c, p_=p_: e.transpose(out=p_[:, 0:128], in_=QS[:, 128 * c:128 * c + 128], identity=idf[:, :]),
                         reads=[b_QS, b_idf], writes=[bp])
                    copy(evac_eng(), QT[:, c, :], p_[:, 0:128], [bp], [b_QT])

                def rec_gen():
                    HD = range(4)
                    for c in range(4):
                        cs_ = slice(128 * c, 128 * c + 128)
                        def nmgL(hd): return NMGB[:, hd, 128 * c + 128:128 * c + 129]
                        def nmgS(hd): return NMGB[:, hd, 128 * c:128 * c + 1]
                        def Acol(hd): return QT[:, c, hd:hd + 1]
                        if own:
                            pS_l = {}; pI_l = {}; pX_l = {}
                            yield
                            for hd in HD:
                                pS, bpS = psr(); pS_l[hd] = (pS, bpS)
                                S.op("pe", lambda e, pS=pS, hd=hd: e.matmul(pS[:, 0:128], lhsT=kT_st[:, hd, cs_], rhs=qT_st[:, hd, cs_], start=True, stop=True),
                                     reads=[b_kT_st, b_qT_st], writes=[bpS])
                            yield
                            for hd in HD:
                                arg, b_arg = arg_l[hd]
                                S.op("dve", lambda e, hd=hd, arg=arg: e.scalar_tensor_tensor(
                                    out=arg[:, :], in0=NMGB[:, hd, 128 * c + 1:128 * c + 129], scalar=Acol(hd), in1=c_tri[:, :],
                                    op0=ALU.add, op1=ALU.add), reads=[b_NMGB, b_QT, bC1], writes=[b_arg])
                            yield
                            for hd in HD:
                                arg, b_arg = arg_l[hd]
                                S.op("act", lambda e, arg=arg: e.activation(out=arg[:, :], in_=arg[:, :], func=AF.Exp), reads=[b_arg], writes=[b_arg])
                            yield
                            for hd in HD:
                                arg, b_arg = arg_l[hd]; Pt, b_Pt = Pt_l[hd]; pS, bpS = pS_l[hd]
                                S.op("dve", lambda e, pS=pS, Pt=Pt, arg=arg: e.scalar_tensor_tensor(out=Pt[:, :], in0=pS[:, 0:128], scalar=128 ** -0.5,
                                                                                                  in1=arg[:, :], op0=ALU.mult, op1=ALU.mult),
                                     reads=[bpS, b_arg], writes=[b_Pt])
                            yield
                            for hd in HD:
                                Pt, b_Pt = Pt_l[hd]
                                pI, bpI = psr(); pI_l[hd] = (pI, bpI)
                                S.op("pe", lambda e, pI=pI, hd=hd, Pt=Pt: e.matmul(pI[:, 0:258], lhsT=Pt[:, :], rhs=vext[:, c, hd, :], start=True, stop=True),
                                     reads=[b_Pt, b_vext], writes=[bpI])
                            yield
                            for hd in HD:
                                intra, b_intra = intra_l[hd]; pI, bpI = pI_l[hd]
                                S.op("act", lambda e, pI=pI, intra=intra: e.copy(out=intra[:, :], in_=pI[:, 0:258]), reads=[bpI], writes=[b_intra])
                            yield
                            for hd in HD:
                                pX, bpX = psr(); pX_l[hd] = (pX, bpX)
                                S.op("pe", lambda e, pX=pX, hd=hd: e.matmul(pX[:, 0:258], lhsT=qT_st[:, hd, cs_], rhs=Cbf[:, hd, :], start=True, stop=True),
                                     reads=[b_qT_st, b_Cbf], writes=[bpX])
                            yield
                            for hd in HD:
                                sc, b_sc = sc_l[hd]
                                S.op("dve", lambda e, hd=hd, sc=sc: e.tensor_scalar(out=sc[:, 0:1], in0=nmgS(hd), scalar1=-1.0, scalar2=None, op0=ALU.mult),
                                     reads=[b_NMGB], writes=[b_sc])
                            yield
                            for hd in HD:
                                sc, b_sc = sc_l[hd]; intra, b_intra = intra_l[hd]; pI, bpI = pI_l[hd]
                                S.op("act", lambda e, hd=hd, sc=sc: e.activation(out=sc[:, 1:2], in_=QT[:, c, 32 + hd:33 + hd], func=AF.Exp, bias=sc[:, 0:1]),
                                     reads=[b_QT, b_sc], writes=[b_sc])
                            yield
                            for hd in HD:
                                sc, b_sc = sc_l[hd]
                                S.op("dve", lambda e, sc=sc: e.tensor_scalar(out=sc[:, 1:2], in0=sc[:, 1:2], scalar1=128 ** -0.5, scalar2=None, op0=ALU.mult),
                                     reads=[b_sc], writes=[b_sc])
                            yield
                            for hd in HD:
                                sc, b_sc = sc_l[hd]; intra, b_intra = intra_l[hd]; Rn, b_Rn = Rn_l[hd]; pX, bpX = pX_l[hd]
                                S.op("dve", lambda e, pX=pX, sc=sc, intra=intra, Rn=Rn: e.scalar_tensor_tensor(out=Rn[:, :], in0=pX[:, 0:258], scalar=sc[:, 1:2],
                                                                                                             in1=intra[:, :], op0=ALU.mult, op1=ALU.add),
                                     reads=[bpX, b_sc, b_intra], writes=[b_Rn])
                            yield
                            for hd in HD:
                                sc, b_sc = sc_l[hd]; Rn, b_Rn = Rn_l[hd]
                                S.op("dve", lambda e, sc=sc, Rn=Rn: e.tensor_scalar(out=sc[:, 3:4], in0=Rn[:, 256:257], scalar1=-1.0, scalar2=None, op0=ALU.mult),
                                     reads=[b_Rn], writes=[b_sc])
                            yield
                            for hd in HD:
                                sc, b_sc = sc_l[hd]; Rn, b_Rn = Rn_l[hd]
                                S.op("dve", lambda e, sc=sc, Rn=Rn: e.tensor_tensor(out=sc[:, 2:3], in0=sc[:, 3:4], in1=Rn[:, 256:257], op=ALU.max),
                                     reads=[b_Rn, b_sc], writes=[b_sc])
                            yield
                            for hd in HD:
                                sc, b_sc = sc_l[hd]
                                S.op("dve", lambda e, hd=hd, sc=sc: e.tensor_tensor(out=sc[:, 2:3], in0=sc[:, 2:3], in1=QT[:, c, 64 + hd:65 + hd], op=ALU.max),
                                     reads=[b_sc, b_QT], writes=[b_sc])
                            yield
                            for hd in HD:
                                sc, b_sc = sc_l[hd]
                                S.op("dve", lambda e, sc=sc: e.reciprocal(out=sc[:, 2:3], in_=sc[:, 2:3]), reads=[b_sc], writes=[b_sc])
                            yield
                            for hd in HD:
                                sc, b_sc = sc_l[hd]; Rn, b_Rn = Rn_l[hd]; hraw, b_hraw = hraw_l[hd]
                                S.op("dve", lambda e, sc=sc, Rn=Rn, hraw=hraw: e.tensor_scalar(out=hraw[:, :], in0=Rn[:, 0:256], scalar1=sc[:, 2:3], scalar2=None, op0=ALU.mult),
                                     reads=[b_Rn, b_sc], writes=[b_hraw])
                            yield
                            for hd in HD:
                                hraw, b_hraw = hraw_l[hd]; st2, b_st2 = st2_l[hd]
                                S.op("dve", lambda e, hraw=hraw, st2=st2: e.bn_stats(out=st2[:, 0:6], in_=hraw[:, :]), reads=[b_hraw], writes=[b_st2])
                            yield
                            for hd in HD:
                                st2, b_st2 = st2_l[hd]; mv2, b_mv2 = mv2_l[hd]
                                S.op("dve", lambda e, st2=st2, mv2=mv2: e.bn_aggr(out=mv2[:, 0:2], in_=st2[:, 0:6]), reads=[b_st2], writes=[b_mv2])
                            yield
                            for hd in HD:
                                mv2, b_mv2 = mv2_l[hd]; sd2, b_sd2 = sd2_l[hd]
                                S.op("dve", lambda e, mv2=mv2, sd2=sd2: e.tensor_scalar(out=sd2[:, 0:1], in0=mv2[:, 1:2], scalar1=EPS, scalar2=None, op0=ALU.add),
                                     reads=[b_mv2], writes=[b_sd2])
                            yield
                            for hd in HD:
                                sd2, b_sd2 = sd2_l[hd]
                                S.op("act", lambda e, sd2=sd2: e.activation(out=sd2[:, 0:1], in_=sd2[:, 0:1], func=AF.Sqrt), reads=[b_sd2], writes=[b_sd2])
                            yield
                            for hd in HD:
                                sd2, b_sd2 = sd2_l[hd]
                                S.op("dve", lambda e, sd2=sd2: e.reciprocal(out=sd2[:, 0:1], in_=sd2[:, 0:1]), reads=[b_sd2], writes=[b_sd2])
                            yield
                            for hd in HD:
                                mv2, b_mv2 = mv2_l[hd]; sd2, b_sd2 = sd2_l[hd]
                                S.op("dve", lambda e, mv2=mv2, sd2=sd2: e.tensor_scalar(out=sd2[:, 1:2], in0=mv2[:, 0:1], scalar1=sd2[:, 0:1], scalar2=-1.0,
                                                                                      op0=ALU.mult, op1=ALU.mult), reads=[b_mv2, b_sd2], writes=[b_sd2])
                            yield
                            for hd in HD:
                                hraw, b_hraw = hraw_l[hd]; sd2, b_sd2 = sd2_l[hd]
                                S.op("act", lambda e, hraw=hraw, sd2=sd2: e.activation(out=hraw[:, :], in_=hraw[:, :], func=AF.Identity, scale=sd2[:, 0:1], bias=sd2[:, 1:2]),
                                     reads=[b_hraw, b_sd2], writes=[b_hraw])
                            yield
                            for hd in HD:
                                hraw, b_hraw = hraw_l[hd]
                                S.op("dve", lambda e, hd=hd, hraw=hraw: e.tensor_tensor(out=hm_t[:, 256 * hd:256 * hd + 256], in0=hraw[:, :],
                                                                                      in1=c_mnw[:, 256 * hd:256 * hd + 256], op=ALU.mult),
                                     reads=[b_hraw, bC1], writes=[b_hm_t])
                        pK_l = {}; pU_l = {}
                        yield
                        for hd in HD:
                            sc, b_sc = sc_l[hd]
                            S.op("act", lambda e, hd=hd, sc=sc: e.activation(out=sc[:, 4:5], in_=Acol(hd), func=AF.Exp, bias=nmgL(hd)),
                                 reads=[b_QT, b_NMGB], writes=[b_sc])
                            S.op("act", lambda e, hd=hd, sc=sc: e.activation(out=sc[:, 5:6], in_=nmgS(hd), func=AF.Exp, scale=-1.0, bias=nmgL(hd)),
                                 reads=[b_NMGB], writes=[b_sc])
                        yield
                        for hd in HD:
                            pK, bpK = psr(); pKh = pK.bitcast(BF16); pK_l[hd] = (pKh, bpK)
                            S.op("pe", lambda e, pKh=pKh, hd=hd: e.transpose(out=pKh[:, 0:128], in_=kT_st[:, hd, cs_], identity=idh[:, :]),
                                 reads=[b_kT_st, b_idh], writes=[bpK])
                        yield
                        for hd in HD:
                            sc, b_sc = sc_l[hd]; kw, b_kw = kw_l[hd]; pKh, bpK = pK_l[hd]
                            S.op("dve", lambda e, pKh=pKh, sc=sc, kw=kw: e.tensor_scalar(out=kw[:, :], in0=pKh[:, 0:128], scalar1=sc[:, 4:5], scalar2=None, op0=ALU.mult),
                                 reads=[bpK, b_sc], writes=[b_kw])
                        yield
                        for hd in HD:
                            kw, b_kw = kw_l[hd]
                            pU, bpU = psr(); pU_l[hd] = (pU, bpU)
                            S.op("pe", lambda e, pU=pU, hd=hd, kw=kw: e.matmul(pU[:, 0:258], lhsT=kw[:, :], rhs=vext[:, c, hd, :], start=True, stop=True),
                                 reads=[b_kw, b_vext], writes=[bpU])
                        yield
                        for hd in HD:
                            sc, b_sc = sc_l[hd]; pU, bpU = pU_l[hd]
                            S.op("dve", lambda e, pU=pU, hd=hd, sc=sc: e.scalar_tensor_tensor(out=Cst[:, hd, :], in0=Cst[:, hd, :], scalar=sc[:, 5:6],
                                                                                            in1=pU[:, 0:258], op0=ALU.mult, op1=ALU.add),
                                 reads=[b_Cst, b_sc, bpU], writes=[b_Cst])
                        if sti >= 5:
                            S.op("act", lambda e: e.copy(out=Cbf[:, :, :], in_=Cst[:, :, :]), reads=[b_Cst], writes=[b_Cbf])
                        if own:
                            S.op("dve", lambda e, c=c: e.tensor_tensor(out=hm_t[:, :], in0=hm_t[:, :], in1=smo[:, c, :], op=ALU.mult),
                                 reads=[b_hm_t, b_smo], writes=[b_hm_t])
                            o0 = t0 - 3072 + 128 * c
                            if DEBUG:
                                S.dma("sp", dbg["dbg_hm"][o0:o0 + 128, :], hm_t[:, :], reads=[b_hm_t], final=True)
                            S.op("act", lambda e: e.copy(out=hm_h[:, :], in_=hm_t[:, :]), reads=[b_hm_t], writes=[b_hm_h])
                            for half in range(2):
                                p_, bp = psr()
                                ph = p_.bitcast(BF16)
                                for j in range(4):
                                    kc = 4 * half + j
                                    S.op("pe", lambda e, ph=ph, kc=kc, j=j: e.transpose(out=ph[:, 128 * j:128 * j + 128],
                                                                                      in_=hm_h[:, 128 * kc:128 * kc + 128], identity=idh[:, :]),
                                         reads=[b_hm_h, b_idh], writes=[bp])
                                copy(evac_eng(), hmT_c[:, 4 * half:4 * half + 4, :],
                                     ph[:, 0:512].rearrange("p (k t) -> p k t", t=128), [bp], [b_hmT_c])
                            S.dma("sp", hmT_d[:, :, o0:o0 + 128].rearrange("k p t -> p k t"), hmT_c[:, :, :], reads=[b_hmT_c], writes=[b_hmT_d])
                    yield
                rec = rec_gen()
                rec_box[0] = rec
                ps_split[0] = True; psi[0] = 0; psr_i[0] = 0
                att_groups = [g for g in range(3) if t0 >= (1024 if g == 2 else 2560)]
                for g in att_groups:
                    wt, bw = wload(C_AK + 512 * g, 512)
                    def cons_k(cc, p_, bp, g=g):
                        copy(evac_eng(), stage_o[:, cc, :], p_[:, :], [bp], [b_stage_o])
                    proj_fm(wt, bw, 4, cons_k)
                    S.dma("sp", kT_d[4 * g:4 * g + 4, :, t0:t0 + 512].rearrange("h p t -> p h t"), stage_o[:, :, :],
                          reads=[b_stage_o], writes=[b_kT_d])
                    wt, bw = wload(C_AV + 512 * g, 512)
                    def cons_v(tt, p_, bp, g=g):
                        copy(evac_eng(), stage_t[:, tt, :], p_[:, :], [bp], [b_stage_t])
                    proj_tm(wt, bw, 512, cons_v)
                    S.dma("sp", v_d[t0:t0 + 512, 512 * g:512 * g + 512].rearrange("(t p) c -> p t c", p=128), stage_t[:, :, :],
                          reads=[b_stage_t], writes=[b_v_d])
                    if own:
                        wt, bw = wload(C_AQ + 512 * g, 512)
                        proj_fm(wt, bw, 4, cons_k)
                        o0 = t0 - 3072
                        S.dma("sp", qT_d[4 * g:4 * g + 4, :, o0:o0 + 512].rearrange("h p t -> p h t"), stage_o[:, :, :],
                              reads=[b_stage_o], writes=[b_qT_d])

                hook_box[0] = None
                rec_box[0] = None
                for _ in rec:
                    pass
                ps_split[0] = False; psi[0] = 0
                for _ in ln_next:
                    pass
        S.barrier()
        e4w = ExitStack()
        if upto >= 4:
            hTo, b_hTo = sb(e4w, [128, 16, NOWN], BF16)
            S.dma("sp", hTo[:, :, :], hT_d[:, :, :].rearrange("k p t -> p k t"), reads=[b_hT_d], writes=[b_hTo])
            wpa, b_wpa = sb(e4w, [128, 4, D], BF16); wpm, b_wpm = sb(e4w, [128, 8, D], BF16)
            for kc in range(4):
                S.dma("pool", wpa[:, kc, :], w_pa[128 * kc:128 * kc + 128, :], writes=[b_wpa])
            for kc in range(8):
                S.dma("pool", wpm[:, kc, :], w_pm[128 * kc:128 * kc + 128, :], writes=[b_wpm])
        if upto >= 2:
          with ExitStack() as e2:
            qT_sb, b_qT_sb = sb(e2, [128, 4, NOWN], BF16)
            kT_sb, b_kT_sb = sb(e2, [128, 4, 3072], BF16)
            pad_sb, b_pad_sb = sb(e2, [128, 3072])
            S.dma("sp", pad_sb[:, :], padneg_b[:, 1024:4096], writes=[b_pad_sb])
            ab_sb, b_ab_sb = sb(e2, [128, 4, 256])
            Vb = [sb(e2, [128, 2, 512], BF16) for _ in range(2)]
            S2_l = [sb(e2, [128, 256]) for _ in range(4)]; Pb_l = [sb(e2, [128, 256], BF16) for _ in range(4)]
            PT_l = [sb(e2, [128, 2, 128], BF16) for _ in range(4)]
            a_sc_l = [sb(e2, [128, 8]) for _ in range(4)]
            o_sb = [sb(e2, [128, 4, 128]) for _ in range(2)]
            l_sb = [sb(e2, [128, 4]) for _ in range(2)]
            ui = 0
            for g in range(3):
                r = (1, 4, 16)[g]
                w_lo = 1024 if g == 2 else 2560
                nq = min(128, 1024 // r)
                nk = 128 + nq
                nch = (1024 // r) // nq
                S.dma("sp", qT_sb[:, :, :], qT_d[4 * g:4 * g + 4, :, :].rearrange("h p t -> p h t"), reads=[b_qT_d], writes=[b_qT_sb])
                S.dma("sp", kT_sb[:, :, 0:4096 - w_lo], kT_d[4 * g:4 * g + 4, :, w_lo:4096].rearrange("h p t -> p h t"),
                      reads=[b_kT_d], writes=[b_kT_sb])
                S.dma("sp", ab_sb[:, :, :], abias[4 * g:4 * g + 4, :, :].rearrange("h p k -> p h k"), writes=[b_ab_sb])
                for p in range(r):
                    for ci in range(nch):
                        j0 = 3072 // r + ci * nq
                        wk0 = p + r * (j0 - 128)
                        kc0 = wk0 - w_lo
                        qc0 = p + r * j0 - 3072
                        vt, b_vt = Vb[ui % 2]
                        ot, b_ot = o_sb[ui % 2]
                        lt, b_lt = l_sb[ui % 2]
                        ui += 1
                        S.dma("sp", vt[:, 0, :], v_d[ss(wk0, 128, r), 512 * g:512 * g + 512], reads=[b_v_d], writes=[b_vt])
                        S.dma("sp", vt[0:nq, 1, :], v_d[ss(wk0 + 128 * r, nq, r), 512 * g:512 * g + 512], reads=[b_v_d], writes=[b_vt])
                        HD = range(4)
                        pS_l = {}; pT_l = {}; pO_l = {}
                        for hd in HD:
                            pS, bpS = ps(); pS_l[hd] = (pS, bpS)
                            S.op("pe", lambda e, pS=pS, hd=hd: e.matmul(pS[0:nq, 0:nk], lhsT=qT_sb[:, hd, ss(qc0, nq, r)],
                                                                      rhs=kT_sb[:, hd, ss(kc0, nk, r)], start=True, stop=True),
                                 reads=[b_qT_sb, b_kT_sb], writes=[bpS])
                        for hd in HD:
                            S2, b_S2 = S2_l[hd]; pS, bpS = pS_l[hd]
                            S.op("dve", lambda e, pS=pS, hd=hd, S2=S2: e.scalar_tensor_tensor(out=S2[0:nq, 0:nk], in0=pS[0:nq, 0:nk], scalar=128 ** -0.5,
                                                                                            in1=ab_sb[0:nq, hd, 0:nk], op0=ALU.mult, op1=ALU.add),
                                 reads=[bpS, b_ab_sb], writes=[b_S2])
                            S.op("pool", lambda e, S2=S2: e.tensor_tensor(out=S2[0:nq, 0:nk], in0=S2[0:nq, 0:nk], in1=pad_sb[0:nq, ss(wk0 - 1024, nk, r)], op=ALU.add),
                                 reads=[b_S2, b_pad_sb], writes=[b_S2])
                        for hd in HD:
                            S2, b_S2 = S2_l[hd]; a_sc, b_a_sc = a_sc_l[hd]
                            S.op("dve", lambda e, S2=S2, a_sc=a_sc: e.tensor_reduce(out=a_sc[0:nq, 1:2], in_=S2[0:nq, 0:nk], axis=AX.X, op=ALU.max, negate=True),
                                 reads=[b_S2], writes=[b_a_sc])
                        for hd in HD:
                            S2, b_S2 = S2_l[hd]; a_sc, b_a_sc = a_sc_l[hd]; Pb, b_Pb = Pb_l[hd]
                            S.op("act", lambda e, S2=S2, a_sc=a_sc, Pb=Pb: e.activation(out=Pb[0:nq, 0:nk], in_=S2[0:nq, 0:nk], func=AF.Exp, bias=a_sc[0:nq, 1:2],
                                                                                       accum_out=a_sc[0:nq, 2:3]), reads=[b_S2, b_a_sc], writes=[b_Pb, b_a_sc])
                        for hd in HD:
                            Pb, b_Pb = Pb_l[hd]
                            pT_, bpT = ps(); pTh = pT_.bitcast(BF16); pT_l[hd] = (pTh, bpT)
                            S.op("pe", lambda e, pTh=pTh, Pb=Pb: e.transpose(out=pTh[:, 0:nq], in_=Pb[0:nq, 0:128], identity=idh[0:nq, 0:nq]),
                                 reads=[b_Pb, b_idh], writes=[bpT])
                            S.op("pe", lambda e, pTh=pTh, Pb=Pb: e.transpose(out=pTh[0:nq, 128:128 + nq], in_=Pb[0:nq, 128:128 + nq], identity=idh[0:nq, 0:nq]),
                                 reads=[b_Pb, b_idh], writes=[bpT])
                        for hd in HD:
                            PT, b_PT = PT_l[hd]; pTh, bpT = pT_l[hd]
                            copy("act", PT[:, 0, 0:nq], pTh[:, 0:nq], [bpT], [b_PT])
                            copy("dve", PT[0:nq, 1, 0:nq], pTh[0:nq, 128:128 + nq], [bpT], [b_PT])
                        for hd in HD:
                            PT, b_PT = PT_l[hd]
                            pO, bpO = ps(); pO_l[hd] = (pO, bpO)
                            S.op("pe", lambda e, pO=pO, hd=hd, vt=vt, PT=PT: e.matmul(pO[0:nq, 0:128], lhsT=PT[:, 0, 0:nq], rhs=vt[:, 0, 128 * hd:128 * hd + 128],
                                                                                    start=True, stop=False), reads=[b_PT, b_vt], writes=[bpO])
                            S.op("pe", lambda e, pO=pO, hd=hd, vt=vt, PT=PT: e.matmul(pO[0:nq, 0:128], lhsT=PT[0:nq, 1, 0:nq], rhs=vt[0:nq, 1, 128 * hd:128 * hd + 128],
                                                                                    start=False, stop=True), reads=[b_PT, b_vt], writes=[bpO])
                        for hd in HD:
                            a_sc, b_a_sc = a_sc_l[hd]
                            S.op("dve", lambda e, a_sc=a_sc: e.reciprocal(out=a_sc[0:nq, 3:4], in_=a_sc[0:nq, 2:3]), reads=[b_a_sc], writes=[b_a_sc])
                            S.op("act", lambda e, a_sc=a_sc: e.activation(out=a_sc[0:nq, 4:5], in_=a_sc[0:nq, 2:3], func=AF.Ln), reads=[b_a_sc], writes=[b_a_sc])
                        for hd in HD:
                            a_sc, b_a_sc = a_sc_l[hd]; pO, bpO = pO_l[hd]
                            S.op("dve", lambda e, pO=pO, hd=hd, ot=ot, a_sc=a_sc: e.tensor_scalar(out=ot[0:nq, hd, :], in0=pO[0:nq, 0:128], scalar1=a_sc[0:nq, 3:4],
                                                                                                scalar2=None, op0=ALU.mult), reads=[bpO, b_a_sc], writes=[b_ot])
                            S.op("dve", lambda e, hd=hd, lt=lt, a_sc=a_sc: e.tensor_tensor(out=lt[0:nq, hd:hd + 1], in0=a_sc[0:nq, 4:5], in1=a_sc[0:nq, 1:2], op=ALU.subtract),
                                 reads=[b_a_sc], writes=[b_lt])
                        S.dma("sp", o_d[g, ss(qc0, nq, r), :], ot[0:nq, :, :].rearrange("p h d -> p (h d)"), reads=[b_ot], writes=[b_o_d])
                        S.dma("sp", lse_d[g, ss(qc0, nq, r), :], lt[0:nq, :], reads=[b_lt], writes=[b_lse_d])
            o3 = [sb(e2, [128, 3, 512]) for _ in range(2)]
            l3, b_l3 = sb(e2, [128, 3, 4]); e3, b_e3 = sb(e2, [128, 3, 4]); m_sc, b_m_sc = sb(e2, [128, 12])
            att_t, b_att_t = sb(e2, [128, 512]); att_h, b_att_h = sb(e2, [128, 512], BF16)
            attT_c, b_attT_c = sb(e2, [128, 4, 128], BF16)
            for t in range(8):
                ot3, b_ot3 = o3[t % 2]
                S.dma("sp", ot3[:, :, :], o_d[:, 128 * t:128 * t + 128, :].rearrange("g p c -> p g c"), reads=[b_o_d], writes=[b_ot3])
                S.dma("sp", l3[:, :, :], lse_d[:, 128 * t:128 * t + 128, :].rearrange("g p c -> p g c"), reads=[b_lse_d], writes=[b_l3])
                S.op("dve", lambda e: e.tensor_tensor(out=m_sc[:, 0:4], in0=l3[:, 0, :], in1=l3[:, 1, :], op=ALU.max), reads=[b_l3], writes=[b_m_sc])
                S.op("dve", lambda e: e.tensor_tensor(out=m_sc[:, 0:4], in0=m_sc[:, 0:4], in1=l3[:, 2, :], op=ALU.max), reads=[b_l3, b_m_sc], writes=[b_m_sc])
                for g in range(3):
                    S.op("dve", lambda e, g=g: e.tensor_tensor(out=e3[:, g, :], in0=l3[:, g, :], in1=m_sc[:, 0:4], op=ALU.subtract),
                         reads=[b_l3, b_m_sc], writes=[b_e3])
                S.op("act", lambda e: e.activation(out=e3[:, :, :], in_=e3[:, :, :], func=AF.Exp), reads=[b_e3], writes=[b_e3])
                S.op("dve", lambda e: e.tensor_tensor(out=m_sc[:, 4:8], in0=e3[:, 0, :], in1=e3[:, 1, :], op=ALU.add), reads=[b_e3], writes=[b_m_sc])
                S.op("dve", lambda e: e.tensor_tensor(out=m_sc[:, 4:8], in0=m_sc[:, 4:8], in1=e3[:, 2, :], op=ALU.add), reads=[b_e3, b_m_sc], writes=[b_m_sc])
                S.op("dve", lambda e: e.reciprocal(out=m_sc[:, 8:12], in_=m_sc[:, 4:8]), reads=[b_m_sc], writes=[b_m_sc])
                for g in range(3):
                    S.op("dve", lambda e, g=g: e.tensor_tensor(out=e3[:, g, :], in0=e3[:, g, :], in1=m_sc[:, 8:12], op=ALU.mult),
                         reads=[b_e3, b_m_sc], writes=[b_e3])
                for sl in range(4):
                    S.op("dve", lambda e, sl=sl, ot3=ot3: e.tensor_scalar(out=att_t[:, 128 * sl:128 * sl + 128], in0=ot3[:, 0, 128 * sl:128 * sl + 128],
                                                                        scalar1=e3[:, 0, sl:sl + 1], scalar2=None, op0=ALU.mult),
                         reads=[b_ot3, b_e3], writes=[b_att_t])
                    for g in (1, 2):
                        S.op("dve", lambda e, sl=sl, g=g, ot3=ot3: e.scalar_tensor_tensor(
                            out=att_t[:, 128 * sl:128 * sl + 128], in0=ot3[:, g, 128 * sl:128 * sl + 128], scalar=e3[:, g, sl:sl + 1],
                            in1=att_t[:, 128 * sl:128 * sl + 128], op0=ALU.mult, op1=ALU.add), reads=[b_ot3, b_e3, b_att_t], writes=[b_att_t])
                if DEBUG:
                    S.dma("sp", dbg["dbg_att"][128 * t:128 * t + 128, :], att_t[:, :], reads=[b_att_t], final=True)
                S.op("act", lambda e: e.copy(out=att_h[:, :], in_=att_t[:, :]), reads=[b_att_t], writes=[b_att_h])
                p_, bp = ps()
                ph = p_.bitcast(BF16)
                for j in range(4):
                    S.op("pe", lambda e, ph=ph, j=j: e.transpose(out=ph[:, 128 * j:128 * j + 128], in_=att_h[:, 128 * j:128 * j + 128], identity=idh[:, :]),
                         reads=[b_att_h, b_idh], writes=[bp])
                copy(evac_eng(), attT_c[:, :, :], ph[:, 0:512].rearrange("p (k t) -> p k t", t=128), [bp], [b_attT_c])
                S.dma("sp", attT_d[:, :, 128 * t:128 * t + 128].rearrange("k p t -> p k t"), attT_c[:, :, :], reads=[b_attT_c], writes=[b_attT_d])
          S.barrier()

        if upto >= 4:
          if True:
            with ExitStack() as e4a:
                mergedT, b_mergedT = sb(e4a, [128, 16, NOWN], BF16)
                hmT, b_hmT = sb(e4a, [128, 8, NOWN], BF16); attT, b_attT = sb(e4a, [128, 4, NOWN], BF16)
                S.dma("sp", hmT[:, :, :], hmT_d[:, :, :].rearrange("k p t -> p k t"), reads=[b_hmT_d], writes=[b_hmT])
                S.dma("sp", attT[:, :, :], attT_d[:, :, :].rearrange("k p t -> p k t"), reads=[b_attT_d], writes=[b_attT])
                wg = [sb(e4a, [128, 2, 16, 256], BF16) for _ in range(2)]
                sg_l = [(sb(e4a, [128, 512]), sb(e4a, [128, 512])) for _ in range(2)]
                sgi = [0]
                for blk in range(8):
                    wgt, b_wgt = wg[blk % 2]
                    for a_, c0 in enumerate((C_GA, C_GM)):
                        S.dma("pool", wgt[:, a_, :, :], w_in[:, c0 + 256 * blk:c0 + 256 * blk + 256].rearrange("(k p) n -> p k n", p=128), writes=[b_wgt])
                    for sub in range(2):
                        cc = 2 * blk + sub
                        for th in range(2):
                            tk = slice(512 * th, 512 * th + 512)
                            pA, bpA = ps(); pM, bpM = ps(); pGA, bpGA = ps(); pGM, bpGM = ps()
                            (sga, b_sga), (sgm, b_sgm) = sg_l[sgi[0] % 2]; sgi[0] += 1
                            for kc in range(4):
                                S.op("pe", lambda e, kc=kc, pA=pA, cc=cc, tk=tk: e.matmul(pA[:, :], lhsT=wpa[:, kc, 128 * cc:128 * cc + 128], rhs=attT[:, kc, tk],
                                                                                       start=(kc == 0), stop=(kc == 3)), reads=[b_wpa, b_attT], writes=[bpA])
                            for kc in range(8):
                                S.op("pe", lambda e, kc=kc, pM=pM, cc=cc, tk=tk: e.matmul(pM[:, :], lhsT=wpm[:, kc, 128 * cc:128 * cc + 128], rhs=hmT[:, kc, tk],
                                                                                       start=(kc == 0), stop=(kc == 7)), reads=[b_wpm, b_hmT], writes=[bpM])
                            for a_, (pG, bpG) in enumerate(((pGA, bpGA), (pGM, bpGM))):
                                for kc in range(16):
                                    S.op("pe", lambda e, kc=kc, pG=pG, a_=a_, sub=sub, tk=tk, wgt=wgt: e.matmul(
                                        pG[:, :], lhsT=wgt[:, a_, kc, 128 * sub:128 * sub + 128], rhs=hTo[:, kc, tk], start=(kc == 0), stop=(kc == 15)),
                                         reads=[b_wgt, b_hTo], writes=[bpG])
                            S.op("act", lambda e, pGA=pGA: e.activation(out=sga[:, :], in_=pGA[:, :], func=AF.Sigmoid), reads=[bpGA], writes=[b_sga])
                            S.op("act", lambda e, pGM=pGM: e.activation(out=sgm[:, :], in_=pGM[:, :], func=AF.Sigmoid), reads=[bpGM], writes=[b_sgm])
                            S.op("dve", lambda e, pA=pA: e.tensor_tensor(out=sga[:, :], in0=sga[:, :], in1=pA[:, :], op=ALU.mult), reads=[b_sga, bpA], writes=[b_sga])
                            S.op("dve", lambda e, pM=pM: e.tensor_tensor(out=sgm[:, :], in0=sgm[:, :], in1=pM[:, :], op=ALU.mult), reads=[b_sgm, bpM], writes=[b_sgm])
                            S.op("dve", lambda e, cc=cc, tk=tk: e.tensor_tensor(out=mergedT[:, cc, tk], in0=sga[:, :], in1=sgm[:, :], op=ALU.add),
                                 reads=[b_sga, b_sgm], writes=[b_mergedT])
                S.dma("sp", mT_d[:, :, :].rearrange("k p t -> p k t"), mergedT[:, :, :], reads=[b_mergedT], writes=[b_mT_d])
            e4w.close()
            S.barrier()
            with ExitStack() as e5:
                mergedT, b_mergedT = sb(e5, [128, 16, NOWN], BF16)
                S.dma("sp", mergedT[:, :, :], mT_d[:, :, :].rearrange("k p t -> p k t"), reads=[b_mT_d], writes=[b_mergedT])
                h1c, b_h1c = sb(e5, [128, D], BF16)
                wo, b_wo = sb(e5, [128, 16, D], BF16)
                wo_b = [Buf() for _ in range(4)]
                for cb_ in range(4):
                    S.dma("pool", wo[:, :, 512 * cb_:512 * cb_ + 512], w_out[:, 512 * cb_:512 * cb_ + 512].rearrange("(k p) n -> p k n", p=128), writes=[wo_b[cb_]])
                cA, b_cA = sb(e5, [128, D]); cB, b_cB = sb(e5, [128, D])
                cG1, b_cG1 = sb(e5, [128, D]); cB1, b_cB1 = sb(e5, [128, D])
                S.dma("sp", cA[:, :], lng_b, writes=[b_cA]); S.dma("sp", cB[:, :], lnb_b, writes=[b_cB])
                S.dma("sp", cG1[:, :], ln1g_b, writes=[b_cG1]); S.dma("sp", cB1[:, :], ln1b_b, writes=[b_cB1])
                h1t, b_h1t = None, None
                c_wr, b_c_wr = sb(e5, [128, 16, 36]); c_br, b_c_br = sb(e5, [128, 36])
                S.dma("sp", c_wr[:, :, :], wr, writes=[b_c_wr]); S.dma("sp", c_br[:, :], br_b, writes=[b_c_br])
                xz_l = [(sb(e5, [128, D]), sb(e5, [128, D])) for _ in range(2)]
                h1T, b_h1T = sb(e5, [128, 16, 128])
                st, b_st = sb(e5, [128, 24]); mvt, b_mv = sb(e5, [128, 2]); sd, b_sd = sb(e5, [128, 2])
                lg, b_lg = sb(e5, [128, 36]); r_sc, b_r_sc = sb(e5, [128, 16]); goh, b_goh = sb(e5, [128, 4])
                esel, b_esel = sb(e5, [128, 8]); top8, b_top8 = sb(e5, [128, 8]); oh, b_oh = sb(e5, [128, 2, 8]); wsel, b_wsel = sb(e5, [128, 8])
                def s5_mm(t):
                        tk = slice(128 * t, 128 * t + 128)
                        (xnt, b_xnt), (z, b_z) = xz_l[t % 2]
                        h1t, b_h1t = z, b_z
                        S.dma("sp", xnt[:, :], h_d[tk, :], reads=[b_h_d], writes=[b_xnt])
                        S.op("dve", lambda e: e.tensor_tensor(out=xnt[:, :], in0=xnt[:, :], in1=cA[:, :], op=ALU.mult), reads=[b_xnt, b_cA], writes=[b_xnt])
                        S.op("dve", lambda e: e.tensor_tensor(out=xnt[:, :], in0=xnt[:, :], in1=cB[:, :], op=ALU.add), reads=[b_xnt, b_cB], writes=[b_xnt])
                        for cb_ in range(4):
                            cs = slice(512 * cb_, 512 * cb_ + 512)
                            pY, bpY = ps()
                            for kc in range(16):
                                S.op("pe", lambda e, kc=kc, pY=pY, cs=cs, tk=tk: e.matmul(pY[:, :], lhsT=mergedT[:, kc, tk], rhs=wo[:, kc, cs],
                                                                                       start=(kc == 0), stop=(kc == 15)), reads=[b_mergedT, wo_b[cb_]], writes=[bpY])
                            S.op("dve", lambda e, pY=pY, cs=cs: e.scalar_tensor_tensor(out=z[:, cs], in0=xnt[:, cs], scalar=ALPHA, in1=pY[:, :],
                                                                                     op0=ALU.mult, op1=ALU.add), reads=[b_xnt, bpY], writes=[b_z])

                def s5_rest(t):
                        tk = slice(128 * t, 128 * t + 128)
                        (xnt, b_xnt), (z, b_z) = xz_l[t % 2]
                        h1t, b_h1t = z, b_z
                        layer_norm_stats(z, b_z, st, b_st, mvt, b_mv, sd, b_sd)
                        S.op("act", lambda e: e.activation(out=z[:, :], in_=z[:, :], func=AF.Identity, scale=sd[:, 0:1], bias=sd[:, 1:2]),
                             reads=[b_z, b_sd], writes=[b_z])
                        S.op("dve", lambda e: e.tensor_tensor(out=z[:, :], in0=z[:, :], in1=cG1[:, :], op=ALU.mult), reads=[b_z, b_cG1], writes=[b_z])
                        S.op("dve", lambda e: e.tensor_tensor(out=h1t[:, :], in0=h1t[:, :], in1=cB1[:, :], op=ALU.add), reads=[b_h1t, b_cB1], writes=[b_h1t])
                        S.dma("sp", h1_d[tk, :], h1t[:, :], reads=[b_h1t], writes=[b_h1_d])
                        if DEBUG:
                            S.dma("sp", dbg["dbg_h1"][tk, :], h1t[:, :], reads=[b_h1t], final=True)
                        S.op("act", lambda e: e.copy(out=h1c[:, :], in_=h1t[:, :]), reads=[b_h1t], writes=[b_h1c])
                        S.dma("sp", h1bf_d[tk, :], h1c[:, :], reads=[b_h1c], writes=[b_h1bf_d])
                        for g4 in range(4):
                            p_, bp = ps()
                            for j in range(4):
                                kc = 4 * g4 + j
                                S.op("pe", lambda e, p_=p_, kc=kc, j=j: e.transpose(out=p_[:, 128 * j:128 * j + 128], in_=h1t[:, 128 * kc:128 * kc + 128], identity=idf[:, :]),
                                     reads=[b_h1t, b_idf], writes=[bp])
                            copy(evac_eng(), h1T[:, 4 * g4:4 * g4 + 4, :], p_[:, :].rearrange("p (k t) -> p k t", t=128), [bp], [b_h1T])
                        pL, bpL = ps()
                        for kc in range(16):
                            S.op("pe", lambda e, kc=kc, pL=pL: e.matmul(pL[:, 0:36], lhsT=h1T[:, kc, :], rhs=c_wr[:, kc, :], start=(kc == 0), stop=(kc == 15)),
                                 reads=[b_h1T, b_c_wr], writes=[bpL])
                        S.op("dve", lambda e, pL=pL: e.tensor_tensor(out=lg[:, :], in0=pL[:, 0:36], in1=c_br[:, :], op=ALU.add), reads=[bpL, b_c_br], writes=[b_lg])
                        S.op("dve", lambda e: e.tensor_reduce(out=r_sc[:, 0:1], in_=lg[:, 0:4], axis=AX.X, op=ALU.max), reads=[b_lg], writes=[b_r_sc])
                        S.op("dve", lambda e: e.tensor_scalar(out=goh[:, :], in0=lg[:, 0:4], scalar1=r_sc[:, 0:1], scalar2=None, op0=ALU.is_equal),
                             reads=[b_lg, b_r_sc], writes=[b_goh])
                        S.op("dve", lambda e: e.tensor_scalar(out=r_sc[:, 1:2], in0=r_sc[:, 0:1], scalar1=-1.0, scalar2=None, op0=ALU.mult), reads=[b_r_sc], writes=[b_r_sc])
                        S.op("act", lambda e: e.activation(out=wsel[:, 0:4], in_=lg[:, 0:4], func=AF.Exp, bias=r_sc[:, 1:2], accum_out=r_sc[:, 2:3]),
                             reads=[b_lg, b_r_sc], writes=[b_wsel, b_r_sc])
                        S.op("dve", lambda e: e.reciprocal(out=r_sc[:, 3:4], in_=r_sc[:, 2:3]), reads=[b_r_sc], writes=[b_r_sc])
                        S.op("dve", lambda e: e.tensor_scalar(out=esel[:, :], in0=lg[:, 4:12], scalar1=goh[:, 0:1], scalar2=None, op0=ALU.mult),
                             reads=[b_lg, b_goh], writes=[b_esel])
                        for g in (1, 2, 3):
                            S.op("dve", lambda e, g=g: e.scalar_tensor_tensor(out=esel[:, :], in0=lg[:, 4 + 8 * g:12 + 8 * g], scalar=goh[:, g:g + 1], in1=esel[:, :],
                                                                              op0=ALU.mult, op1=ALU.add), reads=[b_lg, b_goh, b_esel], writes=[b_esel])
                        S.op("dve", lambda e: e.max(out=top8[:, :], in_=esel[:, :]), reads=[b_esel], writes=[b_top8])
                        S.op("dve", lambda e: e.tensor_tensor(out=r_sc[:, 4:5], in0=top8[:, 1:2], in1=top8[:, 0:1], op=ALU.subtract), reads=[b_top8], writes=[b_r_sc])
                        S.op("act", lambda e: e.activation(out=r_sc[:, 5:6], in_=r_sc[:, 4:5], func=AF.Exp), reads=[b_r_sc], writes=[b_r_sc])
                        S.op("dve", lambda e: e.tensor_scalar(out=r_sc[:, 6:7], in0=r_sc[:, 5:6], scalar1=1.0, scalar2=None, op0=ALU.add), reads=[b_r_sc], writes=[b_r_sc])
                        S.op("dve", lambda e: e.reciprocal(out=r_sc[:, 6:7], in_=r_sc[:, 6:7]), reads=[b_r_sc], writes=[b_r_sc])
                        S.op("dve", lambda e: e.tensor_tensor(out=r_sc[:, 7:8], in0=r_sc[:, 5:6], in1=r_sc[:, 6:7], op=ALU.mult), reads=[b_r_sc], writes=[b_r_sc])
                        S.op("dve", lambda e: e.tensor_scalar(out=r_sc[:, 6:8], in0=r_sc[:, 6:8], scalar1=r_sc[:, 3:4], scalar2=None, op0=ALU.mult),
                             reads=[b_r_sc], writes=[b_r_sc])
                        for j in range(2):
                            S.op("dve", lambda e, j=j: e.tensor_scalar(out=oh[:, j, :], in0=esel[:, :], scalar1=top8[:, j:j + 1], scalar2=None, op0=ALU.is_equal),
                                 reads=[b_esel, b_top8], writes=[b_oh])
                        S.op("dve", lambda e: e.tensor_scalar(out=wsel[:, :], in0=oh[:, 0, :], scalar1=r_sc[:, 6:7], scalar2=None, op0=ALU.mult),
                             reads=[b_oh, b_r_sc], writes=[b_wsel])
                        S.op("dve", lambda e: e.scalar_tensor_tensor(out=wsel[:, :], in0=oh[:, 1, :], scalar=r_sc[:, 7:8], in1=wsel[:, :], op0=ALU.mult, op1=ALU.add),
                             reads=[b_oh, b_r_sc, b_wsel], writes=[b_wsel])
                        for g in range(4):
                            S.op("dve", lambda e, g=g, t=t: e.tensor_scalar(out=Wt_all[:, t, 8 * g:8 * g + 8], in0=wsel[:, :], scalar1=goh[:, g:g + 1], scalar2=None, op0=ALU.mult),
                                 reads=[b_wsel, b_goh], writes=[b_Wt])
                        S.op("dve", lambda e, t=t: e.tensor_scalar(out=mk_all[:, t, :], in0=Wt_all[:, t, :], scalar1=0.0, scalar2=None, op0=ALU.is_gt),
                             reads=[b_Wt], writes=[b_mk])

                s5_mm(0)
                for t in range(8):
                    if t + 1 < 8:
                        s5_mm(t + 1)
                    s5_rest(t)
                c_ones, b_c_ones = sb(e5, [128, 128]); c_ltri, b_c_ltri = sb(e5, [128, 128])
                S.dma("sp", c_ones[:, :], ones_f, writes=[b_c_ones]); S.dma("sp", c_ltri[:, :], ltri, writes=[b_c_ltri])
                for t in range(8):
                    pC, bpC = ps()
                    for t2 in range(t):
                        S.op("pe", lambda e, pC=pC, t2=t2: e.matmul(pC[:, 0:32], lhsT=c_ones[:, :], rhs=mk_all[:, t2, :], start=(t2 == 0), stop=False),
                             reads=[b_c_ones, b_mk], writes=[bpC])
                    S.op("pe", lambda e, pC=pC, t=t: e.matmul(pC[:, 0:32], lhsT=c_ltri[:, :], rhs=mk_all[:, t, :], start=(t == 0), stop=True),
                         reads=[b_c_ltri, b_mk], writes=[bpC])
                    S.op("dve", lambda e, pC=pC, t=t: e.tensor_tensor(out=cumm[:, t, :], in0=pC[:, 0:32], in1=mk_all[:, t, :], op=ALU.mult),
                         reads=[bpC, b_mk], writes=[b_cumm])
          S.barrier()

        if upto >= 6:
          with ExitStack() as e6:
            y_acc, b_y_acc = sb(e6, [128, 8, D])
            S.op("pool", lambda e: e.memset(y_acc[:, :, :], 0.0), writes=[b_y_acc])
            e6a = ExitStack()
            e6_outer = e6
            e6 = e6a
            h1_bf, b_h1_bf = sb(e6, [128, 8, D], BF16)
            S.dma("sp", h1_bf[:, :, :], h1bf_d.rearrange("(t p) c -> p t c", p=128), reads=[b_h1bf_d], writes=[b_h1_bf])
            c_iota, b_c_iota = sb(e6, [128, 128])
            S.dma("sp", c_iota[:, :], iota1, writes=[b_c_iota])
            wgu = [sb(e6, [128, 16, 512], BF16) for _ in range(3)]
            wdn = [sb(e6, [128, 11, 512], BF16) for _ in range(2)]
            Sel, b_Sel = sb(e6, [128, 8, 128], BF16); SelT_l = [sb(e6, [128, NOWN], BF16) for _ in range(2)]
            XeT, b_XeT = sb(e6, [128, 16, 128], BF16)
            sg, b_sg = sb(e6, [128, DFF]); Hs, b_Hs = sb(e6, [128, DFF], BF16)
            HT, b_HT = sb(e6, [128, 11, 128], BF16); Yb_l = [sb(e6, [128, D], BF16) for _ in range(2)]
            pending = []
            gi_ = [0]; di_ = [0]
            for ex in range(NEXP):
                SelT, b_SelT = SelT_l[ex % 2]; Yb, b_Yb = Yb_l[ex % 2]
                for t in range(8):
                    S.op("dve", lambda e, t=t, ex=ex: e.tensor_scalar(out=Sel[:, t, :], in0=c_iota[:, :], scalar1=cumm[:, t, ex:ex + 1], scalar2=None, op0=ALU.is_equal),
                         reads=[b_c_iota, b_cumm], writes=[b_Sel])
                pB, bpB = ps()
                pBh = pB.bitcast(BF16)
                for t in range(8):
                    S.op("pe", lambda e, pBh=pBh, t=t: e.transpose(out=pBh[:, 128 * t:128 * t + 128], in_=Sel[:, t, :], identity=idh[:, :]),
                         reads=[b_Sel, b_idh], writes=[bpB])
                copy(evac_eng(), SelT[:, :], pBh[:, :], [bpB], [b_SelT])
                for g4 in range(4):
                    pX_, bpX_ = ps()
                    for j in range(4):
                        kc = 4 * g4 + j
                        for t in range(8):
                            S.op("pe", lambda e, pX_=pX_, kc=kc, j=j, t=t: e.matmul(pX_[:, 128 * j:128 * j + 128], lhsT=h1_bf[:, t, 128 * kc:128 * kc + 128], rhs=Sel[:, t, :],
                                                                                 start=(t == 0), stop=(t == 7)), reads=[b_h1_bf, b_Sel], writes=[bpX_])
                    copy(evac_eng(), XeT[:, 4 * g4:4 * g4 + 4, :], pX_[:, :].rearrange("p (k t) -> p k t", t=128), [bpX_], [b_XeT])
                for which, wsrc in enumerate((w_gate, w_up)):
                    for blk in range(3):
                        c0 = 512 * blk
                        ncol = min(512, DFF - c0)
                        wt_, bw_ = wgu[gi_[0] % 3]; gi_[0] += 1
                        S.dma("pool", wt_[:, :, 0:ncol], wsrc[ex, :, c0:c0 + ncol].rearrange("(k p) n -> p k n", p=128), writes=[bw_])
                        pG, bpG = ps()
                        for kc in range(16):
                            S.op("pe", lambda e, pG=pG, wt_=wt_, kc=kc: e.matmul(pG[:, 0:ncol], lhsT=XeT[:, kc, :], rhs=wt_[:, kc, 0:ncol], start=(kc == 0), stop=(kc == 15)),
                                 reads=[b_XeT, bw_], writes=[bpG])
                        if which == 0:
                            S.op("act", lambda e, pG=pG, c0=c0: e.activation(out=sg[:, c0:c0 + ncol], in_=pG[:, 0:ncol], func=AF.Silu), reads=[bpG], writes=[b_sg])
                            if blk == 2:
                                while pending:
                                    pending.pop(0)()
                        else:
                            S.op("dve", lambda e, pG=pG, c0=c0: e.tensor_tensor(out=Hs[:, c0:c0 + ncol], in0=sg[:, c0:c0 + ncol], in1=pG[:, 0:ncol], op=ALU.mult),
                                 reads=[b_sg, bpG], writes=[b_Hs])
                for g3 in range(3):
                    pH, bpH = ps()
                    pHh = pH.bitcast(BF16)
                    nj = min(4, 11 - 4 * g3)
                    for j in range(nj):
                        fc = 4 * g3 + j
                        S.op("pe", lambda e, pHh=pHh, j=j, fc=fc: e.transpose(out=pHh[:, 128 * j:128 * j + 128], in_=Hs[:, 128 * fc:128 * fc + 128], identity=idh[:, :]),
                             reads=[b_Hs, b_idh], writes=[bpH])
                    copy(evac_eng(), HT[:, 4 * g3:4 * g3 + nj, :], pHh[:, 0:128 * nj].rearrange("p (k t) -> p k t", t=128), [bpH], [b_HT])
                for cb_ in range(4):
                    cs = slice(512 * cb_, 512 * cb_ + 512)
                    wd_, bwd_ = wdn[di_[0] % 2]; di_[0] += 1
                    S.dma("pool", wd_[:, :, :], w_down[ex, :, cs].rearrange("(k p) n -> p k n", p=128), writes=[bwd_])
                    pY, bpY = ps()
                    for fc in range(11):
                        S.op("pe", lambda e, pY=pY, fc=fc, wd_=wd_: e.matmul(pY[:, :], lhsT=HT[:, fc, :], rhs=wd_[:, fc, :], start=(fc == 0), stop=(fc == 10)),
                             reads=[b_HT, bwd_], writes=[bpY])
                    copy(evac_eng(), Yb[:, cs], pY[:, :], [bpY], [b_Yb])
                def combine(ex=ex, SelT=SelT, b_SelT=b_SelT, Yb=Yb, b_Yb=b_Yb):
                    for t in range(8):
                        for cb_ in range(4):
                            cs = slice(512 * cb_, 512 * cb_ + 512)
                            pZ, bpZ = ps()
                            S.op("pe", lambda e, pZ=pZ, t=t, cs=cs: e.matmul(pZ[:, :], lhsT=SelT[:, 128 * t:128 * t + 128], rhs=Yb[:, cs], start=True, stop=True),
                                 reads=[b_SelT, b_Yb], writes=[bpZ])
                            S.op("dve", lambda e, pZ=pZ, t=t, cs=cs: e.scalar_tensor_tensor(out=y_acc[:, t, cs], in0=pZ[:, :], scalar=Wt_all[:, t, ex:ex + 1],
                                                                                          in1=y_acc[:, t, cs], op0=ALU.mult, op1=ALU.add),
                                 reads=[bpZ, b_Wt, b_y_acc], writes=[b_y_acc])
                pending.append(combine)
            while pending:
                pending.pop(0)()
            e6a.close()
            e6 = e6_outer
            S.barrier()
            cG2, b_cG2 = sb(e6, [128, D]); cB2, b_cB2 = sb(e6, [128, D])
            S.dma("sp", cG2[:, :], ln2g_b, writes=[b_cG2]); S.dma("sp", cB2[:, :], ln2b_b, writes=[b_cB2])
            h1r = [sb(e6, [128, D]) for _ in range(2)]
            st, b_st = sb(e6, [128, 24]); mvt, b_mv = sb(e6, [128, 2]); sd, b_sd = sb(e6, [128, 2])
            for t in range(8):
                tk = slice(128 * t, 128 * t + 128)
                ht_, b_ht_ = h1r[t % 2]
                if DEBUG:
                    S.dma("sp", dbg["dbg_y2"][tk, :], y_acc[:, t, :], reads=[b_y_acc], final=True)
                S.dma("sp", ht_[:, :], h1_d[tk, :], reads=[b_h1_d], writes=[b_ht_])
                S.op("dve", lambda e, t=t, ht_=ht_: e.scalar_tensor_tensor(out=ht_[:, :], in0=ht_[:, :], scalar=ALPHA, in1=y_acc[:, t, :], op0=ALU.mult, op1=ALU.add),
                     reads=[b_ht_, b_y_acc], writes=[b_ht_])
                layer_norm_stats(ht_, b_ht_, st, b_st, mvt, b_mv, sd, b_sd)
                S.op("act", lambda e, ht_=ht_: e.activation(out=ht_[:, :], in_=ht_[:, :], func=AF.Identity, scale=sd[:, 0:1], bias=sd[:, 1:2]),
                     reads=[b_ht_, b_sd], writes=[b_ht_])
                S.op("dve", lambda e, ht_=ht_: e.tensor_tensor(out=ht_[:, :], in0=ht_[:, :], in1=cG2[:, :], op=ALU.mult), reads=[b_ht_, b_cG2], writes=[b_ht_])
                S.op("dve", lambda e, ht_=ht_: e.tensor_tensor(out=ht_[:, :], in0=ht_[:, :], in1=cB2[:, :], op=ALU.add), reads=[b_ht_, b_cB2], writes=[b_ht_])
                S.dma("sp", out_d[tk, :], ht_[:, :], reads=[b_ht_], final=True)
        S.barrier()
        S.finish()
    return nc


def _bcast(v, n=128):
    v = np.asarray(v, np.float32).reshape(1, -1)
    return np.ascontiguousarray(np.broadcast_to(v, (n, v.shape[1])))


def prep_inputs(inp):
    f32 = np.float32
    x = np.asarray(inp["x"], f32)
    shared = {}
    shared["w_in"] = np.ascontiguousarray(np.asarray(inp["w_in"], f32)[0])
    g = np.asarray(inp["ln_in_g"], f32); b = np.asarray(inp["ln_in_b"], f32)
    shared["lng_col"] = np.ascontiguousarray(g.reshape(16, 128).T); shared["lnb_col"] = np.ascontiguousarray(b.reshape(16, 128).T)
    shared["lng_b"] = _bcast(g); shared["lnb_b"] = _bcast(b)
    cwv = np.asarray(inp["m_conv_w"], f32)[0]
    shared["cw"] = np.ascontiguousarray(cwv.reshape(4, 8, 128).transpose(2, 1, 0))
    shared["cb"] = np.ascontiguousarray(np.asarray(inp["m_conv_b"], f32)[0].reshape(8, 128).T)
    shared["ifb"] = np.ascontiguousarray(np.asarray(inp["m_if_bias"], f32)[0].reshape(2, 4).T)
    shared["mnw_b"] = _bcast(np.asarray(inp["m_norm_w"], f32)[0].reshape(-1))
    shared["w_pa"] = np.ascontiguousarray(np.asarray(inp["w_proj_att"], f32)[0])
    shared["w_pm"] = np.ascontiguousarray(np.asarray(inp["w_proj_mlstm"], f32)[0])
    shared["w_out"] = np.ascontiguousarray(np.asarray(inp["w_out"], f32)[0])
    shared["ln1g_b"] = _bcast(np.asarray(inp["ln1_g"], f32)[0]); shared["ln1b_b"] = _bcast(np.asarray(inp["ln1_b"], f32)[0])
    shared["ln2g_b"] = _bcast(np.asarray(inp["ln2_g"], f32)[0]); shared["ln2b_b"] = _bcast(np.asarray(inp["ln2_b"], f32)[0])
    wrc = np.concatenate([np.asarray(inp["w_router_group"], f32)[0], np.asarray(inp["w_router_expert"], f32)[0]], axis=1)
    shared["wr"] = np.ascontiguousarray(wrc.reshape(16, 128, 36).transpose(1, 0, 2))
    shared["br_b"] = _bcast(np.concatenate([np.asarray(inp["b_router_group"], f32)[0], np.asarray(inp["b_router_expert"], f32)[0]]))
    shared["w_gate"] = np.ascontiguousarray(np.asarray(inp["w_gate"], f32)[0])
    shared["w_up"] = np.ascontiguousarray(np.asarray(inp["w_up"], f32)[0])
    shared["w_down"] = np.ascontiguousarray(np.asarray(inp["w_down"], f32)[0])
    slopes = alibi_slopes(12)
    qi = np.arange(128)[:, None]; ki = np.arange(256)[None, :]
    delta = 128 + qi - ki
    ab = np.zeros((12, 128, 256), f32)
    for h in range(12):
        r = (1, 4, 16)[h // 4]
        ab[h] = np.where((delta >= 0) & (delta <= 128), -slopes[h] * (delta * r).astype(f32), NEG)
    shared["abias"] = ab
    shared["ident_f"] = np.eye(128, dtype=f32)
    shared["ident_h"] = np.eye(128, dtype=f32).astype(ml_dtypes.bfloat16)
    s_ = np.arange(128)[:, None]; l_ = np.arange(128)[None, :]
    shared["tri_neg"] = np.where(s_ <= l_, 0.0, NEG).astype(f32)
    shared["ltri"] = (s_ <= l_).astype(f32)
    shared["ones_f"] = np.ones((128, 128), f32)
    shared["iota1"] = np.ascontiguousarray(np.broadcast_to(np.arange(1, 129, dtype=f32)[None, :], (128, 128)))
    shared["iota1c"] = np.arange(1, 129, dtype=f32).reshape(128, 1)
    sel4 = np.zeros((4, 4, 128), f32)
    for k in range(4):
        sel4[k, k, :] = 1.0
    shared["sel4"] = sel4
    sel32 = np.zeros((32, 32, 128), f32)
    for k in range(32):
        sel32[k, k, :] = 1.0
    shared["sel32"] = sel32
    in_maps = []
    for c in range(8):
        b_, k_ = c // 4, c % 4
        base = 1024 * k_ - 3072
        xw = np.zeros((NW, D), f32)
        lo = max(0, -base)
        xw[lo:] = x[b_, base + lo: base + NW]
        valid = (np.arange(NW) + base >= 0).astype(f32)
        m = dict(shared)
        m["x_win"] = xw
        m["padneg_b"] = _bcast((valid - 1.0) * (-NEG))
        m["valid_b"] = _bcast(valid)
        gm = np.zeros((4, 2, NW), f32)
        gm[:, 0, :] = (1.0 - valid) * NEG
        gm[:, 1, :] = (1.0 - valid) * (-NEG)
        m["gmask"] = gm
        in_maps.append(m)
    return in_maps


_NC = None


def kernel(**inputs):
    global _NC
    if _NC is None:
        _NC = build_program()
    in_maps = prep_inputs(inputs)
    res = run_bass_kernel_spmd(_NC, in_maps, core_ids=list(range(8)))
    out = np.zeros((2, 4096, D), np.float32)
    for c in range(8):
        b_, k_ = c // 4, c % 4
        out[b_, 1024 * k_:1024 * k_ + 1024] = res.results[c]["out"]
    return out
```
